# Optimizing a Trainium2 kernel written in Bass

```python
import jax
import jax.numpy as jnp
from jax import lax
import numpy as np

D_MODEL = 1024
BATCH = 8
SEQ = 2048
DEPTH = 2

GRID_W = 64
CTX_LEN = 256
EPS = 1e-6

FNET_WIDTH = 256
FNET_GROUPS = 4
FNET_GDIM = FNET_WIDTH // FNET_GROUPS

GLA_HEADS = 4
GLA_DV = 64
GLA_DK = 32
GLA_WIDTH = GLA_HEADS * GLA_DV
GLA_QK = GLA_HEADS * GLA_DK
GLA_GATE_RANK = 16
GLA_GATE_NORM = 16.0
GLA_CHUNK = 64

ATT_HEADS = 8
ATT_KV_HEADS = 2
ATT_HDIM = 64
ATT_WIDTH = ATT_HEADS * ATT_HDIM
ATT_KV_WIDTH = ATT_KV_HEADS * ATT_HDIM
ROPE_AXIS_DIM = ATT_HDIM // 2
ROPE_FREQS = ROPE_AXIS_DIM // 2
ROPE_THETA = 10000.0
Q_BLOCK = 128

D_MIX = FNET_WIDTH + GLA_WIDTH + ATT_WIDTH
IN_SIZES = (FNET_WIDTH, GLA_QK, GLA_QK, GLA_WIDTH, GLA_WIDTH, GLA_GATE_RANK, GLA_GATE_RANK, ATT_WIDTH, ATT_KV_WIDTH, ATT_KV_WIDTH)
D_IN = FNET_WIDTH + 2 * GLA_QK + 2 * GLA_WIDTH + 2 * GLA_GATE_RANK + ATT_WIDTH + 2 * ATT_KV_WIDTH

N_EXPERTS = 16
N_GROUPS = 4
EXPERTS_PER_GROUP = N_EXPERTS // N_GROUPS
TOP_K = 2
D_EXPERT = 512

kernel_name = 'hybrid_fnet_gla_gqa_moe_dit'


def rms_norm(x, w):
    xf = x.astype(jnp.float32)
    y = xf * lax.rsqrt(jnp.mean(xf * xf, axis=-1, keepdims=True) + EPS)
    return (y * w.astype(jnp.float32)).astype(x.dtype)


def split_proj(p):
    idx = []
    acc = 0
    for s in IN_SIZES[:-1]:
        acc += s
        idx.append(acc)
    return jnp.split(p, idx, axis=-1)


def axial_rope_tables(row, col):
    inv = ROPE_THETA ** (-jnp.arange(ROPE_FREQS, dtype=jnp.float32) * 2.0 / ROPE_AXIS_DIM)
    ang = jnp.stack([row.astype(jnp.float32)[:, None] * inv, col.astype(jnp.float32)[:, None] * inv], axis=1)
    return jnp.cos(ang)[:, None], jnp.sin(ang)[:, None]


def apply_axial_rope(x, cos, sin):
    xf = x.astype(jnp.float32).reshape(x.shape[:-1] + (2, 2, ROPE_FREQS))
    x1 = xf[..., 0, :]
    x2 = xf[..., 1, :]
    out = jnp.stack([x1 * cos - x2 * sin, x2 * cos + x1 * sin], axis=-2)
    return out.reshape(x.shape).astype(x.dtype)


def fourier_mix(u):
    b, n, _ = u.shape
    ug = u.astype(jnp.float32).reshape(b, n, FNET_GROUPS, FNET_GDIM).transpose(0, 2, 1, 3)
    f = jnp.fft.fft2(ug, norm='ortho').real
    return f.transpose(0, 2, 1, 3).reshape(b, n, FNET_WIDTH).astype(u.dtype)


def gla_heads(parts, w_up, b_up):
    gq, gk, gv, gdf, gdb = parts[1], parts[2], parts[3], parts[5], parts[6]
    b, n, _ = gq.shape

    def heads(t, d):
        return t.astype(jnp.float32).reshape(b, n, GLA_HEADS, d).transpose(0, 2, 1, 3)

    q = heads(gq, GLA_DK) * (GLA_DK ** -0.5)
    k = heads(gk, GLA_DK)
    v = heads(gv, GLA_DV)
    la_f = heads(jax.nn.log_sigmoid((gdf @ w_up[0] + b_up[0]).astype(jnp.float32)) / GLA_GATE_NORM, GLA_DK)
    la_b = heads(jax.nn.log_sigmoid((gdb @ w_up[1] + b_up[1]).astype(jnp.float32)) / GLA_GATE_NORM, GLA_DK)
    return q, k, v, la_f, la_b


def gla_chunk_scan(q, k, v, log_a, s0):
    b, h, n, dk = q.shape
    dv = v.shape[-1]
    nc = n // GLA_CHUNK

    def to_chunks(t):
        return t.reshape(b, h, nc, GLA_CHUNK, t.shape[-1]).transpose(2, 0, 1, 3, 4)

    mask = jnp.tril(jnp.ones((GLA_CHUNK, GLA_CHUNK), dtype=bool))

    def step(s, inp):
        qi, ki, vi, ai = inp
        cum = jnp.cumsum(ai, axis=-2)
        last = cum[..., -1:, :]
        q_t = qi * jnp.exp(cum)
        k_t = ki * jnp.exp(-cum)
        att = jnp.where(mask, jnp.einsum('bhik,bhjk->bhij', q_t, k_t), 0.0)
        o = jnp.einsum('bhij,bhjv->bhiv', att, vi) + jnp.einsum('bhik,bhkv->bhiv', q_t, s)
        s_new = jnp.exp(last)[..., 0, :, None] * s + jnp.einsum('bhjk,bhjv->bhkv', ki * jnp.exp(last - cum), vi)
        return s_new, o

    s_fin, oc = lax.scan(step, s0, (to_chunks(q), to_chunks(k), to_chunks(v), to_chunks(log_a)))
    o = oc.transpose(1, 2, 0, 3, 4).reshape(b, h, n, dv)
    return o, s_fin


def gla_bidir(q, k, v, la_f, la_b, s0_f, s0_b):
    o_f, s_f = gla_chunk_scan(q, k, v, la_f, s0_f)
    o_b, s_b = gla_chunk_scan(jnp.flip(q, 2), jnp.flip(k, 2), jnp.flip(v, 2), jnp.flip(la_b, 2), s0_b)
    return o_f + jnp.flip(o_b, 2), s_f, s_b


def gla_out(o, g, w_norm):
    b, h, n, dv = o.shape
    o = rms_norm(o, w_norm).transpose(0, 2, 1, 3).reshape(b, n, h * dv)
    return o.astype(g.dtype) * jax.nn.silu(g)


def attn_heads(parts, qn, kn, rope):
    aq, ak, av = parts[7], parts[8], parts[9]
    b, n, _ = aq.shape
    q = rms_norm(aq.reshape(b, n, ATT_HEADS, ATT_HDIM), qn)
    k = rms_norm(ak.reshape(b, n, ATT_KV_HEADS, ATT_HDIM), kn)
    v = av.reshape(b, n, ATT_KV_HEADS, ATT_HDIM)
    if rope is not None:
        q = apply_axial_rope(q, rope[0], rope[1])
        k = apply_axial_rope(k, rope[0], rope[1])
    return q, k, v


def gqa_blocks(q, k, v):
    b, n, h, hd = q.shape
    g = h // ATT_KV_HEADS
    nb = n // Q_BLOCK
    qb = q.reshape(b, nb, Q_BLOCK, ATT_KV_HEADS, g, hd).transpose(1, 0, 3, 4, 2, 5)
    kt = k.transpose(0, 2, 1, 3)
    vt = v.transpose(0, 2, 1, 3)
    scale = hd ** -0.5

    def one_block(qi):
        s = jnp.einsum('bkgqd,bkmd->bkgqm', qi, kt).astype(jnp.float32) * scale
        p = jax.nn.softmax(s, axis=-1).astype(vt.dtype)
        return jnp.einsum('bkgqm,bkmd->bkgqd', p, vt)

    ob = lax.map(one_block, qb)
    return ob.transpose(1, 0, 4, 2, 3, 5).reshape(b, n, h * hd)


def moe_ffn(h, w_router, b_router, w_gate, w_up, w_down):
    n_tok = h.shape[0]
    s = jax.nn.sigmoid(jnp.matmul(h, w_router).astype(jnp.float32))
    sel = (s + b_router.astype(jnp.float32)).reshape(n_tok, N_GROUPS, EXPERTS_PER_GROUP)
    grp_score = jnp.sum(lax.top_k(sel, TOP_K)[0], axis=-1)
    g_idx = jnp.argmax(grp_score, axis=-1)
    in_grp = sel[jnp.arange(n_tok), g_idx]
    loc = lax.top_k(in_grp, TOP_K)[1]
    e_idx = g_idx[:, None] * EXPERTS_PER_GROUP + loc
    w = jnp.take_along_axis(s, e_idx, axis=-1)
    w = w / jnp.sum(w, axis=-1, keepdims=True)
    gate = jnp.sum(w[..., None] * jax.nn.one_hot(e_idx, N_EXPERTS, dtype=jnp.float32), axis=1).astype(h.dtype)
    out = jnp.zeros_like(h)
    for e in range(N_EXPERTS):
        a = jax.nn.silu(h @ w_gate[e]) * (h @ w_up[e])
        out = out + gate[:, e:e + 1] * (a @ w_down[e])
    return out


def setup_inputs(seed: int = 0) -> dict:
    key = jax.random.key(seed)
    ks = jax.random.split(key, 24)

    def nrm(k, shape, s):
        return jax.random.normal(k, shape, jnp.float32) * s

    def gain(k, shape):
        return 1.0 + 0.02 * jax.random.normal(k, shape, jnp.float32)

    return {
        'x': nrm(ks[0], (BATCH, SEQ, D_MODEL), 1.0),
        'c': nrm(ks[1], (BATCH, D_MODEL), 1.0),
        'ctx': nrm(ks[2], (BATCH, CTX_LEN, D_MODEL), 1.0),
        'c_ctx': nrm(ks[3], (D_MODEL,), 1.0),
        'w_ada': nrm(ks[4], (DEPTH, D_MODEL, 6 * D_MODEL), 0.5 * D_MODEL ** -0.5),
        'b_ada': nrm(ks[5], (DEPTH, 6 * D_MODEL), 0.02),
        'norm_mix': gain(ks[6], (DEPTH, D_MODEL)),
        'norm_ffn': gain(ks[7], (DEPTH, D_MODEL)),
        'w_in': nrm(ks[8], (DEPTH, D_MODEL, D_IN), D_MODEL ** -0.5),
        'w_gla_gate_up': nrm(ks[9], (DEPTH, 2, GLA_GATE_RANK, GLA_QK), GLA_GATE_RANK ** -0.5),
        'b_gla_gate': nrm(ks[10], (DEPTH, 2, GLA_QK), 0.1),
        'gla_norm': gain(ks[11], (DEPTH, GLA_DV)),
        'q_norm': gain(ks[12], (DEPTH, ATT_HDIM)),
        'k_norm': gain(ks[13], (DEPTH, ATT_HDIM)),
        'w_out': nrm(ks[14], (DEPTH, D_MIX, D_MODEL), D_MIX ** -0.5),
        'w_router': nrm(ks[15], (D_MODEL, N_EXPERTS), D_MODEL ** -0.5),
        'b_router': nrm(ks[16], (N_EXPERTS,), 0.01),
        'w_exp_gate': nrm(ks[17], (DEPTH, N_EXPERTS, D_MODEL, D_EXPERT), D_MODEL ** -0.5),
        'w_exp_up': nrm(ks[18], (DEPTH, N_EXPERTS, D_MODEL, D_EXPERT), D_MODEL ** -0.5),
        'w_exp_down': nrm(ks[19], (DEPTH, N_EXPERTS, D_EXPERT, D_MODEL), D_EXPERT ** -0.5),
        'final_norm': gain(ks[20], (D_MODEL,)),
    }


def reference(x, c, ctx, c_ctx, w_ada, b_ada, norm_mix, norm_ffn, w_in, w_gla_gate_up, b_gla_gate, gla_norm, q_norm, k_norm, w_out, w_router, b_router, w_exp_gate, w_exp_up, w_exp_down, final_norm):
    b, n, d = x.shape
    m = ctx.shape[1]
    rows = n // GRID_W
    row = jnp.repeat(jnp.arange(rows), GRID_W)
    col = jnp.tile(jnp.arange(GRID_W), rows)
    rope = axial_rope_tables(row, col)
    silu_c = jax.nn.silu(c)
    silu_cc = jax.nn.silu(c_ctx)
    xc = ctx
    for l in range(DEPTH):
        ctx_out = l < DEPTH - 1
        mod_x = (silu_c @ w_ada[l] + b_ada[l])[:, None, :]
        mod_k = (silu_cc @ w_ada[l] + b_ada[l])[None, None, :]
        shx1, scx1, gx1, shx2, scx2, gx2 = jnp.split(mod_x, 6, axis=-1)
        shk1, sck1, gk1, shk2, sck2, gk2 = jnp.split(mod_k, 6, axis=-1)

        hx = rms_norm(x, norm_mix[l]) * (1.0 + scx1) + shx1
        hk = rms_norm(xc, norm_mix[l]) * (1.0 + sck1) + shk1
        px = split_proj(hx @ w_in[l])
        pk = split_proj(hk @ w_in[l])

        gla_x = gla_heads(px, w_gla_gate_up[l], b_gla_gate[l])
        gla_k = gla_heads(pk, w_gla_gate_up[l], b_gla_gate[l])
        s_zero = jnp.zeros((b, GLA_HEADS, GLA_DK, GLA_DV), jnp.float32)
        ok_gla, s_f, s_b = gla_bidir(*gla_k, s_zero, s_zero)
        ox_gla, _, _ = gla_bidir(*gla_x, s_f, s_b)

        qx, kx, vx = attn_heads(px, q_norm[l], k_norm[l], rope)
        qk_, kk_, vk_ = attn_heads(pk, q_norm[l], k_norm[l], None)
        ox_att = gqa_blocks(qx, jnp.concatenate([kk_, kx], axis=1), jnp.concatenate([vk_, vx], axis=1))

        ox = jnp.concatenate([fourier_mix(px[0]), gla_out(ox_gla, px[4], gla_norm[l]), ox_att], axis=-1) @ w_out[l]
        x = x + gx1 * ox
        if ctx_out:
            ok = jnp.concatenate([fourier_mix(pk[0]), gla_out(ok_gla, pk[4], gla_norm[l]), gqa_blocks(qk_, kk_, vk_)], axis=-1) @ w_out[l]
            xc = xc + gk1 * ok

        hx2 = rms_norm(x, norm_ffn[l]) * (1.0 + scx2) + shx2
        if ctx_out:
            hk2 = rms_norm(xc, norm_ffn[l]) * (1.0 + sck2) + shk2
            tokens = jnp.concatenate([hx2.reshape(b * n, d), hk2.reshape(b * m, d)], axis=0)
            y = moe_ffn(tokens, w_router, b_router, w_exp_gate[l], w_exp_up[l], w_exp_down[l])
            x = x + gx2 * y[:b * n].reshape(b, n, d)
            xc = xc + gk2 * y[b * n:].reshape(b, m, d)
        else:
            y = moe_ffn(hx2.reshape(b * n, d), w_router, b_router, w_exp_gate[l], w_exp_up[l], w_exp_down[l])
            x = x + gx2 * y.reshape(b, n, d)
    return rms_norm(x, final_norm)
```

```python
import numpy as np
import ml_dtypes
import concourse.bass as bass
import concourse.mybir as mybir
from concourse.bass_utils import run_bass_kernel_spmd

F32 = mybir.dt.float32
BF16 = mybir.dt.bfloat16
AF = mybir.ActivationFunctionType
ALU = mybir.AluOpType
AX = mybir.AxisListType

_DSZ = {F32: 4, BF16: 2}


def _dsz(dt):
    if dt in _DSZ:
        return _DSZ[dt]
    s = str(dt)
    if "32" in s:
        return 4
    if "16" in s:
        return 2
    if "64" in s:
        return 8
    return 1


def _region(ap):
    t = ap.tensor
    name = t.name
    dsz = _dsz(ap.dtype)
    space = str(ap.space)
    off = int(ap.offset)
    if "DRAM" in space.upper() or "HBM" in space.upper():
        ext = 0
        for (s, c) in ap.ap:
            ext += abs(int(s)) * (int(c) - 1)
        return (name, 0, 1, off * dsz, (off + ext + 1) * dsz)
    shape = list(t.shape)
    fsz = 1
    for d in shape[1:]:
        fsz *= int(d)
    p0 = off // fsz
    f0 = off % fsz
    pext = 0
    fext = 0
    for (s, c) in ap.ap:
        s = int(s)
        c = int(c)
        if c <= 1 or s == 0:
            continue
        if s % fsz == 0:
            pext += (s // fsz) * (c - 1)
        else:
            fext += abs(s) * (c - 1)
    b0 = f0 * dsz
    b1 = (f0 + fext + 1) * dsz
    if "PSUM" in space.upper():
        return (name, 0, 128, (b0 // 2048) * 2048, ((b1 + 2047) // 2048) * 2048)
    return (name, p0, p0 + pext + 1, b0, b1)


class _Op:
    __slots__ = ("eng", "fn", "k", "is_dma", "deps_eng", "deps_dma", "need_signal", "sig_val",
                 "dma_sem", "dma_val", "dma_prev", "name")

    def __init__(self, eng, fn, is_dma, name=""):
        self.eng = eng
        self.fn = fn
        self.is_dma = is_dma
        self.k = -1
        self.deps_eng = {}
        self.deps_dma = []
        self.need_signal = False
        self.sig_val = 0
        self.dma_sem = None
        self.dma_val = 0
        self.dma_prev = None
        self.name = name


class Prog:
    ENGS = ("pe", "act", "dve", "pool", "sp")
    NDMA = {"sp": 40, "pool": 8, "act": 8}

    def __init__(self, nc):
        self.nc = nc
        self.eng_ops = {e: [] for e in self.ENGS}
        self.recs = {}
        self.dma_count = {q: 0 for q in self.NDMA}
        self.dma_last = {q: [None] * n for q, n in self.NDMA.items()}
        self.nops = 0

    def _dep(self, x, y):
        if y is x:
            return
        if y.is_dma:
            if y not in x.deps_dma:
                x.deps_dma.append(y)
            return
        if (not x.is_dma) and y.eng == x.eng:
            if x.eng == "pe":
                return
            if len(self.eng_ops[x.eng]) - y.k > 3:
                return
        cur = x.deps_eng.get(y.eng, -1)
        if y.k > cur:
            x.deps_eng[y.eng] = y.k
        y.need_signal = True

    def add(self, eng, fn, reads, writes, is_dma=False, name=""):
        op = _Op(eng, fn, is_dma, name)
        rr = [_region(a) for a in reads if a is not None]
        ww = [_region(a) for a in writes if a is not None]
        for (nm, p0, p1, b0, b1) in rr:
            is_ps = (nm == "ps")
            for rec in self.recs.get(nm, ()):
                if rec[0] < p1 and p0 < rec[1] and rec[2] < b1 and b0 < rec[3]:
                    if rec[4] or (is_ps and rec[5].eng != eng):
                        self._dep(op, rec[5])
        for (nm, p0, p1, b0, b1) in ww:
            for rec in self.recs.get(nm, ()):
                if rec[0] < p1 and p0 < rec[1] and rec[2] < b1 and b0 < rec[3]:
                    self._dep(op, rec[5])
        for (nm, p0, p1, b0, b1) in ww:
            lst = self.recs.setdefault(nm, [])
            lst[:] = [r for r in lst if not (p0 <= r[0] and r[1] <= p1 and b0 <= r[2] and r[3] <= b1)]
            lst.append((p0, p1, b0, b1, True, op))
        for (nm, p0, p1, b0, b1) in rr:
            lst = self.recs.setdefault(nm, [])
            if not is_dma:
                lst[:] = [r for r in lst if not ((not r[4]) and (not r[5].is_dma) and r[5].eng == eng
                                                 and p0 <= r[0] and r[1] <= p1 and b0 <= r[2] and r[3] <= b1)]
            lst.append((p0, p1, b0, b1, False, op))
        op.k = len(self.eng_ops[eng])
        self.eng_ops[eng].append(op)
        if is_dma:
            n = self.NDMA[eng]
            slot = self.dma_count[eng] % n
            self.dma_count[eng] += 1
            prev = self.dma_last[eng][slot]
            op.dma_sem = (eng, slot)
            op.dma_prev = prev
            op.dma_val = (prev.dma_val if prev is not None else 0) + 16
            self.dma_last[eng][slot] = op
        self.nops += 1
        return op

    def barrier(self):
        bop = _Op("sp", lambda e: e.nop(), False, "barrier")
        for e in self.ENGS:
            lst = [o for o in self.eng_ops[e] if not o.is_dma]
            if lst:
                y = lst[-1]
                if e == "sp":
                    continue
                bop.deps_eng[e] = y.k
                y.need_signal = True
        for q in self.NDMA:
            for y in self.dma_last[q]:
                if y is not None:
                    bop.deps_dma.append(y)
        bop.k = len(self.eng_ops["sp"])
        bop.need_signal = True
        self.eng_ops["sp"].append(bop)
        self.recs = {"__barrier__": [(0, 1, 0, 1, True, bop)]}
        self._barrier_op = bop
        self._barrier_seen = set()
        return bop

    def _barrier_dep(self, op):
        b = getattr(self, "_barrier_op", None)
        if b is None or op is b or op.eng == "sp" or op.eng in self._barrier_seen:
            return
        self._barrier_seen.add(op.eng)
        if b.k > op.deps_eng.get("sp", -1):
            op.deps_eng["sp"] = b.k

    def _rec(self, eng, fn, reads, writes, is_dma=False, name=""):
        op = self.add(eng, fn, reads, writes, is_dma, name)
        self._barrier_dep(op)
        if eng == "pe":
            l = reads[0]
            rr = lambda v: 32 if v <= 32 else (64 if v <= 64 else 128)
            op.name = (rr(int(l.shape[0])), rr(int(l.shape[-1])))
        return op

    def mm(self, out, lhsT, rhs, start=True, stop=True):
        return self._rec("pe", lambda e: e.matmul(out, lhsT, rhs, start=start, stop=stop), [lhsT, rhs], [out])

    def transpose(self, out, in_, ident):
        return self._rec("pe", lambda e: e.transpose(out, in_, ident), [in_, ident], [out])

    def act(self, out, in_, func, bias=None, scale=1.0, accum_out=None):
        reads = [in_]
        kw = {}
        if bias is not None:
            kw["bias"] = bias
            if not isinstance(bias, (int, float)):
                reads.append(bias)
        if not isinstance(scale, (int, float)):
            reads.append(scale)
        kw["scale"] = scale
        writes = [out]
        if accum_out is not None:
            kw["accum_out"] = accum_out
            writes.append(accum_out)
        return self._rec("act", lambda e: e.activation(out, in_, func, **kw), reads, writes)

    def tt(self, out, in0, in1, op, eng="dve"):
        return self._rec(eng, lambda e: e.tensor_tensor(out, in0, in1, op), [in0, in1], [out])

    def ts(self, out, in0, s1, s2, op0, op1=None, eng="dve", accum_out=None):
        reads = [in0]
        if not isinstance(s1, (int, float)):
            reads.append(s1)
        if s2 is not None and not isinstance(s2, (int, float)):
            reads.append(s2)
        writes = [out]
        kw = {}
        if accum_out is not None:
            kw["accum_out"] = accum_out
            writes.append(accum_out)
        if op1 is None:
            return self._rec(eng, lambda e: e.tensor_scalar(out, in0, s1, s2, op0, **kw), reads, writes)
        return self._rec(eng, lambda e: e.tensor_scalar(out, in0, s1, s2, op0, op1, **kw), reads, writes)

    def stt(self, out, in0, scalar, in1, op0, op1, eng="dve"):
        reads = [in0, in1]
        if not isinstance(scalar, (int, float)):
            reads.append(scalar)
        return self._rec(eng, lambda e: e.scalar_tensor_tensor(out, in0, scalar, in1, op0, op1), reads, [out])

    def copy(self, out, in_, eng="dve"):
        if eng == "act":
            return self._rec("act", lambda e: e.copy(out, in_), [in_], [out])
        return self._rec(eng, lambda e: e.tensor_copy(out, in_), [in_], [out])

    def recip(self, out, in_):
        return self._rec("dve", lambda e: e.reciprocal(out, in_), [in_], [out])

    def reduce(self, out, in_, op, axis=None, eng="dve"):
        ax = axis if axis is not None else AX.X
        return self._rec(eng, lambda e: e.tensor_reduce(out, in_, ax, op), [in_], [out])

    def memset(self, out, val, eng="dve"):
        return self._rec(eng, lambda e: e.memset(out, val), [], [out])

    def dma(self, out, in_, q="sp"):
        return self._rec(q, lambda e: e.dma_start(out=out, in_=in_), [in_], [out], is_dma=True)

    def emit(self):
        nc = self.nc
        self.barrier()
        from contextlib import ExitStack
        with ExitStack() as st:
            esem = {e: st.enter_context(nc.semaphore("c_" + e)) for e in self.ENGS}
            dsem = {}
            for q, n in self.NDMA.items():
                for i in range(n):
                    dsem[(q, i)] = st.enter_context(nc.semaphore("d_%s%d" % (q, i)))
            for e in self.ENGS:
                v = 0
                for op in self.eng_ops[e]:
                    if (not op.is_dma) and op.need_signal:
                        v += 1
                        op.sig_val = v
            prog = self

            pstate = {}

            def run(ename, eng):
                waited = {}
                for op in prog.eng_ops[ename]:
                    waits = []
                    for e2, k2 in op.deps_eng.items():
                        y = prog.eng_ops[e2][k2]
                        waits.append((("c", e2), esem[e2], y.sig_val))
                    for y in op.deps_dma:
                        waits.append((y.dma_sem, dsem[y.dma_sem], y.dma_val))
                    if op.is_dma and op.dma_prev is not None:
                        waits.append((op.dma_sem, dsem[op.dma_sem], op.dma_prev.dma_val))
                    for key, sem, val in waits:
                        if waited.get(key, 0) >= val:
                            continue
                        waited[key] = val
                        eng.wait_ge(sem, val)
                    if ename == "pe":
                        pass
                        pstate["mode"] = op.name
                    ins = op.fn(eng)
                    if op.is_dma:
                        ins.then_inc(dsem[op.dma_sem], 16)
                    elif op.need_signal:
                        ins.then_inc(esem[ename], 1)

            with nc.Block() as block:
                @block.tensor
                def _(eng):
                    run("pe", eng)

                @block.scalar
                def _(eng):
                    run("act", eng)

                @block.vector
                def _(eng):
                    run("dve", eng)

                @block.gpsimd
                def _(eng):
                    run("pool", eng)

                @block.sync
                def _(eng):
                    run("sp", eng)


T = 2304
NT = 18
KT = 8
EPS = 1e-6
CHUNKS = [(0, 256, 1), (256, 512, 0), (768, 512, 0), (1280, 512, 0), (1792, 512, 0)]


def _bc(ap, pos, n):
    shp = list(ap.shape)
    v = ap.unsqueeze(pos)
    shp.insert(pos, n)
    return v.to_broadcast(shp)


_DBG = {}


class _Rot:
    def __init__(self, items):
        self.items = list(items)
        self.i = 0

    def next(self):
        v = self.items[self.i % len(self.items)]
        self.i += 1
        return v


def build(n_layers=2, stop_at=None, dbg=False, skip=(), sub=None):
    from contextlib import ExitStack
    nc = bass.Bass("TRN2", target_bir_lowering=False)

    def din(name, shape, dt=F32):
        return nc.dram_tensor(name, shape, dt, kind="ExternalInput").ap()

    x_d = din("x", [2048, 1024])
    ctx_d = din("ctx", [256, 1024])
    vecs0_d = din("vecs0", [112, 128])
    vecs1_d = din("vecs1", [46, 128])
    wada_d = din("w_ada", [2, 1024, 6144])
    win_d = din("w_in", [2, 1024, 1824])
    wout_d = din("w_out", [2, 1024, 1024])
    wup_d = din("wup", [2, 2, 33, 128])
    wr_d = din("w_router", [1024, 16])
    brep_d = din("brep", [128, 288])
    need_moe = stop_at is None or stop_at[0] == "I" or stop_at[1] >= 1
    weg_d = weu_d = wed_d = None
    if need_moe:
        weg_d = din("w_exp_gate", [2, 16, 1024, 512])
        weu_d = din("w_exp_up", [2, 16, 1024, 512])
        wed_d = din("w_exp_down", [2, 16, 512, 1024])
    ident_d = din("ident", [128, 128])
    cs64_d = din("cs64", [128, 256], BF16)
    cn_d = din("cn", [2048, 2048], BF16)
    sn_d = din("sn", [2048, 2048], BF16)
    c256_d = din("c256", [256, 256], BF16)
    s256_d = din("s256", [256, 256], BF16)
    ropec_d = din("ropec", [128, T])
    ropes_d = din("ropes", [128, T])
    psw_d = din("psw", [128, 128])
    masks_d = din("masks", [128, 6, 128])
    bo64_d = din("bo64", [128, 128], BF16)
    esel_d = din("esel", [16, 16, 128], BF16)
    y_d = nc.dram_tensor("y", [2048, 1024], F32, kind="ExternalOutput").ap()
    hT_d = nc.dram_tensor("hT_scr", [128, 8, T], BF16).ap()
    dbg_d = None
    if dbg:
        dbg_d = nc.dram_tensor("dbg_x", [8, 128, 8, T], F32, kind="ExternalOutput").ap()

    st = ExitStack()
    with st:
        ARENA_BYTES = 210944
        arena_t = st.enter_context(nc.sbuf_tensor("arena", [128, ARENA_BYTES // 2], BF16))
        astate = {"off": 0, "peak": 0}

        def _release(m):
            astate["off"] = m

        def sb(name, shape, dt, stack=None):
            n = _dsz(dt)
            for d in shape[1:]:
                n *= int(d)
            n = (n + 63) // 64 * 64
            off = astate["off"]
            if stack is not None:
                stack.callback(_release, off)
            assert off + n <= ARENA_BYTES, ("arena overflow", name, off, n)
            astate["off"] = off + n
            astate["peak"] = max(astate["peak"], off + n)
            v = arena_t[:, off // 2:(off + n) // 2]
            if dt != BF16:
                v = v.bitcast(dt)
            tot = 1
            for d in shape[1:]:
                tot *= int(d)
            v = v[:, 0:tot]
            if len(shape) > 2:
                names = ["d%d" % i for i in range(len(shape) - 1)]
                v = v.rearrange("p (%s) -> p %s" % (" ".join(names), " ".join(names)),
                                **{nm: int(s) for nm, s in zip(names, shape[1:])})
            return v

        P = Prog(nc)
        ps = st.enter_context(nc.psum_tensor("ps", [128, 8, 512], F32))
        xT = sb("xT", [128, 8, T], F32)
        stage = sb("stage", [128, 4, 2048], F32)
        ident = sb("ident", [128, 128], F32)
        ones_bf = sb("ones_bf", [128, 128], BF16)
        bo64 = sb("bo64", [128, 128], BF16)
        V0T = sb("V0T", [128, 112], F32)
        V1T = sb("V1T", [128, 46], F32)
        cvec = sb("cvec", [128, 8, 2], F32)
        modT = sb("modT", [128, 2, 48, 2], F32)
        drv = sb("drv", [128, 2, 2, 8, 2], F32)

        P.dma(ident[:], ident_d)
        P.dma(bo64[:], bo64_d)
        P.memset(ones_bf[:], 1.0)
        P.dma(stage[0:112, 0, 0:128], vecs0_d)
        P.dma(stage[0:46, 1, 0:128], vecs1_d)
        P.transpose(ps[:, 0, 0:112], stage[0:112, 0, 0:128], ident[0:112, 0:112])
        P.transpose(ps[:, 1, 0:46], stage[0:46, 1, 0:128], ident[0:46, 0:46])
        P.copy(V0T[:], ps[:, 0, 0:112])
        P.copy(V1T[:], ps[:, 1, 0:46])
        for wh_ in range(2):
            P.act(cvec[:, :, wh_], V0T[:, 8 * wh_:8 * wh_ + 8], AF.Exp, scale=-1.0)
            P.ts(cvec[:, :, wh_], cvec[:, :, wh_], 1.0, None, ALU.add)
            P.recip(cvec[:, :, wh_], cvec[:, :, wh_])
            P.tt(cvec[:, :, wh_], V0T[:, 8 * wh_:8 * wh_ + 8], cvec[:, :, wh_], ALU.mult)

        for l in range(n_layers):
            for s in range(12):
                slot = (s % 2) * 2
                for hlf in range(2):
                    P.dma(stage[:, slot + hlf, :].rearrange("p (k n) -> p k n", k=4),
                          wada_d[l, hlf * 512:(hlf + 1) * 512, s * 512:(s + 1) * 512].rearrange("(k p) n -> p k n", p=128))
                for ft in range(4):
                    jg = s * 4 + ft
                    for k in range(8):
                        wv = stage[:, slot + k // 4, :].rearrange("p (k n) -> p k n", k=4)
                        P.mm(ps[:, 2, 2 * jg:2 * jg + 2], wv[:, k % 4, ft * 128:(ft + 1) * 128], cvec[:, k, :],
                             k == 0, k == 7)
            pv = ps[:, 2, 0:96].rearrange("p (a b) -> p a b", b=2)
            bias = V0T[:, 16 + 48 * l:16 + 48 * (l + 1)]
            P.tt(modT[:, l], pv, _bc(bias, 2, 2), ALU.add)
            for which in range(2):
                sc = modT[:, l, (1 + 3 * which) * 8:(2 + 3 * which) * 8, :]
                nw = V1T[:, (16 * which + 8 * l):(16 * which + 8 * l + 8)]
                P.ts(drv[:, l, which], sc, 1.0, 32.0, ALU.add, ALU.mult)
                P.tt(drv[:, l, which], drv[:, l, which], _bc(nw, 2, 2), ALU.mult)
        P.barrier()

        def A_of(l, which, j, who):
            return drv[:, l, which, j, who:who + 1]

        def B_of(l, which, j, who):
            sec = 0 if which == 0 else 3
            return modT[:, l, sec * 8 + j, who:who + 1]

        def G_of(l, which, j, who):
            sec = 2 if which == 0 else 5
            return modT[:, l, sec * 8 + j, who:who + 1]

        with ExitStack() as s0:
            xin = sb("xin", [128, 2, 1024], F32, s0)
            ev = 0
            for t in range(NT):
                src = ctx_d[t * 128:(t + 1) * 128, :] if t < 2 else x_d[(t - 2) * 128:(t - 1) * 128, :]
                P.dma(xin[:, t % 2, :], src)
                for half in range(2):
                    b = (2 * t + half) % 4 + 3
                    for jj in range(4):
                        j = half * 4 + jj
                        P.transpose(ps[:, b, jj * 128:(jj + 1) * 128], xin[:, t % 2, j * 128:(j + 1) * 128], ident[:])
                    dst = xT[:, half * 4:half * 4 + 4, t * 128:(t + 1) * 128]
                    srcp = ps[:, b, :].rearrange("p (a b) -> p a b", b=128)
                    if ev % 2 == 0:
                        P.copy(dst, srcp)
                    else:
                        P.copy(dst, srcp, eng="act")
                    ev += 1
            P.barrier()

        def norm_chunk(l, which, c0, w, who, sq, xn, rs, bank):
            P.act(sq[:, :, :w], xT[:, :, c0:c0 + w], AF.Square)
            for j in range(8):
                P.mm(ps[:, bank, :w], ones_bf[:], sq[:, j, :w], j == 0, j == 7)
            P.act(rs[:, :w], ps[:, bank, :w], AF.Sqrt, bias=EPS * 1024.0, scale=1.0)
            P.recip(rs[:, :w], rs[:, :w])
            P.tt(xn[:, :, :w], xT[:, :, c0:c0 + w], _bc(rs[:, :w], 1, 8), ALU.mult)

        def load_w(dst_bf, src_rows_by_cols, ncols, slot_rot, eng="pool"):
            kt = dst_bf.shape[1]
            per = max(1, 2048 // ncols)
            k = 0
            while k < kt:
                n = min(per, kt - k)
                slot = slot_rot.next()
                sv = stage[:, slot, 0:n * ncols].rearrange("p (k n) -> p k n", n=ncols)
                P.dma(sv, src_rows_by_cols[k * 128:(k + n) * 128, :].rearrange("(k p) n -> p k n", p=128))
                P.copy(dst_bf[:, k:k + n, :], sv, eng=eng)
                k += n

        srot = _Rot([0, 1, 2, 3])

        def out_proj(l, Wo, mix, nk, chunks, banks):
            for (c0, w, who) in chunks:
                for m in range(8):
                    b = banks.next()
                    for k in range(nk):
                        P.mm(ps[:, b, :w], Wo[:, k, m * 128:(m + 1) * 128], mix[:, k, c0:c0 + w], k == 0, k == nk - 1)
                    P.stt(xT[:, m, c0:c0 + w], ps[:, b, :w], G_of(l, 0, m, who), xT[:, m, c0:c0 + w], ALU.mult, ALU.add)

        def dump_dbg(idx):
            if dbg_d is not None:
                for j in range(8):
                    P.dma(dbg_d[idx, :, j, :], xT[:, j, :])

        def gla_phase(l, last, lchunks):
            with ExitStack() as s1:
                Wo = sb("g_Wo", [128, 2, 1024], BF16, s1)
                wup = sb("g_wup", [128, 2, 128], BF16, s1)
                msk = sb("g_msk", [128, 6, 128], F32, s1)
                qT = sb("g_qT", [128, T], BF16, s1)
                kT = sb("g_kT", [128, T], BF16, s1)
                ktok = sb("g_ktok", [128, NT, 128], BF16, s1)
                vtok = sb("g_vtok", [128, NT, 256], BF16, s1)
                sgT = sb("g_sg", [128, 2, T], BF16, s1)
                gd = sb("g_gd", [128, T], BF16, s1)
                oT = sb("g_oT", [128, 2, T], F32, s1)
                g8 = sb("g_g8", [128, 1], F32, s1)
                rb = _Rot([0, 1, 2, 3, 4, 5, 6, 7])
                P.dma(msk[:], masks_d)
                P.ts(g8[:], V1T[:, 40 + l:41 + l], 8.0, None, ALU.mult)
                load_w(Wo, wout_d[l][256:512, :], 1024, srot)
                sl = srot.next()
                P.dma(stage[0:33, sl, 0:256].rearrange("p (d c) -> p d c", d=2), wup_d[l].rearrange("d r c -> r d c"))
                P.copy(wup[0:33], stage[0:33, sl, 0:256].rearrange("p (d c) -> p d c", d=2))
                P.memset(gd[0:64], 1.0)
                if sub == "E1a":
                    return
                with ExitStack() as s2:
                    Wg = sb("g_W", [128, 8, 800], BF16, s2)
                    hcb = sb("g_hc", [128, 8, 512], BF16, s2)
                    sge = sb("g_sge", [128, 2, 512], F32, s2)
                    load_w(Wg[:, :, 0:512], win_d[l][:, 256:768], 512, srot)
                    load_w(Wg[:, :, 512:800], win_d[l][:, 768:1056], 288, srot)
                    if sub == "E1b":
                        return
                    for ci, (c0, w, who) in enumerate(CHUNKS):
                        P.dma(hcb[:, :, :w], hT_d[:, :, c0:c0 + w])
                        for (m0, msz, kind) in ((0, 128, "q"), (128, 128, "k"), (512, 128, "g0"), (640, 128, "g1"),
                                                (768, 32, "df")):
                            if _DBG.get("kinds") is not None and kind not in _DBG["kinds"]:
                                continue
                            b = rb.next()
                            for k in range(8):
                                P.mm(ps[0:msz, b, :w], Wg[:, k, m0:m0 + msz], hcb[:, k, :w], k == 0, k == 7)
                            if kind == "q":
                                P.ts(qT[:, c0:c0 + w], ps[:, b, :w], float(32.0 ** -0.5), None, ALU.mult)
                            elif kind == "k":
                                P.copy(kT[:, c0:c0 + w], ps[:, b, :w])
                            elif kind in ("g0", "g1"):
                                gi = 0 if kind == "g0" else 1
                                P.act(sge[:, gi, :w], ps[:, b, :w], AF.Exp, scale=-1.0)
                                P.ts(sge[:, gi, :w], sge[:, gi, :w], 1.0, None, ALU.add, eng="pool")
                                P.recip(sge[:, gi, :w], sge[:, gi, :w])
                                P.tt(sgT[:, gi, c0:c0 + w], ps[:, b, :w], sge[:, gi, :w], ALU.mult)
                            else:
                                P.copy(gd[0:32, c0:c0 + w], ps[0:32, b, :w])
                        if sub == "E1c" or _DBG.get("notm"):
                            continue
                        for tt in range(w // 128):
                            tg = c0 // 128 + tt
                            b = rb.next()
                            for k in range(8):
                                P.mm(ps[:, b, 0:384], hcb[:, k, tt * 128:(tt + 1) * 128], Wg[:, k, 128:512], k == 0, k == 7)
                            P.copy(ktok[:, tg, :], ps[:, b, 0:128])
                            P.copy(vtok[:, tg, :], ps[:, b, 128:384], eng="act")
                if sub == "E1":
                    return
                sflat = stage.rearrange("p a b -> p (a b)")
                sp_tok = sflat[:, 0:2304].rearrange("p (t c) -> p t c", c=128)
                sbf = sflat[:, 2304:8192].bitcast(BF16)
                q_t = sbf[:, 0:2304]
                k_t = sbf[:, 2304:4608]
                kk = sbf[:, 4608:6912].rearrange("p (t c) -> p t c", c=128)
                Sb = sbf[:, 6912:9216].rearrange("p (i c) -> p i c", c=64)
                with ExitStack() as s2:
                    te = sb("g_te", [128, 512], F32, s2)
                    ec = sb("g_ec", [128, 512], F32, s2)
                    en = sb("g_en", [128, 512], F32, s2)
                    esf = sb("g_esf", [128, 512], F32, s2)
                    dec = sb("g_dec", [128, 36], F32, s2)
                    S = sb("g_S", [128, 64], F32, s2)
                    attb = sb("g_attb", [128, 2, 4, 128], BF16, s2)
                    groups = [(0, 4), (4, 4), (8, 4), (12, 4), (16, 2)]
                    for d in range(2):
                        for (t0, n) in groups:
                            b = rb.next()
                            for i in range(n):
                                t = t0 + i
                                P.mm(ps[:, b, i * 128:(i + 1) * 128], gd[0:33, t * 128:(t + 1) * 128], wup[0:33, d, :], True, True)
                            P.act(te[:, 0:n * 128], ps[:, b, 0:n * 128], AF.Exp, scale=-1.0)
                            P.act(sp_tok[:, t0:t0 + n, :], te[:, 0:n * 128].rearrange("p (t c) -> p t c", c=128), AF.Ln, bias=1.0)
                        for (t0, n) in groups:
                            bC = rb.next()
                            bS = rb.next()
                            for i in range(n):
                                t = t0 + i
                                P.mm(ps[:, bC, i * 128:(i + 1) * 128], sp_tok[:, t, :], msk[:, d, :], True, True)
                                P.mm(ps[:, bS, i * 128:(i + 1) * 128], msk[:, 2 + d, :], sp_tok[:, t, :], True, True)
                            nn = n * 128
                            cols = slice(t0 * 128, t0 * 128 + nn)
                            P.act(ec[:, 0:nn], ps[:, bC, 0:nn], AF.Exp)
                            P.act(en[:, 0:nn], ps[:, bC, 0:nn], AF.Exp, scale=-1.0)
                            P.act(esf[:, 0:nn], ps[:, bS, 0:nn], AF.Exp)
                            P.tt(q_t[:, cols], qT[:, cols], ec[:, 0:nn], ALU.mult)
                            P.tt(k_t[:, cols], kT[:, cols], en[:, 0:nn], ALU.mult, eng="pool")
                            P.tt(kk[:, t0:t0 + n, :], ktok[:, t0:t0 + n, :], esf[:, 0:nn].rearrange("p (t c) -> p t c", c=128), ALU.mult)
                            ecv = ec[:, 0:nn].rearrange("p (i c) -> p i c", c=64)
                            pick = 63 if d == 0 else 0
                            P.copy(dec[:, 2 * t0:2 * t0 + 2 * n], ecv[:, :, pick], eng="pool")
                        if sub == "E2":
                            continue
                        P.memset(S[:], 0.0)
                        order = list(range(36)) if d == 0 else ([3, 2, 1, 0] + list(range(35, 3, -1)))
                        for ci in order:
                            t, c = ci // 2, ci % 2
                            r0 = 64 * c
                            P.copy(Sb[:, ci, :], S[:], eng="pool")
                            b = rb.next()
                            for h in range(4):
                                o_ap = ps[32 * h:32 * h + 32, b, 0:64]
                                l_ap = kk[r0:r0 + 64, t, 32 * h:32 * h + 32]
                                r_ap = vtok[r0:r0 + 64, t, 64 * h:64 * h + 64]
                                P._rec("pe", (lambda e, o_ap=o_ap, l_ap=l_ap, r_ap=r_ap, r0=r0, h=h:
                                              e.matmul(o_ap, l_ap, r_ap, start=True, stop=True, tile_position=(r0, 32 * h))),
                                       [l_ap, r_ap], [o_ap])
                            P.stt(S[:], S[:], dec[:, ci:ci + 1], ps[:, b, 0:64], ALU.mult, ALU.add)
                        if sub == "E3":
                            continue
                        rbo = _Rot([4, 5, 6, 7])
                        for t in range(NT):
                            if last and t < 2:
                                continue
                            tc_ = slice(t * 128, (t + 1) * 128)
                            bA = 0
                            for h in range(4):
                                o_ap = ps[:, bA + h, 0:128]
                                l_ap = k_t[32 * h:32 * h + 32, tc_]
                                r_ap = q_t[32 * h:32 * h + 32, tc_]
                                P._rec("pe", (lambda e, o_ap=o_ap, l_ap=l_ap, r_ap=r_ap, h=h:
                                              e.matmul(o_ap, l_ap, r_ap, start=True, stop=True, tile_position=(32 * h, 0))),
                                       [l_ap, r_ap], [o_ap])
                            ab = attb[:, t % 2]
                            P.tt(ab, ps[:, bA:bA + 4, 0:128], _bc(msk[:, 4 + d, :], 1, 4), ALU.mult)
                            bO = rbo.next()
                            for h in range(4):
                                po = 64 * (h % 2)
                                cb = (h // 2) * 128
                                o_ap = ps[po:po + 64, bO, cb:cb + 128]
                                l_ap = vtok[:, t, 64 * h:64 * h + 64]
                                r_ap = ab[:, h, :]
                                P._rec("pe", (lambda e, o_ap=o_ap, l_ap=l_ap, r_ap=r_ap, po=po:
                                              e.matmul(o_ap, l_ap, r_ap, start=True, stop=False, tile_position=(0, po))),
                                       [l_ap, r_ap], [o_ap])
                                for c in range(2):
                                    ci = 2 * t + c
                                    o2 = ps[po:po + 64, bO, cb + 64 * c:cb + 64 * c + 64]
                                    l2 = Sb[32 * h:32 * h + 32, ci, :]
                                    r2 = q_t[32 * h:32 * h + 32, t * 128 + 64 * c:t * 128 + 64 * c + 64]
                                    P._rec("pe", (lambda e, o2=o2, l2=l2, r2=r2, h=h, po=po, c=c:
                                                  e.matmul(o2, l2, r2, start=False, stop=(c == 1), tile_position=(32 * h, po))),
                                           [l2, r2], [o2])
                            pso = ps[:, bO, 0:256].rearrange("p (a c) -> p a c", c=128)
                            if d == 0:
                                P.copy(oT[:, :, tc_], pso, eng="act")
                            else:
                                P.tt(oT[:, :, tc_], oT[:, :, tc_], pso, ALU.add)
                    if sub in ("E2", "E3"):
                        return
                    sq = sb("g_sq", [128, 2, 512], BF16, s2)
                    rs = sb("g_rs", [128, 512], F32, s2)
                    tmp = sb("g_tmp", [128, 512], F32, s2)
                    for (c0, w, who) in lchunks:
                        P.act(sq[:, :, :w], oT[:, :, c0:c0 + w], AF.Square)
                        for m in range(2):
                            b = rb.next()
                            P.mm(ps[:, b, :w], bo64[:], sq[:, m, :w], True, True)
                            P.act(rs[:, :w], ps[:, b, :w], AF.Sqrt, bias=EPS * 64.0, scale=1.0)
                            P.recip(rs[:, :w], rs[:, :w])
                            P.stt(tmp[:, :w], oT[:, m, c0:c0 + w], g8[:, 0:1], rs[:, :w], ALU.mult, ALU.mult)
                            P.tt(sgT[:, m, c0:c0 + w], tmp[:, :w], sgT[:, m, c0:c0 + w], ALU.mult, eng="pool")
                out_proj(l, Wo, sgT, 2, lchunks, rb)
                P.barrier()

        def att_phase(l, last, lchunks):
            with ExitStack() as s1:
                Wo = sb("a_Wo", [128, 4, 1024], BF16, s1)
                qr = sb("a_qr", [128, 4, T], BF16, s1)
                kd = sb("a_kd", [128, 2, T], BF16, s1)
                vt = sb("a_vt", [128, NT, 128], BF16, s1)
                g8 = sb("a_g8", [128, 2], F32, s1)
                rb = _Rot([0, 1, 2, 3, 4, 5, 6, 7])
                P.ts(g8[:, 0:1], V1T[:, 42 + l:43 + l], 8.0, None, ALU.mult)
                P.ts(g8[:, 1:2], V1T[:, 44 + l:45 + l], 8.0, None, ALU.mult)
                load_w(Wo, wout_d[l][512:1024, :], 1024, srot)
                with ExitStack() as s2:
                    Wq = sb("a_Wq", [128, 8, 512], BF16, s2)
                    Wk = sb("a_Wk", [128, 8, 256], BF16, s2)
                    Wv = sb("a_Wv", [128, 8, 128], BF16, s2)
                    psw = sb("a_psw", [128, 128], F32, s2)
                    rc = sb("a_rc", [128, T], F32, s2)
                    rsn = sb("a_rs", [128, T], F32, s2)
                    hcb = sb("a_hc", [128, 8, 512], BF16, s2)
                    qg = sb("a_qg", [128, 512], F32, s2)
                    sq = sb("a_sq", [128, 512], BF16, s2)
                    rs = sb("a_rsd", [128, 512], F32, s2)
                    t1 = sb("a_t1", [128, 512], F32, s2)
                    t2 = sb("a_t2", [128, 512], F32, s2)
                    P.dma(psw[:], psw_d)
                    P.dma(rc[:], ropec_d)
                    P.dma(rsn[:], ropes_d)
                    load_w(Wq, win_d[l][:, 1056:1568], 512, srot)
                    load_w(Wv, win_d[l][:, 1696:1824], 128, srot)
                    sl = srot.next()
                    sv = stage[:, sl, :].rearrange("p (k n) -> p k n", n=256)
                    for g in range(2):
                        for r in range(2):
                            P.dma(sv[:, :, (2 * g + r) * 64:(2 * g + r + 1) * 64],
                                  win_d[l][:, 1568 + 64 * g:1568 + 64 * g + 64].rearrange("(k p) n -> p k n", p=128))
                    P.copy(Wk[:], sv, eng="pool")
                    for ci, (c0, w, who) in enumerate(CHUNKS):
                        P.dma(hcb[:, :, :w], hT_d[:, :, c0:c0 + w])
                        for i in range(6):
                            isq = i < 4
                            b0 = rb.next()
                            for k in range(8):
                                wsl = Wq[:, k, i * 128:(i + 1) * 128] if isq else Wk[:, k, (i - 4) * 128:(i - 3) * 128]
                                P.mm(ps[:, b0, :w], wsl, hcb[:, k, :w], k == 0, k == 7)
                            gcol = g8[:, 0:1] if isq else g8[:, 1:2]
                            P.act(qg[:, :w], ps[:, b0, :w], AF.Identity, scale=gcol)
                            P.act(sq[:, :w], ps[:, b0, :w], AF.Square)
                            b1 = rb.next()
                            P.mm(ps[:, b1, :w], bo64[:], sq[:, :w], True, True)
                            P.act(rs[:, :w], ps[:, b1, :w], AF.Sqrt, bias=EPS * 64.0, scale=1.0)
                            P.recip(rs[:, :w], rs[:, :w])
                            b2 = rb.next()
                            P.mm(ps[:, b2, :w], psw[:], qg[:, :w], True, True)
                            P.tt(t1[:, :w], qg[:, :w], rc[:, c0:c0 + w], ALU.mult, eng="pool")
                            P.tt(t2[:, :w], ps[:, b2, :w], rsn[:, c0:c0 + w], ALU.mult)
                            P.tt(t1[:, :w], t1[:, :w], t2[:, :w], ALU.add, eng="pool")
                            dst = qr[:, i, c0:c0 + w] if isq else kd[:, i - 4, c0:c0 + w]
                            P.tt(dst, t1[:, :w], rs[:, :w], ALU.mult)
                        for tt in range(w // 128):
                            tg = c0 // 128 + tt
                            b = rb.next()
                            for k in range(8):
                                P.mm(ps[:, b, 0:128], hcb[:, k, tt * 128:(tt + 1) * 128], Wv[:, k, :], k == 0, k == 7)
                            P.copy(vt[:, tg, :], ps[:, b, 0:128], eng="act")
                with ExitStack() as s2:
                    amix = sb("a_mix", [128, 4, T], BF16, s2)
                    PT = sb("a_PT", [128, 4, 512], BF16, s2)
                    rd = sb("a_rd", [128, 2, 512], F32, s2)
                    rS = _Rot([0, 1, 2, 3])
                    rO = _Rot([(4, 5), (6, 7)])
                    rP = _Rot([0, 1, 2, 3])
                    for (c0, w, who) in lchunks:
                        kts = list(range(2)) if who == 1 else list(range(NT))
                        for h in range(8):
                            g = h // 4
                            qt = h // 2
                            po = 64 * (h % 2)
                            bO, bD = rO.next()
                            for idx, kt in enumerate(kts):
                                bS = rS.next()
                                P.mm(ps[:, bS, :w], kd[po:po + 64, g, kt * 128:(kt + 1) * 128], qr[po:po + 64, qt, c0:c0 + w], True, True)
                                pt = PT[:, rP.next(), :w]
                                P.act(pt, ps[:, bS, :w], AF.Exp, scale=0.125)
                                P.mm(ps[po:po + 64, bO, :w], vt[:, kt, g * 64:(g + 1) * 64], pt, idx == 0, idx == len(kts) - 1)
                                P.mm(ps[po:po + 64, bD, :w], ones_bf[:, 0:64], pt, idx == 0, idx == len(kts) - 1)
                            P.recip(rd[po:po + 64, h % 2, :w], ps[po:po + 64, bD, :w])
                            P.tt(amix[po:po + 64, qt, c0:c0 + w], ps[po:po + 64, bO, :w], rd[po:po + 64, h % 2, :w], ALU.mult)
                    out_proj(l, Wo, amix, 4, lchunks, rb)
                P.barrier()

        def moe_phase(l, last, lchunks):
            with ExitStack() as s1:
                h2T = sb("m_h2T", [128, 8, T], BF16, s1)
                GT = sb("m_GT", [128, T], BF16, s1)
                esel = sb("m_esel", [128, 16, 128], BF16, s1)
                P.dma(esel[0:16], esel_d)
                tiles = list(range(2, NT)) if last else list(range(NT))
                with ExitStack() as s2:
                    sq = sb("m_sq", [128, 8, 512], BF16, s2)
                    xn = sb("m_xn", [128, 8, 512], F32, s2)
                    rs = sb("m_rs", [128, 512], F32, s2)
                    wr = sb("m_wr", [128, 8, 16], F32, s2)
                    brep = sb("m_brep", [128, 288], F32, s2)
                    s_tok = sb("m_s", [128, NT, 16], F32, s2)
                    sel2 = sb("m_sel2", [128, 72, 8], F32, s2)
                    p1 = sb("m_p1", [128, 72, 4], F32, s2)
                    p2 = sb("m_p2", [128, 72, 2], F32, s2)
                    gs = sb("m_gs", [128, 72], F32, s2)
                    gs2 = sb("m_gs2", [128, 72], F32, s2)
                    gmax = sb("m_gmax", [128, NT], F32, s2)
                    oh = sb("m_oh", [128, 72], F32, s2)
                    cnt = sb("m_cnt", [128, 72, 4], F32, s2)
                    c2 = sb("m_c2", [128, 72, 4], F32, s2)
                    wsum = sb("m_wsum", [128, NT], F32, s2)
                    gate = sb("m_gate", [128, NT, 16], F32, s2)
                    P.dma(wr[:], wr_d.rearrange("(k p) e -> p k e", p=128))
                    P.dma(brep[:], brep_d)
                    P.memset(s_tok[:], 0.0)
                    for ci, (c0, w, who) in enumerate(lchunks):
                        norm_chunk(l, 1, c0, w, who, sq, xn, rs, ci % 2)
                        for j in range(8):
                            P.ts(xn[:, j, :w], xn[:, j, :w], A_of(l, 1, j, who), B_of(l, 1, j, who), ALU.mult, ALU.add,
                                 eng=("dve" if j % 2 == 0 else "pool"))
                        P.copy(h2T[:, :, c0:c0 + w], xn[:, :, :w], eng="act")
                        b = 2 + ci % 2
                        for tt in range(w // 128):
                            tg = c0 // 128 + tt
                            for k in range(8):
                                P.mm(ps[:, b, tt * 16:(tt + 1) * 16], xn[:, k, tt * 128:(tt + 1) * 128], wr[:, k, :], k == 0, k == 7)
                        nt_ = w // 128
                        tg0 = c0 // 128
                        P.act(s_tok[:, tg0:tg0 + nt_, :], ps[:, b, 0:nt_ * 16].rearrange("p (t e) -> p t e", e=16), AF.Exp, scale=-1.0)
                        P.ts(s_tok[:, tg0:tg0 + nt_, :], s_tok[:, tg0:tg0 + nt_, :], 1.0, None, ALU.add)
                        P.recip(s_tok[:, tg0:tg0 + nt_, :], s_tok[:, tg0:tg0 + nt_, :])
                    sv = s_tok.rearrange("p t (g e) -> p (t g) e", e=4)
                    bv = brep.rearrange("p (a e) -> p a e", e=4)
                    P.tt(sel2[:, :, 0:4], sv, bv, ALU.add)
                    P.tt(sel2[:, :, 4:8], sv, bv, ALU.add)
                    P.tt(p1[:], sel2[:, :, 0:4], sel2[:, :, 1:5], ALU.add)
                    P.tt(p2[:], sel2[:, :, 0:2], sel2[:, :, 2:4], ALU.add)
                    P.reduce(gs[:], p1[:], ALU.max)
                    P.reduce(gs2[:], p2[:], ALU.max)
                    P.tt(gs[:], gs[:], gs2[:], ALU.max)
                    P.reduce(gmax[:], gs.rearrange("p (t g) -> p t g", g=4), ALU.max)
                    P.tt(oh.rearrange("p (t g) -> p t g", g=4), gs.rearrange("p (t g) -> p t g", g=4), _bc(gmax[:], 2, 4), ALU.is_equal)
                    P.tt(cnt[:], sel2[:, :, 1:5], sel2[:, :, 0:4], ALU.is_gt)
                    P.tt(c2[:], sel2[:, :, 2:6], sel2[:, :, 0:4], ALU.is_gt)
                    P.tt(cnt[:], cnt[:], c2[:], ALU.add)
                    P.tt(c2[:], sel2[:, :, 3:7], sel2[:, :, 0:4], ALU.is_gt)
                    P.tt(cnt[:], cnt[:], c2[:], ALU.add)
                    P.ts(cnt[:], cnt[:], 1.0, None, ALU.is_le)
                    P.tt(cnt[:], cnt[:], _bc(oh[:], 2, 4), ALU.mult)
                    gv = gate.rearrange("p t (g e) -> p (t g) e", e=4)
                    P.tt(gv, sv, cnt[:], ALU.mult)
                    P.reduce(wsum[:], gate[:], ALU.add)
                    P.ts(wsum[:], wsum[:], 1e-30, None, ALU.max)
                    P.recip(wsum[:], wsum[:])
                    P.tt(gate[:], gate[:], _bc(wsum[:], 2, 16), ALU.mult)
                    for t in tiles:
                        b = 4 + (t // 4) % 2
                        P.transpose(ps[0:16, b, (t % 4) * 128:(t % 4 + 1) * 128], gate[:, t, :], ident[:])
                        P.copy(GT[0:16, t * 128:(t + 1) * 128], ps[0:16, b, (t % 4) * 128:(t % 4 + 1) * 128])
                with ExitStack() as s2:
                    wbuf = sb("m_wbuf", [128, 3, 3, 2048], BF16, s2)
                    sg = sb("m_sg", [128, 2, 512], F32, s2)
                    t1 = sb("m_t1", [128, 2, 512], F32, s2)
                    abuf = sb("m_a", [128, 2, 2, 512], BF16, s2)
                    rG = _Rot([0, 1])
                    rU = _Rot([2, 3])
                    rD = _Rot([5, 6, 7])
                    it = 0
                    for e in range(16):
                        for fh in range(2):
                            u = e * 2 + fh
                            ws = u % 3
                            Wg_ = wbuf[:, ws, 0].rearrange("p (k f) -> p k f", f=256)
                            Wu_ = wbuf[:, ws, 1].rearrange("p (k f) -> p k f", f=256)
                            Wd_ = wbuf[:, ws, 2].rearrange("p (k d) -> p k d", d=1024)
                            for mi, (dst, srcw) in enumerate(((Wg_, weg_d[l, e][:, fh * 256:(fh + 1) * 256]),
                                                              (Wu_, weu_d[l, e][:, fh * 256:(fh + 1) * 256]))):
                                sl = srot.next()
                                sv_ = stage[:, sl, :].rearrange("p (k f) -> p k f", f=256)
                                P.dma(sv_, srcw.rearrange("(k p) f -> p k f", p=128))
                                P.copy(dst, sv_, eng="pool")
                            sl = srot.next()
                            sv_ = stage[:, sl, :].rearrange("p (k d) -> p k d", d=1024)
                            P.dma(sv_, wed_d[l, e][fh * 256:(fh + 1) * 256, :].rearrange("(k p) d -> p k d", p=128))
                            P.copy(Wd_, sv_, eng="pool")
                            for (c0, w, who) in lchunks:
                                ab = abuf[:, it % 2]
                                it += 1
                                P.mm(ps[:, 4, :w], esel[0:16, e, :], GT[0:16, c0:c0 + w], True, True)
                                for ft in range(2):
                                    bG = rG.next()
                                    bU = rU.next()
                                    for k in range(8):
                                        P.mm(ps[:, bG, :w], Wg_[:, k, ft * 128:(ft + 1) * 128], h2T[:, k, c0:c0 + w], k == 0, k == 7)
                                    for k in range(8):
                                        P.mm(ps[:, bU, :w], Wu_[:, k, ft * 128:(ft + 1) * 128], h2T[:, k, c0:c0 + w], k == 0, k == 7)
                                    P.act(sg[:, ft, :w], ps[:, bG, :w], AF.Exp, scale=-1.0)
                                    P.ts(sg[:, ft, :w], sg[:, ft, :w], 1.0, None, ALU.add, eng="pool")
                                    P.recip(sg[:, ft, :w], sg[:, ft, :w])
                                    P.tt(sg[:, ft, :w], ps[:, bG, :w], sg[:, ft, :w], ALU.mult)
                                    P.tt(t1[:, ft, :w], sg[:, ft, :w], ps[:, bU, :w], ALU.mult)
                                    P.tt(ab[:, ft, :w], t1[:, ft, :w], ps[:, 4, :w], ALU.mult)
                                for m in range(8):
                                    bD = rD.next()
                                    for ft in range(2):
                                        P.mm(ps[:, bD, :w], Wd_[:, ft, m * 128:(m + 1) * 128], ab[:, ft, :w], ft == 0, ft == 1)
                                    P.stt(xT[:, m, c0:c0 + w], ps[:, bD, :w], G_of(l, 1, m, who), xT[:, m, c0:c0 + w], ALU.mult, ALU.add)
                P.barrier()

        for l in range(n_layers):
            last = (l == n_layers - 1) and (n_layers == 2)
            lchunks = CHUNKS[1:] if last else CHUNKS

            with ExitStack() as s1:
                sq = sb("b_sq", [128, 8, 512], BF16, s1)
                xn = sb("b_xn", [128, 8, 512], F32, s1)
                rs = sb("b_rs", [128, 512], F32, s1)
                hc = sb("b_hc", [128, 2, 8, 512], BF16, s1)
                for ci, (c0, w, who) in enumerate(CHUNKS):
                    norm_chunk(l, 0, c0, w, who, sq, xn, rs, ci % 2)
                    for j in range(8):
                        P.ts(hc[:, ci % 2, j, :w], xn[:, j, :w], A_of(l, 0, j, who), B_of(l, 0, j, who), ALU.mult, ALU.add,
                             eng=("dve" if j % 2 == 0 else "pool"))
                    P.dma(hT_d[:, :, c0:c0 + w], hc[:, ci % 2, :, :w])
                P.barrier()
            if stop_at == ("B", l):
                break

            with ExitStack() as s1:
                Wu = sb("f_Wu", [128, 8, 256], BF16, s1)
                Wo = sb("f_Wo", [128, 2, 1024], BF16, s1)
                cs64 = sb("f_cs64", [128, 256], BF16, s1)
                uT = sb("f_uT", [128, 2, T], BF16, s1)
                ucs = sb("f_ucs", [128, NT, 512], BF16, s1)
                fmix = sb("f_mix", [128, 2, T], BF16, s1)
                hcb = sb("f_hc", [128, 2, 8, 512], BF16, s1)
                tab = sb("f_tab", [128, 1, 2, 16, 512], BF16, s1)
                P.dma(cs64[:], cs64_d)
                load_w(Wu, win_d[l][:, 0:256], 256, srot)
                load_w(Wo, wout_d[l][0:256, :], 1024, srot)
                rb = _Rot([0, 1, 2, 3])
                for ci, (c0, w, who) in enumerate(CHUNKS):
                    P.dma(hcb[:, ci % 2, :, :w], hT_d[:, :, c0:c0 + w])
                    for m in range(2):
                        b = rb.next()
                        for k in range(8):
                            P.mm(ps[:, b, :w], Wu[:, k, m * 128:(m + 1) * 128], hcb[:, ci % 2, k, :w], k == 0, k == 7)
                        P.copy(uT[:, m, c0:c0 + w], ps[:, b, :w], eng=("act" if m == 0 else "dve"))
                t_lo = 2 if last else 0
                for t in range(t_lo, NT):
                    b = rb.next()
                    for j in range(2):
                        P.mm(ps[:, b, j * 256:(j + 1) * 256], uT[:, j, t * 128:(t + 1) * 128], cs64[:], True, True)
                    P.copy(ucs[:, t, :], ps[:, b, :], eng=("act" if t % 2 == 0 else "dve"))
                for pc in range(4):
                    bufi = 0
                    for hh in range(2):
                        P.dma(tab[:, bufi, 0, hh * 8:(hh + 1) * 8], cn_d[hh * 1024:(hh + 1) * 1024, pc * 512:(pc + 1) * 512].rearrange("(t p) c -> p t c", p=128))
                        P.dma(tab[:, bufi, 1, hh * 8:(hh + 1) * 8], sn_d[hh * 1024:(hh + 1) * 1024, pc * 512:(pc + 1) * 512].rearrange("(t p) c -> p t c", p=128))
                    for j in range(2):
                        b = rb.next()
                        for t in range(16):
                            P.mm(ps[:, b, :], ucs[:, 2 + t, j * 256:j * 256 + 128], tab[:, bufi, 0, t, :], t == 0, False)
                            P.mm(ps[:, b, :], ucs[:, 2 + t, j * 256 + 128:j * 256 + 256], tab[:, bufi, 1, t, :], False, t == 15)
                        P.copy(fmix[:, j, 256 + pc * 512:256 + (pc + 1) * 512], ps[:, b, :], eng=("act" if j == 0 else "dve"))
                if not last:
                    c2 = sb("f_c2", [128, 2, 2, 256], BF16, s1)
                    P.dma(c2[:, 0], c256_d.rearrange("(t p) c -> p t c", p=128))
                    P.dma(c2[:, 1], s256_d.rearrange("(t p) c -> p t c", p=128))
                    for j in range(2):
                        b = rb.next()
                        for t in range(2):
                            P.mm(ps[:, b, 0:256], ucs[:, t, j * 256:j * 256 + 128], c2[:, 0, t, :], t == 0, False)
                            P.mm(ps[:, b, 0:256], ucs[:, t, j * 256 + 128:j * 256 + 256], c2[:, 1, t, :], False, t == 1)
                        P.copy(fmix[:, j, 0:256], ps[:, b, 0:256])
                out_proj(l, Wo, fmix, 2, lchunks, rb)
                P.barrier()
            dump_dbg(4 * l + 0)
            if stop_at == ("D", l):
                break

            if 'E' not in skip:
                gla_phase(l, last, lchunks)
            dump_dbg(4 * l + 1)
            if stop_at == ("E", l):
                break

            if 'F' not in skip:
                att_phase(l, last, lchunks)
            dump_dbg(4 * l + 2)
            if stop_at == ("F", l):
                break

            moe_phase(l, last, lchunks)
            dump_dbg(4 * l + 3)
            if stop_at == ("I", l):
                break

        if stop_at is None:
            with ExitStack() as s1:
                sq = sb("o_sq", [128, 8, 512], BF16, s1)
                xn = sb("o_xn", [128, 8, 512], F32, s1)
                rs = sb("o_rs", [128, 512], F32, s1)
                ot = sb("o_ot", [128, 2, 1024], F32, s1)
                fn32 = sb("o_fn", [128, 8], F32, s1)
                P.ts(fn32[:], V1T[:, 32:40], 32.0, None, ALU.mult)
                ev = 0
                for ci, (c0, w, who) in enumerate(CHUNKS[1:]):
                    norm_chunk(0, 0, c0, w, who, sq, xn, rs, 0)
                    for j in range(8):
                        P.ts(xn[:, j, :w], xn[:, j, :w], fn32[:, j:j + 1], None, ALU.mult, eng=("dve" if j % 2 == 0 else "pool"))
                    for tt in range(4):
                        tg = ci * 4 + tt
                        for half in range(2):
                            b = 1 + (2 * tg + half) % 4
                            for jj in range(4):
                                j = half * 4 + jj
                                P.transpose(ps[:, b, jj * 128:(jj + 1) * 128], xn[:, j, tt * 128:(tt + 1) * 128], ident[:])
                            dst = ot[:, tg % 2, half * 512:(half + 1) * 512]
                            if ev % 2 == 0:
                                P.copy(dst, ps[:, b, :])
                            else:
                                P.copy(dst, ps[:, b, :], eng="act")
                            ev += 1
                        P.dma(y_d[tg * 128:(tg + 1) * 128, :], ot[:, tg % 2, :])
        P.emit()
        print("arena peak bytes", astate["peak"], "ops", P.nops)
    return nc


_CONST_CACHE = {}


def _consts():
    if _CONST_CACHE:
        return _CONST_CACHE
    bf = ml_dtypes.bfloat16
    c = {}
    c["ident"] = np.eye(128, dtype=np.float32)
    k = np.arange(64)
    ang = 2 * np.pi * np.outer(k, k) / 64.0
    C64 = np.cos(ang) / 8.0
    S64 = np.sin(ang) / 8.0
    cs = np.zeros((128, 256), np.float64)
    for g in range(2):
        cs[g * 64:(g + 1) * 64, g * 64:(g + 1) * 64] = C64
        cs[g * 64:(g + 1) * 64, 128 + g * 64:128 + (g + 1) * 64] = S64
    c["cs64"] = cs.astype(bf)
    for n, cn, sn in ((2048, "cn", "sn"), (256, "c256", "s256")):
        i = np.arange(n)
        a = 2 * np.pi * (np.outer(i, i) % n) / float(n)
        c[cn] = (np.cos(a) / np.sqrt(n)).astype(bf)
        c[sn] = (-np.sin(a) / np.sqrt(n)).astype(bf)
    inv = 10000.0 ** (-np.arange(16, dtype=np.float64) * 2.0 / 32.0)
    tok = np.arange(2048)
    row = tok // 64
    col = tok % 64
    rc = np.ones((128, T), np.float64)
    rs = np.zeros((128, T), np.float64)
    for hd in range(128):
        a = (hd % 64) // 32
        f = hd % 16
        pos = row if a == 0 else col
        rc[hd, 256:] = np.cos(pos * inv[f])
        rs[hd, 256:] = np.sin(pos * inv[f])
    c["ropec"] = rc.astype(np.float32)
    c["ropes"] = rs.astype(np.float32)
    psw = np.zeros((128, 128), np.float32)
    for hdp in range(128):
        half = (hdp % 32) // 16
        if half == 0:
            psw[hdp + 16, hdp] = -1.0
        else:
            psw[hdp - 16, hdp] = 1.0
    c["psw"] = psw
    j = np.arange(128)[:, None]
    i = np.arange(128)[None, :]
    same = (j // 64) == (i // 64)
    m = np.zeros((128, 6, 128), np.float32)
    m[:, 0, :] = np.where(same & (j <= i), -1.0 / 16.0, 0.0)
    m[:, 1, :] = np.where(same & (j >= i), -1.0 / 16.0, 0.0)
    m[:, 2, :] = np.where(same & (j > i), -1.0 / 16.0, 0.0)
    m[:, 3, :] = np.where(same & (j < i), -1.0 / 16.0, 0.0)
    m[:, 4, :] = np.where(same & (j <= i), 1.0, 0.0)
    m[:, 5, :] = np.where(same & (j >= i), 1.0, 0.0)
    c["masks"] = m
    c["bo64"] = same.astype(np.float32).astype(bf)
    es = np.zeros((16, 16, 128), np.float32)
    for e in range(16):
        es[e, e, :] = 1.0
    c["esel"] = es.astype(bf)
    _CONST_CACHE.update(c)
    return _CONST_CACHE


_NC_CACHE = {}


def _prep_inputs(inputs):
    f = lambda a: np.ascontiguousarray(np.asarray(a, dtype=np.float32))
    x = f(inputs["x"])
    c = f(inputs["c"])
    ctx = f(inputs["ctx"])
    c_ctx = f(inputs["c_ctx"])
    b_ada = f(inputs["b_ada"])
    cst = _consts()
    vecs1 = np.concatenate([
        f(inputs["norm_mix"]).reshape(16, 128),
        f(inputs["norm_ffn"]).reshape(16, 128),
        f(inputs["final_norm"]).reshape(8, 128),
        np.tile(f(inputs["gla_norm"]), (1, 2)),
        np.tile(f(inputs["q_norm"]), (1, 2)),
        np.tile(f(inputs["k_norm"]), (1, 2)),
    ], axis=0)
    wg_ = f(inputs["w_gla_gate_up"])
    bg_ = f(inputs["b_gla_gate"])
    wup = np.zeros((2, 2, 33, 128), np.float32)
    for l_ in range(2):
        for d_ in range(2):
            wup[l_, d_, 16 * d_:16 * d_ + 16, :] = wg_[l_, d_]
            wup[l_, d_, 32, :] = bg_[l_, d_]
    brep = np.ascontiguousarray(np.broadcast_to(np.tile(f(inputs["b_router"]), 18)[None, :], (128, 288)))
    shared = {
        "vecs1": np.ascontiguousarray(vecs1),
        "w_ada": f(inputs["w_ada"]), "w_in": f(inputs["w_in"]), "w_out": f(inputs["w_out"]),
        "wup": np.ascontiguousarray(wup), "w_router": f(inputs["w_router"]), "brep": brep,
        "w_exp_gate": f(inputs["w_exp_gate"]), "w_exp_up": f(inputs["w_exp_up"]), "w_exp_down": f(inputs["w_exp_down"]),
    }
    for k_ in ("ident", "cs64", "cn", "sn", "c256", "s256", "ropec", "ropes", "psw", "masks", "bo64", "esel"):
        shared[k_] = cst[k_]
    in_maps = []
    for b in range(8):
        vecs0 = np.concatenate([c[b].reshape(8, 128), c_ctx.reshape(8, 128),
                                b_ada[0].reshape(48, 128), b_ada[1].reshape(48, 128)], axis=0)
        m = dict(shared)
        m["x"] = x[b]
        m["ctx"] = ctx[b]
        m["vecs0"] = np.ascontiguousarray(vecs0)
        in_maps.append(m)
    return in_maps


def kernel(**inputs):
    in_maps = _prep_inputs(inputs)
    if "nc" not in _NC_CACHE:
        _NC_CACHE["nc"] = build()
    nc = _NC_CACHE["nc"]
    res = run_bass_kernel_spmd(nc, in_maps, core_ids=list(range(8)))
    out = np.stack([np.asarray(r["y"], dtype=np.float32) for r in res.results], axis=0)
    return out
```

```python
import numpy as np
import ml_dtypes
import concourse.bass as bass
import concourse.mybir as mybir
from concourse.bass_utils import run_bass_kernel_spmd

F32 = mybir.dt.float32
BF16 = mybir.dt.bfloat16
AF = mybir.ActivationFunctionType
ALU = mybir.AluOpType
AX = mybir.AxisListType

_DSZ = {F32: 4, BF16: 2}


def _dsz(dt):
    if dt in _DSZ:
        return _DSZ[dt]
    s = str(dt)
    if "32" in s:
        return 4
    if "16" in s:
        return 2
    if "64" in s:
        return 8
    return 1


def _region(ap):
    t = ap.tensor
    name = t.name
    dsz = _dsz(ap.dtype)
    space = str(ap.space)
    off = int(ap.offset)
    if "DRAM" in space.upper() or "HBM" in space.upper():
        ext = 0
        for (s, c) in ap.ap:
            ext += abs(int(s)) * (int(c) - 1)
        return (name, 0, 1, off * dsz, (off + ext + 1) * dsz)
    shape = list(t.shape)
    fsz = 1
    for d in shape[1:]:
        fsz *= int(d)
    p0 = off // fsz
    f0 = off % fsz
    pext = 0
    fext = 0
    for (s, c) in ap.ap:
        s = int(s)
        c = int(c)
        if c <= 1 or s == 0:
            continue
        if s % fsz == 0:
            pext += (s // fsz) * (c - 1)
        else:
            fext += abs(s) * (c - 1)
    b0 = f0 * dsz
    b1 = (f0 + fext + 1) * dsz
    if "PSUM" in space.upper():
        return (name, 0, 128, (b0 // 2048) * 2048, ((b1 + 2047) // 2048) * 2048)
    return (name, p0, p0 + pext + 1, b0, b1)


class _Op:
    __slots__ = ("eng", "fn", "k", "is_dma", "deps_eng", "deps_dma", "need_signal", "sig_val",
                 "dma_sem", "dma_val", "dma_prev", "name")

    def __init__(self, eng, fn, is_dma, name=""):
        self.eng = eng
        self.fn = fn
        self.is_dma = is_dma
        self.k = -1
        self.deps_eng = {}
        self.deps_dma = []
        self.need_signal = False
        self.sig_val = 0
        self.dma_sem = None
        self.dma_val = 0
        self.dma_prev = None
        self.name = name


class Prog:
    ENGS = ("pe", "act", "dve", "pool", "sp")
    NDMA = {"sp": 40, "pool": 8, "act": 8}

    def __init__(self, nc):
        self.nc = nc
        self.eng_ops = {e: [] for e in self.ENGS}
        self.recs = {}
        self.dma_count = {q: 0 for q in self.NDMA}
        self.dma_last = {q: [None] * n for q, n in self.NDMA.items()}
        self.nops = 0

    def _dep(self, x, y):
        if y is x:
            return
        if y.is_dma:
            if y not in x.deps_dma:
                x.deps_dma.append(y)
            return
        if (not x.is_dma) and y.eng == x.eng:
            if x.eng == "pe":
                return
            if len(self.eng_ops[x.eng]) - y.k > 3:
                return
        cur = x.deps_eng.get(y.eng, -1)
        if y.k > cur:
            x.deps_eng[y.eng] = y.k
        y.need_signal = True

    def add(self, eng, fn, reads, writes, is_dma=False, name=""):
        op = _Op(eng, fn, is_dma, name)
        rr = [_region(a) for a in reads if a is not None]
        ww = [_region(a) for a in writes if a is not None]
        for (nm, p0, p1, b0, b1) in rr:
            is_ps = (nm == "ps")
            for rec in self.recs.get(nm, ()):
                if rec[0] < p1 and p0 < rec[1] and rec[2] < b1 and b0 < rec[3]:
                    if rec[4] or (is_ps and rec[5].eng != eng):
                        self._dep(op, rec[5])
        for (nm, p0, p1, b0, b1) in ww:
            for rec in self.recs.get(nm, ()):
                if rec[0] < p1 and p0 < rec[1] and rec[2] < b1 and b0 < rec[3]:
                    self._dep(op, rec[5])
        for (nm, p0, p1, b0, b1) in ww:
            lst = self.recs.setdefault(nm, [])
            lst[:] = [r for r in lst if not (p0 <= r[0] and r[1] <= p1 and b0 <= r[2] and r[3] <= b1)]
            lst.append((p0, p1, b0, b1, True, op))
        for (nm, p0, p1, b0, b1) in rr:
            lst = self.recs.setdefault(nm, [])
            if not is_dma:
                lst[:] = [r for r in lst if not ((not r[4]) and (not r[5].is_dma) and r[5].eng == eng
                                                 and p0 <= r[0] and r[1] <= p1 and b0 <= r[2] and r[3] <= b1)]
            lst.append((p0, p1, b0, b1, False, op))
        op.k = len(self.eng_ops[eng])
        self.eng_ops[eng].append(op)
        if is_dma:
            n = self.NDMA[eng]
            slot = self.dma_count[eng] % n
            self.dma_count[eng] += 1
            prev = self.dma_last[eng][slot]
            op.dma_sem = (eng, slot)
            op.dma_prev = prev
            op.dma_val = (prev.dma_val if prev is not None else 0) + 16
            self.dma_last[eng][slot] = op
        self.nops += 1
        return op

    def barrier(self):
        bop = _Op("sp", lambda e: e.nop(), False, "barrier")
        for e in self.ENGS:
            lst = [o for o in self.eng_ops[e] if not o.is_dma]
            if lst:
                y = lst[-1]
                if e == "sp":
                    continue
                bop.deps_eng[e] = y.k
                y.need_signal = True
        for q in self.NDMA:
            for y in self.dma_last[q]:
                if y is not None:
                    bop.deps_dma.append(y)
        bop.k = len(self.eng_ops["sp"])
        bop.need_signal = True
        self.eng_ops["sp"].append(bop)
        self.recs = {"__barrier__": [(0, 1, 0, 1, True, bop)]}
        self._barrier_op = bop
        self._barrier_seen = set()
        return bop

    def _barrier_dep(self, op):
        b = getattr(self, "_barrier_op", None)
        if b is None or op is b or op.eng == "sp" or op.eng in self._barrier_seen:
            return
        self._barrier_seen.add(op.eng)
        if b.k > op.deps_eng.get("sp", -1):
            op.deps_eng["sp"] = b.k

    def _rec(self, eng, fn, reads, writes, is_dma=False, name=""):
        op = self.add(eng, fn, reads, writes, is_dma, name)
        self._barrier_dep(op)
        if eng == "pe":
            l = reads[0]
            rr = lambda v: 32 if v <= 32 else (64 if v <= 64 else 128)
            op.name = (rr(int(l.shape[0])), rr(int(l.shape[-1])))
        return op

    def mm(self, out, lhsT, rhs, start=True, stop=True):
        return self._rec("pe", lambda e: e.matmul(out, lhsT, rhs, start=start, stop=stop), [lhsT, rhs], [out])

    def transpose(self, out, in_, ident):
        return self._rec("pe", lambda e: e.transpose(out, in_, ident), [in_, ident], [out])

    def act(self, out, in_, func, bias=None, scale=1.0, accum_out=None):
        reads = [in_]
        kw = {}
        if bias is not None:
            kw["bias"] = bias
            if not isinstance(bias, (int, float)):
                reads.append(bias)
        if not isinstance(scale, (int, float)):
            reads.append(scale)
        kw["scale"] = scale
        writes = [out]
        if accum_out is not None:
            kw["accum_out"] = accum_out
            writes.append(accum_out)
        return self._rec("act", lambda e: e.activation(out, in_, func, **kw), reads, writes)

    def tt(self, out, in0, in1, op, eng="dve"):
        return self._rec(eng, lambda e: e.tensor_tensor(out, in0, in1, op), [in0, in1], [out])

    def ts(self, out, in0, s1, s2, op0, op1=None, eng="dve", accum_out=None):
        reads = [in0]
        if not isinstance(s1, (int, float)):
            reads.append(s1)
        if s2 is not None and not isinstance(s2, (int, float)):
            reads.append(s2)
        writes = [out]
        kw = {}
        if accum_out is not None:
            kw["accum_out"] = accum_out
            writes.append(accum_out)
        if op1 is None:
            return self._rec(eng, lambda e: e.tensor_scalar(out, in0, s1, s2, op0, **kw), reads, writes)
        return self._rec(eng, lambda e: e.tensor_scalar(out, in0, s1, s2, op0, op1, **kw), reads, writes)

    def stt(self, out, in0, scalar, in1, op0, op1, eng="dve"):
        reads = [in0, in1]
        if not isinstance(scalar, (int, float)):
            reads.append(scalar)
        return self._rec(eng, lambda e: e.scalar_tensor_tensor(out, in0, scalar, in1, op0, op1), reads, [out])

    def copy(self, out, in_, eng="dve"):
        if eng == "act":
            return self._rec("act", lambda e: e.copy(out, in_), [in_], [out])
        return self._rec(eng, lambda e: e.tensor_copy(out, in_), [in_], [out])

    def recip(self, out, in_):
        return self._rec("dve", lambda e: e.reciprocal(out, in_), [in_], [out])

    def reduce(self, out, in_, op, axis=None, eng="dve"):
        ax = axis if axis is not None else AX.X
        return self._rec(eng, lambda e: e.tensor_reduce(out, in_, ax, op), [in_], [out])

    def memset(self, out, val, eng="dve"):
        return self._rec(eng, lambda e: e.memset(out, val), [], [out])

    def dma(self, out, in_, q="sp"):
        return self._rec(q, lambda e: e.dma_start(out=out, in_=in_), [in_], [out], is_dma=True)

    def emit(self):
        nc = self.nc
        self.barrier()
        from contextlib import ExitStack
        with ExitStack() as st:
            esem = {e: st.enter_context(nc.semaphore("c_" + e)) for e in self.ENGS}
            dsem = {}
            for q, n in self.NDMA.items():
                for i in range(n):
                    dsem[(q, i)] = st.enter_context(nc.semaphore("d_%s%d" % (q, i)))
            for e in self.ENGS:
                v = 0
                for op in self.eng_ops[e]:
                    if (not op.is_dma) and op.need_signal:
                        v += 1
                        op.sig_val = v
            prog = self

            pstate = {}

            def run(ename, eng):
                waited = {}
                for op in prog.eng_ops[ename]:
                    waits = []
                    for e2, k2 in op.deps_eng.items():
                        y = prog.eng_ops[e2][k2]
                        waits.append((("c", e2), esem[e2], y.sig_val))
                    for y in op.deps_dma:
                        waits.append((y.dma_sem, dsem[y.dma_sem], y.dma_val))
                    if op.is_dma and op.dma_prev is not None:
                        waits.append((op.dma_sem, dsem[op.dma_sem], op.dma_prev.dma_val))
                    for key, sem, val in waits:
                        if waited.get(key, 0) >= val:
                            continue
                        waited[key] = val
                        eng.wait_ge(sem, val)
                    if ename == "pe":
                        pass
                        pstate["mode"] = op.name
                    ins = op.fn(eng)
                    if op.is_dma:
                        ins.then_inc(dsem[op.dma_sem], 16)
                    elif op.need_signal:
                        ins.then_inc(esem[ename], 1)

            with nc.Block() as block:
                @block.tensor
                def _(eng):
                    run("pe", eng)

                @block.scalar
                def _(eng):
                    run("act", eng)

                @block.vector
                def _(eng):
                    run("dve", eng)

                @block.gpsimd
                def _(eng):
                    run("pool", eng)

                @block.sync
                def _(eng):
                    run("sp", eng)


T = 2304
NT = 18
KT = 8
EPS = 1e-6
CHUNKS = [(0, 256, 1), (256, 512, 0), (768, 512, 0), (1280, 512, 0), (1792, 512, 0)]


def _bc(ap, pos, n):
    shp = list(ap.shape)
    v = ap.unsqueeze(pos)
    shp.insert(pos, n)
    return v.to_broadcast(shp)


_DBG = {}


class _Rot:
    def __init__(self, items):
        self.items = list(items)
        self.i = 0

    def next(self):
        v = self.items[self.i % len(self.items)]
        self.i += 1
        return v


def build(n_layers=2, stop_at=None, dbg=False, skip=(), sub=None):
    from contextlib import ExitStack
    nc = bass.Bass("TRN2", target_bir_lowering=False)

    def din(name, shape, dt=F32):
        return nc.dram_tensor(name, shape, dt, kind="ExternalInput").ap()

    x_d = din("x", [2048, 1024])
    ctx_d = din("ctx", [256, 1024])
    vecs0_d = din("vecs0", [112, 128])
    vecs1_d = din("vecs1", [46, 128])
    wada_d = din("w_ada", [2, 1024, 6144])
    win_d = din("w_in", [2, 1024, 1824])
    wout_d = din("w_out", [2, 1024, 1024])
    wup_d = din("wup", [2, 2, 33, 128])
    wr_d = din("w_router", [1024, 16])
    brep_d = din("brep", [128, 288])
    need_moe = stop_at is None or stop_at[0] == "I" or stop_at[1] >= 1
    weg_d = weu_d = wed_d = None
    if need_moe:
        weg_d = din("w_exp_gate", [2, 16, 1024, 512])
        weu_d = din("w_exp_up", [2, 16, 1024, 512])
        wed_d = din("w_exp_down", [2, 16, 512, 1024])
    ident_d = din("ident", [128, 128])
    cs64_d = din("cs64", [128, 256], BF16)
    cn_d = din("cn", [2048, 2048], BF16)
    sn_d = din("sn", [2048, 2048], BF16)
    c256_d = din("c256", [256, 256], BF16)
    s256_d = din("s256", [256, 256], BF16)
    ropec_d = din("ropec", [128, T])
    ropes_d = din("ropes", [128, T])
    psw_d = din("psw", [128, 128])
    masks_d = din("masks", [128, 6, 128])
    bo64_d = din("bo64", [128, 128], BF16)
    esel_d = din("esel", [16, 16, 128], BF16)
    y_d = nc.dram_tensor("y", [2048, 1024], F32, kind="ExternalOutput").ap()
    hT_d = nc.dram_tensor("hT_scr", [128, 8, T], BF16).ap()
    dbg_d = None
    if dbg:
        dbg_d = nc.dram_tensor("dbg_x", [8, 128, 8, T], F32, kind="ExternalOutput").ap()

    st = ExitStack()
    with st:
        ARENA_BYTES = 210944
        arena_t = st.enter_context(nc.sbuf_tensor("arena", [128, ARENA_BYTES // 2], BF16))
        astate = {"off": 0, "peak": 0}

        def _release(m):
            astate["off"] = m

        def sb(name, shape, dt, stack=None):
            n = _dsz(dt)
            for d in shape[1:]:
                n *= int(d)
            n = (n + 63) // 64 * 64
            off = astate["off"]
            if stack is not None:
                stack.callback(_release, off)
            assert off + n <= ARENA_BYTES, ("arena overflow", name, off, n)
            astate["off"] = off + n
            astate["peak"] = max(astate["peak"], off + n)
            v = arena_t[:, off // 2:(off + n) // 2]
            if dt != BF16:
                v = v.bitcast(dt)
            tot = 1
            for d in shape[1:]:
                tot *= int(d)
            v = v[:, 0:tot]
            if len(shape) > 2:
                names = ["d%d" % i for i in range(len(shape) - 1)]
                v = v.rearrange("p (%s) -> p %s" % (" ".join(names), " ".join(names)),
                                **{nm: int(s) for nm, s in zip(names, shape[1:])})
            return v

        P = Prog(nc)
        ps = st.enter_context(nc.psum_tensor("ps", [128, 8, 512], F32))
        xT = sb("xT", [128, 8, T], F32)
        stage = sb("stage", [128, 4, 2048], F32)
        ident = sb("ident", [128, 128], F32)
        ones_bf = sb("ones_bf", [128, 128], BF16)
        bo64 = sb("bo64", [128, 128], BF16)
        V0T = sb("V0T", [128, 112], F32)
        V1T = sb("V1T", [128, 46], F32)
        cvec = sb("cvec", [128, 8, 2], F32)
        modT = sb("modT", [128, 2, 48, 2], F32)
        drv = sb("drv", [128, 2, 2, 8, 2], F32)

        P.dma(ident[:], ident_d)
        P.dma(bo64[:], bo64_d)
        P.memset(ones_bf[:], 1.0)
        P.dma(stage[0:112, 0, 0:128], vecs0_d)
        P.dma(stage[0:46, 1, 0:128], vecs1_d)
        P.transpose(ps[:, 0, 0:112], stage[0:112, 0, 0:128], ident[0:112, 0:112])
        P.transpose(ps[:, 1, 0:46], stage[0:46, 1, 0:128], ident[0:46, 0:46])
        P.copy(V0T[:], ps[:, 0, 0:112])
        P.copy(V1T[:], ps[:, 1, 0:46])
        for wh_ in range(2):
            P.act(cvec[:, :, wh_], V0T[:, 8 * wh_:8 * wh_ + 8], AF.Exp, scale=-1.0)
            P.ts(cvec[:, :, wh_], cvec[:, :, wh_], 1.0, None, ALU.add)
            P.recip(cvec[:, :, wh_], cvec[:, :, wh_])
            P.tt(cvec[:, :, wh_], V0T[:, 8 * wh_:8 * wh_ + 8], cvec[:, :, wh_], ALU.mult)

        for l in range(n_layers):
            for s in range(12):
                slot = (s % 2) * 2
                for hlf in range(2):
                    P.dma(stage[:, slot + hlf, :].rearrange("p (k n) -> p k n", k=4),
                          wada_d[l, hlf * 512:(hlf + 1) * 512, s * 512:(s + 1) * 512].rearrange("(k p) n -> p k n", p=128))
                for ft in range(4):
                    jg = s * 4 + ft
                    for k in range(8):
                        wv = stage[:, slot + k // 4, :].rearrange("p (k n) -> p k n", k=4)
                        P.mm(ps[:, 2, 2 * jg:2 * jg + 2], wv[:, k % 4, ft * 128:(ft + 1) * 128], cvec[:, k, :],
                             k == 0, k == 7)
            pv = ps[:, 2, 0:96].rearrange("p (a b) -> p a b", b=2)
            bias = V0T[:, 16 + 48 * l:16 + 48 * (l + 1)]
            P.tt(modT[:, l], pv, _bc(bias, 2, 2), ALU.add)
            for which in range(2):
                sc = modT[:, l, (1 + 3 * which) * 8:(2 + 3 * which) * 8, :]
                nw = V1T[:, (16 * which + 8 * l):(16 * which + 8 * l + 8)]
                P.ts(drv[:, l, which], sc, 1.0, 32.0, ALU.add, ALU.mult)
                P.tt(drv[:, l, which], drv[:, l, which], _bc(nw, 2, 2), ALU.mult)
        P.barrier()

        def A_of(l, which, j, who):
            return drv[:, l, which, j, who:who + 1]

        def B_of(l, which, j, who):
            sec = 0 if which == 0 else 3
            return modT[:, l, sec * 8 + j, who:who + 1]

        def G_of(l, which, j, who):
            sec = 2 if which == 0 else 5
            return modT[:, l, sec * 8 + j, who:who + 1]

        with ExitStack() as s0:
            xin = sb("xin", [128, 2, 1024], F32, s0)
            ev = 0
            for t in range(NT):
                src = ctx_d[t * 128:(t + 1) * 128, :] if t < 2 else x_d[(t - 2) * 128:(t - 1) * 128, :]
                P.dma(xin[:, t % 2, :], src)
                for half in range(2):
                    b = (2 * t + half) % 4 + 3
                    for jj in range(4):
                        j = half * 4 + jj
                        P.transpose(ps[:, b, jj * 128:(jj + 1) * 128], xin[:, t % 2, j * 128:(j + 1) * 128], ident[:])
                    dst = xT[:, half * 4:half * 4 + 4, t * 128:(t + 1) * 128]
                    srcp = ps[:, b, :].rearrange("p (a b) -> p a b", b=128)
                    if ev % 2 == 0:
                        P.copy(dst, srcp)
                    else:
                        P.copy(dst, srcp, eng="act")
                    ev += 1
            P.barrier()

        def norm_chunk(l, which, c0, w, who, sq, xn, rs, bank):
            P.act(sq[:, :, :w], xT[:, :, c0:c0 + w], AF.Square)
            for j in range(8):
                P.mm(ps[:, bank, :w], ones_bf[:], sq[:, j, :w], j == 0, j == 7)
            P.act(rs[:, :w], ps[:, bank, :w], AF.Sqrt, bias=EPS * 1024.0, scale=1.0)
            P.recip(rs[:, :w], rs[:, :w])
            P.tt(xn[:, :, :w], xT[:, :, c0:c0 + w], _bc(rs[:, :w], 1, 8), ALU.mult)

        def load_w(dst_bf, src_rows_by_cols, ncols, slot_rot, eng="pool"):
            kt = dst_bf.shape[1]
            per = max(1, 2048 // ncols)
            k = 0
            while k < kt:
                n = min(per, kt - k)
                slot = slot_rot.next()
                sv = stage[:, slot, 0:n * ncols].rearrange("p (k n) -> p k n", n=ncols)
                P.dma(sv, src_rows_by_cols[k * 128:(k + n) * 128, :].rearrange("(k p) n -> p k n", p=128))
                P.copy(dst_bf[:, k:k + n, :], sv, eng=eng)
                k += n

        srot = _Rot([0, 1, 2, 3])

        def out_proj(l, Wo, mix, nk, chunks, banks):
            for (c0, w, who) in chunks:
                for m in range(8):
                    b = banks.next()
                    for k in range(nk):
                        P.mm(ps[:, b, :w], Wo[:, k, m * 128:(m + 1) * 128], mix[:, k, c0:c0 + w], k == 0, k == nk - 1)
                    P.stt(xT[:, m, c0:c0 + w], ps[:, b, :w], G_of(l, 0, m, who), xT[:, m, c0:c0 + w], ALU.mult, ALU.add)

        def dump_dbg(idx):
            if dbg_d is not None:
                for j in range(8):
                    P.dma(dbg_d[idx, :, j, :], xT[:, j, :])

        def gla_phase(l, last, lchunks):
            with ExitStack() as s1:
                Wo = sb("g_Wo", [128, 2, 1024], BF16, s1)
                wup = sb("g_wup", [128, 2, 128], BF16, s1)
                msk = sb("g_msk", [128, 6, 128], F32, s1)
                qT = sb("g_qT", [128, T], BF16, s1)
                kT = sb("g_kT", [128, T], BF16, s1)
                ktok = sb("g_ktok", [128, NT, 128], BF16, s1)
                vtok = sb("g_vtok", [128, NT, 256], BF16, s1)
                sgT = sb("g_sg", [128, 2, T], BF16, s1)
                gd = sb("g_gd", [128, T], BF16, s1)
                oT = sb("g_oT", [128, 2, T], F32, s1)
                g8 = sb("g_g8", [128, 1], F32, s1)
                rb = _Rot([0, 1, 2, 3, 4, 5, 6, 7])
                P.dma(msk[:], masks_d)
                P.ts(g8[:], V1T[:, 40 + l:41 + l], 8.0, None, ALU.mult)
                load_w(Wo, wout_d[l][256:512, :], 1024, srot)
                sl = srot.next()
                P.dma(stage[0:33, sl, 0:256].rearrange("p (d c) -> p d c", d=2), wup_d[l].rearrange("d r c -> r d c"))
                P.copy(wup[0:33], stage[0:33, sl, 0:256].rearrange("p (d c) -> p d c", d=2))
                P.memset(gd[0:64], 1.0)
                if sub == "E1a":
                    return
                with ExitStack() as s2:
                    Wg = sb("g_W", [128, 8, 800], BF16, s2)
                    hcb = sb("g_hc", [128, 8, 512], BF16, s2)
                    sge = sb("g_sge", [128, 2, 512], F32, s2)
                    load_w(Wg[:, :, 0:512], win_d[l][:, 256:768], 512, srot)
                    load_w(Wg[:, :, 512:800], win_d[l][:, 768:1056], 288, srot)
                    if sub == "E1b":
                        return
                    for ci, (c0, w, who) in enumerate(CHUNKS):
                        P.dma(hcb[:, :, :w], hT_d[:, :, c0:c0 + w])
                        for (m0, msz, kind) in ((0, 128, "q"), (128, 128, "k"), (512, 128, "g0"), (640, 128, "g1"),
                                                (768, 32, "df")):
                            if _DBG.get("kinds") is not None and kind not in _DBG["kinds"]:
                                continue
                            b = rb.next()
                            for k in range(8):
                                P.mm(ps[0:msz, b, :w], Wg[:, k, m0:m0 + msz], hcb[:, k, :w], k == 0, k == 7)
                            if kind == "q":
                                P.ts(qT[:, c0:c0 + w], ps[:, b, :w], float(32.0 ** -0.5), None, ALU.mult)
                            elif kind == "k":
                                P.copy(kT[:, c0:c0 + w], ps[:, b, :w])
                            elif kind in ("g0", "g1"):
                                gi = 0 if kind == "g0" else 1
                                P.act(sge[:, gi, :w], ps[:, b, :w], AF.Exp, scale=-1.0)
                                P.ts(sge[:, gi, :w], sge[:, gi, :w], 1.0, None, ALU.add, eng="pool")
                                P.recip(sge[:, gi, :w], sge[:, gi, :w])
                                P.tt(sgT[:, gi, c0:c0 + w], ps[:, b, :w], sge[:, gi, :w], ALU.mult)
                            else:
                                P.copy(gd[0:32, c0:c0 + w], ps[0:32, b, :w])
                        if sub == "E1c" or _DBG.get("notm"):
                            continue
                        for tt in range(w // 128):
                            tg = c0 // 128 + tt
                            b = rb.next()
                            for k in range(8):
                                P.mm(ps[:, b, 0:384], hcb[:, k, tt * 128:(tt + 1) * 128], Wg[:, k, 128:512], k == 0, k == 7)
                            P.copy(ktok[:, tg, :], ps[:, b, 0:128])
                            P.copy(vtok[:, tg, :], ps[:, b, 128:384], eng="act")
                if sub == "E1":
                    return
                sflat = stage.rearrange("p a b -> p (a b)")
                sp_tok = sflat[:, 0:2304].rearrange("p (t c) -> p t c", c=128)
                sbf = sflat[:, 2304:8192].bitcast(BF16)
                q_t = sbf[:, 0:2304]
                k_t = sbf[:, 2304:4608]
                kk = sbf[:, 4608:6912].rearrange("p (t c) -> p t c", c=128)
                Sb = sbf[:, 6912:9216].rearrange("p (i c) -> p i c", c=64)
                with ExitStack() as s2:
                    te = sb("g_te", [128, 512], F32, s2)
                    ec = sb("g_ec", [128, 512], F32, s2)
                    en = sb("g_en", [128, 512], F32, s2)
                    esf = sb("g_esf", [128, 512], F32, s2)
                    dec = sb("g_dec", [128, 36], F32, s2)
                    S = sb("g_S", [128, 64], F32, s2)
                    attb = sb("g_attb", [128, 2, 4, 128], BF16, s2)
                    groups = [(0, 4), (4, 4), (8, 4), (12, 4), (16, 2)]
                    for d in range(2):
                        for (t0, n) in groups:
                            b = rb.next()
                            for i in range(n):
                                t = t0 + i
                                P.mm(ps[:, b, i * 128:(i + 1) * 128], gd[0:33, t * 128:(t + 1) * 128], wup[0:33, d, :], True, True)
                            P.act(te[:, 0:n * 128], ps[:, b, 0:n * 128], AF.Exp, scale=-1.0)
                            P.act(sp_tok[:, t0:t0 + n, :], te[:, 0:n * 128].rearrange("p (t c) -> p t c", c=128), AF.Ln, bias=1.0)
                        for (t0, n) in groups:
                            bC = rb.next()
                            bS = rb.next()
                            for i in range(n):
                                t = t0 + i
                                P.mm(ps[:, bC, i * 128:(i + 1) * 128], sp_tok[:, t, :], msk[:, d, :], True, True)
                                P.mm(ps[:, bS, i * 128:(i + 1) * 128], msk[:, 2 + d, :], sp_tok[:, t, :], True, True)
                            nn = n * 128
                            cols = slice(t0 * 128, t0 * 128 + nn)
                            P.act(ec[:, 0:nn], ps[:, bC, 0:nn], AF.Exp)
                            P.act(en[:, 0:nn], ps[:, bC, 0:nn], AF.Exp, scale=-1.0)
                            P.act(esf[:, 0:nn], ps[:, bS, 0:nn], AF.Exp)
                            P.tt(q_t[:, cols], qT[:, cols], ec[:, 0:nn], ALU.mult)
                            P.tt(k_t[:, cols], kT[:, cols], en[:, 0:nn], ALU.mult, eng="pool")
                            P.tt(kk[:, t0:t0 + n, :], ktok[:, t0:t0 + n, :], esf[:, 0:nn].rearrange("p (t c) -> p t c", c=128), ALU.mult)
                            ecv = ec[:, 0:nn].rearrange("p (i c) -> p i c", c=64)
                            pick = 63 if d == 0 else 0
                            P.copy(dec[:, 2 * t0:2 * t0 + 2 * n], ecv[:, :, pick], eng="pool")
                        if sub == "E2":
                            continue
                        P.memset(S[:], 0.0)
                        order = list(range(36)) if d == 0 else ([3, 2, 1, 0] + list(range(35, 3, -1)))
                        for ci in order:
                            t, c = ci // 2, ci % 2
                            r0 = 64 * c
                            P.copy(Sb[:, ci, :], S[:], eng="pool")
                            b = rb.next()
                            for h in range(4):
                                o_ap = ps[32 * h:32 * h + 32, b, 0:64]
                                l_ap = kk[r0:r0 + 64, t, 32 * h:32 * h + 32]
                                r_ap = vtok[r0:r0 + 64, t, 64 * h:64 * h + 64]
                                P._rec("pe", (lambda e, o_ap=o_ap, l_ap=l_ap, r_ap=r_ap, r0=r0, h=h:
                                              e.matmul(o_ap, l_ap, r_ap, start=True, stop=True, tile_position=(r0, 32 * h))),
                                       [l_ap, r_ap], [o_ap])
                            P.stt(S[:], S[:], dec[:, ci:ci + 1], ps[:, b, 0:64], ALU.mult, ALU.add)
                        if sub == "E3":
                            continue
                        rbo = _Rot([4, 5, 6, 7])
                        for t in range(NT):
                            if last and t < 2:
                                continue
                            tc_ = slice(t * 128, (t + 1) * 128)
                            bA = 0
                            for h in range(4):
                                o_ap = ps[:, bA + h, 0:128]
                                l_ap = k_t[32 * h:32 * h + 32, tc_]
                                r_ap = q_t[32 * h:32 * h + 32, tc_]
                                P._rec("pe", (lambda e, o_ap=o_ap, l_ap=l_ap, r_ap=r_ap, h=h:
                                              e.matmul(o_ap, l_ap, r_ap, start=True, stop=True, tile_position=(32 * h, 0))),
                                       [l_ap, r_ap], [o_ap])
                            ab = attb[:, t % 2]
                            P.tt(ab, ps[:, bA:bA + 4, 0:128], _bc(msk[:, 4 + d, :], 1, 4), ALU.mult)
                            bO = rbo.next()
                            for h in range(4):
                                po = 64 * (h % 2)
                                cb = (h // 2) * 128
                                o_ap = ps[po:po + 64, bO, cb:cb + 128]
                                l_ap = vtok[:, t, 64 * h:64 * h + 64]
                                r_ap = ab[:, h, :]
                                P._rec("pe", (lambda e, o_ap=o_ap, l_ap=l_ap, r_ap=r_ap, po=po:
                                              e.matmul(o_ap, l_ap, r_ap, start=True, stop=False, tile_position=(0, po))),
                                       [l_ap, r_ap], [o_ap])
                                for c in range(2):
                                    ci = 2 * t + c
                                    o2 = ps[po:po + 64, bO, cb + 64 * c:cb + 64 * c + 64]
                                    l2 = Sb[32 * h:32 * h + 32, ci, :]
                                    r2 = q_t[32 * h:32 * h + 32, t * 128 + 64 * c:t * 128 + 64 * c + 64]
                                    P._rec("pe", (lambda e, o2=o2, l2=l2, r2=r2, h=h, po=po, c=c:
                                                  e.matmul(o2, l2, r2, start=False, stop=(c == 1), tile_position=(32 * h, po))),
                                           [l2, r2], [o2])
                            pso = ps[:, bO, 0:256].rearrange("p (a c) -> p a c", c=128)
                            if d == 0:
                                P.copy(oT[:, :, tc_], pso, eng="act")
                            else:
                                P.tt(oT[:, :, tc_], oT[:, :, tc_], pso, ALU.add)
                    if sub in ("E2", "E3"):
                        return
                    sq = sb("g_sq", [128, 2, 512], BF16, s2)
                    rs = sb("g_rs", [128, 512], F32, s2)
                    tmp = sb("g_tmp", [128, 512], F32, s2)
                    for (c0, w, who) in lchunks:
                        P.act(sq[:, :, :w], oT[:, :, c0:c0 + w], AF.Square)
                        for m in range(2):
                            b = rb.next()
                            P.mm(ps[:, b, :w], bo64[:], sq[:, m, :w], True, True)
                            P.act(rs[:, :w], ps[:, b, :w], AF.Sqrt, bias=EPS * 64.0, scale=1.0)
                            P.recip(rs[:, :w], rs[:, :w])
                            P.stt(tmp[:, :w], oT[:, m, c0:c0 + w], g8[:, 0:1], rs[:, :w], ALU.mult, ALU.mult)
                            P.tt(sgT[:, m, c0:c0 + w], tmp[:, :w], sgT[:, m, c0:c0 + w], ALU.mult, eng="pool")
                out_proj(l, Wo, sgT, 2, lchunks, rb)
                P.barrier()

        def att_phase(l, last, lchunks):
            with ExitStack() as s1:
                Wo = sb("a_Wo", [128, 4, 1024], BF16, s1)
                qr = sb("a_qr", [128, 4, T], BF16, s1)
                kd = sb("a_kd", [128, 2, T], BF16, s1)
                vt = sb("a_vt", [128, NT, 128], BF16, s1)
                g8 = sb("a_g8", [128, 2], F32, s1)
                rb = _Rot([0, 1, 2, 3, 4, 5, 6, 7])
                P.ts(g8[:, 0:1], V1T[:, 42 + l:43 + l], 8.0, None, ALU.mult)
                P.ts(g8[:, 1:2], V1T[:, 44 + l:45 + l], 8.0, None, ALU.mult)
                load_w(Wo, wout_d[l][512:1024, :], 1024, srot)
                with ExitStack() as s2:
                    Wq = sb("a_Wq", [128, 8, 512], BF16, s2)
                    Wk = sb("a_Wk", [128, 8, 256], BF16, s2)
                    Wv = sb("a_Wv", [128, 8, 128], BF16, s2)
                    psw = sb("a_psw", [128, 128], F32, s2)
                    rc = sb("a_rc", [128, T], F32, s2)
                    rsn = sb("a_rs", [128, T], F32, s2)
                    hcb = sb("a_hc", [128, 8, 512], BF16, s2)
                    qg = sb("a_qg", [128, 512], F32, s2)
                    sq = sb("a_sq", [128, 512], BF16, s2)
                    rs = sb("a_rsd", [128, 512], F32, s2)
                    t1 = sb("a_t1", [128, 512], F32, s2)
                    t2 = sb("a_t2", [128, 512], F32, s2)
                    P.dma(psw[:], psw_d)
                    P.dma(rc[:], ropec_d)
                    P.dma(rsn[:], ropes_d)
                    load_w(Wq, win_d[l][:, 1056:1568], 512, srot)
                    load_w(Wv, win_d[l][:, 1696:1824], 128, srot)
                    sl = srot.next()
                    sv = stage[:, sl, :].rearrange("p (k n) -> p k n", n=256)
                    for g in range(2):
                        for r in range(2):
                            P.dma(sv[:, :, (2 * g + r) * 64:(2 * g + r + 1) * 64],
                                  win_d[l][:, 1568 + 64 * g:1568 + 64 * g + 64].rearrange("(k p) n -> p k n", p=128))
                    P.copy(Wk[:], sv, eng="pool")
                    for ci, (c0, w, who) in enumerate(CHUNKS):
                        P.dma(hcb[:, :, :w], hT_d[:, :, c0:c0 + w])
                        for i in range(6):
                            isq = i < 4
                            b0 = rb.next()
                            for k in range(8):
                                wsl = Wq[:, k, i * 128:(i + 1) * 128] if isq else Wk[:, k, (i - 4) * 128:(i - 3) * 128]
                                P.mm(ps[:, b0, :w], wsl, hcb[:, k, :w], k == 0, k == 7)
                            gcol = g8[:, 0:1] if isq else g8[:, 1:2]
                            P.act(qg[:, :w], ps[:, b0, :w], AF.Identity, scale=gcol)
                            P.act(sq[:, :w], ps[:, b0, :w], AF.Square)
                            b1 = rb.next()
                            P.mm(ps[:, b1, :w], bo64[:], sq[:, :w], True, True)
                            P.act(rs[:, :w], ps[:, b1, :w], AF.Sqrt, bias=EPS * 64.0, scale=1.0)
                            P.recip(rs[:, :w], rs[:, :w])
                            b2 = rb.next()
                            P.mm(ps[:, b2, :w], psw[:], qg[:, :w], True, True)
                            P.tt(t1[:, :w], qg[:, :w], rc[:, c0:c0 + w], ALU.mult, eng="pool")
                            P.tt(t2[:, :w], ps[:, b2, :w], rsn[:, c0:c0 + w], ALU.mult)
                            P.tt(t1[:, :w], t1[:, :w], t2[:, :w], ALU.add, eng="pool")
                            dst = qr[:, i, c0:c0 + w] if isq else kd[:, i - 4, c0:c0 + w]
                            P.tt(dst, t1[:, :w], rs[:, :w], ALU.mult)
                        for tt in range(w // 128):
                            tg = c0 // 128 + tt
                            b = rb.next()
                            for k in range(8):
                                P.mm(ps[:, b, 0:128], hcb[:, k, tt * 128:(tt + 1) * 128], Wv[:, k, :], k == 0, k == 7)
                            P.copy(vt[:, tg, :], ps[:, b, 0:128], eng="act")
                with ExitStack() as s2:
                    amix = sb("a_mix", [128, 4, T], BF16, s2)
                    PT = sb("a_PT", [128, 4, 512], BF16, s2)
                    rd = sb("a_rd", [128, 2, 512], F32, s2)
                    rS = _Rot([0, 1, 2, 3])
                    rO = _Rot([(4, 5), (6, 7)])
                    rP = _Rot([0, 1, 2, 3])
                    for (c0, w, who) in lchunks:
                        kts = list(range(2)) if who == 1 else list(range(NT))
                        for h in range(8):
                            g = h // 4
                            qt = h // 2
                            po = 64 * (h % 2)
                            bO, bD = rO.next()
                            sbank = {}

                            def score(kt_):
                                bS_ = rS.next()
                                sbank[kt_] = bS_
                                P.mm(ps[:, bS_, :w], kd[po:po + 64, g, kt_ * 128:(kt_ + 1) * 128], qr[po:po + 64, qt, c0:c0 + w], True, True)

                            for kt_ in kts[:2]:
                                score(kt_)
                            for idx, kt in enumerate(kts):
                                bS = sbank[kt]
                                pt = PT[:, rP.next(), :w]
                                P.act(pt, ps[:, bS, :w], AF.Exp, scale=0.125)
                                if idx + 2 < len(kts):
                                    score(kts[idx + 2])
                                P.mm(ps[po:po + 64, bO, :w], vt[:, kt, g * 64:(g + 1) * 64], pt, idx == 0, idx == len(kts) - 1)
                                P.mm(ps[po:po + 64, bD, :w], ones_bf[:, 0:64], pt, idx == 0, idx == len(kts) - 1)
                            P.recip(rd[po:po + 64, h % 2, :w], ps[po:po + 64, bD, :w])
                            P.tt(amix[po:po + 64, qt, c0:c0 + w], ps[po:po + 64, bO, :w], rd[po:po + 64, h % 2, :w], ALU.mult)
                    out_proj(l, Wo, amix, 4, lchunks, rb)
                P.barrier()

        def moe_phase(l, last, lchunks):
            with ExitStack() as s1:
                h2T = sb("m_h2T", [128, 8, T], BF16, s1)
                GT = sb("m_GT", [128, T], BF16, s1)
                esel = sb("m_esel", [128, 16, 128], BF16, s1)
                P.dma(esel[0:16], esel_d)
                tiles = list(range(2, NT)) if last else list(range(NT))
                with ExitStack() as s2:
                    sq = sb("m_sq", [128, 8, 512], BF16, s2)
                    xn = sb("m_xn", [128, 8, 512], F32, s2)
                    rs = sb("m_rs", [128, 512], F32, s2)
                    wr = sb("m_wr", [128, 8, 16], F32, s2)
                    brep = sb("m_brep", [128, 288], F32, s2)
                    s_tok = sb("m_s", [128, NT, 16], F32, s2)
                    sel2 = sb("m_sel2", [128, 72, 8], F32, s2)
                    p1 = sb("m_p1", [128, 72, 4], F32, s2)
                    p2 = sb("m_p2", [128, 72, 2], F32, s2)
                    gs = sb("m_gs", [128, 72], F32, s2)
                    gs2 = sb("m_gs2", [128, 72], F32, s2)
                    gmax = sb("m_gmax", [128, NT], F32, s2)
                    oh = sb("m_oh", [128, 72], F32, s2)
                    cnt = sb("m_cnt", [128, 72, 4], F32, s2)
                    c2 = sb("m_c2", [128, 72, 4], F32, s2)
                    wsum = sb("m_wsum", [128, NT], F32, s2)
                    gate = sb("m_gate", [128, NT, 16], F32, s2)
                    P.dma(wr[:], wr_d.rearrange("(k p) e -> p k e", p=128))
                    P.dma(brep[:], brep_d)
                    P.memset(s_tok[:], 0.0)
                    for ci, (c0, w, who) in enumerate(lchunks):
                        norm_chunk(l, 1, c0, w, who, sq, xn, rs, ci % 2)
                        for j in range(8):
                            P.ts(xn[:, j, :w], xn[:, j, :w], A_of(l, 1, j, who), B_of(l, 1, j, who), ALU.mult, ALU.add,
                                 eng=("dve" if j % 2 == 0 else "pool"))
                        P.copy(h2T[:, :, c0:c0 + w], xn[:, :, :w], eng="act")
                        b = 2 + ci % 2
                        for tt in range(w // 128):
                            tg = c0 // 128 + tt
                            for k in range(8):
                                P.mm(ps[:, b, tt * 16:(tt + 1) * 16], xn[:, k, tt * 128:(tt + 1) * 128], wr[:, k, :], k == 0, k == 7)
                        nt_ = w // 128
                        tg0 = c0 // 128
                        P.act(s_tok[:, tg0:tg0 + nt_, :], ps[:, b, 0:nt_ * 16].rearrange("p (t e) -> p t e", e=16), AF.Exp, scale=-1.0)
                        P.ts(s_tok[:, tg0:tg0 + nt_, :], s_tok[:, tg0:tg0 + nt_, :], 1.0, None, ALU.add)
                        P.recip(s_tok[:, tg0:tg0 + nt_, :], s_tok[:, tg0:tg0 + nt_, :])
                    sv = s_tok.rearrange("p t (g e) -> p (t g) e", e=4)
                    bv = brep.rearrange("p (a e) -> p a e", e=4)
                    P.tt(sel2[:, :, 0:4], sv, bv, ALU.add)
                    P.tt(sel2[:, :, 4:8], sv, bv, ALU.add)
                    P.tt(p1[:], sel2[:, :, 0:4], sel2[:, :, 1:5], ALU.add)
                    P.tt(p2[:], sel2[:, :, 0:2], sel2[:, :, 2:4], ALU.add)
                    P.reduce(gs[:], p1[:], ALU.max)
                    P.reduce(gs2[:], p2[:], ALU.max)
                    P.tt(gs[:], gs[:], gs2[:], ALU.max)
                    P.reduce(gmax[:], gs.rearrange("p (t g) -> p t g", g=4), ALU.max)
                    P.tt(oh.rearrange("p (t g) -> p t g", g=4), gs.rearrange("p (t g) -> p t g", g=4), _bc(gmax[:], 2, 4), ALU.is_equal)
                    P.tt(cnt[:], sel2[:, :, 1:5], sel2[:, :, 0:4], ALU.is_gt)
                    P.tt(c2[:], sel2[:, :, 2:6], sel2[:, :, 0:4], ALU.is_gt)
                    P.tt(cnt[:], cnt[:], c2[:], ALU.add)
                    P.tt(c2[:], sel2[:, :, 3:7], sel2[:, :, 0:4], ALU.is_gt)
                    P.tt(cnt[:], cnt[:], c2[:], ALU.add)
                    P.ts(cnt[:], cnt[:], 1.0, None, ALU.is_le)
                    P.tt(cnt[:], cnt[:], _bc(oh[:], 2, 4), ALU.mult)
                    gv = gate.rearrange("p t (g e) -> p (t g) e", e=4)
                    P.tt(gv, sv, cnt[:], ALU.mult)
                    P.reduce(wsum[:], gate[:], ALU.add)
                    P.ts(wsum[:], wsum[:], 1e-30, None, ALU.max)
                    P.recip(wsum[:], wsum[:])
                    P.tt(gate[:], gate[:], _bc(wsum[:], 2, 16), ALU.mult)
                    for t in tiles:
                        b = 4 + (t // 4) % 2
                        P.transpose(ps[0:16, b, (t % 4) * 128:(t % 4 + 1) * 128], gate[:, t, :], ident[:])
                        P.copy(GT[0:16, t * 128:(t + 1) * 128], ps[0:16, b, (t % 4) * 128:(t % 4 + 1) * 128])
                with ExitStack() as s2:
                    wbuf = sb("m_wbuf", [128, 3, 3, 2048], BF16, s2)
                    sg = sb("m_sg", [128, 2, 512], F32, s2)
                    t1 = sb("m_t1", [128, 2, 512], F32, s2)
                    abuf = sb("m_a", [128, 2, 2, 512], BF16, s2)
                    rG = _Rot([0, 1])
                    rU = _Rot([2, 3])
                    rD = _Rot([5, 6, 7])
                    it = 0
                    pending = None

                    def emit_down(pd):
                        Wd_p, ab_p, c0p, wp, whop = pd
                        for m in range(8):
                            bD = rD.next()
                            for ft in range(2):
                                P.mm(ps[:, bD, :wp], Wd_p[:, ft, m * 128:(m + 1) * 128], ab_p[:, ft, :wp], ft == 0, ft == 1)
                            P.stt(xT[:, m, c0p:c0p + wp], ps[:, bD, :wp], G_of(l, 1, m, whop), xT[:, m, c0p:c0p + wp], ALU.mult, ALU.add)

                    for e in range(16):
                        for fh in range(2):
                            u = e * 2 + fh
                            ws = u % 3
                            Wg_ = wbuf[:, ws, 0].rearrange("p (k f) -> p k f", f=256)
                            Wu_ = wbuf[:, ws, 1].rearrange("p (k f) -> p k f", f=256)
                            Wd_ = wbuf[:, ws, 2].rearrange("p (k d) -> p k d", d=1024)
                            for mi, (dst, srcw) in enumerate(((Wg_, weg_d[l, e][:, fh * 256:(fh + 1) * 256]),
                                                              (Wu_, weu_d[l, e][:, fh * 256:(fh + 1) * 256]))):
                                sl = srot.next()
                                sv_ = stage[:, sl, :].rearrange("p (k f) -> p k f", f=256)
                                P.dma(sv_, srcw.rearrange("(k p) f -> p k f", p=128))
                                P.copy(dst, sv_, eng="pool")
                            sl = srot.next()
                            sv_ = stage[:, sl, :].rearrange("p (k d) -> p k d", d=1024)
                            P.dma(sv_, wed_d[l, e][fh * 256:(fh + 1) * 256, :].rearrange("(k p) d -> p k d", p=128))
                            P.copy(Wd_, sv_, eng="pool")
                            for (c0, w, who) in lchunks:
                                ab = abuf[:, it % 2]
                                it += 1
                                P.mm(ps[:, 4, :w], esel[0:16, e, :], GT[0:16, c0:c0 + w], True, True)
                                for ft in range(2):
                                    bG = rG.next()
                                    bU = rU.next()
                                    for k in range(8):
                                        P.mm(ps[:, bG, :w], Wg_[:, k, ft * 128:(ft + 1) * 128], h2T[:, k, c0:c0 + w], k == 0, k == 7)
                                    for k in range(8):
                                        P.mm(ps[:, bU, :w], Wu_[:, k, ft * 128:(ft + 1) * 128], h2T[:, k, c0:c0 + w], k == 0, k == 7)
                                    P.act(sg[:, ft, :w], ps[:, bG, :w], AF.Silu)
                                    P.tt(t1[:, ft, :w], sg[:, ft, :w], ps[:, bU, :w], ALU.mult)
                                    P.tt(ab[:, ft, :w], t1[:, ft, :w], ps[:, 4, :w], ALU.mult)
                                if pending is not None:
                                    emit_down(pending)
                                pending = (Wd_, ab, c0, w, who)
                    emit_down(pending)
                P.barrier()

        for l in range(n_layers):
            last = (l == n_layers - 1) and (n_layers == 2)
            lchunks = CHUNKS[1:] if last else CHUNKS

            with ExitStack() as s1:
                sq = sb("b_sq", [128, 8, 512], BF16, s1)
                xn = sb("b_xn", [128, 8, 512], F32, s1)
                rs = sb("b_rs", [128, 512], F32, s1)
                hc = sb("b_hc", [128, 2, 8, 512], BF16, s1)
                for ci, (c0, w, who) in enumerate(CHUNKS):
                    norm_chunk(l, 0, c0, w, who, sq, xn, rs, ci % 2)
                    for j in range(8):
                        P.ts(hc[:, ci % 2, j, :w], xn[:, j, :w], A_of(l, 0, j, who), B_of(l, 0, j, who), ALU.mult, ALU.add,
                             eng=("dve" if j % 2 == 0 else "pool"))
                    P.dma(hT_d[:, :, c0:c0 + w], hc[:, ci % 2, :, :w])
                P.barrier()
            if stop_at == ("B", l):
                break

            with ExitStack() as s1:
                Wu = sb("f_Wu", [128, 8, 256], BF16, s1)
                Wo = sb("f_Wo", [128, 2, 1024], BF16, s1)
                cs64 = sb("f_cs64", [128, 256], BF16, s1)
                uT = sb("f_uT", [128, 2, T], BF16, s1)
                ucs = sb("f_ucs", [128, NT, 512], BF16, s1)
                fmix = sb("f_mix", [128, 2, T], BF16, s1)
                hcb = sb("f_hc", [128, 2, 8, 512], BF16, s1)
                tab = sb("f_tab", [128, 1, 2, 16, 512], BF16, s1)
                P.dma(cs64[:], cs64_d)
                load_w(Wu, win_d[l][:, 0:256], 256, srot)
                load_w(Wo, wout_d[l][0:256, :], 1024, srot)
                rb = _Rot([0, 1, 2, 3])
                for ci, (c0, w, who) in enumerate(CHUNKS):
                    P.dma(hcb[:, ci % 2, :, :w], hT_d[:, :, c0:c0 + w])
                    for m in range(2):
                        b = rb.next()
                        for k in range(8):
                            P.mm(ps[:, b, :w], Wu[:, k, m * 128:(m + 1) * 128], hcb[:, ci % 2, k, :w], k == 0, k == 7)
                        P.copy(uT[:, m, c0:c0 + w], ps[:, b, :w], eng=("act" if m == 0 else "dve"))
                t_lo = 2 if last else 0
                for t in range(t_lo, NT):
                    b = rb.next()
                    for j in range(2):
                        P.mm(ps[:, b, j * 256:(j + 1) * 256], uT[:, j, t * 128:(t + 1) * 128], cs64[:], True, True)
                    P.copy(ucs[:, t, :], ps[:, b, :], eng=("act" if t % 2 == 0 else "dve"))
                for pc in range(4):
                    bufi = 0
                    for hh in range(2):
                        P.dma(tab[:, bufi, 0, hh * 8:(hh + 1) * 8], cn_d[hh * 1024:(hh + 1) * 1024, pc * 512:(pc + 1) * 512].rearrange("(t p) c -> p t c", p=128))
                        P.dma(tab[:, bufi, 1, hh * 8:(hh + 1) * 8], sn_d[hh * 1024:(hh + 1) * 1024, pc * 512:(pc + 1) * 512].rearrange("(t p) c -> p t c", p=128))
                    for j in range(2):
                        b = rb.next()
                        for t in range(16):
                            P.mm(ps[:, b, :], ucs[:, 2 + t, j * 256:j * 256 + 128], tab[:, bufi, 0, t, :], t == 0, False)
                            P.mm(ps[:, b, :], ucs[:, 2 + t, j * 256 + 128:j * 256 + 256], tab[:, bufi, 1, t, :], False, t == 15)
                        P.copy(fmix[:, j, 256 + pc * 512:256 + (pc + 1) * 512], ps[:, b, :], eng=("act" if j == 0 else "dve"))
                if not last:
                    c2 = sb("f_c2", [128, 2, 2, 256], BF16, s1)
                    P.dma(c2[:, 0], c256_d.rearrange("(t p) c -> p t c", p=128))
                    P.dma(c2[:, 1], s256_d.rearrange("(t p) c -> p t c", p=128))
                    for j in range(2):
                        b = rb.next()
                        for t in range(2):
                            P.mm(ps[:, b, 0:256], ucs[:, t, j * 256:j * 256 + 128], c2[:, 0, t, :], t == 0, False)
                            P.mm(ps[:, b, 0:256], ucs[:, t, j * 256 + 128:j * 256 + 256], c2[:, 1, t, :], False, t == 1)
                        P.copy(fmix[:, j, 0:256], ps[:, b, 0:256])
                out_proj(l, Wo, fmix, 2, lchunks, rb)
                P.barrier()
            dump_dbg(4 * l + 0)
            if stop_at == ("D", l):
                break

            if 'E' not in skip:
                gla_phase(l, last, lchunks)
            dump_dbg(4 * l + 1)
            if stop_at == ("E", l):
                break

            if 'F' not in skip:
                att_phase(l, last, lchunks)
            dump_dbg(4 * l + 2)
            if stop_at == ("F", l):
                break

            moe_phase(l, last, lchunks)
            dump_dbg(4 * l + 3)
            if stop_at == ("I", l):
                break

        if stop_at is None:
            with ExitStack() as s1:
                sq = sb("o_sq", [128, 8, 512], BF16, s1)
                xn = sb("o_xn", [128, 8, 512], F32, s1)
                rs = sb("o_rs", [128, 512], F32, s1)
                ot = sb("o_ot", [128, 2, 1024], F32, s1)
                fn32 = sb("o_fn", [128, 8], F32, s1)
                P.ts(fn32[:], V1T[:, 32:40], 32.0, None, ALU.mult)
                ev = 0
                for ci, (c0, w, who) in enumerate(CHUNKS[1:]):
                    norm_chunk(0, 0, c0, w, who, sq, xn, rs, 0)
                    for j in range(8):
                        P.ts(xn[:, j, :w], xn[:, j, :w], fn32[:, j:j + 1], None, ALU.mult, eng=("dve" if j % 2 == 0 else "pool"))
                    for tt in range(4):
                        tg = ci * 4 + tt
                        for half in range(2):
                            b = 1 + (2 * tg + half) % 4
                            for jj in range(4):
                                j = half * 4 + jj
                                P.transpose(ps[:, b, jj * 128:(jj + 1) * 128], xn[:, j, tt * 128:(tt + 1) * 128], ident[:])
                            dst = ot[:, tg % 2, half * 512:(half + 1) * 512]
                            if ev % 2 == 0:
                                P.copy(dst, ps[:, b, :])
                            else:
                                P.copy(dst, ps[:, b, :], eng="act")
                            ev += 1
                        P.dma(y_d[tg * 128:(tg + 1) * 128, :], ot[:, tg % 2, :])
        P.emit()
        print("arena peak bytes", astate["peak"], "ops", P.nops)
    return nc


_CONST_CACHE = {}


def _consts():
    if _CONST_CACHE:
        return _CONST_CACHE
    bf = ml_dtypes.bfloat16
    c = {}
    c["ident"] = np.eye(128, dtype=np.float32)
    k = np.arange(64)
    ang = 2 * np.pi * np.outer(k, k) / 64.0
    C64 = np.cos(ang) / 8.0
    S64 = np.sin(ang) / 8.0
    cs = np.zeros((128, 256), np.float64)
    for g in range(2):
        cs[g * 64:(g + 1) * 64, g * 64:(g + 1) * 64] = C64
        cs[g * 64:(g + 1) * 64, 128 + g * 64:128 + (g + 1) * 64] = S64
    c["cs64"] = cs.astype(bf)
    for n, cn, sn in ((2048, "cn", "sn"), (256, "c256", "s256")):
        i = np.arange(n)
        a = 2 * np.pi * (np.outer(i, i) % n) / float(n)
        c[cn] = (np.cos(a) / np.sqrt(n)).astype(bf)
        c[sn] = (-np.sin(a) / np.sqrt(n)).astype(bf)
    inv = 10000.0 ** (-np.arange(16, dtype=np.float64) * 2.0 / 32.0)
    tok = np.arange(2048)
    row = tok // 64
    col = tok % 64
    rc = np.ones((128, T), np.float64)
    rs = np.zeros((128, T), np.float64)
    for hd in range(128):
        a = (hd % 64) // 32
        f = hd % 16
        pos = row if a == 0 else col
        rc[hd, 256:] = np.cos(pos * inv[f])
        rs[hd, 256:] = np.sin(pos * inv[f])
    c["ropec"] = rc.astype(np.float32)
    c["ropes"] = rs.astype(np.float32)
    psw = np.zeros((128, 128), np.float32)
    for hdp in range(128):
        half = (hdp % 32) // 16
        if half == 0:
            psw[hdp + 16, hdp] = -1.0
        else:
            psw[hdp - 16, hdp] = 1.0
    c["psw"] = psw
    j = np.arange(128)[:, None]
    i = np.arange(128)[None, :]
    same = (j // 64) == (i // 64)
    m = np.zeros((128, 6, 128), np.float32)
    m[:, 0, :] = np.where(same & (j <= i), -1.0 / 16.0, 0.0)
    m[:, 1, :] = np.where(same & (j >= i), -1.0 / 16.0, 0.0)
    m[:, 2, :] = np.where(same & (j > i), -1.0 / 16.0, 0.0)
    m[:, 3, :] = np.where(same & (j < i), -1.0 / 16.0, 0.0)
    m[:, 4, :] = np.where(same & (j <= i), 1.0, 0.0)
    m[:, 5, :] = np.where(same & (j >= i), 1.0, 0.0)
    c["masks"] = m
    c["bo64"] = same.astype(np.float32).astype(bf)
    es = np.zeros((16, 16, 128), np.float32)
    for e in range(16):
        es[e, e, :] = 1.0
    c["esel"] = es.astype(bf)
    _CONST_CACHE.update(c)
    return _CONST_CACHE


_NC_CACHE = {}


def _prep_inputs(inputs):
    f = lambda a: np.ascontiguousarray(np.asarray(a, dtype=np.float32))
    x = f(inputs["x"])
    c = f(inputs["c"])
    ctx = f(inputs["ctx"])
    c_ctx = f(inputs["c_ctx"])
    b_ada = f(inputs["b_ada"])
    cst = _consts()
    vecs1 = np.concatenate([
        f(inputs["norm_mix"]).reshape(16, 128),
        f(inputs["norm_ffn"]).reshape(16, 128),
        f(inputs["final_norm"]).reshape(8, 128),
        np.tile(f(inputs["gla_norm"]), (1, 2)),
        np.tile(f(inputs["q_norm"]), (1, 2)),
        np.tile(f(inputs["k_norm"]), (1, 2)),
    ], axis=0)
    wg_ = f(inputs["w_gla_gate_up"])
    bg_ = f(inputs["b_gla_gate"])
    wup = np.zeros((2, 2, 33, 128), np.float32)
    for l_ in range(2):
        for d_ in range(2):
            wup[l_, d_, 16 * d_:16 * d_ + 16, :] = wg_[l_, d_]
            wup[l_, d_, 32, :] = bg_[l_, d_]
    brep = np.ascontiguousarray(np.broadcast_to(np.tile(f(inputs["b_router"]), 18)[None, :], (128, 288)))
    shared = {
        "vecs1": np.ascontiguousarray(vecs1),
        "w_ada": f(inputs["w_ada"]), "w_in": f(inputs["w_in"]), "w_out": f(inputs["w_out"]),
        "wup": np.ascontiguousarray(wup), "w_router": f(inputs["w_router"]), "brep": brep,
        "w_exp_gate": f(inputs["w_exp_gate"]), "w_exp_up": f(inputs["w_exp_up"]), "w_exp_down": f(inputs["w_exp_down"]),
    }
    for k_ in ("ident", "cs64", "cn", "sn", "c256", "s256", "ropec", "ropes", "psw", "masks", "bo64", "esel"):
        shared[k_] = cst[k_]
    in_maps = []
    for b in range(8):
        vecs0 = np.concatenate([c[b].reshape(8, 128), c_ctx.reshape(8, 128),
                                b_ada[0].reshape(48, 128), b_ada[1].reshape(48, 128)], axis=0)
        m = dict(shared)
        m["x"] = x[b]
        m["ctx"] = ctx[b]
        m["vecs0"] = np.ascontiguousarray(vecs0)
        in_maps.append(m)
    return in_maps


def kernel(**inputs):
    in_maps = _prep_inputs(inputs)
    if "nc" not in _NC_CACHE:
        _NC_CACHE["nc"] = build()
    nc = _NC_CACHE["nc"]
    res = run_bass_kernel_spmd(nc, in_maps, core_ids=list(range(8)))
    out = np.stack([np.asarray(r["y"], dtype=np.float32) for r in res.results], axis=0)
    return out
```

```python
import numpy as np
import ml_dtypes
import concourse.bass as bass
import concourse.mybir as mybir
from concourse.bass_utils import run_bass_kernel_spmd

F32 = mybir.dt.float32
BF16 = mybir.dt.bfloat16
AF = mybir.ActivationFunctionType
ALU = mybir.AluOpType
AX = mybir.AxisListType

_DSZ = {F32: 4, BF16: 2}


def _dsz(dt):
    if dt in _DSZ:
        return _DSZ[dt]
    s = str(dt)
    if "32" in s:
        return 4
    if "16" in s:
        return 2
    if "64" in s:
        return 8
    return 1


def _region(ap):
    t = ap.tensor
    name = t.name
    dsz = _dsz(ap.dtype)
    space = str(ap.space)
    off = int(ap.offset)
    if "DRAM" in space.upper() or "HBM" in space.upper():
        ext = 0
        for (s, c) in ap.ap:
            ext += abs(int(s)) * (int(c) - 1)
        return (name, 0, 1, off * dsz, (off + ext + 1) * dsz)
    shape = list(t.shape)
    fsz = 1
    for d in shape[1:]:
        fsz *= int(d)
    p0 = off // fsz
    f0 = off % fsz
    pext = 0
    fext = 0
    for (s, c) in ap.ap:
        s = int(s)
        c = int(c)
        if c <= 1 or s == 0:
            continue
        if s % fsz == 0:
            pext += (s // fsz) * (c - 1)
        else:
            fext += abs(s) * (c - 1)
    b0 = f0 * dsz
    b1 = (f0 + fext + 1) * dsz
    if "PSUM" in space.upper():
        return (name, 0, 128, (b0 // 2048) * 2048, ((b1 + 2047) // 2048) * 2048)
    return (name, p0, p0 + pext + 1, b0, b1)


class _Op:
    __slots__ = ("eng", "fn", "k", "is_dma", "deps_eng", "deps_dma", "need_signal", "sig_val",
                 "dma_sem", "dma_val", "dma_prev", "name")

    def __init__(self, eng, fn, is_dma, name=""):
        self.eng = eng
        self.fn = fn
        self.is_dma = is_dma
        self.k = -1
        self.deps_eng = {}
        self.deps_dma = []
        self.need_signal = False
        self.sig_val = 0
        self.dma_sem = None
        self.dma_val = 0
        self.dma_prev = None
        self.name = name


class Prog:
    ENGS = ("pe", "act", "dve", "pool", "sp")
    NDMA = {"sp": 40, "pool": 8, "act": 8}

    def __init__(self, nc):
        self.nc = nc
        self.eng_ops = {e: [] for e in self.ENGS}
        self.recs = {}
        self.dma_count = {q: 0 for q in self.NDMA}
        self.dma_last = {q: [None] * n for q, n in self.NDMA.items()}
        self.nops = 0

    def _dep(self, x, y):
        if y is x:
            return
        if y.is_dma:
            if y not in x.deps_dma:
                x.deps_dma.append(y)
            return
        if (not x.is_dma) and y.eng == x.eng:
            if x.eng == "pe":
                return
            if len(self.eng_ops[x.eng]) - y.k > 3:
                return
        cur = x.deps_eng.get(y.eng, -1)
        if y.k > cur:
            x.deps_eng[y.eng] = y.k
        y.need_signal = True

    def add(self, eng, fn, reads, writes, is_dma=False, name=""):
        op = _Op(eng, fn, is_dma, name)
        rr = [_region(a) for a in reads if a is not None]
        ww = [_region(a) for a in writes if a is not None]
        for (nm, p0, p1, b0, b1) in rr:
            is_ps = (nm == "ps")
            for rec in self.recs.get(nm, ()):
                if rec[0] < p1 and p0 < rec[1] and rec[2] < b1 and b0 < rec[3]:
                    if rec[4] or (is_ps and rec[5].eng != eng):
                        self._dep(op, rec[5])
        for (nm, p0, p1, b0, b1) in ww:
            for rec in self.recs.get(nm, ()):
                if rec[0] < p1 and p0 < rec[1] and rec[2] < b1 and b0 < rec[3]:
                    self._dep(op, rec[5])
        for (nm, p0, p1, b0, b1) in ww:
            lst = self.recs.setdefault(nm, [])
            lst[:] = [r for r in lst if not (p0 <= r[0] and r[1] <= p1 and b0 <= r[2] and r[3] <= b1)]
            lst.append((p0, p1, b0, b1, True, op))
        for (nm, p0, p1, b0, b1) in rr:
            lst = self.recs.setdefault(nm, [])
            if not is_dma:
                lst[:] = [r for r in lst if not ((not r[4]) and (not r[5].is_dma) and r[5].eng == eng
                                                 and p0 <= r[0] and r[1] <= p1 and b0 <= r[2] and r[3] <= b1)]
            lst.append((p0, p1, b0, b1, False, op))
        op.k = len(self.eng_ops[eng])
        self.eng_ops[eng].append(op)
        if is_dma:
            n = self.NDMA[eng]
            slot = self.dma_count[eng] % n
            self.dma_count[eng] += 1
            prev = self.dma_last[eng][slot]
            op.dma_sem = (eng, slot)
            op.dma_prev = prev
            op.dma_val = (prev.dma_val if prev is not None else 0) + 16
            self.dma_last[eng][slot] = op
        self.nops += 1
        return op

    def barrier(self):
        bop = _Op("sp", lambda e: e.nop(), False, "barrier")
        for e in self.ENGS:
            lst = [o for o in self.eng_ops[e] if not o.is_dma]
            if lst:
                y = lst[-1]
                if e == "sp":
                    continue
                bop.deps_eng[e] = y.k
                y.need_signal = True
        for q in self.NDMA:
            for y in self.dma_last[q]:
                if y is not None:
                    bop.deps_dma.append(y)
        bop.k = len(self.eng_ops["sp"])
        bop.need_signal = True
        self.eng_ops["sp"].append(bop)
        self.recs = {"__barrier__": [(0, 1, 0, 1, True, bop)]}
        self._barrier_op = bop
        self._barrier_seen = set()
        return bop

    def _barrier_dep(self, op):
        b = getattr(self, "_barrier_op", None)
        if b is None or op is b or op.eng == "sp" or op.eng in self._barrier_seen:
            return
        self._barrier_seen.add(op.eng)
        if b.k > op.deps_eng.get("sp", -1):
            op.deps_eng["sp"] = b.k

    def _rec(self, eng, fn, reads, writes, is_dma=False, name=""):
        op = self.add(eng, fn, reads, writes, is_dma, name)
        self._barrier_dep(op)
        if eng == "pe":
            l = reads[0]
            rr = lambda v: 32 if v <= 32 else (64 if v <= 64 else 128)
            op.name = (rr(int(l.shape[0])), rr(int(l.shape[-1])))
        return op

    def mm(self, out, lhsT, rhs, start=True, stop=True):
        return self._rec("pe", lambda e: e.matmul(out, lhsT, rhs, start=start, stop=stop), [lhsT, rhs], [out])

    def transpose(self, out, in_, ident):
        return self._rec("pe", lambda e: e.transpose(out, in_, ident), [in_, ident], [out])

    def act(self, out, in_, func, bias=None, scale=1.0, accum_out=None):
        reads = [in_]
        kw = {}
        if bias is not None:
            kw["bias"] = bias
            if not isinstance(bias, (int, float)):
                reads.append(bias)
        if not isinstance(scale, (int, float)):
            reads.append(scale)
        kw["scale"] = scale
        writes = [out]
        if accum_out is not None:
            kw["accum_out"] = accum_out
            writes.append(accum_out)
        return self._rec("act", lambda e: e.activation(out, in_, func, **kw), reads, writes)

    def tt(self, out, in0, in1, op, eng="dve"):
        return self._rec(eng, lambda e: e.tensor_tensor(out, in0, in1, op), [in0, in1], [out])

    def ts(self, out, in0, s1, s2, op0, op1=None, eng="dve", accum_out=None):
        reads = [in0]
        if not isinstance(s1, (int, float)):
            reads.append(s1)
        if s2 is not None and not isinstance(s2, (int, float)):
            reads.append(s2)
        writes = [out]
        kw = {}
        if accum_out is not None:
            kw["accum_out"] = accum_out
            writes.append(accum_out)
        if op1 is None:
            return self._rec(eng, lambda e: e.tensor_scalar(out, in0, s1, s2, op0, **kw), reads, writes)
        return self._rec(eng, lambda e: e.tensor_scalar(out, in0, s1, s2, op0, op1, **kw), reads, writes)

    def stt(self, out, in0, scalar, in1, op0, op1, eng="dve"):
        reads = [in0, in1]
        if not isinstance(scalar, (int, float)):
            reads.append(scalar)
        return self._rec(eng, lambda e: e.scalar_tensor_tensor(out, in0, scalar, in1, op0, op1), reads, [out])

    def copy(self, out, in_, eng="dve"):
        if eng == "act":
            return self._rec("act", lambda e: e.copy(out, in_), [in_], [out])
        return self._rec(eng, lambda e: e.tensor_copy(out, in_), [in_], [out])

    def recip(self, out, in_):
        return self._rec("dve", lambda e: e.reciprocal(out, in_), [in_], [out])

    def reduce(self, out, in_, op, axis=None, eng="dve"):
        ax = axis if axis is not None else AX.X
        return self._rec(eng, lambda e: e.tensor_reduce(out, in_, ax, op), [in_], [out])

    def memset(self, out, val, eng="dve"):
        return self._rec(eng, lambda e: e.memset(out, val), [], [out])

    def dma(self, out, in_, q="sp"):
        return self._rec(q, lambda e: e.dma_start(out=out, in_=in_), [in_], [out], is_dma=True)

    def emit(self):
        nc = self.nc
        self.barrier()
        from contextlib import ExitStack
        with ExitStack() as st:
            esem = {e: st.enter_context(nc.semaphore("c_" + e)) for e in self.ENGS}
            dsem = {}
            for q, n in self.NDMA.items():
                for i in range(n):
                    dsem[(q, i)] = st.enter_context(nc.semaphore("d_%s%d" % (q, i)))
            for e in self.ENGS:
                v = 0
                for op in self.eng_ops[e]:
                    if (not op.is_dma) and op.need_signal:
                        v += 1
                        op.sig_val = v
            prog = self

            pstate = {}

            def run(ename, eng):
                waited = {}
                for op in prog.eng_ops[ename]:
                    waits = []
                    for e2, k2 in op.deps_eng.items():
                        y = prog.eng_ops[e2][k2]
                        waits.append((("c", e2), esem[e2], y.sig_val))
                    for y in op.deps_dma:
                        waits.append((y.dma_sem, dsem[y.dma_sem], y.dma_val))
                    if op.is_dma and op.dma_prev is not None:
                        waits.append((op.dma_sem, dsem[op.dma_sem], op.dma_prev.dma_val))
                    for key, sem, val in waits:
                        if waited.get(key, 0) >= val:
                            continue
                        waited[key] = val
                        eng.wait_ge(sem, val)
                    if ename == "pe":
                        pass
                        pstate["mode"] = op.name
                    ins = op.fn(eng)
                    if op.is_dma:
                        ins.then_inc(dsem[op.dma_sem], 16)
                    elif op.need_signal:
                        ins.then_inc(esem[ename], 1)

            with nc.Block() as block:
                @block.tensor
                def _(eng):
                    run("pe", eng)

                @block.scalar
                def _(eng):
                    run("act", eng)

                @block.vector
                def _(eng):
                    run("dve", eng)

                @block.gpsimd
                def _(eng):
                    run("pool", eng)

                @block.sync
                def _(eng):
                    run("sp", eng)


T = 2304
NT = 18
KT = 8
EPS = 1e-6
CHUNKS = [(0, 256, 1), (256, 512, 0), (768, 512, 0), (1280, 512, 0), (1792, 512, 0)]


def _bc(ap, pos, n):
    shp = list(ap.shape)
    v = ap.unsqueeze(pos)
    shp.insert(pos, n)
    return v.to_broadcast(shp)


_DBG = {}


class _Rot:
    def __init__(self, items):
        self.items = list(items)
        self.i = 0

    def next(self):
        v = self.items[self.i % len(self.items)]
        self.i += 1
        return v


def build(n_layers=2, stop_at=None, dbg=False, skip=(), sub=None):
    from contextlib import ExitStack
    nc = bass.Bass("TRN2", target_bir_lowering=False)

    def din(name, shape, dt=F32):
        return nc.dram_tensor(name, shape, dt, kind="ExternalInput").ap()

    x_d = din("x", [2048, 1024])
    ctx_d = din("ctx", [256, 1024])
    vecs0_d = din("vecs0", [112, 128])
    vecs1_d = din("vecs1", [46, 128])
    wada_d = din("w_ada", [2, 1024, 6144])
    win_d = din("w_in", [2, 1024, 1824])
    wout_d = din("w_out", [2, 1024, 1024])
    wup_d = din("wup", [2, 2, 33, 128])
    wr_d = din("w_router", [1024, 16])
    brep_d = din("brep", [128, 288])
    need_moe = stop_at is None or stop_at[0] == "I" or stop_at[1] >= 1
    weg_d = weu_d = wed_d = None
    if need_moe:
        weg_d = din("w_exp_gate", [2, 16, 1024, 512])
        weu_d = din("w_exp_up", [2, 16, 1024, 512])
        wed_d = din("w_exp_down", [2, 16, 512, 1024])
    ident_d = din("ident", [128, 128])
    cs64_d = din("cs64", [128, 256], BF16)
    cn_d = din("cn", [2048, 2048], BF16)
    sn_d = din("sn", [2048, 2048], BF16)
    c256_d = din("c256", [256, 256], BF16)
    s256_d = din("s256", [256, 256], BF16)
    ropec_d = din("ropec", [128, T])
    ropes_d = din("ropes", [128, T])
    psw_d = din("psw", [128, 128])
    masks_d = din("masks", [128, 6, 128])
    bo64_d = din("bo64", [128, 128], BF16)
    esel_d = din("esel", [16, 16, 128], BF16)
    y_d = nc.dram_tensor("y", [2048, 1024], F32, kind="ExternalOutput").ap()
    hT_d = nc.dram_tensor("hT_scr", [128, 8, T], BF16).ap()
    dbg_d = None
    if dbg:
        dbg_d = nc.dram_tensor("dbg_x", [8, 128, 8, T], F32, kind="ExternalOutput").ap()

    st = ExitStack()
    with st:
        ARENA_BYTES = 210944
        arena_t = st.enter_context(nc.sbuf_tensor("arena", [128, ARENA_BYTES // 2], BF16))
        astate = {"off": 0, "peak": 0}

        def _release(m):
            astate["off"] = m

        def sb(name, shape, dt, stack=None):
            n = _dsz(dt)
            for d in shape[1:]:
                n *= int(d)
            n = (n + 63) // 64 * 64
            off = astate["off"]
            if stack is not None:
                stack.callback(_release, off)
            assert off + n <= ARENA_BYTES, ("arena overflow", name, off, n)
            astate["off"] = off + n
            astate["peak"] = max(astate["peak"], off + n)
            v = arena_t[:, off // 2:(off + n) // 2]
            if dt != BF16:
                v = v.bitcast(dt)
            tot = 1
            for d in shape[1:]:
                tot *= int(d)
            v = v[:, 0:tot]
            if len(shape) > 2:
                names = ["d%d" % i for i in range(len(shape) - 1)]
                v = v.rearrange("p (%s) -> p %s" % (" ".join(names), " ".join(names)),
                                **{nm: int(s) for nm, s in zip(names, shape[1:])})
            return v

        P = Prog(nc)
        ps = st.enter_context(nc.psum_tensor("ps", [128, 8, 512], F32))
        xT = sb("xT", [128, 8, T], F32)
        stage = sb("stage", [128, 4, 2048], F32)
        ident = sb("ident", [128, 128], F32)
        ones_bf = sb("ones_bf", [128, 128], BF16)
        bo64 = sb("bo64", [128, 128], BF16)
        V0T = sb("V0T", [128, 112], F32)
        V1T = sb("V1T", [128, 46], F32)
        cvec = sb("cvec", [128, 8, 2], F32)
        modT = sb("modT", [128, 2, 48, 2], F32)
        drv = sb("drv", [128, 2, 2, 8, 2], F32)

        P.dma(ident[:], ident_d)
        P.dma(bo64[:], bo64_d)
        P.memset(ones_bf[:], 1.0)
        P.dma(stage[0:112, 0, 0:128], vecs0_d)
        P.dma(stage[0:46, 1, 0:128], vecs1_d)
        P.transpose(ps[:, 0, 0:112], stage[0:112, 0, 0:128], ident[0:112, 0:112])
        P.transpose(ps[:, 1, 0:46], stage[0:46, 1, 0:128], ident[0:46, 0:46])
        P.copy(V0T[:], ps[:, 0, 0:112])
        P.copy(V1T[:], ps[:, 1, 0:46])
        for wh_ in range(2):
            P.act(cvec[:, :, wh_], V0T[:, 8 * wh_:8 * wh_ + 8], AF.Exp, scale=-1.0)
            P.ts(cvec[:, :, wh_], cvec[:, :, wh_], 1.0, None, ALU.add)
            P.recip(cvec[:, :, wh_], cvec[:, :, wh_])
            P.tt(cvec[:, :, wh_], V0T[:, 8 * wh_:8 * wh_ + 8], cvec[:, :, wh_], ALU.mult)

        for l in range(n_layers):
            for s in range(12):
                slot = (s % 2) * 2
                for hlf in range(2):
                    P.dma(stage[:, slot + hlf, :].rearrange("p (k n) -> p k n", k=4),
                          wada_d[l, hlf * 512:(hlf + 1) * 512, s * 512:(s + 1) * 512].rearrange("(k p) n -> p k n", p=128))
                for ft in range(4):
                    jg = s * 4 + ft
                    for k in range(8):
                        wv = stage[:, slot + k // 4, :].rearrange("p (k n) -> p k n", k=4)
                        P.mm(ps[:, 2, 2 * jg:2 * jg + 2], wv[:, k % 4, ft * 128:(ft + 1) * 128], cvec[:, k, :],
                             k == 0, k == 7)
            pv = ps[:, 2, 0:96].rearrange("p (a b) -> p a b", b=2)
            bias = V0T[:, 16 + 48 * l:16 + 48 * (l + 1)]
            P.tt(modT[:, l], pv, _bc(bias, 2, 2), ALU.add)
            for which in range(2):
                sc = modT[:, l, (1 + 3 * which) * 8:(2 + 3 * which) * 8, :]
                nw = V1T[:, (16 * which + 8 * l):(16 * which + 8 * l + 8)]
                P.ts(drv[:, l, which], sc, 1.0, 32.0, ALU.add, ALU.mult)
                P.tt(drv[:, l, which], drv[:, l, which], _bc(nw, 2, 2), ALU.mult)
        P.barrier()

        def A_of(l, which, j, who):
            return drv[:, l, which, j, who:who + 1]

        def B_of(l, which, j, who):
            sec = 0 if which == 0 else 3
            return modT[:, l, sec * 8 + j, who:who + 1]

        def G_of(l, which, j, who):
            sec = 2 if which == 0 else 5
            return modT[:, l, sec * 8 + j, who:who + 1]

        with ExitStack() as s0:
            xin = sb("xin", [128, 2, 1024], F32, s0)
            ev = 0
            for t in range(NT):
                src = ctx_d[t * 128:(t + 1) * 128, :] if t < 2 else x_d[(t - 2) * 128:(t - 1) * 128, :]
                P.dma(xin[:, t % 2, :], src)
                for half in range(2):
                    b = (2 * t + half) % 4 + 3
                    for jj in range(4):
                        j = half * 4 + jj
                        P.transpose(ps[:, b, jj * 128:(jj + 1) * 128], xin[:, t % 2, j * 128:(j + 1) * 128], ident[:])
                    dst = xT[:, half * 4:half * 4 + 4, t * 128:(t + 1) * 128]
                    srcp = ps[:, b, :].rearrange("p (a b) -> p a b", b=128)
                    if ev % 2 == 0:
                        P.copy(dst, srcp)
                    else:
                        P.copy(dst, srcp, eng="act")
                    ev += 1
            P.barrier()

        def norm_chunk(l, which, c0, w, who, sq, xn, rs, bank):
            P.act(sq[:, :, :w], xT[:, :, c0:c0 + w], AF.Square)
            for j in range(8):
                P.mm(ps[:, bank, :w], ones_bf[:], sq[:, j, :w], j == 0, j == 7)
            P.act(rs[:, :w], ps[:, bank, :w], AF.Sqrt, bias=EPS * 1024.0, scale=1.0)
            P.recip(rs[:, :w], rs[:, :w])
            P.tt(xn[:, :, :w], xT[:, :, c0:c0 + w], _bc(rs[:, :w], 1, 8), ALU.mult)

        def load_w(dst_bf, src_rows_by_cols, ncols, slot_rot, eng="pool"):
            kt = dst_bf.shape[1]
            per = max(1, 2048 // ncols)
            k = 0
            while k < kt:
                n = min(per, kt - k)
                slot = slot_rot.next()
                sv = stage[:, slot, 0:n * ncols].rearrange("p (k n) -> p k n", n=ncols)
                P.dma(sv, src_rows_by_cols[k * 128:(k + n) * 128, :].rearrange("(k p) n -> p k n", p=128))
                P.copy(dst_bf[:, k:k + n, :], sv, eng=eng)
                k += n

        srot = _Rot([0, 1, 2, 3])

        def out_proj(l, Wo, mix, nk, chunks, banks):
            for (c0, w, who) in chunks:
                for m in range(8):
                    b = banks.next()
                    for k in range(nk):
                        P.mm(ps[:, b, :w], Wo[:, k, m * 128:(m + 1) * 128], mix[:, k, c0:c0 + w], k == 0, k == nk - 1)
                    P.stt(xT[:, m, c0:c0 + w], ps[:, b, :w], G_of(l, 0, m, who), xT[:, m, c0:c0 + w], ALU.mult, ALU.add)

        def dump_dbg(idx):
            if dbg_d is not None:
                for j in range(8):
                    P.dma(dbg_d[idx, :, j, :], xT[:, j, :])

        def gla_phase(l, last, lchunks):
            with ExitStack() as s1:
                Wo = sb("g_Wo", [128, 2, 1024], BF16, s1)
                wup = sb("g_wup", [128, 2, 128], BF16, s1)
                msk = sb("g_msk", [128, 6, 128], F32, s1)
                qT = sb("g_qT", [128, T], BF16, s1)
                kT = sb("g_kT", [128, T], BF16, s1)
                ktok = sb("g_ktok", [128, NT, 128], BF16, s1)
                vtok = sb("g_vtok", [128, NT, 256], BF16, s1)
                sgT = sb("g_sg", [128, 2, T], BF16, s1)
                gd = sb("g_gd", [128, T], BF16, s1)
                oT = sb("g_oT", [128, 2, T], F32, s1)
                g8 = sb("g_g8", [128, 1], F32, s1)
                rb = _Rot([0, 1, 2, 3, 4, 5, 6, 7])
                P.dma(msk[:], masks_d)
                P.ts(g8[:], V1T[:, 40 + l:41 + l], 8.0, None, ALU.mult)
                load_w(Wo, wout_d[l][256:512, :], 1024, srot)
                sl = srot.next()
                P.dma(stage[0:33, sl, 0:256].rearrange("p (d c) -> p d c", d=2), wup_d[l].rearrange("d r c -> r d c"))
                P.copy(wup[0:33], stage[0:33, sl, 0:256].rearrange("p (d c) -> p d c", d=2))
                P.memset(gd[0:64], 1.0)
                if sub == "E1a":
                    return
                with ExitStack() as s2:
                    Wg = sb("g_W", [128, 8, 800], BF16, s2)
                    hcb = sb("g_hc", [128, 8, 512], BF16, s2)
                    sge = sb("g_sge", [128, 2, 512], F32, s2)
                    load_w(Wg[:, :, 0:512], win_d[l][:, 256:768], 512, srot)
                    load_w(Wg[:, :, 512:800], win_d[l][:, 768:1056], 288, srot)
                    if sub == "E1b":
                        return
                    for ci, (c0, w, who) in enumerate(CHUNKS):
                        P.dma(hcb[:, :, :w], hT_d[:, :, c0:c0 + w])
                        for (m0, msz, kind) in ((0, 128, "q"), (128, 128, "k"), (512, 128, "g0"), (640, 128, "g1"),
                                                (768, 32, "df")):
                            if _DBG.get("kinds") is not None and kind not in _DBG["kinds"]:
                                continue
                            b = rb.next()
                            for k in range(8):
                                P.mm(ps[0:msz, b, :w], Wg[:, k, m0:m0 + msz], hcb[:, k, :w], k == 0, k == 7)
                            if kind == "q":
                                P.ts(qT[:, c0:c0 + w], ps[:, b, :w], float(32.0 ** -0.5), None, ALU.mult)
                            elif kind == "k":
                                P.copy(kT[:, c0:c0 + w], ps[:, b, :w])
                            elif kind in ("g0", "g1"):
                                gi = 0 if kind == "g0" else 1
                                P.act(sge[:, gi, :w], ps[:, b, :w], AF.Exp, scale=-1.0)
                                P.ts(sge[:, gi, :w], sge[:, gi, :w], 1.0, None, ALU.add, eng="pool")
                                P.recip(sge[:, gi, :w], sge[:, gi, :w])
                                P.tt(sgT[:, gi, c0:c0 + w], ps[:, b, :w], sge[:, gi, :w], ALU.mult)
                            else:
                                P.copy(gd[0:32, c0:c0 + w], ps[0:32, b, :w])
                        if sub == "E1c" or _DBG.get("notm"):
                            continue
                        for tt in range(w // 128):
                            tg = c0 // 128 + tt
                            b = rb.next()
                            for k in range(8):
                                P.mm(ps[:, b, 0:384], hcb[:, k, tt * 128:(tt + 1) * 128], Wg[:, k, 128:512], k == 0, k == 7)
                            P.copy(ktok[:, tg, :], ps[:, b, 0:128])
                            P.copy(vtok[:, tg, :], ps[:, b, 128:384], eng="act")
                if sub == "E1":
                    return
                sflat = stage.rearrange("p a b -> p (a b)")
                sp_tok = sflat[:, 0:2304].rearrange("p (t c) -> p t c", c=128)
                sbf = sflat[:, 2304:8192].bitcast(BF16)
                q_t = sbf[:, 0:2304]
                k_t = sbf[:, 2304:4608]
                kk = sbf[:, 4608:6912].rearrange("p (t c) -> p t c", c=128)
                Sb = sbf[:, 6912:9216].rearrange("p (i c) -> p i c", c=64)
                with ExitStack() as s2:
                    te = sb("g_te", [128, 512], F32, s2)
                    ec = sb("g_ec", [128, 512], F32, s2)
                    en = sb("g_en", [128, 512], F32, s2)
                    esf = sb("g_esf", [128, 512], F32, s2)
                    dec = sb("g_dec", [128, 36], F32, s2)
                    Sall = sb("g_Sall", [128, 37, 64], F32, s2)
                    attb = sb("g_attb", [128, 2, 4, 128], BF16, s2)
                    groups = [(0, 4), (4, 4), (8, 4), (12, 4), (16, 2)]
                    for d in range(2):
                        for (t0, n) in groups:
                            b = rb.next()
                            for i in range(n):
                                t = t0 + i
                                P.mm(ps[:, b, i * 128:(i + 1) * 128], gd[0:33, t * 128:(t + 1) * 128], wup[0:33, d, :], True, True)
                            P.act(te[:, 0:n * 128], ps[:, b, 0:n * 128], AF.Exp, scale=-1.0)
                            P.act(sp_tok[:, t0:t0 + n, :], te[:, 0:n * 128].rearrange("p (t c) -> p t c", c=128), AF.Ln, bias=1.0)
                        for (t0, n) in groups:
                            bC = rb.next()
                            bS = rb.next()
                            for i in range(n):
                                t = t0 + i
                                P.mm(ps[:, bC, i * 128:(i + 1) * 128], sp_tok[:, t, :], msk[:, d, :], True, True)
                                P.mm(ps[:, bS, i * 128:(i + 1) * 128], msk[:, 2 + d, :], sp_tok[:, t, :], True, True)
                            nn = n * 128
                            cols = slice(t0 * 128, t0 * 128 + nn)
                            P.act(ec[:, 0:nn], ps[:, bC, 0:nn], AF.Exp)
                            P.act(en[:, 0:nn], ps[:, bC, 0:nn], AF.Exp, scale=-1.0)
                            P.act(esf[:, 0:nn], ps[:, bS, 0:nn], AF.Exp)
                            P.tt(q_t[:, cols], qT[:, cols], ec[:, 0:nn], ALU.mult)
                            P.tt(k_t[:, cols], kT[:, cols], en[:, 0:nn], ALU.mult, eng="pool")
                            P.tt(kk[:, t0:t0 + n, :], ktok[:, t0:t0 + n, :], esf[:, 0:nn].rearrange("p (t c) -> p t c", c=128), ALU.mult)
                            ecv = ec[:, 0:nn].rearrange("p (i c) -> p i c", c=64)
                            pick = 63 if d == 0 else 0
                            P.copy(dec[:, 2 * t0:2 * t0 + 2 * n], ecv[:, :, pick], eng="pool")
                        if sub == "E2":
                            continue
                        P.memset(Sall[:, 0, :], 0.0)
                        order = list(range(36)) if d == 0 else ([3, 2, 1, 0] + list(range(35, 3, -1)))
                        pos_of = {ci_: i_ for i_, ci_ in enumerate(order)}
                        for idx_, ci in enumerate(order):
                            t, c = ci // 2, ci % 2
                            r0 = 64 * c
                            b = rb.next()
                            for h in range(4):
                                o_ap = ps[32 * h:32 * h + 32, b, 0:64]
                                l_ap = kk[r0:r0 + 64, t, 32 * h:32 * h + 32]
                                r_ap = vtok[r0:r0 + 64, t, 64 * h:64 * h + 64]
                                P._rec("pe", (lambda e, o_ap=o_ap, l_ap=l_ap, r_ap=r_ap, r0=r0, h=h:
                                              e.matmul(o_ap, l_ap, r_ap, start=True, stop=True, tile_position=(r0, 32 * h))),
                                       [l_ap, r_ap], [o_ap])
                            P.stt(Sall[:, idx_ + 1, :], Sall[:, idx_, :], dec[:, ci:ci + 1], ps[:, b, 0:64], ALU.mult, ALU.add)
                        P.copy(Sb[:, :, :], Sall[:, 0:36, :], eng="pool")
                        if sub == "E3":
                            continue
                        rbo = _Rot([4, 5, 6, 7])
                        for t in range(NT):
                            if last and t < 2:
                                continue
                            tc_ = slice(t * 128, (t + 1) * 128)
                            bA = 0
                            for h in range(4):
                                o_ap = ps[:, bA + h, 0:128]
                                l_ap = k_t[32 * h:32 * h + 32, tc_]
                                r_ap = q_t[32 * h:32 * h + 32, tc_]
                                P._rec("pe", (lambda e, o_ap=o_ap, l_ap=l_ap, r_ap=r_ap, h=h:
                                              e.matmul(o_ap, l_ap, r_ap, start=True, stop=True, tile_position=(32 * h, 0))),
                                       [l_ap, r_ap], [o_ap])
                            ab = attb[:, t % 2]
                            P.tt(ab, ps[:, bA:bA + 4, 0:128], _bc(msk[:, 4 + d, :], 1, 4), ALU.mult)
                            bO = rbo.next()
                            for h in range(4):
                                po = 64 * (h % 2)
                                cb = (h // 2) * 128
                                o_ap = ps[po:po + 64, bO, cb:cb + 128]
                                l_ap = vtok[:, t, 64 * h:64 * h + 64]
                                r_ap = ab[:, h, :]
                                P._rec("pe", (lambda e, o_ap=o_ap, l_ap=l_ap, r_ap=r_ap, po=po:
                                              e.matmul(o_ap, l_ap, r_ap, start=True, stop=False, tile_position=(0, po))),
                                       [l_ap, r_ap], [o_ap])
                                for c in range(2):
                                    ci = 2 * t + c
                                    o2 = ps[po:po + 64, bO, cb + 64 * c:cb + 64 * c + 64]
                                    l2 = Sb[32 * h:32 * h + 32, pos_of[ci], :]
                                    r2 = q_t[32 * h:32 * h + 32, t * 128 + 64 * c:t * 128 + 64 * c + 64]
                                    P._rec("pe", (lambda e, o2=o2, l2=l2, r2=r2, h=h, po=po, c=c:
                                                  e.matmul(o2, l2, r2, start=False, stop=(c == 1), tile_position=(32 * h, po))),
                                           [l2, r2], [o2])
                            pso = ps[:, bO, 0:256].rearrange("p (a c) -> p a c", c=128)
                            if d == 0:
                                P.copy(oT[:, :, tc_], pso, eng="act")
                            else:
                                P.tt(oT[:, :, tc_], oT[:, :, tc_], pso, ALU.add)
                    if sub in ("E2", "E3"):
                        return
                    sq = sb("g_sq", [128, 2, 512], BF16, s2)
                    rs = sb("g_rs", [128, 512], F32, s2)
                    tmp = sb("g_tmp", [128, 512], F32, s2)
                    for (c0, w, who) in lchunks:
                        P.act(sq[:, :, :w], oT[:, :, c0:c0 + w], AF.Square)
                        for m in range(2):
                            b = rb.next()
                            P.mm(ps[:, b, :w], bo64[:], sq[:, m, :w], True, True)
                            P.act(rs[:, :w], ps[:, b, :w], AF.Sqrt, bias=EPS * 64.0, scale=1.0)
                            P.recip(rs[:, :w], rs[:, :w])
                            P.stt(tmp[:, :w], oT[:, m, c0:c0 + w], g8[:, 0:1], rs[:, :w], ALU.mult, ALU.mult)
                            P.tt(sgT[:, m, c0:c0 + w], tmp[:, :w], sgT[:, m, c0:c0 + w], ALU.mult, eng="pool")
                out_proj(l, Wo, sgT, 2, lchunks, rb)
                P.barrier()

        def att_phase(l, last, lchunks):
            with ExitStack() as s1:
                Wo = sb("a_Wo", [128, 4, 1024], BF16, s1)
                qr = sb("a_qr", [128, 4, T], BF16, s1)
                kd = sb("a_kd", [128, 2, T], BF16, s1)
                vt = sb("a_vt", [128, NT, 128], BF16, s1)
                g8 = sb("a_g8", [128, 2], F32, s1)
                rb = _Rot([0, 1, 2, 3, 4, 5, 6, 7])
                P.ts(g8[:, 0:1], V1T[:, 42 + l:43 + l], 8.0, None, ALU.mult)
                P.ts(g8[:, 1:2], V1T[:, 44 + l:45 + l], 8.0, None, ALU.mult)
                load_w(Wo, wout_d[l][512:1024, :], 1024, srot)
                with ExitStack() as s2:
                    Wq = sb("a_Wq", [128, 8, 512], BF16, s2)
                    Wk = sb("a_Wk", [128, 8, 256], BF16, s2)
                    Wv = sb("a_Wv", [128, 8, 128], BF16, s2)
                    psw = sb("a_psw", [128, 128], F32, s2)
                    sflat_a = stage.rearrange("p a b -> p (a b)")
                    rc = sflat_a[:, 0:T]
                    rsn = sflat_a[:, T:2 * T]
                    hcb = sb("a_hc", [128, 8, 512], BF16, s2)
                    qg2 = sb("a_qg", [128, 2, 512], F32, s2)
                    sq2 = sb("a_sq", [128, 2, 512], BF16, s2)
                    rs2 = sb("a_rsd", [128, 2, 512], F32, s2)
                    t12 = sb("a_t1", [128, 2, 512], F32, s2)
                    t22 = sb("a_t2", [128, 2, 512], F32, s2)
                    P.dma(psw[:], psw_d)
                    load_w(Wq, win_d[l][:, 1056:1568], 512, srot)
                    load_w(Wv, win_d[l][:, 1696:1824], 128, srot)
                    sl = srot.next()
                    sv = stage[:, sl, :].rearrange("p (k n) -> p k n", n=256)
                    for g in range(2):
                        for r in range(2):
                            P.dma(sv[:, :, (2 * g + r) * 64:(2 * g + r + 1) * 64],
                                  win_d[l][:, 1568 + 64 * g:1568 + 64 * g + 64].rearrange("(k p) n -> p k n", p=128))
                    P.copy(Wk[:], sv, eng="pool")
                    P.dma(rc, ropec_d)
                    P.dma(rsn, ropes_d)
                    tcnt = 0
                    for ci, (c0, w, who) in enumerate(CHUNKS):
                        P.dma(hcb[:, :, :w], hT_d[:, :, c0:c0 + w])
                        for i in range(6):
                            qg = qg2[:, tcnt % 2]
                            sq = sq2[:, tcnt % 2]
                            rs = rs2[:, tcnt % 2]
                            t1 = t12[:, tcnt % 2]
                            t2 = t22[:, tcnt % 2]
                            tcnt += 1
                            isq = i < 4
                            b0 = rb.next()
                            for k in range(8):
                                wsl = Wq[:, k, i * 128:(i + 1) * 128] if isq else Wk[:, k, (i - 4) * 128:(i - 3) * 128]
                                P.mm(ps[:, b0, :w], wsl, hcb[:, k, :w], k == 0, k == 7)
                            gcol = g8[:, 0:1] if isq else g8[:, 1:2]
                            P.act(qg[:, :w], ps[:, b0, :w], AF.Identity, scale=gcol)
                            P.act(sq[:, :w], ps[:, b0, :w], AF.Square)
                            b1 = rb.next()
                            P.mm(ps[:, b1, :w], bo64[:], sq[:, :w], True, True)
                            P.act(rs[:, :w], ps[:, b1, :w], AF.Sqrt, bias=EPS * 64.0, scale=1.0)
                            P.recip(rs[:, :w], rs[:, :w])
                            b2 = rb.next()
                            P.mm(ps[:, b2, :w], psw[:], qg[:, :w], True, True)
                            P.tt(t1[:, :w], qg[:, :w], rc[:, c0:c0 + w], ALU.mult, eng="pool")
                            P.tt(t2[:, :w], ps[:, b2, :w], rsn[:, c0:c0 + w], ALU.mult)
                            P.tt(t1[:, :w], t1[:, :w], t2[:, :w], ALU.add, eng="pool")
                            dst = qr[:, i, c0:c0 + w] if isq else kd[:, i - 4, c0:c0 + w]
                            P.tt(dst, t1[:, :w], rs[:, :w], ALU.mult)
                        for tt in range(w // 128):
                            tg = c0 // 128 + tt
                            b = rb.next()
                            for k in range(8):
                                P.mm(ps[:, b, 0:128], hcb[:, k, tt * 128:(tt + 1) * 128], Wv[:, k, :], k == 0, k == 7)
                            P.copy(vt[:, tg, :], ps[:, b, 0:128], eng="act")
                with ExitStack() as s2:
                    amix = sb("a_mix", [128, 4, T], BF16, s2)
                    PT = sb("a_PT", [128, 4, 512], BF16, s2)
                    rd = sb("a_rd", [128, 2, 512], F32, s2)
                    rS = _Rot([0, 1, 2, 3])
                    rO = _Rot([(4, 5), (6, 7)])
                    rP = _Rot([0, 1, 2, 3])
                    for (c0, w, who) in lchunks:
                        kts = list(range(2)) if who == 1 else list(range(NT))
                        for h in range(8):
                            g = h // 4
                            qt = h // 2
                            po = 64 * (h % 2)
                            bO, bD = rO.next()
                            sbank = {}

                            def score(kt_):
                                bS_ = rS.next()
                                sbank[kt_] = bS_
                                P.mm(ps[:, bS_, :w], kd[po:po + 64, g, kt_ * 128:(kt_ + 1) * 128], qr[po:po + 64, qt, c0:c0 + w], True, True)

                            for kt_ in kts[:2]:
                                score(kt_)
                            for idx, kt in enumerate(kts):
                                bS = sbank[kt]
                                pt = PT[:, rP.next(), :w]
                                P.act(pt, ps[:, bS, :w], AF.Exp, scale=0.125)
                                if idx + 2 < len(kts):
                                    score(kts[idx + 2])
                                P.mm(ps[po:po + 64, bO, :w], vt[:, kt, g * 64:(g + 1) * 64], pt, idx == 0, idx == len(kts) - 1)
                                P.mm(ps[po:po + 64, bD, :w], ones_bf[:, 0:64], pt, idx == 0, idx == len(kts) - 1)
                            P.recip(rd[po:po + 64, h % 2, :w], ps[po:po + 64, bD, :w])
                            P.tt(amix[po:po + 64, qt, c0:c0 + w], ps[po:po + 64, bO, :w], rd[po:po + 64, h % 2, :w], ALU.mult)
                    out_proj(l, Wo, amix, 4, lchunks, rb)
                P.barrier()

        def moe_phase(l, last, lchunks):
            with ExitStack() as s1:
                h2T = sb("m_h2T", [128, 8, T], BF16, s1)
                GT = sb("m_GT", [128, T], BF16, s1)
                esel = sb("m_esel", [128, 16, 128], BF16, s1)
                P.dma(esel[0:16], esel_d)
                tiles = list(range(2, NT)) if last else list(range(NT))
                with ExitStack() as s2:
                    sq = sb("m_sq", [128, 8, 512], BF16, s2)
                    xn = sb("m_xn", [128, 8, 512], F32, s2)
                    rs = sb("m_rs", [128, 512], F32, s2)
                    wr = sb("m_wr", [128, 8, 16], F32, s2)
                    brep = sb("m_brep", [128, 288], F32, s2)
                    s_tok = sb("m_s", [128, NT, 16], F32, s2)
                    sel2 = sb("m_sel2", [128, 72, 8], F32, s2)
                    p1 = sb("m_p1", [128, 72, 4], F32, s2)
                    p2 = sb("m_p2", [128, 72, 2], F32, s2)
                    gs = sb("m_gs", [128, 72], F32, s2)
                    gs2 = sb("m_gs2", [128, 72], F32, s2)
                    gmax = sb("m_gmax", [128, NT], F32, s2)
                    oh = sb("m_oh", [128, 72], F32, s2)
                    cnt = sb("m_cnt", [128, 72, 4], F32, s2)
                    c2 = sb("m_c2", [128, 72, 4], F32, s2)
                    wsum = sb("m_wsum", [128, NT], F32, s2)
                    gate = sb("m_gate", [128, NT, 16], F32, s2)
                    P.dma(wr[:], wr_d.rearrange("(k p) e -> p k e", p=128))
                    P.dma(brep[:], brep_d)
                    P.memset(s_tok[:], 0.0)
                    for ci, (c0, w, who) in enumerate(lchunks):
                        norm_chunk(l, 1, c0, w, who, sq, xn, rs, ci % 2)
                        for j in range(8):
                            P.ts(xn[:, j, :w], xn[:, j, :w], A_of(l, 1, j, who), B_of(l, 1, j, who), ALU.mult, ALU.add,
                                 eng=("dve" if j % 2 == 0 else "pool"))
                        P.copy(h2T[:, :, c0:c0 + w], xn[:, :, :w], eng="act")
                        b = 2 + ci % 2
                        for tt in range(w // 128):
                            tg = c0 // 128 + tt
                            for k in range(8):
                                P.mm(ps[:, b, tt * 16:(tt + 1) * 16], xn[:, k, tt * 128:(tt + 1) * 128], wr[:, k, :], k == 0, k == 7)
                        nt_ = w // 128
                        tg0 = c0 // 128
                        P.act(s_tok[:, tg0:tg0 + nt_, :], ps[:, b, 0:nt_ * 16].rearrange("p (t e) -> p t e", e=16), AF.Exp, scale=-1.0)
                        P.ts(s_tok[:, tg0:tg0 + nt_, :], s_tok[:, tg0:tg0 + nt_, :], 1.0, None, ALU.add)
                        P.recip(s_tok[:, tg0:tg0 + nt_, :], s_tok[:, tg0:tg0 + nt_, :])
                    sv = s_tok.rearrange("p t (g e) -> p (t g) e", e=4)
                    bv = brep.rearrange("p (a e) -> p a e", e=4)
                    P.tt(sel2[:, :, 0:4], sv, bv, ALU.add)
                    P.tt(sel2[:, :, 4:8], sv, bv, ALU.add)
                    P.tt(p1[:], sel2[:, :, 0:4], sel2[:, :, 1:5], ALU.add)
                    P.tt(p2[:], sel2[:, :, 0:2], sel2[:, :, 2:4], ALU.add)
                    P.reduce(gs[:], p1[:], ALU.max)
                    P.reduce(gs2[:], p2[:], ALU.max)
                    P.tt(gs[:], gs[:], gs2[:], ALU.max)
                    P.reduce(gmax[:], gs.rearrange("p (t g) -> p t g", g=4), ALU.max)
                    P.tt(oh.rearrange("p (t g) -> p t g", g=4), gs.rearrange("p (t g) -> p t g", g=4), _bc(gmax[:], 2, 4), ALU.is_equal)
                    P.tt(cnt[:], sel2[:, :, 1:5], sel2[:, :, 0:4], ALU.is_gt)
                    P.tt(c2[:], sel2[:, :, 2:6], sel2[:, :, 0:4], ALU.is_gt)
                    P.tt(cnt[:], cnt[:], c2[:], ALU.add)
                    P.tt(c2[:], sel2[:, :, 3:7], sel2[:, :, 0:4], ALU.is_gt)
                    P.tt(cnt[:], cnt[:], c2[:], ALU.add)
                    P.ts(cnt[:], cnt[:], 1.0, None, ALU.is_le)
                    P.tt(cnt[:], cnt[:], _bc(oh[:], 2, 4), ALU.mult)
                    gv = gate.rearrange("p t (g e) -> p (t g) e", e=4)
                    P.tt(gv, sv, cnt[:], ALU.mult)
                    P.reduce(wsum[:], gate[:], ALU.add)
                    P.ts(wsum[:], wsum[:], 1e-30, None, ALU.max)
                    P.recip(wsum[:], wsum[:])
                    P.tt(gate[:], gate[:], _bc(wsum[:], 2, 16), ALU.mult)
                    for t in tiles:
                        b = 4 + (t // 4) % 2
                        P.transpose(ps[0:16, b, (t % 4) * 128:(t % 4 + 1) * 128], gate[:, t, :], ident[:])
                        P.copy(GT[0:16, t * 128:(t + 1) * 128], ps[0:16, b, (t % 4) * 128:(t % 4 + 1) * 128])
                with ExitStack() as s2:
                    wbuf = sb("m_wbuf", [128, 3, 3, 2048], BF16, s2)
                    sg = sb("m_sg", [128, 2, 512], F32, s2)
                    t1 = sb("m_t1", [128, 2, 512], F32, s2)
                    abuf = sb("m_a", [128, 2, 2, 512], BF16, s2)
                    rG = _Rot([0, 1])
                    rU = _Rot([2, 3])
                    rD = _Rot([5, 6, 7])
                    it = 0
                    pending = None

                    def emit_down(pd):
                        Wd_p, ab_p, c0p, wp, whop = pd
                        for m in range(8):
                            bD = rD.next()
                            for ft in range(2):
                                P.mm(ps[:, bD, :wp], Wd_p[:, ft, m * 128:(m + 1) * 128], ab_p[:, ft, :wp], ft == 0, ft == 1)
                            P.stt(xT[:, m, c0p:c0p + wp], ps[:, bD, :wp], G_of(l, 1, m, whop), xT[:, m, c0p:c0p + wp], ALU.mult, ALU.add)

                    for e in range(16):
                        for fh in range(2):
                            u = e * 2 + fh
                            ws = u % 3
                            Wg_ = wbuf[:, ws, 0].rearrange("p (k f) -> p k f", f=256)
                            Wu_ = wbuf[:, ws, 1].rearrange("p (k f) -> p k f", f=256)
                            Wd_ = wbuf[:, ws, 2].rearrange("p (k d) -> p k d", d=1024)
                            for mi, (dst, srcw) in enumerate(((Wg_, weg_d[l, e][:, fh * 256:(fh + 1) * 256]),
                                                              (Wu_, weu_d[l, e][:, fh * 256:(fh + 1) * 256]))):
                                sl = srot.next()
                                sv_ = stage[:, sl, :].rearrange("p (k f) -> p k f", f=256)
                                P.dma(sv_, srcw.rearrange("(k p) f -> p k f", p=128))
                                P.copy(dst, sv_, eng="act")
                            sl = srot.next()
                            sv_ = stage[:, sl, :].rearrange("p (k d) -> p k d", d=1024)
                            P.dma(sv_, wed_d[l, e][fh * 256:(fh + 1) * 256, :].rearrange("(k p) d -> p k d", p=128))
                            P.copy(Wd_, sv_, eng="act")
                            for (c0, w, who) in lchunks:
                                ab = abuf[:, it % 2]
                                it += 1
                                P.mm(ps[:, 4, :w], esel[0:16, e, :], GT[0:16, c0:c0 + w], True, True)
                                for ft in range(2):
                                    bG = rG.next()
                                    bU = rU.next()
                                    for k in range(8):
                                        P.mm(ps[:, bG, :w], Wg_[:, k, ft * 128:(ft + 1) * 128], h2T[:, k, c0:c0 + w], k == 0, k == 7)
                                    for k in range(8):
                                        P.mm(ps[:, bU, :w], Wu_[:, k, ft * 128:(ft + 1) * 128], h2T[:, k, c0:c0 + w], k == 0, k == 7)
                                    P.act(sg[:, ft, :w], ps[:, bG, :w], AF.Silu)
                                    P.tt(t1[:, ft, :w], sg[:, ft, :w], ps[:, bU, :w], ALU.mult)
                                    P.tt(ab[:, ft, :w], t1[:, ft, :w], ps[:, 4, :w], ALU.mult)
                                if pending is not None:
                                    emit_down(pending)
                                pending = (Wd_, ab, c0, w, who)
                    emit_down(pending)
                P.barrier()

        for l in range(n_layers):
            last = (l == n_layers - 1) and (n_layers == 2)
            lchunks = CHUNKS[1:] if last else CHUNKS

            with ExitStack() as s1:
                sq = sb("b_sq", [128, 8, 512], BF16, s1)
                xn = sb("b_xn", [128, 8, 512], F32, s1)
                rs = sb("b_rs", [128, 512], F32, s1)
                hc = sb("b_hc", [128, 2, 8, 512], BF16, s1)
                for ci, (c0, w, who) in enumerate(CHUNKS):
                    norm_chunk(l, 0, c0, w, who, sq, xn, rs, ci % 2)
                    for j in range(8):
                        P.ts(hc[:, ci % 2, j, :w], xn[:, j, :w], A_of(l, 0, j, who), B_of(l, 0, j, who), ALU.mult, ALU.add,
                             eng=("dve" if j % 2 == 0 else "pool"))
                    P.dma(hT_d[:, :, c0:c0 + w], hc[:, ci % 2, :, :w])
                P.barrier()
            if stop_at == ("B", l):
                break

            with ExitStack() as s1:
                Wu = sb("f_Wu", [128, 8, 256], BF16, s1)
                Wo = sb("f_Wo", [128, 2, 1024], BF16, s1)
                cs64 = sb("f_cs64", [128, 256], BF16, s1)
                uT = sb("f_uT", [128, 2, T], BF16, s1)
                ucs = sb("f_ucs", [128, NT, 512], BF16, s1)
                fmix = sb("f_mix", [128, 2, T], BF16, s1)
                hcb = sb("f_hc", [128, 2, 8, 512], BF16, s1)
                tab = sb("f_tab", [128, 1, 2, 16, 512], BF16, s1)
                P.dma(cs64[:], cs64_d)
                load_w(Wu, win_d[l][:, 0:256], 256, srot)
                load_w(Wo, wout_d[l][0:256, :], 1024, srot)
                rb = _Rot([0, 1, 2, 3])
                for ci, (c0, w, who) in enumerate(CHUNKS):
                    P.dma(hcb[:, ci % 2, :, :w], hT_d[:, :, c0:c0 + w])
                    for m in range(2):
                        b = rb.next()
                        for k in range(8):
                            P.mm(ps[:, b, :w], Wu[:, k, m * 128:(m + 1) * 128], hcb[:, ci % 2, k, :w], k == 0, k == 7)
                        P.copy(uT[:, m, c0:c0 + w], ps[:, b, :w], eng=("act" if m == 0 else "dve"))
                t_lo = 2 if last else 0
                for t in range(t_lo, NT):
                    b = rb.next()
                    for j in range(2):
                        P.mm(ps[:, b, j * 256:(j + 1) * 256], uT[:, j, t * 128:(t + 1) * 128], cs64[:], True, True)
                    P.copy(ucs[:, t, :], ps[:, b, :], eng=("act" if t % 2 == 0 else "dve"))
                for pc in range(4):
                    bufi = 0
                    for hh in range(2):
                        P.dma(tab[:, bufi, 0, hh * 8:(hh + 1) * 8], cn_d[hh * 1024:(hh + 1) * 1024, pc * 512:(pc + 1) * 512].rearrange("(t p) c -> p t c", p=128))
                        P.dma(tab[:, bufi, 1, hh * 8:(hh + 1) * 8], sn_d[hh * 1024:(hh + 1) * 1024, pc * 512:(pc + 1) * 512].rearrange("(t p) c -> p t c", p=128))
                    for j in range(2):
                        b = rb.next()
                        for t in range(16):
                            P.mm(ps[:, b, :], ucs[:, 2 + t, j * 256:j * 256 + 128], tab[:, bufi, 0, t, :], t == 0, False)
                            P.mm(ps[:, b, :], ucs[:, 2 + t, j * 256 + 128:j * 256 + 256], tab[:, bufi, 1, t, :], False, t == 15)
                        P.copy(fmix[:, j, 256 + pc * 512:256 + (pc + 1) * 512], ps[:, b, :], eng=("act" if j == 0 else "dve"))
                if not last:
                    c2 = sb("f_c2", [128, 2, 2, 256], BF16, s1)
                    P.dma(c2[:, 0], c256_d.rearrange("(t p) c -> p t c", p=128))
                    P.dma(c2[:, 1], s256_d.rearrange("(t p) c -> p t c", p=128))
                    for j in range(2):
                        b = rb.next()
                        for t in range(2):
                            P.mm(ps[:, b, 0:256], ucs[:, t, j * 256:j * 256 + 128], c2[:, 0, t, :], t == 0, False)
                            P.mm(ps[:, b, 0:256], ucs[:, t, j * 256 + 128:j * 256 + 256], c2[:, 1, t, :], False, t == 1)
                        P.copy(fmix[:, j, 0:256], ps[:, b, 0:256])
                out_proj(l, Wo, fmix, 2, lchunks, rb)
                P.barrier()
            dump_dbg(4 * l + 0)
            if stop_at == ("D", l):
                break

            if 'E' not in skip:
                gla_phase(l, last, lchunks)
            dump_dbg(4 * l + 1)
            if stop_at == ("E", l):
                break

            if 'F' not in skip:
                att_phase(l, last, lchunks)
            dump_dbg(4 * l + 2)
            if stop_at == ("F", l):
                break

            moe_phase(l, last, lchunks)
            dump_dbg(4 * l + 3)
            if stop_at == ("I", l):
                break

        if stop_at is None:
            with ExitStack() as s1:
                sq = sb("o_sq", [128, 8, 512], BF16, s1)
                xn = sb("o_xn", [128, 8, 512], F32, s1)
                rs = sb("o_rs", [128, 512], F32, s1)
                ot = sb("o_ot", [128, 2, 1024], F32, s1)
                fn32 = sb("o_fn", [128, 8], F32, s1)
                P.ts(fn32[:], V1T[:, 32:40], 32.0, None, ALU.mult)
                ev = 0
                for ci, (c0, w, who) in enumerate(CHUNKS[1:]):
                    norm_chunk(0, 0, c0, w, who, sq, xn, rs, 0)
                    for j in range(8):
                        P.ts(xn[:, j, :w], xn[:, j, :w], fn32[:, j:j + 1], None, ALU.mult, eng=("dve" if j % 2 == 0 else "pool"))
                    for tt in range(4):
                        tg = ci * 4 + tt
                        for half in range(2):
                            b = 1 + (2 * tg + half) % 4
                            for jj in range(4):
                                j = half * 4 + jj
                                P.transpose(ps[:, b, jj * 128:(jj + 1) * 128], xn[:, j, tt * 128:(tt + 1) * 128], ident[:])
                            dst = ot[:, tg % 2, half * 512:(half + 1) * 512]
                            if ev % 2 == 0:
                                P.copy(dst, ps[:, b, :])
                            else:
                                P.copy(dst, ps[:, b, :], eng="act")
                            ev += 1
                        P.dma(y_d[tg * 128:(tg + 1) * 128, :], ot[:, tg % 2, :])
        P.emit()
        print("arena peak bytes", astate["peak"], "ops", P.nops)
    return nc


_CONST_CACHE = {}


def _consts():
    if _CONST_CACHE:
        return _CONST_CACHE
    bf = ml_dtypes.bfloat16
    c = {}
    c["ident"] = np.eye(128, dtype=np.float32)
    k = np.arange(64)
    ang = 2 * np.pi * np.outer(k, k) / 64.0
    C64 = np.cos(ang) / 8.0
    S64 = np.sin(ang) / 8.0
    cs = np.zeros((128, 256), np.float64)
    for g in range(2):
        cs[g * 64:(g + 1) * 64, g * 64:(g + 1) * 64] = C64
        cs[g * 64:(g + 1) * 64, 128 + g * 64:128 + (g + 1) * 64] = S64
    c["cs64"] = cs.astype(bf)
    for n, cn, sn in ((2048, "cn", "sn"), (256, "c256", "s256")):
        i = np.arange(n)
        a = 2 * np.pi * (np.outer(i, i) % n) / float(n)
        c[cn] = (np.cos(a) / np.sqrt(n)).astype(bf)
        c[sn] = (-np.sin(a) / np.sqrt(n)).astype(bf)
    inv = 10000.0 ** (-np.arange(16, dtype=np.float64) * 2.0 / 32.0)
    tok = np.arange(2048)
    row = tok // 64
    col = tok % 64
    rc = np.ones((128, T), np.float64)
    rs = np.zeros((128, T), np.float64)
    for hd in range(128):
        a = (hd % 64) // 32
        f = hd % 16
        pos = row if a == 0 else col
        rc[hd, 256:] = np.cos(pos * inv[f])
        rs[hd, 256:] = np.sin(pos * inv[f])
    c["ropec"] = rc.astype(np.float32)
    c["ropes"] = rs.astype(np.float32)
    psw = np.zeros((128, 128), np.float32)
    for hdp in range(128):
        half = (hdp % 32) // 16
        if half == 0:
            psw[hdp + 16, hdp] = -1.0
        else:
            psw[hdp - 16, hdp] = 1.0
    c["psw"] = psw
    j = np.arange(128)[:, None]
    i = np.arange(128)[None, :]
    same = (j // 64) == (i // 64)
    m = np.zeros((128, 6, 128), np.float32)
    m[:, 0, :] = np.where(same & (j <= i), -1.0 / 16.0, 0.0)
    m[:, 1, :] = np.where(same & (j >= i), -1.0 / 16.0, 0.0)
    m[:, 2, :] = np.where(same & (j > i), -1.0 / 16.0, 0.0)
    m[:, 3, :] = np.where(same & (j < i), -1.0 / 16.0, 0.0)
    m[:, 4, :] = np.where(same & (j <= i), 1.0, 0.0)
    m[:, 5, :] = np.where(same & (j >= i), 1.0, 0.0)
    c["masks"] = m
    c["bo64"] = same.astype(np.float32).astype(bf)
    es = np.zeros((16, 16, 128), np.float32)
    for e in range(16):
        es[e, e, :] = 1.0
    c["esel"] = es.astype(bf)
    _CONST_CACHE.update(c)
    return _CONST_CACHE


_NC_CACHE = {}


def _prep_inputs(inputs):
    f = lambda a: np.ascontiguousarray(np.asarray(a, dtype=np.float32))
    x = f(inputs["x"])
    c = f(inputs["c"])
    ctx = f(inputs["ctx"])
    c_ctx = f(inputs["c_ctx"])
    b_ada = f(inputs["b_ada"])
    cst = _consts()
    vecs1 = np.concatenate([
        f(inputs["norm_mix"]).reshape(16, 128),
        f(inputs["norm_ffn"]).reshape(16, 128),
        f(inputs["final_norm"]).reshape(8, 128),
        np.tile(f(inputs["gla_norm"]), (1, 2)),
        np.tile(f(inputs["q_norm"]), (1, 2)),
        np.tile(f(inputs["k_norm"]), (1, 2)),
    ], axis=0)
    wg_ = f(inputs["w_gla_gate_up"])
    bg_ = f(inputs["b_gla_gate"])
    wup = np.zeros((2, 2, 33, 128), np.float32)
    for l_ in range(2):
        for d_ in range(2):
            wup[l_, d_, 16 * d_:16 * d_ + 16, :] = wg_[l_, d_]
            wup[l_, d_, 32, :] = bg_[l_, d_]
    brep = np.ascontiguousarray(np.broadcast_to(np.tile(f(inputs["b_router"]), 18)[None, :], (128, 288)))
    shared = {
        "vecs1": np.ascontiguousarray(vecs1),
        "w_ada": f(inputs["w_ada"]), "w_in": f(inputs["w_in"]), "w_out": f(inputs["w_out"]),
        "wup": np.ascontiguousarray(wup), "w_router": f(inputs["w_router"]), "brep": brep,
        "w_exp_gate": f(inputs["w_exp_gate"]), "w_exp_up": f(inputs["w_exp_up"]), "w_exp_down": f(inputs["w_exp_down"]),
    }
    for k_ in ("ident", "cs64", "cn", "sn", "c256", "s256", "ropec", "ropes", "psw", "masks", "bo64", "esel"):
        shared[k_] = cst[k_]
    in_maps = []
    for b in range(8):
        vecs0 = np.concatenate([c[b].reshape(8, 128), c_ctx.reshape(8, 128),
                                b_ada[0].reshape(48, 128), b_ada[1].reshape(48, 128)], axis=0)
        m = dict(shared)
        m["x"] = x[b]
        m["ctx"] = ctx[b]
        m["vecs0"] = np.ascontiguousarray(vecs0)
        in_maps.append(m)
    return in_maps


def kernel(**inputs):
    in_maps = _prep_inputs(inputs)
    if "nc" not in _NC_CACHE:
        _NC_CACHE["nc"] = build()
    nc = _NC_CACHE["nc"]
    res = run_bass_kernel_spmd(nc, in_maps, core_ids=list(range(8)))
    out = np.stack([np.asarray(r["y"], dtype=np.float32) for r in res.results], axis=0)
    return out
```

```python
import numpy as np
import ml_dtypes
import concourse.bass as bass
import concourse.mybir as mybir
from concourse.bass_utils import run_bass_kernel_spmd

F32 = mybir.dt.float32
BF16 = mybir.dt.bfloat16
AF = mybir.ActivationFunctionType
ALU = mybir.AluOpType
AX = mybir.AxisListType

_DSZ = {F32: 4, BF16: 2}


def _dsz(dt):
    if dt in _DSZ:
        return _DSZ[dt]
    s = str(dt)
    if "32" in s:
        return 4
    if "16" in s:
        return 2
    if "64" in s:
        return 8
    return 1


def _region(ap):
    t = ap.tensor
    name = t.name
    dsz = _dsz(ap.dtype)
    space = str(ap.space)
    off = int(ap.offset)
    if "DRAM" in space.upper() or "HBM" in space.upper():
        ext = 0
        for (s, c) in ap.ap:
            ext += abs(int(s)) * (int(c) - 1)
        return (name, 0, 1, off * dsz, (off + ext + 1) * dsz)
    shape = list(t.shape)
    fsz = 1
    for d in shape[1:]:
        fsz *= int(d)
    p0 = off // fsz
    f0 = off % fsz
    pext = 0
    fext = 0
    for (s, c) in ap.ap:
        s = int(s)
        c = int(c)
        if c <= 1 or s == 0:
            continue
        if s % fsz == 0:
            pext += (s // fsz) * (c - 1)
        else:
            fext += abs(s) * (c - 1)
    b0 = f0 * dsz
    b1 = (f0 + fext + 1) * dsz
    if "PSUM" in space.upper():
        return (name, 0, 128, (b0 // 2048) * 2048, ((b1 + 2047) // 2048) * 2048)
    return (name, p0, p0 + pext + 1, b0, b1)


class _Op:
    __slots__ = ("eng", "fn", "k", "is_dma", "deps_eng", "deps_dma", "need_signal", "sig_val",
                 "dma_sem", "dma_val", "dma_prev", "name")

    def __init__(self, eng, fn, is_dma, name=""):
        self.eng = eng
        self.fn = fn
        self.is_dma = is_dma
        self.k = -1
        self.deps_eng = {}
        self.deps_dma = []
        self.need_signal = False
        self.sig_val = 0
        self.dma_sem = None
        self.dma_val = 0
        self.dma_prev = None
        self.name = name


class Prog:
    ENGS = ("pe", "act", "dve", "pool", "sp")
    NDMA = {"sp": 40, "pool": 8, "act": 8}

    def __init__(self, nc):
        self.nc = nc
        self.eng_ops = {e: [] for e in self.ENGS}
        self.recs = {}
        self.dma_count = {q: 0 for q in self.NDMA}
        self.dma_last = {q: [None] * n for q, n in self.NDMA.items()}
        self.nops = 0

    def _dep(self, x, y):
        if y is x:
            return
        if y.is_dma:
            if y not in x.deps_dma:
                x.deps_dma.append(y)
            return
        if (not x.is_dma) and y.eng == x.eng:
            if x.eng == "pe":
                return
        cur = x.deps_eng.get(y.eng, -1)
        if y.k > cur:
            x.deps_eng[y.eng] = y.k
        y.need_signal = True

    def add(self, eng, fn, reads, writes, is_dma=False, name=""):
        op = _Op(eng, fn, is_dma, name)
        rr = [_region(a) for a in reads if a is not None]
        ww = [_region(a) for a in writes if a is not None]
        for (nm, p0, p1, b0, b1) in rr:
            is_ps = (nm == "ps")
            for rec in self.recs.get(nm, ()):
                if rec[0] < p1 and p0 < rec[1] and rec[2] < b1 and b0 < rec[3]:
                    if rec[4] or (is_ps and rec[5].eng != eng):
                        self._dep(op, rec[5])
        for (nm, p0, p1, b0, b1) in ww:
            for rec in self.recs.get(nm, ()):
                if rec[0] < p1 and p0 < rec[1] and rec[2] < b1 and b0 < rec[3]:
                    self._dep(op, rec[5])
        for (nm, p0, p1, b0, b1) in ww:
            lst = self.recs.setdefault(nm, [])
            lst[:] = [r for r in lst if not (p0 <= r[0] and r[1] <= p1 and b0 <= r[2] and r[3] <= b1)]
            lst.append((p0, p1, b0, b1, True, op))
        for (nm, p0, p1, b0, b1) in rr:
            lst = self.recs.setdefault(nm, [])
            if not is_dma:
                lst[:] = [r for r in lst if not ((not r[4]) and (not r[5].is_dma) and r[5].eng == eng
                                                 and p0 <= r[0] and r[1] <= p1 and b0 <= r[2] and r[3] <= b1)]
            lst.append((p0, p1, b0, b1, False, op))
        op.k = len(self.eng_ops[eng])
        self.eng_ops[eng].append(op)
        if is_dma:
            n = self.NDMA[eng]
            slot = self.dma_count[eng] % n
            self.dma_count[eng] += 1
            prev = self.dma_last[eng][slot]
            op.dma_sem = (eng, slot)
            op.dma_prev = prev
            op.dma_val = (prev.dma_val if prev is not None else 0) + 16
            self.dma_last[eng][slot] = op
        self.nops += 1
        return op

    def barrier(self):
        bop = _Op("sp", lambda e: e.nop(), False, "barrier")
        for e in self.ENGS:
            lst = [o for o in self.eng_ops[e] if not o.is_dma]
            if lst:
                y = lst[-1]
                if e == "sp":
                    continue
                bop.deps_eng[e] = y.k
                y.need_signal = True
        for q in self.NDMA:
            for y in self.dma_last[q]:
                if y is not None:
                    bop.deps_dma.append(y)
        bop.k = len(self.eng_ops["sp"])
        bop.need_signal = True
        self.eng_ops["sp"].append(bop)
        self.recs = {"__barrier__": [(0, 1, 0, 1, True, bop)]}
        self._barrier_op = bop
        self._barrier_seen = set()
        return bop

    def _barrier_dep(self, op):
        b = getattr(self, "_barrier_op", None)
        if b is None or op is b or op.eng == "sp" or op.eng in self._barrier_seen:
            return
        self._barrier_seen.add(op.eng)
        if b.k > op.deps_eng.get("sp", -1):
            op.deps_eng["sp"] = b.k

    def _rec(self, eng, fn, reads, writes, is_dma=False, name=""):
        op = self.add(eng, fn, reads, writes, is_dma, name)
        self._barrier_dep(op)
        if eng == "pe":
            l = reads[0]
            rr = lambda v: 32 if v <= 32 else (64 if v <= 64 else 128)
            op.name = (rr(int(l.shape[0])), rr(int(l.shape[-1])))
        return op

    def mm(self, out, lhsT, rhs, start=True, stop=True):
        return self._rec("pe", lambda e: e.matmul(out, lhsT, rhs, start=start, stop=stop), [lhsT, rhs], [out])

    def transpose(self, out, in_, ident):
        return self._rec("pe", lambda e: e.transpose(out, in_, ident), [in_, ident], [out])

    def act(self, out, in_, func, bias=None, scale=1.0, accum_out=None):
        reads = [in_]
        kw = {}
        if bias is not None:
            kw["bias"] = bias
            if not isinstance(bias, (int, float)):
                reads.append(bias)
        if not isinstance(scale, (int, float)):
            reads.append(scale)
        kw["scale"] = scale
        writes = [out]
        if accum_out is not None:
            kw["accum_out"] = accum_out
            writes.append(accum_out)
        return self._rec("act", lambda e: e.activation(out, in_, func, **kw), reads, writes)

    def tt(self, out, in0, in1, op, eng="dve"):
        return self._rec(eng, lambda e: e.tensor_tensor(out, in0, in1, op), [in0, in1], [out])

    def ts(self, out, in0, s1, s2, op0, op1=None, eng="dve", accum_out=None):
        reads = [in0]
        if not isinstance(s1, (int, float)):
            reads.append(s1)
        if s2 is not None and not isinstance(s2, (int, float)):
            reads.append(s2)
        writes = [out]
        kw = {}
        if accum_out is not None:
            kw["accum_out"] = accum_out
            writes.append(accum_out)
        if op1 is None:
            return self._rec(eng, lambda e: e.tensor_scalar(out, in0, s1, s2, op0, **kw), reads, writes)
        return self._rec(eng, lambda e: e.tensor_scalar(out, in0, s1, s2, op0, op1, **kw), reads, writes)

    def stt(self, out, in0, scalar, in1, op0, op1, eng="dve"):
        reads = [in0, in1]
        if not isinstance(scalar, (int, float)):
            reads.append(scalar)
        return self._rec(eng, lambda e: e.scalar_tensor_tensor(out, in0, scalar, in1, op0, op1), reads, [out])

    def copy(self, out, in_, eng="dve"):
        if eng == "act":
            return self._rec("act", lambda e: e.copy(out, in_), [in_], [out])
        return self._rec(eng, lambda e: e.tensor_copy(out, in_), [in_], [out])

    def recip(self, out, in_):
        return self._rec("dve", lambda e: e.reciprocal(out, in_), [in_], [out])

    def reduce(self, out, in_, op, axis=None, eng="dve"):
        ax = axis if axis is not None else AX.X
        return self._rec(eng, lambda e: e.tensor_reduce(out, in_, ax, op), [in_], [out])

    def memset(self, out, val, eng="dve"):
        return self._rec(eng, lambda e: e.memset(out, val), [], [out])

    def dma(self, out, in_, q="sp"):
        return self._rec(q, lambda e: e.dma_start(out=out, in_=in_), [in_], [out], is_dma=True)

    def emit(self):
        nc = self.nc
        self.barrier()
        from contextlib import ExitStack
        with ExitStack() as st:
            esem = {e: st.enter_context(nc.semaphore("c_" + e)) for e in self.ENGS}
            dsem = {}
            for q, n in self.NDMA.items():
                for i in range(n):
                    dsem[(q, i)] = st.enter_context(nc.semaphore("d_%s%d" % (q, i)))
            for e in self.ENGS:
                v = 0
                for op in self.eng_ops[e]:
                    if (not op.is_dma) and op.need_signal:
                        v += 1
                        op.sig_val = v
            prog = self

            pstate = {}

            def run(ename, eng):
                waited = {}
                for op in prog.eng_ops[ename]:
                    waits = []
                    for e2, k2 in op.deps_eng.items():
                        y = prog.eng_ops[e2][k2]
                        waits.append((("c", e2), esem[e2], y.sig_val))
                    for y in op.deps_dma:
                        waits.append((y.dma_sem, dsem[y.dma_sem], y.dma_val))
                    if op.is_dma and op.dma_prev is not None:
                        waits.append((op.dma_sem, dsem[op.dma_sem], op.dma_prev.dma_val))
                    for key, sem, val in waits:
                        if waited.get(key, 0) >= val:
                            continue
                        waited[key] = val
                        eng.wait_ge(sem, val)
                    if ename == "pe":
                        pass
                        pstate["mode"] = op.name
                    ins = op.fn(eng)
                    if op.is_dma:
                        ins.then_inc(dsem[op.dma_sem], 16)
                    elif op.need_signal:
                        ins.then_inc(esem[ename], 1)

            with nc.Block() as block:
                @block.tensor
                def _(eng):
                    run("pe", eng)

                @block.scalar
                def _(eng):
                    run("act", eng)

                @block.vector
                def _(eng):
                    run("dve", eng)

                @block.gpsimd
                def _(eng):
                    run("pool", eng)

                @block.sync
                def _(eng):
                    run("sp", eng)


T = 2304
NT = 18
KT = 8
EPS = 1e-6
CHUNKS = [(0, 256, 1), (256, 512, 0), (768, 512, 0), (1280, 512, 0), (1792, 512, 0)]


def _bc(ap, pos, n):
    shp = list(ap.shape)
    v = ap.unsqueeze(pos)
    shp.insert(pos, n)
    return v.to_broadcast(shp)


_DBG = {}


class _Rot:
    def __init__(self, items):
        self.items = list(items)
        self.i = 0

    def next(self):
        v = self.items[self.i % len(self.items)]
        self.i += 1
        return v


def build(n_layers=2, stop_at=None, dbg=False, skip=(), sub=None):
    from contextlib import ExitStack
    nc = bass.Bass("TRN2", target_bir_lowering=False)

    def din(name, shape, dt=F32):
        return nc.dram_tensor(name, shape, dt, kind="ExternalInput").ap()

    x_d = din("x", [2048, 1024])
    ctx_d = din("ctx", [256, 1024])
    vecs0_d = din("vecs0", [112, 128])
    vecs1_d = din("vecs1", [46, 128])
    wada_d = din("w_ada", [2, 1024, 6144])
    win_d = din("w_in", [2, 1024, 1824])
    wout_d = din("w_out", [2, 1024, 1024])
    wup_d = din("wup", [2, 2, 33, 128])
    wr_d = din("w_router", [1024, 16])
    brep_d = din("brep", [128, 288])
    need_moe = stop_at is None or stop_at[0] == "I" or stop_at[1] >= 1
    weg_d = weu_d = wed_d = None
    if need_moe:
        weg_d = din("w_exp_gate", [2, 16, 1024, 512])
        weu_d = din("w_exp_up", [2, 16, 1024, 512])
        wed_d = din("w_exp_down", [2, 16, 512, 1024])
    ident_d = din("ident", [128, 128])
    cs64_d = din("cs64", [128, 256], BF16)
    cn_d = din("cn", [2048, 2048], BF16)
    sn_d = din("sn", [2048, 2048], BF16)
    c256_d = din("c256", [256, 256], BF16)
    s256_d = din("s256", [256, 256], BF16)
    ropec_d = din("ropec", [128, T])
    ropes_d = din("ropes", [128, T])
    psw_d = din("psw", [128, 128])
    masks_d = din("masks", [128, 6, 128])
    bo64_d = din("bo64", [128, 128], BF16)
    esel_d = din("esel", [16, 16, 128], BF16)
    y_d = nc.dram_tensor("y", [2048, 1024], F32, kind="ExternalOutput").ap()
    hT_d = nc.dram_tensor("hT_scr", [128, 8, T], BF16).ap()
    dbg_d = None
    if dbg:
        dbg_d = nc.dram_tensor("dbg_x", [8, 128, 8, T], F32, kind="ExternalOutput").ap()

    st = ExitStack()
    with st:
        ARENA_BYTES = 210944
        arena_t = st.enter_context(nc.sbuf_tensor("arena", [128, ARENA_BYTES // 2], BF16))
        astate = {"off": 0, "peak": 0}

        def _release(m):
            astate["off"] = m

        def sb(name, shape, dt, stack=None):
            n = _dsz(dt)
            for d in shape[1:]:
                n *= int(d)
            n = (n + 63) // 64 * 64
            off = astate["off"]
            if stack is not None:
                stack.callback(_release, off)
            assert off + n <= ARENA_BYTES, ("arena overflow", name, off, n)
            astate["off"] = off + n
            astate["peak"] = max(astate["peak"], off + n)
            v = arena_t[:, off // 2:(off + n) // 2]
            if dt != BF16:
                v = v.bitcast(dt)
            tot = 1
            for d in shape[1:]:
                tot *= int(d)
            v = v[:, 0:tot]
            if len(shape) > 2:
                names = ["d%d" % i for i in range(len(shape) - 1)]
                v = v.rearrange("p (%s) -> p %s" % (" ".join(names), " ".join(names)),
                                **{nm: int(s) for nm, s in zip(names, shape[1:])})
            return v

        P = Prog(nc)
        ps = st.enter_context(nc.psum_tensor("ps", [128, 8, 512], F32))
        xT = sb("xT", [128, 8, T], F32)
        stage = sb("stage", [128, 4, 2048], F32)
        ident = sb("ident", [128, 128], F32)
        ones_bf = sb("ones_bf", [128, 128], BF16)
        bo64 = sb("bo64", [128, 128], BF16)
        V0T = sb("V0T", [128, 112], F32)
        V1T = sb("V1T", [128, 46], F32)
        cvec = sb("cvec", [128, 8, 2], F32)
        modT = sb("modT", [128, 2, 48, 2], F32)
        drv = sb("drv", [128, 2, 2, 8, 2], F32)

        P.dma(ident[:], ident_d)
        P.dma(bo64[:], bo64_d)
        P.memset(ones_bf[:], 1.0)
        P.dma(stage[0:112, 0, 0:128], vecs0_d)
        P.dma(stage[0:46, 1, 0:128], vecs1_d)
        P.transpose(ps[:, 0, 0:112], stage[0:112, 0, 0:128], ident[0:112, 0:112])
        P.transpose(ps[:, 1, 0:46], stage[0:46, 1, 0:128], ident[0:46, 0:46])
        P.copy(V0T[:], ps[:, 0, 0:112])
        P.copy(V1T[:], ps[:, 1, 0:46])
        for wh_ in range(2):
            P.act(cvec[:, :, wh_], V0T[:, 8 * wh_:8 * wh_ + 8], AF.Exp, scale=-1.0)
            P.ts(cvec[:, :, wh_], cvec[:, :, wh_], 1.0, None, ALU.add)
            P.recip(cvec[:, :, wh_], cvec[:, :, wh_])
            P.tt(cvec[:, :, wh_], V0T[:, 8 * wh_:8 * wh_ + 8], cvec[:, :, wh_], ALU.mult)

        for l in range(n_layers):
            for s in range(12):
                slot = (s % 2) * 2
                for hlf in range(2):
                    P.dma(stage[:, slot + hlf, :].rearrange("p (k n) -> p k n", k=4),
                          wada_d[l, hlf * 512:(hlf + 1) * 512, s * 512:(s + 1) * 512].rearrange("(k p) n -> p k n", p=128))
                for ft in range(4):
                    jg = s * 4 + ft
                    for k in range(8):
                        wv = stage[:, slot + k // 4, :].rearrange("p (k n) -> p k n", k=4)
                        P.mm(ps[:, 2, 2 * jg:2 * jg + 2], wv[:, k % 4, ft * 128:(ft + 1) * 128], cvec[:, k, :],
                             k == 0, k == 7)
            pv = ps[:, 2, 0:96].rearrange("p (a b) -> p a b", b=2)
            bias = V0T[:, 16 + 48 * l:16 + 48 * (l + 1)]
            P.tt(modT[:, l], pv, _bc(bias, 2, 2), ALU.add)
            for which in range(2):
                sc = modT[:, l, (1 + 3 * which) * 8:(2 + 3 * which) * 8, :]
                nw = V1T[:, (16 * which + 8 * l):(16 * which + 8 * l + 8)]
                P.ts(drv[:, l, which], sc, 1.0, 32.0, ALU.add, ALU.mult)
                P.tt(drv[:, l, which], drv[:, l, which], _bc(nw, 2, 2), ALU.mult)
        P.barrier()

        def A_of(l, which, j, who):
            return drv[:, l, which, j, who:who + 1]

        def B_of(l, which, j, who):
            sec = 0 if which == 0 else 3
            return modT[:, l, sec * 8 + j, who:who + 1]

        def G_of(l, which, j, who):
            sec = 2 if which == 0 else 5
            return modT[:, l, sec * 8 + j, who:who + 1]

        with ExitStack() as s0:
            xin = sb("xin", [128, 2, 1024], F32, s0)
            ev = 0
            for t in range(NT):
                src = ctx_d[t * 128:(t + 1) * 128, :] if t < 2 else x_d[(t - 2) * 128:(t - 1) * 128, :]
                P.dma(xin[:, t % 2, :], src)
                for half in range(2):
                    b = (2 * t + half) % 4 + 3
                    for jj in range(4):
                        j = half * 4 + jj
                        P.transpose(ps[:, b, jj * 128:(jj + 1) * 128], xin[:, t % 2, j * 128:(j + 1) * 128], ident[:])
                    dst = xT[:, half * 4:half * 4 + 4, t * 128:(t + 1) * 128]
                    srcp = ps[:, b, :].rearrange("p (a b) -> p a b", b=128)
                    if ev % 2 == 0:
                        P.copy(dst, srcp)
                    else:
                        P.copy(dst, srcp, eng="act")
                    ev += 1
            P.barrier()

        def norm_chunk(l, which, c0, w, who, sq, xn, rs, bank):
            P.act(sq[:, :, :w], xT[:, :, c0:c0 + w], AF.Square)
            for j in range(8):
                P.mm(ps[:, bank, :w], ones_bf[:], sq[:, j, :w], j == 0, j == 7)
            P.act(rs[:, :w], ps[:, bank, :w], AF.Sqrt, bias=EPS * 1024.0, scale=1.0)
            P.recip(rs[:, :w], rs[:, :w])
            P.tt(xn[:, :, :w], xT[:, :, c0:c0 + w], _bc(rs[:, :w], 1, 8), ALU.mult)

        def load_w(dst_bf, src_rows_by_cols, ncols, slot_rot, eng="pool"):
            kt = dst_bf.shape[1]
            per = max(1, 2048 // ncols)
            k = 0
            while k < kt:
                n = min(per, kt - k)
                slot = slot_rot.next()
                sv = stage[:, slot, 0:n * ncols].rearrange("p (k n) -> p k n", n=ncols)
                P.dma(sv, src_rows_by_cols[k * 128:(k + n) * 128, :].rearrange("(k p) n -> p k n", p=128))
                P.copy(dst_bf[:, k:k + n, :], sv, eng=("act" if cast_rot.next() == 0 else "dve"))
                k += n

        srot = _Rot([0, 1, 2, 3])
        cast_rot = _Rot([0, 1])

        def out_proj(l, Wo, mix, nk, chunks, banks):
            for (c0, w, who) in chunks:
                for m in range(8):
                    b = banks.next()
                    for k in range(nk):
                        P.mm(ps[:, b, :w], Wo[:, k, m * 128:(m + 1) * 128], mix[:, k, c0:c0 + w], k == 0, k == nk - 1)
                    P.stt(xT[:, m, c0:c0 + w], ps[:, b, :w], G_of(l, 0, m, who), xT[:, m, c0:c0 + w], ALU.mult, ALU.add)

        def dump_dbg(idx):
            if dbg_d is not None:
                for j in range(8):
                    P.dma(dbg_d[idx, :, j, :], xT[:, j, :])

        def gla_phase(l, last, lchunks):
            with ExitStack() as s1:
                Wo = sb("g_Wo", [128, 2, 1024], BF16, s1)
                wup = sb("g_wup", [128, 2, 128], BF16, s1)
                msk = sb("g_msk", [128, 6, 128], F32, s1)
                qT = sb("g_qT", [128, T], BF16, s1)
                kT = sb("g_kT", [128, T], BF16, s1)
                ktok = sb("g_ktok", [128, NT, 128], BF16, s1)
                vtok = sb("g_vtok", [128, NT, 256], BF16, s1)
                sgT = sb("g_sg", [128, 2, T], BF16, s1)
                gd = sb("g_gd", [128, T], BF16, s1)
                oT = sb("g_oT", [128, 2, T], F32, s1)
                g8 = sb("g_g8", [128, 1], F32, s1)
                rb = _Rot([0, 1, 2, 3, 4, 5, 6, 7])
                P.dma(msk[:], masks_d)
                P.ts(g8[:], V1T[:, 40 + l:41 + l], 8.0, None, ALU.mult)
                load_w(Wo, wout_d[l][256:512, :], 1024, srot)
                sl = srot.next()
                P.dma(stage[0:33, sl, 0:256].rearrange("p (d c) -> p d c", d=2), wup_d[l].rearrange("d r c -> r d c"))
                P.copy(wup[0:33], stage[0:33, sl, 0:256].rearrange("p (d c) -> p d c", d=2))
                P.memset(gd[0:64], 1.0)
                if sub == "E1a":
                    return
                with ExitStack() as s2:
                    Wg = sb("g_W", [128, 8, 800], BF16, s2)
                    hcb = sb("g_hc", [128, 8, 512], BF16, s2)
                    sge = sb("g_sge", [128, 2, 512], F32, s2)
                    load_w(Wg[:, :, 0:512], win_d[l][:, 256:768], 512, srot)
                    load_w(Wg[:, :, 512:800], win_d[l][:, 768:1056], 288, srot)
                    if sub == "E1b":
                        return
                    for ci, (c0, w, who) in enumerate(CHUNKS):
                        P.dma(hcb[:, :, :w], hT_d[:, :, c0:c0 + w])
                        for (m0, msz, kind) in ((0, 128, "q"), (128, 128, "k"), (512, 128, "g0"), (640, 128, "g1"),
                                                (768, 32, "df")):
                            if _DBG.get("kinds") is not None and kind not in _DBG["kinds"]:
                                continue
                            b = rb.next()
                            for k in range(8):
                                P.mm(ps[0:msz, b, :w], Wg[:, k, m0:m0 + msz], hcb[:, k, :w], k == 0, k == 7)
                            if kind == "q":
                                P.ts(qT[:, c0:c0 + w], ps[:, b, :w], float(32.0 ** -0.5), None, ALU.mult)
                            elif kind == "k":
                                P.copy(kT[:, c0:c0 + w], ps[:, b, :w])
                            elif kind in ("g0", "g1"):
                                gi = 0 if kind == "g0" else 1
                                P.act(sgT[:, gi, c0:c0 + w], ps[:, b, :w], AF.Silu)
                            else:
                                P.copy(gd[0:32, c0:c0 + w], ps[0:32, b, :w])
                        if sub == "E1c" or _DBG.get("notm"):
                            continue
                        for tt in range(w // 128):
                            tg = c0 // 128 + tt
                            b = rb.next()
                            for k in range(8):
                                P.mm(ps[:, b, 0:384], hcb[:, k, tt * 128:(tt + 1) * 128], Wg[:, k, 128:512], k == 0, k == 7)
                            P.copy(ktok[:, tg, :], ps[:, b, 0:128])
                            P.copy(vtok[:, tg, :], ps[:, b, 128:384], eng="act")
                if sub == "E1":
                    return
                sflat = stage.rearrange("p a b -> p (a b)")
                sp_tok = sflat[:, 0:2304].rearrange("p (t c) -> p t c", c=128)
                sbf = sflat[:, 2304:8192].bitcast(BF16)
                q_t = sbf[:, 0:2304]
                k_t = sbf[:, 2304:4608]
                kk = sbf[:, 4608:6912].rearrange("p (t c) -> p t c", c=128)
                Sb = sbf[:, 6912:9216].rearrange("p (i c) -> p i c", c=64)
                with ExitStack() as s2:
                    te = sb("g_te", [128, 512], F32, s2)
                    ec = sb("g_ec", [128, 512], F32, s2)
                    en = sb("g_en", [128, 512], F32, s2)
                    esf = sb("g_esf", [128, 512], F32, s2)
                    dec = sb("g_dec", [128, 36], F32, s2)
                    Sall = sb("g_Sall", [128, 37, 64], F32, s2)
                    attb = sb("g_attb", [128, 2, 4, 128], BF16, s2)
                    groups = [(0, 4), (4, 4), (8, 4), (12, 4), (16, 2)]
                    for d in range(2):
                        for (t0, n) in groups:
                            b = rb.next()
                            for i in range(n):
                                t = t0 + i
                                P.mm(ps[:, b, i * 128:(i + 1) * 128], gd[0:33, t * 128:(t + 1) * 128], wup[0:33, d, :], True, True)
                            P.act(te[:, 0:n * 128], ps[:, b, 0:n * 128], AF.Exp, scale=-1.0)
                            P.act(sp_tok[:, t0:t0 + n, :], te[:, 0:n * 128].rearrange("p (t c) -> p t c", c=128), AF.Ln, bias=1.0)
                        for (t0, n) in groups:
                            bC = rb.next()
                            bS = rb.next()
                            for i in range(n):
                                t = t0 + i
                                P.mm(ps[:, bC, i * 128:(i + 1) * 128], sp_tok[:, t, :], msk[:, d, :], True, True)
                                P.mm(ps[:, bS, i * 128:(i + 1) * 128], msk[:, 2 + d, :], sp_tok[:, t, :], True, True)
                            nn = n * 128
                            cols = slice(t0 * 128, t0 * 128 + nn)
                            P.act(ec[:, 0:nn], ps[:, bC, 0:nn], AF.Exp)
                            P.act(en[:, 0:nn], ps[:, bC, 0:nn], AF.Exp, scale=-1.0)
                            P.act(esf[:, 0:nn], ps[:, bS, 0:nn], AF.Exp)
                            P.tt(q_t[:, cols], qT[:, cols], ec[:, 0:nn], ALU.mult)
                            P.tt(k_t[:, cols], kT[:, cols], en[:, 0:nn], ALU.mult, eng="pool")
                            P.tt(kk[:, t0:t0 + n, :], ktok[:, t0:t0 + n, :], esf[:, 0:nn].rearrange("p (t c) -> p t c", c=128), ALU.mult)
                            ecv = ec[:, 0:nn].rearrange("p (i c) -> p i c", c=64)
                            pick = 63 if d == 0 else 0
                            P.copy(dec[:, 2 * t0:2 * t0 + 2 * n], ecv[:, :, pick], eng="pool")
                        if sub == "E2":
                            continue
                        P.memset(Sall[:, 0, :], 0.0)
                        order = list(range(36)) if d == 0 else ([3, 2, 1, 0] + list(range(35, 3, -1)))
                        pos_of = {ci_: i_ for i_, ci_ in enumerate(order)}
                        for idx_, ci in enumerate(order):
                            t, c = ci // 2, ci % 2
                            r0 = 64 * c
                            b = rb.next()
                            for h in range(4):
                                o_ap = ps[32 * h:32 * h + 32, b, 0:64]
                                l_ap = kk[r0:r0 + 64, t, 32 * h:32 * h + 32]
                                r_ap = vtok[r0:r0 + 64, t, 64 * h:64 * h + 64]
                                P._rec("pe", (lambda e, o_ap=o_ap, l_ap=l_ap, r_ap=r_ap, r0=r0, h=h:
                                              e.matmul(o_ap, l_ap, r_ap, start=True, stop=True, tile_position=(r0, 32 * h))),
                                       [l_ap, r_ap], [o_ap])
                            P.stt(Sall[:, idx_ + 1, :], Sall[:, idx_, :], dec[:, ci:ci + 1], ps[:, b, 0:64], ALU.mult, ALU.add)
                        P.copy(Sb[:, :, :], Sall[:, 0:36, :], eng="pool")
                        if sub == "E3":
                            continue
                        rbo = _Rot([4, 5, 6, 7])
                        for t in range(NT):
                            if last and t < 2:
                                continue
                            tc_ = slice(t * 128, (t + 1) * 128)
                            bA = 0
                            for h in range(4):
                                o_ap = ps[:, bA + h, 0:128]
                                l_ap = k_t[32 * h:32 * h + 32, tc_]
                                r_ap = q_t[32 * h:32 * h + 32, tc_]
                                P._rec("pe", (lambda e, o_ap=o_ap, l_ap=l_ap, r_ap=r_ap, h=h:
                                              e.matmul(o_ap, l_ap, r_ap, start=True, stop=True, tile_position=(32 * h, 0))),
                                       [l_ap, r_ap], [o_ap])
                            ab = attb[:, t % 2]
                            P.tt(ab, ps[:, bA:bA + 4, 0:128], _bc(msk[:, 4 + d, :], 1, 4), ALU.mult)
                            bO = rbo.next()
                            for h in range(4):
                                po = 64 * (h % 2)
                                cb = (h // 2) * 128
                                o_ap = ps[po:po + 64, bO, cb:cb + 128]
                                l_ap = vtok[:, t, 64 * h:64 * h + 64]
                                r_ap = ab[:, h, :]
                                P._rec("pe", (lambda e, o_ap=o_ap, l_ap=l_ap, r_ap=r_ap, po=po:
                                              e.matmul(o_ap, l_ap, r_ap, start=True, stop=False, tile_position=(0, po))),
                                       [l_ap, r_ap], [o_ap])
                                for c in range(2):
                                    ci = 2 * t + c
                                    o2 = ps[po:po + 64, bO, cb + 64 * c:cb + 64 * c + 64]
                                    l2 = Sb[32 * h:32 * h + 32, pos_of[ci], :]
                                    r2 = q_t[32 * h:32 * h + 32, t * 128 + 64 * c:t * 128 + 64 * c + 64]
                                    P._rec("pe", (lambda e, o2=o2, l2=l2, r2=r2, h=h, po=po, c=c:
                                                  e.matmul(o2, l2, r2, start=False, stop=(c == 1), tile_position=(32 * h, po))),
                                           [l2, r2], [o2])
                            pso = ps[:, bO, 0:256].rearrange("p (a c) -> p a c", c=128)
                            if d == 0:
                                P.copy(oT[:, :, tc_], pso, eng="act")
                            else:
                                P.tt(oT[:, :, tc_], oT[:, :, tc_], pso, ALU.add)
                    if sub in ("E2", "E3"):
                        return
                    sq = sb("g_sq", [128, 2, 512], BF16, s2)
                    rs = sb("g_rs", [128, 512], F32, s2)
                    tmp = sb("g_tmp", [128, 512], F32, s2)
                    for (c0, w, who) in lchunks:
                        P.act(sq[:, :, :w], oT[:, :, c0:c0 + w], AF.Square)
                        for m in range(2):
                            b = rb.next()
                            P.mm(ps[:, b, :w], bo64[:], sq[:, m, :w], True, True)
                            P.act(rs[:, :w], ps[:, b, :w], AF.Sqrt, bias=EPS * 64.0, scale=1.0)
                            P.recip(rs[:, :w], rs[:, :w])
                            P.stt(tmp[:, :w], oT[:, m, c0:c0 + w], g8[:, 0:1], rs[:, :w], ALU.mult, ALU.mult)
                            P.tt(sgT[:, m, c0:c0 + w], tmp[:, :w], sgT[:, m, c0:c0 + w], ALU.mult, eng="pool")
                out_proj(l, Wo, sgT, 2, lchunks, rb)
                P.barrier()

        def att_phase(l, last, lchunks):
            with ExitStack() as s1:
                Wo = sb("a_Wo", [128, 4, 1024], BF16, s1)
                qr = sb("a_qr", [128, 4, T], BF16, s1)
                kd = sb("a_kd", [128, 2, T], BF16, s1)
                vt = sb("a_vt", [128, NT, 128], BF16, s1)
                g8 = sb("a_g8", [128, 2], F32, s1)
                rb = _Rot([0, 1, 2, 3, 4, 5, 6, 7])
                P.ts(g8[:, 0:1], V1T[:, 42 + l:43 + l], 8.0, None, ALU.mult)
                P.ts(g8[:, 1:2], V1T[:, 44 + l:45 + l], 8.0, None, ALU.mult)
                load_w(Wo, wout_d[l][512:1024, :], 1024, srot)
                with ExitStack() as s2:
                    Wq = sb("a_Wq", [128, 8, 512], BF16, s2)
                    Wk = sb("a_Wk", [128, 8, 256], BF16, s2)
                    Wv = sb("a_Wv", [128, 8, 128], BF16, s2)
                    psw = sb("a_psw", [128, 128], F32, s2)
                    sflat_a = stage.rearrange("p a b -> p (a b)")
                    rc = sflat_a[:, 0:T]
                    rsn = sflat_a[:, T:2 * T]
                    hcb = sb("a_hc", [128, 8, 512], BF16, s2)
                    qg2 = sb("a_qg", [128, 2, 512], F32, s2)
                    sq2 = sb("a_sq", [128, 2, 512], BF16, s2)
                    rs2 = sb("a_rsd", [128, 2, 512], F32, s2)
                    t12 = sb("a_t1", [128, 2, 512], F32, s2)
                    t22 = sb("a_t2", [128, 2, 512], F32, s2)
                    P.dma(psw[:], psw_d)
                    load_w(Wq, win_d[l][:, 1056:1568], 512, srot)
                    load_w(Wv, win_d[l][:, 1696:1824], 128, srot)
                    sl = srot.next()
                    sv = stage[:, sl, :].rearrange("p (k n) -> p k n", n=256)
                    for g in range(2):
                        for r in range(2):
                            P.dma(sv[:, :, (2 * g + r) * 64:(2 * g + r + 1) * 64],
                                  win_d[l][:, 1568 + 64 * g:1568 + 64 * g + 64].rearrange("(k p) n -> p k n", p=128))
                    P.copy(Wk[:], sv, eng="act")
                    P.dma(rc, ropec_d)
                    P.dma(rsn, ropes_d)
                    tcnt = 0
                    for ci, (c0, w, who) in enumerate(CHUNKS):
                        P.dma(hcb[:, :, :w], hT_d[:, :, c0:c0 + w])
                        for i in range(6):
                            qg = qg2[:, tcnt % 2]
                            sq = sq2[:, tcnt % 2]
                            rs = rs2[:, tcnt % 2]
                            t1 = t12[:, tcnt % 2]
                            t2 = t22[:, tcnt % 2]
                            tcnt += 1
                            isq = i < 4
                            b0 = rb.next()
                            for k in range(8):
                                wsl = Wq[:, k, i * 128:(i + 1) * 128] if isq else Wk[:, k, (i - 4) * 128:(i - 3) * 128]
                                P.mm(ps[:, b0, :w], wsl, hcb[:, k, :w], k == 0, k == 7)
                            gcol = g8[:, 0:1] if isq else g8[:, 1:2]
                            P.act(qg[:, :w], ps[:, b0, :w], AF.Identity, scale=gcol)
                            P.act(sq[:, :w], ps[:, b0, :w], AF.Square)
                            b1 = rb.next()
                            P.mm(ps[:, b1, :w], bo64[:], sq[:, :w], True, True)
                            P.act(rs[:, :w], ps[:, b1, :w], AF.Sqrt, bias=EPS * 64.0, scale=1.0)
                            P.recip(rs[:, :w], rs[:, :w])
                            b2 = rb.next()
                            P.mm(ps[:, b2, :w], psw[:], qg[:, :w], True, True)
                            P.tt(t1[:, :w], qg[:, :w], rc[:, c0:c0 + w], ALU.mult, eng="pool")
                            P.tt(t2[:, :w], ps[:, b2, :w], rsn[:, c0:c0 + w], ALU.mult)
                            P.tt(t1[:, :w], t1[:, :w], t2[:, :w], ALU.add, eng="pool")
                            dst = qr[:, i, c0:c0 + w] if isq else kd[:, i - 4, c0:c0 + w]
                            P.tt(dst, t1[:, :w], rs[:, :w], ALU.mult)
                        for tt in range(w // 128):
                            tg = c0 // 128 + tt
                            b = rb.next()
                            for k in range(8):
                                P.mm(ps[:, b, 0:128], hcb[:, k, tt * 128:(tt + 1) * 128], Wv[:, k, :], k == 0, k == 7)
                            P.copy(vt[:, tg, :], ps[:, b, 0:128], eng="act")
                with ExitStack() as s2:
                    amix = sb("a_mix", [128, 4, T], BF16, s2)
                    PT = sb("a_PT", [128, 4, 512], BF16, s2)
                    rd = sb("a_rd", [128, 2, 512], F32, s2)
                    rS = _Rot([0, 1, 2, 3])
                    rO = _Rot([(4, 5), (6, 7)])
                    rP = _Rot([0, 1, 2, 3])
                    for (c0, w, who) in lchunks:
                        kts = list(range(2)) if who == 1 else list(range(NT))
                        for h in range(8):
                            g = h // 4
                            qt = h // 2
                            po = 64 * (h % 2)
                            bO, bD = rO.next()
                            sbank = {}

                            def score(kt_):
                                bS_ = rS.next()
                                sbank[kt_] = bS_
                                P.mm(ps[:, bS_, :w], kd[po:po + 64, g, kt_ * 128:(kt_ + 1) * 128], qr[po:po + 64, qt, c0:c0 + w], True, True)

                            for kt_ in kts[:3]:
                                score(kt_)
                            for idx, kt in enumerate(kts):
                                bS = sbank[kt]
                                pt = PT[:, rP.next(), :w]
                                P.act(pt, ps[:, bS, :w], AF.Exp, scale=0.125)
                                if idx + 3 < len(kts):
                                    score(kts[idx + 3])
                                P.mm(ps[po:po + 64, bO, :w], vt[:, kt, g * 64:(g + 1) * 64], pt, idx == 0, idx == len(kts) - 1)
                                P.mm(ps[po:po + 64, bD, :w], ones_bf[:, 0:64], pt, idx == 0, idx == len(kts) - 1)
                            P.recip(rd[po:po + 64, h % 2, :w], ps[po:po + 64, bD, :w])
                            P.tt(amix[po:po + 64, qt, c0:c0 + w], ps[po:po + 64, bO, :w], rd[po:po + 64, h % 2, :w], ALU.mult)
                    out_proj(l, Wo, amix, 4, lchunks, rb)
                P.barrier()

        def moe_phase(l, last, lchunks):
            with ExitStack() as s1:
                h2T = sb("m_h2T", [128, 8, T], BF16, s1)
                GT = sb("m_GT", [128, T], BF16, s1)
                esel = sb("m_esel", [128, 16, 128], BF16, s1)
                P.dma(esel[0:16], esel_d)
                tiles = list(range(2, NT)) if last else list(range(NT))
                with ExitStack() as s2:
                    sq = sb("m_sq", [128, 8, 512], BF16, s2)
                    xn = sb("m_xn", [128, 8, 512], F32, s2)
                    rs = sb("m_rs", [128, 512], F32, s2)
                    wr = sb("m_wr", [128, 8, 16], F32, s2)
                    brep = sb("m_brep", [128, 288], F32, s2)
                    s_tok = sb("m_s", [128, NT, 16], F32, s2)
                    sel2 = sb("m_sel2", [128, 72, 8], F32, s2)
                    p1 = sb("m_p1", [128, 72, 4], F32, s2)
                    p2 = sb("m_p2", [128, 72, 2], F32, s2)
                    gs = sb("m_gs", [128, 72], F32, s2)
                    gs2 = sb("m_gs2", [128, 72], F32, s2)
                    gmax = sb("m_gmax", [128, NT], F32, s2)
                    oh = sb("m_oh", [128, 72], F32, s2)
                    cnt = sb("m_cnt", [128, 72, 4], F32, s2)
                    c2 = sb("m_c2", [128, 72, 4], F32, s2)
                    wsum = sb("m_wsum", [128, NT], F32, s2)
                    gate = sb("m_gate", [128, NT, 16], F32, s2)
                    P.dma(wr[:], wr_d.rearrange("(k p) e -> p k e", p=128))
                    P.dma(brep[:], brep_d)
                    P.memset(s_tok[:], 0.0)
                    for ci, (c0, w, who) in enumerate(lchunks):
                        norm_chunk(l, 1, c0, w, who, sq, xn, rs, ci % 2)
                        for j in range(8):
                            if j % 2 == 0:
                                P.ts(xn[:, j, :w], xn[:, j, :w], A_of(l, 1, j, who), B_of(l, 1, j, who), ALU.mult, ALU.add)
                            else:
                                P.act(xn[:, j, :w], xn[:, j, :w], AF.Identity, bias=B_of(l, 1, j, who), scale=A_of(l, 1, j, who))
                        P.copy(h2T[:, :, c0:c0 + w], xn[:, :, :w], eng="act")
                        b = 2 + ci % 2
                        for tt in range(w // 128):
                            tg = c0 // 128 + tt
                            for k in range(8):
                                P.mm(ps[:, b, tt * 16:(tt + 1) * 16], xn[:, k, tt * 128:(tt + 1) * 128], wr[:, k, :], k == 0, k == 7)
                        nt_ = w // 128
                        tg0 = c0 // 128
                        P.act(s_tok[:, tg0:tg0 + nt_, :], ps[:, b, 0:nt_ * 16].rearrange("p (t e) -> p t e", e=16), AF.Exp, scale=-1.0)
                        P.ts(s_tok[:, tg0:tg0 + nt_, :], s_tok[:, tg0:tg0 + nt_, :], 1.0, None, ALU.add)
                        P.recip(s_tok[:, tg0:tg0 + nt_, :], s_tok[:, tg0:tg0 + nt_, :])
                    sv = s_tok.rearrange("p t (g e) -> p (t g) e", e=4)
                    bv = brep.rearrange("p (a e) -> p a e", e=4)
                    P.tt(sel2[:, :, 0:4], sv, bv, ALU.add)
                    P.tt(sel2[:, :, 4:8], sv, bv, ALU.add)
                    P.tt(p1[:], sel2[:, :, 0:4], sel2[:, :, 1:5], ALU.add)
                    P.tt(p2[:], sel2[:, :, 0:2], sel2[:, :, 2:4], ALU.add)
                    P.reduce(gs[:], p1[:], ALU.max)
                    P.reduce(gs2[:], p2[:], ALU.max)
                    P.tt(gs[:], gs[:], gs2[:], ALU.max)
                    P.reduce(gmax[:], gs.rearrange("p (t g) -> p t g", g=4), ALU.max)
                    P.tt(oh.rearrange("p (t g) -> p t g", g=4), gs.rearrange("p (t g) -> p t g", g=4), _bc(gmax[:], 2, 4), ALU.is_equal)
                    P.tt(cnt[:], sel2[:, :, 1:5], sel2[:, :, 0:4], ALU.is_gt)
                    P.tt(c2[:], sel2[:, :, 2:6], sel2[:, :, 0:4], ALU.is_gt)
                    P.tt(cnt[:], cnt[:], c2[:], ALU.add)
                    P.tt(c2[:], sel2[:, :, 3:7], sel2[:, :, 0:4], ALU.is_gt)
                    P.tt(cnt[:], cnt[:], c2[:], ALU.add)
                    P.ts(cnt[:], cnt[:], 1.0, None, ALU.is_le)
                    P.tt(cnt[:], cnt[:], _bc(oh[:], 2, 4), ALU.mult)
                    gv = gate.rearrange("p t (g e) -> p (t g) e", e=4)
                    P.tt(gv, sv, cnt[:], ALU.mult)
                    P.reduce(wsum[:], gate[:], ALU.add)
                    P.ts(wsum[:], wsum[:], 1e-30, None, ALU.max)
                    P.recip(wsum[:], wsum[:])
                    P.tt(gate[:], gate[:], _bc(wsum[:], 2, 16), ALU.mult)
                    for t in tiles:
                        b = 4 + (t // 4) % 2
                        P.transpose(ps[0:16, b, (t % 4) * 128:(t % 4 + 1) * 128], gate[:, t, :], ident[:])
                        P.copy(GT[0:16, t * 128:(t + 1) * 128], ps[0:16, b, (t % 4) * 128:(t % 4 + 1) * 128])
                with ExitStack() as s2:
                    wbuf = sb("m_wbuf", [128, 3, 3, 2048], BF16, s2)
                    sg = sb("m_sg", [128, 2, 512], F32, s2)
                    t1 = sb("m_t1", [128, 2, 512], F32, s2)
                    abuf = sb("m_a", [128, 2, 2, 512], BF16, s2)
                    rG = _Rot([0, 1])
                    rU = _Rot([2, 3])
                    rD = _Rot([5, 6, 7])
                    it = 0
                    pending = None

                    def emit_down(pd):
                        Wd_p, ab_p, c0p, wp, whop = pd
                        for m in range(8):
                            bD = rD.next()
                            for ft in range(2):
                                P.mm(ps[:, bD, :wp], Wd_p[:, ft, m * 128:(m + 1) * 128], ab_p[:, ft, :wp], ft == 0, ft == 1)
                            P.stt(xT[:, m, c0p:c0p + wp], ps[:, bD, :wp], G_of(l, 1, m, whop), xT[:, m, c0p:c0p + wp], ALU.mult, ALU.add)

                    for e in range(16):
                        for fh in range(2):
                            u = e * 2 + fh
                            ws = u % 3
                            Wg_ = wbuf[:, ws, 0].rearrange("p (k f) -> p k f", f=256)
                            Wu_ = wbuf[:, ws, 1].rearrange("p (k f) -> p k f", f=256)
                            Wd_ = wbuf[:, ws, 2].rearrange("p (k d) -> p k d", d=1024)
                            for mi, (dst, srcw) in enumerate(((Wg_, weg_d[l, e][:, fh * 256:(fh + 1) * 256]),
                                                              (Wu_, weu_d[l, e][:, fh * 256:(fh + 1) * 256]))):
                                sl = srot.next()
                                sv_ = stage[:, sl, :].rearrange("p (k f) -> p k f", f=256)
                                P.dma(sv_, srcw.rearrange("(k p) f -> p k f", p=128))
                                P.copy(dst, sv_, eng="act")
                            sl = srot.next()
                            sv_ = stage[:, sl, :].rearrange("p (k d) -> p k d", d=1024)
                            P.dma(sv_, wed_d[l, e][fh * 256:(fh + 1) * 256, :].rearrange("(k p) d -> p k d", p=128))
                            P.copy(Wd_, sv_, eng="act")
                            for (c0, w, who) in lchunks:
                                ab = abuf[:, it % 2]
                                it += 1
                                P.mm(ps[:, 4, :w], esel[0:16, e, :], GT[0:16, c0:c0 + w], True, True)
                                for ft in range(2):
                                    bG = rG.next()
                                    bU = rU.next()
                                    for k in range(8):
                                        P.mm(ps[:, bG, :w], Wg_[:, k, ft * 128:(ft + 1) * 128], h2T[:, k, c0:c0 + w], k == 0, k == 7)
                                    for k in range(8):
                                        P.mm(ps[:, bU, :w], Wu_[:, k, ft * 128:(ft + 1) * 128], h2T[:, k, c0:c0 + w], k == 0, k == 7)
                                    P.act(sg[:, ft, :w], ps[:, bG, :w], AF.Silu)
                                    P.tt(t1[:, ft, :w], sg[:, ft, :w], ps[:, bU, :w], ALU.mult)
                                    P.tt(ab[:, ft, :w], t1[:, ft, :w], ps[:, 4, :w], ALU.mult)
                                if pending is not None:
                                    emit_down(pending)
                                pending = (Wd_, ab, c0, w, who)
                    emit_down(pending)
                P.barrier()

        for l in range(n_layers):
            last = (l == n_layers - 1) and (n_layers == 2)
            lchunks = CHUNKS[1:] if last else CHUNKS

            with ExitStack() as s1:
                sq = sb("b_sq", [128, 8, 512], BF16, s1)
                xn = sb("b_xn", [128, 8, 512], F32, s1)
                rs = sb("b_rs", [128, 512], F32, s1)
                hc = sb("b_hc", [128, 2, 8, 512], BF16, s1)
                for ci, (c0, w, who) in enumerate(CHUNKS):
                    norm_chunk(l, 0, c0, w, who, sq, xn, rs, ci % 2)
                    for j in range(8):
                        if j % 2 == 0:
                            P.ts(hc[:, ci % 2, j, :w], xn[:, j, :w], A_of(l, 0, j, who), B_of(l, 0, j, who), ALU.mult, ALU.add)
                        else:
                            P.act(hc[:, ci % 2, j, :w], xn[:, j, :w], AF.Identity, bias=B_of(l, 0, j, who), scale=A_of(l, 0, j, who))
                    P.dma(hT_d[:, :, c0:c0 + w], hc[:, ci % 2, :, :w])
                P.barrier()
            if stop_at == ("B", l):
                break

            with ExitStack() as s1:
                Wu = sb("f_Wu", [128, 8, 256], BF16, s1)
                Wo = sb("f_Wo", [128, 2, 1024], BF16, s1)
                cs64 = sb("f_cs64", [128, 256], BF16, s1)
                uT = sb("f_uT", [128, 2, T], BF16, s1)
                ucs = sb("f_ucs", [128, NT, 512], BF16, s1)
                fmix = sb("f_mix", [128, 2, T], BF16, s1)
                hcb = sb("f_hc", [128, 2, 8, 512], BF16, s1)
                tab = sb("f_tab", [128, 1, 2, 16, 512], BF16, s1)
                P.dma(cs64[:], cs64_d)
                load_w(Wu, win_d[l][:, 0:256], 256, srot)
                load_w(Wo, wout_d[l][0:256, :], 1024, srot)
                rb = _Rot([0, 1, 2, 3])
                for ci, (c0, w, who) in enumerate(CHUNKS):
                    P.dma(hcb[:, ci % 2, :, :w], hT_d[:, :, c0:c0 + w])
                    for m in range(2):
                        b = rb.next()
                        for k in range(8):
                            P.mm(ps[:, b, :w], Wu[:, k, m * 128:(m + 1) * 128], hcb[:, ci % 2, k, :w], k == 0, k == 7)
                        P.copy(uT[:, m, c0:c0 + w], ps[:, b, :w], eng=("act" if m == 0 else "dve"))
                t_lo = 2 if last else 0
                for t in range(t_lo, NT):
                    b = rb.next()
                    for j in range(2):
                        P.mm(ps[:, b, j * 256:(j + 1) * 256], uT[:, j, t * 128:(t + 1) * 128], cs64[:], True, True)
                    P.copy(ucs[:, t, :], ps[:, b, :], eng=("act" if t % 2 == 0 else "dve"))
                tab2 = tab.rearrange("p a c t n -> p (a c t n)").rearrange("p (b c t n) -> p b c t n", b=2, c=2, t=16)
                for pc in range(8):
                    bufi = pc % 2
                    P.dma(tab2[:, bufi, 0], cn_d[:, pc * 256:(pc + 1) * 256].rearrange("(t p) c -> p t c", p=128))
                    P.dma(tab2[:, bufi, 1], sn_d[:, pc * 256:(pc + 1) * 256].rearrange("(t p) c -> p t c", p=128))
                    for j in range(2):
                        b = rb.next()
                        for t in range(16):
                            P.mm(ps[:, b, 0:256], ucs[:, 2 + t, j * 256:j * 256 + 128], tab2[:, bufi, 0, t, :], t == 0, False)
                            P.mm(ps[:, b, 0:256], ucs[:, 2 + t, j * 256 + 128:j * 256 + 256], tab2[:, bufi, 1, t, :], False, t == 15)
                        P.copy(fmix[:, j, 256 + pc * 256:256 + (pc + 1) * 256], ps[:, b, 0:256], eng=("act" if j == 0 else "dve"))
                if not last:
                    c2 = sb("f_c2", [128, 2, 2, 256], BF16, s1)
                    P.dma(c2[:, 0], c256_d.rearrange("(t p) c -> p t c", p=128))
                    P.dma(c2[:, 1], s256_d.rearrange("(t p) c -> p t c", p=128))
                    for j in range(2):
                        b = rb.next()
                        for t in range(2):
                            P.mm(ps[:, b, 0:256], ucs[:, t, j * 256:j * 256 + 128], c2[:, 0, t, :], t == 0, False)
                            P.mm(ps[:, b, 0:256], ucs[:, t, j * 256 + 128:j * 256 + 256], c2[:, 1, t, :], False, t == 1)
                        P.copy(fmix[:, j, 0:256], ps[:, b, 0:256])
                out_proj(l, Wo, fmix, 2, lchunks, rb)
                P.barrier()
            dump_dbg(4 * l + 0)
            if stop_at == ("D", l):
                break

            if 'E' not in skip:
                gla_phase(l, last, lchunks)
            dump_dbg(4 * l + 1)
            if stop_at == ("E", l):
                break

            if 'F' not in skip:
                att_phase(l, last, lchunks)
            dump_dbg(4 * l + 2)
            if stop_at == ("F", l):
                break

            moe_phase(l, last, lchunks)
            dump_dbg(4 * l + 3)
            if stop_at == ("I", l):
                break

        if stop_at is None:
            with ExitStack() as s1:
                sq = sb("o_sq", [128, 8, 512], BF16, s1)
                xn = sb("o_xn", [128, 8, 512], F32, s1)
                rs = sb("o_rs", [128, 512], F32, s1)
                ot = sb("o_ot", [128, 2, 1024], F32, s1)
                fn32 = sb("o_fn", [128, 8], F32, s1)
                P.ts(fn32[:], V1T[:, 32:40], 32.0, None, ALU.mult)
                ev = 0
                for ci, (c0, w, who) in enumerate(CHUNKS[1:]):
                    norm_chunk(0, 0, c0, w, who, sq, xn, rs, 0)
                    for j in range(8):
                        if j % 2 == 0:
                            P.ts(xn[:, j, :w], xn[:, j, :w], fn32[:, j:j + 1], None, ALU.mult)
                        else:
                            P.act(xn[:, j, :w], xn[:, j, :w], AF.Identity, scale=fn32[:, j:j + 1])
                    for tt in range(4):
                        tg = ci * 4 + tt
                        for half in range(2):
                            b = 1 + (2 * tg + half) % 4
                            for jj in range(4):
                                j = half * 4 + jj
                                P.transpose(ps[:, b, jj * 128:(jj + 1) * 128], xn[:, j, tt * 128:(tt + 1) * 128], ident[:])
                            dst = ot[:, tg % 2, half * 512:(half + 1) * 512]
                            if ev % 2 == 0:
                                P.copy(dst, ps[:, b, :])
                            else:
                                P.copy(dst, ps[:, b, :], eng="act")
                            ev += 1
                        P.dma(y_d[tg * 128:(tg + 1) * 128, :], ot[:, tg % 2, :])
        P.emit()
        print("arena peak bytes", astate["peak"], "ops", P.nops)
    return nc


_CONST_CACHE = {}


def _consts():
    if _CONST_CACHE:
        return _CONST_CACHE
    bf = ml_dtypes.bfloat16
    c = {}
    c["ident"] = np.eye(128, dtype=np.float32)
    k = np.arange(64)
    ang = 2 * np.pi * np.outer(k, k) / 64.0
    C64 = np.cos(ang) / 8.0
    S64 = np.sin(ang) / 8.0
    cs = np.zeros((128, 256), np.float64)
    for g in range(2):
        cs[g * 64:(g + 1) * 64, g * 64:(g + 1) * 64] = C64
        cs[g * 64:(g + 1) * 64, 128 + g * 64:128 + (g + 1) * 64] = S64
    c["cs64"] = cs.astype(bf)
    for n, cn, sn in ((2048, "cn", "sn"), (256, "c256", "s256")):
        i = np.arange(n)
        a = 2 * np.pi * (np.outer(i, i) % n) / float(n)
        c[cn] = (np.cos(a) / np.sqrt(n)).astype(bf)
        c[sn] = (-np.sin(a) / np.sqrt(n)).astype(bf)
    inv = 10000.0 ** (-np.arange(16, dtype=np.float64) * 2.0 / 32.0)
    tok = np.arange(2048)
    row = tok // 64
    col = tok % 64
    rc = np.ones((128, T), np.float64)
    rs = np.zeros((128, T), np.float64)
    for hd in range(128):
        a = (hd % 64) // 32
        f = hd % 16
        pos = row if a == 0 else col
        rc[hd, 256:] = np.cos(pos * inv[f])
        rs[hd, 256:] = np.sin(pos * inv[f])
    c["ropec"] = rc.astype(np.float32)
    c["ropes"] = rs.astype(np.float32)
    psw = np.zeros((128, 128), np.float32)
    for hdp in range(128):
        half = (hdp % 32) // 16
        if half == 0:
            psw[hdp + 16, hdp] = -1.0
        else:
            psw[hdp - 16, hdp] = 1.0
    c["psw"] = psw
    j = np.arange(128)[:, None]
    i = np.arange(128)[None, :]
    same = (j // 64) == (i // 64)
    m = np.zeros((128, 6, 128), np.float32)
    m[:, 0, :] = np.where(same & (j <= i), -1.0 / 16.0, 0.0)
    m[:, 1, :] = np.where(same & (j >= i), -1.0 / 16.0, 0.0)
    m[:, 2, :] = np.where(same & (j > i), -1.0 / 16.0, 0.0)
    m[:, 3, :] = np.where(same & (j < i), -1.0 / 16.0, 0.0)
    m[:, 4, :] = np.where(same & (j <= i), 1.0, 0.0)
    m[:, 5, :] = np.where(same & (j >= i), 1.0, 0.0)
    c["masks"] = m
    c["bo64"] = same.astype(np.float32).astype(bf)
    es = np.zeros((16, 16, 128), np.float32)
    for e in range(16):
        es[e, e, :] = 1.0
    c["esel"] = es.astype(bf)
    _CONST_CACHE.update(c)
    return _CONST_CACHE


_NC_CACHE = {}


def _prep_inputs(inputs):
    f = lambda a: np.ascontiguousarray(np.asarray(a, dtype=np.float32))
    x = f(inputs["x"])
    c = f(inputs["c"])
    ctx = f(inputs["ctx"])
    c_ctx = f(inputs["c_ctx"])
    b_ada = f(inputs["b_ada"])
    cst = _consts()
    vecs1 = np.concatenate([
        f(inputs["norm_mix"]).reshape(16, 128),
        f(inputs["norm_ffn"]).reshape(16, 128),
        f(inputs["final_norm"]).reshape(8, 128),
        np.tile(f(inputs["gla_norm"]), (1, 2)),
        np.tile(f(inputs["q_norm"]), (1, 2)),
        np.tile(f(inputs["k_norm"]), (1, 2)),
    ], axis=0)
    wg_ = f(inputs["w_gla_gate_up"])
    bg_ = f(inputs["b_gla_gate"])
    wup = np.zeros((2, 2, 33, 128), np.float32)
    for l_ in range(2):
        for d_ in range(2):
            wup[l_, d_, 16 * d_:16 * d_ + 16, :] = wg_[l_, d_]
            wup[l_, d_, 32, :] = bg_[l_, d_]
    brep = np.ascontiguousarray(np.broadcast_to(np.tile(f(inputs["b_router"]), 18)[None, :], (128, 288)))
    shared = {
        "vecs1": np.ascontiguousarray(vecs1),
        "w_ada": f(inputs["w_ada"]), "w_in": f(inputs["w_in"]), "w_out": f(inputs["w_out"]),
        "wup": np.ascontiguousarray(wup), "w_router": f(inputs["w_router"]), "brep": brep,
        "w_exp_gate": f(inputs["w_exp_gate"]), "w_exp_up": f(inputs["w_exp_up"]), "w_exp_down": f(inputs["w_exp_down"]),
    }
    for k_ in ("ident", "cs64", "cn", "sn", "c256", "s256", "ropec", "ropes", "psw", "masks", "bo64", "esel"):
        shared[k_] = cst[k_]
    in_maps = []
    for b in range(8):
        vecs0 = np.concatenate([c[b].reshape(8, 128), c_ctx.reshape(8, 128),
                                b_ada[0].reshape(48, 128), b_ada[1].reshape(48, 128)], axis=0)
        m = dict(shared)
        m["x"] = x[b]
        m["ctx"] = ctx[b]
        m["vecs0"] = np.ascontiguousarray(vecs0)
        in_maps.append(m)
    return in_maps


def kernel(**inputs):
    in_maps = _prep_inputs(inputs)
    if "nc" not in _NC_CACHE:
        _NC_CACHE["nc"] = build()
    nc = _NC_CACHE["nc"]
    res = run_bass_kernel_spmd(nc, in_maps, core_ids=list(range(8)))
    out = np.stack([np.asarray(r["y"], dtype=np.float32) for r in res.results], axis=0)
    return out
```

```python
import numpy as np
import ml_dtypes
import concourse.bass as bass
import concourse.mybir as mybir
from concourse.bass_utils import run_bass_kernel_spmd

F32 = mybir.dt.float32
BF16 = mybir.dt.bfloat16
AF = mybir.ActivationFunctionType
ALU = mybir.AluOpType
AX = mybir.AxisListType

_DSZ = {F32: 4, BF16: 2}


def _dsz(dt):
    if dt in _DSZ:
        return _DSZ[dt]
    s = str(dt)
    if "32" in s:
        return 4
    if "16" in s:
        return 2
    if "64" in s:
        return 8
    return 1


def _region(ap):
    t = ap.tensor
    name = t.name
    dsz = _dsz(ap.dtype)
    space = str(ap.space)
    off = int(ap.offset)
    if "DRAM" in space.upper() or "HBM" in space.upper():
        ext = 0
        for (s, c) in ap.ap:
            ext += abs(int(s)) * (int(c) - 1)
        return (name, 0, 1, off * dsz, (off + ext + 1) * dsz)
    shape = list(t.shape)
    fsz = 1
    for d in shape[1:]:
        fsz *= int(d)
    p0 = off // fsz
    f0 = off % fsz
    pext = 0
    fext = 0
    for (s, c) in ap.ap:
        s = int(s)
        c = int(c)
        if c <= 1 or s == 0:
            continue
        if s % fsz == 0:
            pext += (s // fsz) * (c - 1)
        else:
            fext += abs(s) * (c - 1)
    b0 = f0 * dsz
    b1 = (f0 + fext + 1) * dsz
    if "PSUM" in space.upper():
        return (name, 0, 128, (b0 // 2048) * 2048, ((b1 + 2047) // 2048) * 2048)
    return (name, p0, p0 + pext + 1, b0, b1)


class _Op:
    __slots__ = ("eng", "fn", "k", "is_dma", "deps_eng", "deps_dma", "need_signal", "sig_val",
                 "dma_sem", "dma_val", "dma_prev", "name")

    def __init__(self, eng, fn, is_dma, name=""):
        self.eng = eng
        self.fn = fn
        self.is_dma = is_dma
        self.k = -1
        self.deps_eng = {}
        self.deps_dma = []
        self.need_signal = False
        self.sig_val = 0
        self.dma_sem = None
        self.dma_val = 0
        self.dma_prev = None
        self.name = name


class Prog:
    ENGS = ("pe", "act", "dve", "pool", "sp")
    NDMA = {"sp": 40, "pool": 8, "act": 8}

    def __init__(self, nc):
        self.nc = nc
        self.eng_ops = {e: [] for e in self.ENGS}
        self.recs = {}
        self.dma_count = {q: 0 for q in self.NDMA}
        self.dma_last = {q: [None] * n for q, n in self.NDMA.items()}
        self.nops = 0

    def _dep(self, x, y):
        if y is x:
            return
        if y.is_dma:
            if y not in x.deps_dma:
                x.deps_dma.append(y)
            return
        if (not x.is_dma) and y.eng == x.eng:
            if x.eng == "pe":
                return
        cur = x.deps_eng.get(y.eng, -1)
        if y.k > cur:
            x.deps_eng[y.eng] = y.k
        y.need_signal = True

    def add(self, eng, fn, reads, writes, is_dma=False, name=""):
        op = _Op(eng, fn, is_dma, name)
        rr = [_region(a) for a in reads if a is not None]
        ww = [_region(a) for a in writes if a is not None]
        for (nm, p0, p1, b0, b1) in rr:
            is_ps = (nm == "ps")
            for rec in self.recs.get(nm, ()):
                if rec[0] < p1 and p0 < rec[1] and rec[2] < b1 and b0 < rec[3]:
                    if rec[4] or (is_ps and rec[5].eng != eng):
                        self._dep(op, rec[5])
        for (nm, p0, p1, b0, b1) in ww:
            for rec in self.recs.get(nm, ()):
                if rec[0] < p1 and p0 < rec[1] and rec[2] < b1 and b0 < rec[3]:
                    self._dep(op, rec[5])
        for (nm, p0, p1, b0, b1) in ww:
            lst = self.recs.setdefault(nm, [])
            lst[:] = [r for r in lst if not (p0 <= r[0] and r[1] <= p1 and b0 <= r[2] and r[3] <= b1)]
            lst.append((p0, p1, b0, b1, True, op))
        for (nm, p0, p1, b0, b1) in rr:
            lst = self.recs.setdefault(nm, [])
            if not is_dma:
                lst[:] = [r for r in lst if not ((not r[4]) and (not r[5].is_dma) and r[5].eng == eng
                                                 and p0 <= r[0] and r[1] <= p1 and b0 <= r[2] and r[3] <= b1)]
            lst.append((p0, p1, b0, b1, False, op))
        op.k = len(self.eng_ops[eng])
        self.eng_ops[eng].append(op)
        if is_dma:
            n = self.NDMA[eng]
            slot = self.dma_count[eng] % n
            self.dma_count[eng] += 1
            prev = self.dma_last[eng][slot]
            op.dma_sem = (eng, slot)
            op.dma_prev = prev
            op.dma_val = (prev.dma_val if prev is not None else 0) + 16
            self.dma_last[eng][slot] = op
        self.nops += 1
        return op

    def barrier(self):
        bop = _Op("sp", lambda e: e.nop(), False, "barrier")
        for e in self.ENGS:
            lst = [o for o in self.eng_ops[e] if not o.is_dma]
            if lst:
                y = lst[-1]
                if e == "sp":
                    continue
                bop.deps_eng[e] = y.k
                y.need_signal = True
        for q in self.NDMA:
            for y in self.dma_last[q]:
                if y is not None:
                    bop.deps_dma.append(y)
        bop.k = len(self.eng_ops["sp"])
        bop.need_signal = True
        self.eng_ops["sp"].append(bop)
        self.recs = {"__barrier__": [(0, 1, 0, 1, True, bop)]}
        self._barrier_op = bop
        self._barrier_seen = set()
        return bop

    def _barrier_dep(self, op):
        b = getattr(self, "_barrier_op", None)
        if b is None or op is b or op.eng == "sp" or op.eng in self._barrier_seen:
            return
        self._barrier_seen.add(op.eng)
        if b.k > op.deps_eng.get("sp", -1):
            op.deps_eng["sp"] = b.k

    def _rec(self, eng, fn, reads, writes, is_dma=False, name=""):
        op = self.add(eng, fn, reads, writes, is_dma, name)
        self._barrier_dep(op)
        if eng == "pe":
            l = reads[0]
            rr = lambda v: 32 if v <= 32 else (64 if v <= 64 else 128)
            op.name = (rr(int(l.shape[0])), rr(int(l.shape[-1])))
        return op

    def mm(self, out, lhsT, rhs, start=True, stop=True):
        return self._rec("pe", lambda e: e.matmul(out, lhsT, rhs, start=start, stop=stop), [lhsT, rhs], [out])

    def transpose(self, out, in_, ident):
        return self._rec("pe", lambda e: e.transpose(out, in_, ident), [in_, ident], [out])

    def act(self, out, in_, func, bias=None, scale=1.0, accum_out=None):
        reads = [in_]
        kw = {}
        if bias is not None:
            kw["bias"] = bias
            if not isinstance(bias, (int, float)):
                reads.append(bias)
        if not isinstance(scale, (int, float)):
            reads.append(scale)
        kw["scale"] = scale
        writes = [out]
        if accum_out is not None:
            kw["accum_out"] = accum_out
            writes.append(accum_out)
        return self._rec("act", lambda e: e.activation(out, in_, func, **kw), reads, writes)

    def tt(self, out, in0, in1, op, eng="dve"):
        return self._rec(eng, lambda e: e.tensor_tensor(out, in0, in1, op), [in0, in1], [out])

    def ts(self, out, in0, s1, s2, op0, op1=None, eng="dve", accum_out=None):
        reads = [in0]
        if not isinstance(s1, (int, float)):
            reads.append(s1)
        if s2 is not None and not isinstance(s2, (int, float)):
            reads.append(s2)
        writes = [out]
        kw = {}
        if accum_out is not None:
            kw["accum_out"] = accum_out
            writes.append(accum_out)
        if op1 is None:
            return self._rec(eng, lambda e: e.tensor_scalar(out, in0, s1, s2, op0, **kw), reads, writes)
        return self._rec(eng, lambda e: e.tensor_scalar(out, in0, s1, s2, op0, op1, **kw), reads, writes)

    def stt(self, out, in0, scalar, in1, op0, op1, eng="dve"):
        reads = [in0, in1]
        if not isinstance(scalar, (int, float)):
            reads.append(scalar)
        return self._rec(eng, lambda e: e.scalar_tensor_tensor(out, in0, scalar, in1, op0, op1), reads, [out])

    def copy(self, out, in_, eng="dve"):
        if eng == "act":
            return self._rec("act", lambda e: e.copy(out, in_), [in_], [out])
        return self._rec(eng, lambda e: e.tensor_copy(out, in_), [in_], [out])

    def recip(self, out, in_):
        return self._rec("dve", lambda e: e.reciprocal(out, in_), [in_], [out])

    def reduce(self, out, in_, op, axis=None, eng="dve"):
        ax = axis if axis is not None else AX.X
        return self._rec(eng, lambda e: e.tensor_reduce(out, in_, ax, op), [in_], [out])

    def memset(self, out, val, eng="dve"):
        return self._rec(eng, lambda e: e.memset(out, val), [], [out])

    def dma(self, out, in_, q="sp"):
        return self._rec(q, lambda e: e.dma_start(out=out, in_=in_), [in_], [out], is_dma=True)

    def emit(self):
        nc = self.nc
        self.barrier()
        from contextlib import ExitStack
        with ExitStack() as st:
            esem = {e: st.enter_context(nc.semaphore("c_" + e)) for e in self.ENGS}
            dsem = {}
            for q, n in self.NDMA.items():
                for i in range(n):
                    dsem[(q, i)] = st.enter_context(nc.semaphore("d_%s%d" % (q, i)))
            for e in self.ENGS:
                v = 0
                for op in self.eng_ops[e]:
                    if (not op.is_dma) and op.need_signal:
                        v += 1
                        op.sig_val = v
            prog = self

            pstate = {}

            def run(ename, eng):
                waited = {}
                for op in prog.eng_ops[ename]:
                    waits = []
                    for e2, k2 in op.deps_eng.items():
                        y = prog.eng_ops[e2][k2]
                        waits.append((("c", e2), esem[e2], y.sig_val))
                    for y in op.deps_dma:
                        waits.append((y.dma_sem, dsem[y.dma_sem], y.dma_val))
                    if op.is_dma and op.dma_prev is not None:
                        waits.append((op.dma_sem, dsem[op.dma_sem], op.dma_prev.dma_val))
                    for key, sem, val in waits:
                        if waited.get(key, 0) >= val:
                            continue
                        waited[key] = val
                        eng.wait_ge(sem, val)
                    if ename == "pe":
                        pass
                        pstate["mode"] = op.name
                    ins = op.fn(eng)
                    if op.is_dma:
                        ins.then_inc(dsem[op.dma_sem], 16)
                    elif op.need_signal:
                        ins.then_inc(esem[ename], 1)

            with nc.Block() as block:
                @block.tensor
                def _(eng):
                    run("pe", eng)

                @block.scalar
                def _(eng):
                    run("act", eng)

                @block.vector
                def _(eng):
                    run("dve", eng)

                @block.gpsimd
                def _(eng):
                    run("pool", eng)

                @block.sync
                def _(eng):
                    run("sp", eng)


T = 2304
NT = 18
KT = 8
EPS = 1e-6
CHUNKS = [(0, 256, 1), (256, 512, 0), (768, 512, 0), (1280, 512, 0), (1792, 512, 0)]


def _bc(ap, pos, n):
    shp = list(ap.shape)
    v = ap.unsqueeze(pos)
    shp.insert(pos, n)
    return v.to_broadcast(shp)


_DBG = {}


class _Rot:
    def __init__(self, items):
        self.items = list(items)
        self.i = 0

    def next(self):
        v = self.items[self.i % len(self.items)]
        self.i += 1
        return v


def build(n_layers=2, stop_at=None, dbg=False, skip=(), sub=None):
    from contextlib import ExitStack
    nc = bass.Bass("TRN2", target_bir_lowering=False)

    def din(name, shape, dt=F32):
        return nc.dram_tensor(name, shape, dt, kind="ExternalInput").ap()

    x_d = din("x", [2048, 1024])
    ctx_d = din("ctx", [256, 1024])
    vecs0_d = din("vecs0", [112, 128])
    vecs1_d = din("vecs1", [46, 128])
    wada_d = din("w_ada", [2, 1024, 6144])
    win_d = din("w_in", [2, 1024, 1824])
    wout_d = din("w_out", [2, 1024, 1024])
    wup_d = din("wup", [2, 2, 33, 128])
    wr_d = din("w_router", [1024, 16])
    brep_d = din("brep", [128, 288])
    need_moe = stop_at is None or stop_at[0] == "I" or stop_at[1] >= 1
    weg_d = weu_d = wed_d = None
    if need_moe:
        weg_d = din("w_exp_gate", [2, 16, 1024, 512])
        weu_d = din("w_exp_up", [2, 16, 1024, 512])
        wed_d = din("w_exp_down", [2, 16, 512, 1024])
    ident_d = din("ident", [128, 128])
    cs64_d = din("cs64", [128, 256], BF16)
    cn_d = din("cn", [2048, 2048], BF16)
    sn_d = din("sn", [2048, 2048], BF16)
    c256_d = din("c256", [256, 256], BF16)
    s256_d = din("s256", [256, 256], BF16)
    ropec_d = din("ropec", [128, T])
    ropes_d = din("ropes", [128, T])
    psw_d = din("psw", [128, 128])
    masks_d = din("masks", [128, 6, 128])
    bo64_d = din("bo64", [128, 128], BF16)
    esel_d = din("esel", [16, 16, 128], BF16)
    y_d = nc.dram_tensor("y", [2048, 1024], F32, kind="ExternalOutput").ap()
    hT_d = nc.dram_tensor("hT_scr", [128, 8, T], BF16).ap()
    dbg_d = None
    if dbg:
        dbg_d = nc.dram_tensor("dbg_x", [8, 128, 8, T], F32, kind="ExternalOutput").ap()

    st = ExitStack()
    with st:
        ARENA_BYTES = 210944
        arena_t = st.enter_context(nc.sbuf_tensor("arena", [128, ARENA_BYTES // 2], BF16))
        astate = {"off": 0, "peak": 0}

        def _release(m):
            astate["off"] = m

        def sb(name, shape, dt, stack=None):
            n = _dsz(dt)
            for d in shape[1:]:
                n *= int(d)
            n = (n + 63) // 64 * 64
            off = astate["off"]
            if stack is not None:
                stack.callback(_release, off)
            assert off + n <= ARENA_BYTES, ("arena overflow", name, off, n)
            astate["off"] = off + n
            astate["peak"] = max(astate["peak"], off + n)
            v = arena_t[:, off // 2:(off + n) // 2]
            if dt != BF16:
                v = v.bitcast(dt)
            tot = 1
            for d in shape[1:]:
                tot *= int(d)
            v = v[:, 0:tot]
            if len(shape) > 2:
                names = ["d%d" % i for i in range(len(shape) - 1)]
                v = v.rearrange("p (%s) -> p %s" % (" ".join(names), " ".join(names)),
                                **{nm: int(s) for nm, s in zip(names, shape[1:])})
            return v

        P = Prog(nc)
        ps = st.enter_context(nc.psum_tensor("ps", [128, 8, 512], F32))
        xT = sb("xT", [128, 8, T], F32)
        stage = sb("stage", [128, 4, 2048], F32)
        ident = sb("ident", [128, 128], F32)
        ones_bf = sb("ones_bf", [128, 128], BF16)
        bo64 = sb("bo64", [128, 128], BF16)
        V0T = sb("V0T", [128, 112], F32)
        V1T = sb("V1T", [128, 46], F32)
        cvec = sb("cvec", [128, 8, 2], F32)
        modT = sb("modT", [128, 2, 48, 2], F32)
        drv = sb("drv", [128, 2, 2, 8, 2], F32)

        P.dma(ident[:], ident_d)
        P.dma(bo64[:], bo64_d)
        P.memset(ones_bf[:], 1.0)
        P.dma(stage[0:112, 0, 0:128], vecs0_d)
        P.dma(stage[0:46, 1, 0:128], vecs1_d)
        P.transpose(ps[:, 0, 0:112], stage[0:112, 0, 0:128], ident[0:112, 0:112])
        P.transpose(ps[:, 1, 0:46], stage[0:46, 1, 0:128], ident[0:46, 0:46])
        P.copy(V0T[:], ps[:, 0, 0:112])
        P.copy(V1T[:], ps[:, 1, 0:46])
        for wh_ in range(2):
            P.act(cvec[:, :, wh_], V0T[:, 8 * wh_:8 * wh_ + 8], AF.Exp, scale=-1.0)
            P.ts(cvec[:, :, wh_], cvec[:, :, wh_], 1.0, None, ALU.add)
            P.recip(cvec[:, :, wh_], cvec[:, :, wh_])
            P.tt(cvec[:, :, wh_], V0T[:, 8 * wh_:8 * wh_ + 8], cvec[:, :, wh_], ALU.mult)

        s_ada = ExitStack()
        modrow = sb("modrow", [128, 6144], F32, s_ada)
        for l in range(n_layers):
            for s in range(12):
                slot = (s % 2) * 2
                for hlf in range(2):
                    P.dma(stage[:, slot + hlf, :].rearrange("p (k n) -> p k n", k=4),
                          wada_d[l, hlf * 512:(hlf + 1) * 512, s * 512:(s + 1) * 512].rearrange("(k p) n -> p k n", p=128))
                bA = s % 2
                for k in range(8):
                    wv = stage[:, slot + k // 4, :].rearrange("p (k n) -> p k n", k=4)
                    P.mm(ps[0:2, bA, 0:512], cvec[:, k, :], wv[:, k % 4, :], k == 0, k == 7)
                P.copy(modrow[0:2, s * 512:(s + 1) * 512], ps[0:2, bA, 0:512])
            for jg in range(48):
                P.transpose(ps[:, 2, 2 * jg:2 * jg + 2], modrow[0:2, jg * 128:(jg + 1) * 128], ident[0:2, 0:2])
            pv = ps[:, 2, 0:96].rearrange("p (a b) -> p a b", b=2)
            bias = V0T[:, 16 + 48 * l:16 + 48 * (l + 1)]
            P.tt(modT[:, l], pv, _bc(bias, 2, 2), ALU.add)
            for which in range(2):
                sc = modT[:, l, (1 + 3 * which) * 8:(2 + 3 * which) * 8, :]
                nw = V1T[:, (16 * which + 8 * l):(16 * which + 8 * l + 8)]
                P.ts(drv[:, l, which], sc, 1.0, 32.0, ALU.add, ALU.mult)
                P.tt(drv[:, l, which], drv[:, l, which], _bc(nw, 2, 2), ALU.mult)
        s_ada.close()
        P.barrier()

        def A_of(l, which, j, who):
            return drv[:, l, which, j, who:who + 1]

        def B_of(l, which, j, who):
            sec = 0 if which == 0 else 3
            return modT[:, l, sec * 8 + j, who:who + 1]

        def G_of(l, which, j, who):
            sec = 2 if which == 0 else 5
            return modT[:, l, sec * 8 + j, who:who + 1]

        with ExitStack() as s0:
            xin = sb("xin", [128, 2, 1024], F32, s0)
            ev = 0
            for t in range(NT):
                src = ctx_d[t * 128:(t + 1) * 128, :] if t < 2 else x_d[(t - 2) * 128:(t - 1) * 128, :]
                P.dma(xin[:, t % 2, :], src)
                for half in range(2):
                    b = (2 * t + half) % 4 + 3
                    for jj in range(4):
                        j = half * 4 + jj
                        P.transpose(ps[:, b, jj * 128:(jj + 1) * 128], xin[:, t % 2, j * 128:(j + 1) * 128], ident[:])
                    dst = xT[:, half * 4:half * 4 + 4, t * 128:(t + 1) * 128]
                    srcp = ps[:, b, :].rearrange("p (a b) -> p a b", b=128)
                    if ev % 2 == 0:
                        P.copy(dst, srcp)
                    else:
                        P.copy(dst, srcp, eng="act")
                    ev += 1
            P.barrier()

        def norm_chunk(l, which, c0, w, who, sq, xn, rs, bank):
            P.act(sq[:, :, :w], xT[:, :, c0:c0 + w], AF.Square)
            for j in range(8):
                P.mm(ps[:, bank, :w], ones_bf[:], sq[:, j, :w], j == 0, j == 7)
            P.act(rs[:, :w], ps[:, bank, :w], AF.Sqrt, bias=EPS * 1024.0, scale=1.0)
            P.recip(rs[:, :w], rs[:, :w])
            P.tt(xn[:, :, :w], xT[:, :, c0:c0 + w], _bc(rs[:, :w], 1, 8), ALU.mult)

        def load_w(dst_bf, src_rows_by_cols, ncols, slot_rot, eng="pool"):
            kt = dst_bf.shape[1]
            per = max(1, 2048 // ncols)
            k = 0
            while k < kt:
                n = min(per, kt - k)
                slot = slot_rot.next()
                sv = stage[:, slot, 0:n * ncols].rearrange("p (k n) -> p k n", n=ncols)
                P.dma(sv, src_rows_by_cols[k * 128:(k + n) * 128, :].rearrange("(k p) n -> p k n", p=128))
                P.copy(dst_bf[:, k:k + n, :], sv, eng=("act" if cast_rot.next() == 0 else "dve"))
                k += n

        srot = _Rot([0, 1, 2, 3])
        cast_rot = _Rot([0, 1])

        def out_proj(l, Wo, mix, nk, chunks, banks):
            for (c0, w, who) in chunks:
                for m in range(8):
                    b = banks.next()
                    for k in range(nk):
                        P.mm(ps[:, b, :w], Wo[:, k, m * 128:(m + 1) * 128], mix[:, k, c0:c0 + w], k == 0, k == nk - 1)
                    P.stt(xT[:, m, c0:c0 + w], ps[:, b, :w], G_of(l, 0, m, who), xT[:, m, c0:c0 + w], ALU.mult, ALU.add)

        def dump_dbg(idx):
            if dbg_d is not None:
                for j in range(8):
                    P.dma(dbg_d[idx, :, j, :], xT[:, j, :])

        def gla_phase(l, last, lchunks):
            with ExitStack() as s1:
                Wo = sb("g_Wo", [128, 2, 1024], BF16, s1)
                wup = sb("g_wup", [128, 2, 128], BF16, s1)
                msk = sb("g_msk", [128, 6, 128], F32, s1)
                qT = sb("g_qT", [128, T], BF16, s1)
                kT = sb("g_kT", [128, T], BF16, s1)
                ktok = sb("g_ktok", [128, NT, 128], BF16, s1)
                vtok = sb("g_vtok", [128, NT, 256], BF16, s1)
                sgT = sb("g_sg", [128, 2, T], BF16, s1)
                gd = sb("g_gd", [128, T], BF16, s1)
                oT = sb("g_oT", [128, 2, T], F32, s1)
                g8 = sb("g_g8", [128, 1], F32, s1)
                rb = _Rot([0, 1, 2, 3, 4, 5, 6, 7])
                P.dma(msk[:], masks_d)
                P.ts(g8[:], V1T[:, 40 + l:41 + l], 8.0, None, ALU.mult)
                load_w(Wo, wout_d[l][256:512, :], 1024, srot)
                sl = srot.next()
                P.dma(stage[0:33, sl, 0:256].rearrange("p (d c) -> p d c", d=2), wup_d[l].rearrange("d r c -> r d c"))
                P.copy(wup[0:33], stage[0:33, sl, 0:256].rearrange("p (d c) -> p d c", d=2))
                P.memset(gd[0:64], 1.0)
                if sub == "E1a":
                    return
                with ExitStack() as s2:
                    Wg = sb("g_W", [128, 8, 800], BF16, s2)
                    hcb = sb("g_hc", [128, 8, 512], BF16, s2)
                    sge = sb("g_sge", [128, 2, 512], F32, s2)
                    load_w(Wg[:, :, 0:512], win_d[l][:, 256:768], 512, srot)
                    load_w(Wg[:, :, 512:800], win_d[l][:, 768:1056], 288, srot)
                    if sub == "E1b":
                        return
                    for ci, (c0, w, who) in enumerate(CHUNKS):
                        P.dma(hcb[:, :, :w], hT_d[:, :, c0:c0 + w])
                        for (m0, msz, kind) in ((0, 128, "q"), (128, 128, "k"), (512, 128, "g0"), (640, 128, "g1"),
                                                (768, 32, "df")):
                            if _DBG.get("kinds") is not None and kind not in _DBG["kinds"]:
                                continue
                            b = rb.next()
                            for k in range(8):
                                P.mm(ps[0:msz, b, :w], Wg[:, k, m0:m0 + msz], hcb[:, k, :w], k == 0, k == 7)
                            if kind == "q":
                                P.ts(qT[:, c0:c0 + w], ps[:, b, :w], float(32.0 ** -0.5), None, ALU.mult)
                            elif kind == "k":
                                P.copy(kT[:, c0:c0 + w], ps[:, b, :w])
                            elif kind in ("g0", "g1"):
                                gi = 0 if kind == "g0" else 1
                                P.act(sgT[:, gi, c0:c0 + w], ps[:, b, :w], AF.Silu)
                            else:
                                P.copy(gd[0:32, c0:c0 + w], ps[0:32, b, :w])
                        if sub == "E1c" or _DBG.get("notm"):
                            continue
                        for tt in range(w // 128):
                            tg = c0 // 128 + tt
                            b = rb.next()
                            for k in range(8):
                                P.mm(ps[:, b, 0:384], hcb[:, k, tt * 128:(tt + 1) * 128], Wg[:, k, 128:512], k == 0, k == 7)
                            P.copy(ktok[:, tg, :], ps[:, b, 0:128])
                            P.copy(vtok[:, tg, :], ps[:, b, 128:384], eng="act")
                if sub == "E1":
                    return
                sflat = stage.rearrange("p a b -> p (a b)")
                sp_tok = sflat[:, 0:2304].rearrange("p (t c) -> p t c", c=128)
                sbf = sflat[:, 2304:8192].bitcast(BF16)
                q_t = sbf[:, 0:2304]
                k_t = sbf[:, 2304:4608]
                kk = sbf[:, 4608:6912].rearrange("p (t c) -> p t c", c=128)
                Sb = sbf[:, 6912:9216].rearrange("p (i c) -> p i c", c=64)
                with ExitStack() as s2:
                    te = sb("g_te", [128, 512], F32, s2)
                    ec = sb("g_ec", [128, 512], F32, s2)
                    en = sb("g_en", [128, 512], F32, s2)
                    esf = sb("g_esf", [128, 512], F32, s2)
                    dec = sb("g_dec", [128, 36], F32, s2)
                    Sall = sb("g_Sall", [128, 37, 64], F32, s2)
                    attb = sb("g_attb", [128, 2, 4, 128], BF16, s2)
                    groups = [(0, 4), (4, 4), (8, 4), (12, 4), (16, 2)]
                    for d in range(2):
                        for (t0, n) in groups:
                            b = rb.next()
                            for i in range(n):
                                t = t0 + i
                                P.mm(ps[:, b, i * 128:(i + 1) * 128], gd[0:33, t * 128:(t + 1) * 128], wup[0:33, d, :], True, True)
                            P.act(te[:, 0:n * 128], ps[:, b, 0:n * 128], AF.Exp, scale=-1.0)
                            P.act(sp_tok[:, t0:t0 + n, :], te[:, 0:n * 128].rearrange("p (t c) -> p t c", c=128), AF.Ln, bias=1.0)
                        for (t0, n) in groups:
                            bC = rb.next()
                            bS = rb.next()
                            for i in range(n):
                                t = t0 + i
                                P.mm(ps[:, bC, i * 128:(i + 1) * 128], sp_tok[:, t, :], msk[:, d, :], True, True)
                                P.mm(ps[:, bS, i * 128:(i + 1) * 128], msk[:, 2 + d, :], sp_tok[:, t, :], True, True)
                            nn = n * 128
                            cols = slice(t0 * 128, t0 * 128 + nn)
                            P.act(ec[:, 0:nn], ps[:, bC, 0:nn], AF.Exp)
                            P.act(en[:, 0:nn], ps[:, bC, 0:nn], AF.Exp, scale=-1.0)
                            P.act(esf[:, 0:nn], ps[:, bS, 0:nn], AF.Exp)
                            P.tt(q_t[:, cols], qT[:, cols], ec[:, 0:nn], ALU.mult)
                            P.tt(k_t[:, cols], kT[:, cols], en[:, 0:nn], ALU.mult, eng="pool")
                            P.tt(kk[:, t0:t0 + n, :], ktok[:, t0:t0 + n, :], esf[:, 0:nn].rearrange("p (t c) -> p t c", c=128), ALU.mult)
                            ecv = ec[:, 0:nn].rearrange("p (i c) -> p i c", c=64)
                            pick = 63 if d == 0 else 0
                            P.copy(dec[:, 2 * t0:2 * t0 + 2 * n], ecv[:, :, pick], eng="pool")
                        if sub == "E2":
                            continue
                        P.memset(Sall[:, 0, :], 0.0)
                        order = list(range(36)) if d == 0 else ([3, 2, 1, 0] + list(range(35, 3, -1)))
                        pos_of = {ci_: i_ for i_, ci_ in enumerate(order)}
                        for idx_, ci in enumerate(order):
                            t, c = ci // 2, ci % 2
                            r0 = 64 * c
                            b = rb.next()
                            for h in range(4):
                                o_ap = ps[32 * h:32 * h + 32, b, 0:64]
                                l_ap = kk[r0:r0 + 64, t, 32 * h:32 * h + 32]
                                r_ap = vtok[r0:r0 + 64, t, 64 * h:64 * h + 64]
                                P._rec("pe", (lambda e, o_ap=o_ap, l_ap=l_ap, r_ap=r_ap, r0=r0, h=h:
                                              e.matmul(o_ap, l_ap, r_ap, start=True, stop=True, tile_position=(r0, 32 * h))),
                                       [l_ap, r_ap], [o_ap])
                            P.stt(Sall[:, idx_ + 1, :], Sall[:, idx_, :], dec[:, ci:ci + 1], ps[:, b, 0:64], ALU.mult, ALU.add)
                        P.copy(Sb[:, :, :], Sall[:, 0:36, :], eng="pool")
                        if sub == "E3":
                            continue
                        rbo = _Rot([4, 5, 6, 7])
                        for t in range(NT):
                            if last and t < 2:
                                continue
                            tc_ = slice(t * 128, (t + 1) * 128)
                            bA = 0
                            for h in range(4):
                                o_ap = ps[:, bA + h, 0:128]
                                l_ap = k_t[32 * h:32 * h + 32, tc_]
                                r_ap = q_t[32 * h:32 * h + 32, tc_]
                                P._rec("pe", (lambda e, o_ap=o_ap, l_ap=l_ap, r_ap=r_ap, h=h:
                                              e.matmul(o_ap, l_ap, r_ap, start=True, stop=True, tile_position=(32 * h, 0))),
                                       [l_ap, r_ap], [o_ap])
                            ab = attb[:, t % 2]
                            P.tt(ab, ps[:, bA:bA + 4, 0:128], _bc(msk[:, 4 + d, :], 1, 4), ALU.mult)
                            bO = rbo.next()
                            for h in range(4):
                                po = 64 * (h % 2)
                                cb = (h // 2) * 128
                                o_ap = ps[po:po + 64, bO, cb:cb + 128]
                                l_ap = vtok[:, t, 64 * h:64 * h + 64]
                                r_ap = ab[:, h, :]
                                P._rec("pe", (lambda e, o_ap=o_ap, l_ap=l_ap, r_ap=r_ap, po=po:
                                              e.matmul(o_ap, l_ap, r_ap, start=True, stop=False, tile_position=(0, po))),
                                       [l_ap, r_ap], [o_ap])
                                for c in range(2):
                                    ci = 2 * t + c
                                    o2 = ps[po:po + 64, bO, cb + 64 * c:cb + 64 * c + 64]
                                    l2 = Sb[32 * h:32 * h + 32, pos_of[ci], :]
                                    r2 = q_t[32 * h:32 * h + 32, t * 128 + 64 * c:t * 128 + 64 * c + 64]
                                    P._rec("pe", (lambda e, o2=o2, l2=l2, r2=r2, h=h, po=po, c=c:
                                                  e.matmul(o2, l2, r2, start=False, stop=(c == 1), tile_position=(32 * h, po))),
                                           [l2, r2], [o2])
                            pso = ps[:, bO, 0:256].rearrange("p (a c) -> p a c", c=128)
                            if d == 0:
                                P.copy(oT[:, :, tc_], pso, eng="act")
                            else:
                                P.tt(oT[:, :, tc_], oT[:, :, tc_], pso, ALU.add)
                    if sub in ("E2", "E3"):
                        return
                    sq = sb("g_sq", [128, 2, 512], BF16, s2)
                    rs = sb("g_rs", [128, 512], F32, s2)
                    tmp = sb("g_tmp", [128, 512], F32, s2)
                    for (c0, w, who) in lchunks:
                        P.act(sq[:, :, :w], oT[:, :, c0:c0 + w], AF.Square)
                        for m in range(2):
                            b = rb.next()
                            P.mm(ps[:, b, :w], bo64[:], sq[:, m, :w], True, True)
                            P.act(rs[:, :w], ps[:, b, :w], AF.Sqrt, bias=EPS * 64.0, scale=1.0)
                            P.recip(rs[:, :w], rs[:, :w])
                            P.stt(tmp[:, :w], oT[:, m, c0:c0 + w], g8[:, 0:1], rs[:, :w], ALU.mult, ALU.mult)
                            P.tt(sgT[:, m, c0:c0 + w], tmp[:, :w], sgT[:, m, c0:c0 + w], ALU.mult, eng="pool")
                out_proj(l, Wo, sgT, 2, lchunks, rb)
                P.barrier()

        def att_phase(l, last, lchunks):
            with ExitStack() as s1:
                Wo = sb("a_Wo", [128, 4, 1024], BF16, s1)
                qr = sb("a_qr", [128, 4, T], BF16, s1)
                kd = sb("a_kd", [128, 2, T], BF16, s1)
                vt = sb("a_vt", [128, NT, 128], BF16, s1)
                g8 = sb("a_g8", [128, 2], F32, s1)
                rb = _Rot([0, 1, 2, 3, 4, 5, 6, 7])
                P.ts(g8[:, 0:1], V1T[:, 42 + l:43 + l], 8.0, None, ALU.mult)
                P.ts(g8[:, 1:2], V1T[:, 44 + l:45 + l], 8.0, None, ALU.mult)
                load_w(Wo, wout_d[l][512:1024, :], 1024, srot)
                with ExitStack() as s2:
                    Wq = sb("a_Wq", [128, 8, 512], BF16, s2)
                    Wk = sb("a_Wk", [128, 8, 256], BF16, s2)
                    Wv = sb("a_Wv", [128, 8, 128], BF16, s2)
                    psw = sb("a_psw", [128, 128], F32, s2)
                    sflat_a = stage.rearrange("p a b -> p (a b)")
                    rc = sflat_a[:, 0:T]
                    rsn = sflat_a[:, T:2 * T]
                    hcb = sb("a_hc", [128, 8, 512], BF16, s2)
                    qg2 = sb("a_qg", [128, 2, 512], F32, s2)
                    sq2 = sb("a_sq", [128, 2, 512], BF16, s2)
                    rs2 = sb("a_rsd", [128, 2, 512], F32, s2)
                    t12 = sb("a_t1", [128, 2, 512], F32, s2)
                    t22 = sb("a_t2", [128, 2, 512], F32, s2)
                    P.dma(psw[:], psw_d)
                    load_w(Wq, win_d[l][:, 1056:1568], 512, srot)
                    load_w(Wv, win_d[l][:, 1696:1824], 128, srot)
                    sl = srot.next()
                    sv = stage[:, sl, :].rearrange("p (k n) -> p k n", n=256)
                    for g in range(2):
                        for r in range(2):
                            P.dma(sv[:, :, (2 * g + r) * 64:(2 * g + r + 1) * 64],
                                  win_d[l][:, 1568 + 64 * g:1568 + 64 * g + 64].rearrange("(k p) n -> p k n", p=128))
                    P.copy(Wk[:], sv, eng="act")
                    P.dma(rc, ropec_d)
                    P.dma(rsn, ropes_d)
                    tcnt = 0
                    for ci, (c0, w, who) in enumerate(CHUNKS):
                        P.dma(hcb[:, :, :w], hT_d[:, :, c0:c0 + w])
                        for i in range(6):
                            qg = qg2[:, tcnt % 2]
                            sq = sq2[:, tcnt % 2]
                            rs = rs2[:, tcnt % 2]
                            t1 = t12[:, tcnt % 2]
                            t2 = t22[:, tcnt % 2]
                            tcnt += 1
                            isq = i < 4
                            b0 = rb.next()
                            for k in range(8):
                                wsl = Wq[:, k, i * 128:(i + 1) * 128] if isq else Wk[:, k, (i - 4) * 128:(i - 3) * 128]
                                P.mm(ps[:, b0, :w], wsl, hcb[:, k, :w], k == 0, k == 7)
                            gcol = g8[:, 0:1] if isq else g8[:, 1:2]
                            P.act(qg[:, :w], ps[:, b0, :w], AF.Identity, scale=gcol)
                            P.act(sq[:, :w], ps[:, b0, :w], AF.Square)
                            b1 = rb.next()
                            P.mm(ps[:, b1, :w], bo64[:], sq[:, :w], True, True)
                            P.act(rs[:, :w], ps[:, b1, :w], AF.Sqrt, bias=EPS * 64.0, scale=1.0)
                            P.recip(rs[:, :w], rs[:, :w])
                            b2 = rb.next()
                            P.mm(ps[:, b2, :w], psw[:], qg[:, :w], True, True)
                            P.tt(t1[:, :w], qg[:, :w], rc[:, c0:c0 + w], ALU.mult, eng="pool")
                            P.tt(t2[:, :w], ps[:, b2, :w], rsn[:, c0:c0 + w], ALU.mult)
                            P.tt(t1[:, :w], t1[:, :w], t2[:, :w], ALU.add, eng="pool")
                            dst = qr[:, i, c0:c0 + w] if isq else kd[:, i - 4, c0:c0 + w]
                            P.tt(dst, t1[:, :w], rs[:, :w], ALU.mult)
                        for tt in range(w // 128):
                            tg = c0 // 128 + tt
                            b = rb.next()
                            for k in range(8):
                                P.mm(ps[:, b, 0:128], hcb[:, k, tt * 128:(tt + 1) * 128], Wv[:, k, :], k == 0, k == 7)
                            P.copy(vt[:, tg, :], ps[:, b, 0:128], eng="act")
                with ExitStack() as s2:
                    amix = sb("a_mix", [128, 4, T], BF16, s2)
                    PT = sb("a_PT", [128, 4, 512], BF16, s2)
                    rd = sb("a_rd", [128, 2, 512], F32, s2)
                    rS = _Rot([0, 1, 2, 3])
                    rO = _Rot([(4, 5), (6, 7)])
                    rP = _Rot([0, 1, 2, 3])
                    for (c0, w, who) in lchunks:
                        kts = list(range(2)) if who == 1 else list(range(NT))
                        for h in range(8):
                            g = h // 4
                            qt = h // 2
                            po = 64 * (h % 2)
                            bO, bD = rO.next()
                            sbank = {}

                            def score(kt_):
                                bS_ = rS.next()
                                sbank[kt_] = bS_
                                P.mm(ps[:, bS_, :w], kd[po:po + 64, g, kt_ * 128:(kt_ + 1) * 128], qr[po:po + 64, qt, c0:c0 + w], True, True)

                            for kt_ in kts[:3]:
                                score(kt_)
                            for idx, kt in enumerate(kts):
                                bS = sbank[kt]
                                pt = PT[:, rP.next(), :w]
                                P.act(pt, ps[:, bS, :w], AF.Exp, scale=0.125)
                                if idx + 3 < len(kts):
                                    score(kts[idx + 3])
                                P.mm(ps[po:po + 64, bO, :w], vt[:, kt, g * 64:(g + 1) * 64], pt, idx == 0, idx == len(kts) - 1)
                                P.mm(ps[po:po + 64, bD, :w], ones_bf[:, 0:64], pt, idx == 0, idx == len(kts) - 1)
                            P.recip(rd[po:po + 64, h % 2, :w], ps[po:po + 64, bD, :w])
                            P.tt(amix[po:po + 64, qt, c0:c0 + w], ps[po:po + 64, bO, :w], rd[po:po + 64, h % 2, :w], ALU.mult)
                    out_proj(l, Wo, amix, 4, lchunks, rb)
                P.barrier()

        def moe_phase(l, last, lchunks):
            with ExitStack() as s1:
                h2T = sb("m_h2T", [128, 8, T], BF16, s1)
                GT = sb("m_GT", [128, T], BF16, s1)
                esel = sb("m_esel", [128, 16, 128], BF16, s1)
                P.dma(esel[0:16], esel_d)
                tiles = list(range(2, NT)) if last else list(range(NT))
                with ExitStack() as s2:
                    sq = sb("m_sq", [128, 8, 512], BF16, s2)
                    xn = sb("m_xn", [128, 8, 512], F32, s2)
                    rs = sb("m_rs", [128, 512], F32, s2)
                    wr = sb("m_wr", [128, 8, 16], F32, s2)
                    brep = sb("m_brep", [128, 288], F32, s2)
                    s_tok = sb("m_s", [128, NT, 16], F32, s2)
                    sel2 = sb("m_sel2", [128, 72, 8], F32, s2)
                    p1 = sb("m_p1", [128, 72, 4], F32, s2)
                    p2 = sb("m_p2", [128, 72, 2], F32, s2)
                    gs = sb("m_gs", [128, 72], F32, s2)
                    gs2 = sb("m_gs2", [128, 72], F32, s2)
                    gmax = sb("m_gmax", [128, NT], F32, s2)
                    oh = sb("m_oh", [128, 72], F32, s2)
                    cnt = sb("m_cnt", [128, 72, 4], F32, s2)
                    c2 = sb("m_c2", [128, 72, 4], F32, s2)
                    wsum = sb("m_wsum", [128, NT], F32, s2)
                    gate = sb("m_gate", [128, NT, 16], F32, s2)
                    P.dma(wr[:], wr_d.rearrange("(k p) e -> p k e", p=128))
                    P.dma(brep[:], brep_d)
                    P.memset(s_tok[:], 0.0)
                    for ci, (c0, w, who) in enumerate(lchunks):
                        norm_chunk(l, 1, c0, w, who, sq, xn, rs, ci % 2)
                        for j in range(8):
                            if j % 2 == 0:
                                P.ts(xn[:, j, :w], xn[:, j, :w], A_of(l, 1, j, who), B_of(l, 1, j, who), ALU.mult, ALU.add)
                            else:
                                P.act(xn[:, j, :w], xn[:, j, :w], AF.Identity, bias=B_of(l, 1, j, who), scale=A_of(l, 1, j, who))
                        P.copy(h2T[:, :, c0:c0 + w], xn[:, :, :w], eng="act")
                        b = 2 + ci % 2
                        for tt in range(w // 128):
                            tg = c0 // 128 + tt
                            for k in range(8):
                                P.mm(ps[:, b, tt * 16:(tt + 1) * 16], xn[:, k, tt * 128:(tt + 1) * 128], wr[:, k, :], k == 0, k == 7)
                        nt_ = w // 128
                        tg0 = c0 // 128
                        P.act(s_tok[:, tg0:tg0 + nt_, :], ps[:, b, 0:nt_ * 16].rearrange("p (t e) -> p t e", e=16), AF.Exp, scale=-1.0)
                        P.ts(s_tok[:, tg0:tg0 + nt_, :], s_tok[:, tg0:tg0 + nt_, :], 1.0, None, ALU.add)
                        P.recip(s_tok[:, tg0:tg0 + nt_, :], s_tok[:, tg0:tg0 + nt_, :])
                    sv = s_tok.rearrange("p t (g e) -> p (t g) e", e=4)
                    bv = brep.rearrange("p (a e) -> p a e", e=4)
                    P.tt(sel2[:, :, 0:4], sv, bv, ALU.add)
                    P.tt(sel2[:, :, 4:8], sv, bv, ALU.add)
                    P.tt(p1[:], sel2[:, :, 0:4], sel2[:, :, 1:5], ALU.add)
                    P.tt(p2[:], sel2[:, :, 0:2], sel2[:, :, 2:4], ALU.add)
                    P.reduce(gs[:], p1[:], ALU.max)
                    P.reduce(gs2[:], p2[:], ALU.max)
                    P.tt(gs[:], gs[:], gs2[:], ALU.max)
                    P.reduce(gmax[:], gs.rearrange("p (t g) -> p t g", g=4), ALU.max)
                    P.tt(oh.rearrange("p (t g) -> p t g", g=4), gs.rearrange("p (t g) -> p t g", g=4), _bc(gmax[:], 2, 4), ALU.is_equal)
                    P.tt(cnt[:], sel2[:, :, 1:5], sel2[:, :, 0:4], ALU.is_gt)
                    P.tt(c2[:], sel2[:, :, 2:6], sel2[:, :, 0:4], ALU.is_gt)
                    P.tt(cnt[:], cnt[:], c2[:], ALU.add)
                    P.tt(c2[:], sel2[:, :, 3:7], sel2[:, :, 0:4], ALU.is_gt)
                    P.tt(cnt[:], cnt[:], c2[:], ALU.add)
                    P.ts(cnt[:], cnt[:], 1.0, None, ALU.is_le)
                    P.tt(cnt[:], cnt[:], _bc(oh[:], 2, 4), ALU.mult)
                    gv = gate.rearrange("p t (g e) -> p (t g) e", e=4)
                    P.tt(gv, sv, cnt[:], ALU.mult)
                    P.reduce(wsum[:], gate[:], ALU.add)
                    P.ts(wsum[:], wsum[:], 1e-30, None, ALU.max)
                    P.recip(wsum[:], wsum[:])
                    P.tt(gate[:], gate[:], _bc(wsum[:], 2, 16), ALU.mult)
                    for t in tiles:
                        b = 4 + (t // 4) % 2
                        P.transpose(ps[0:16, b, (t % 4) * 128:(t % 4 + 1) * 128], gate[:, t, :], ident[:])
                        P.copy(GT[0:16, t * 128:(t + 1) * 128], ps[0:16, b, (t % 4) * 128:(t % 4 + 1) * 128])
                with ExitStack() as s2:
                    wbuf = sb("m_wbuf", [128, 3, 3, 2048], BF16, s2)
                    sg = sb("m_sg", [128, 2, 512], F32, s2)
                    t1 = sb("m_t1", [128, 2, 512], F32, s2)
                    abuf = sb("m_a", [128, 2, 2, 512], BF16, s2)
                    rG = _Rot([0, 1])
                    rU = _Rot([2, 3])
                    rD = _Rot([5, 6, 7])
                    it = 0
                    pending = None

                    def emit_down(pd):
                        Wd_p, ab_p, c0p, wp, whop = pd
                        for m in range(8):
                            bD = rD.next()
                            for ft in range(2):
                                P.mm(ps[:, bD, :wp], Wd_p[:, ft, m * 128:(m + 1) * 128], ab_p[:, ft, :wp], ft == 0, ft == 1)
                            P.stt(xT[:, m, c0p:c0p + wp], ps[:, bD, :wp], G_of(l, 1, m, whop), xT[:, m, c0p:c0p + wp], ALU.mult, ALU.add)

                    for e in range(16):
                        for fh in range(2):
                            u = e * 2 + fh
                            ws = u % 3
                            Wg_ = wbuf[:, ws, 0].rearrange("p (k f) -> p k f", f=256)
                            Wu_ = wbuf[:, ws, 1].rearrange("p (k f) -> p k f", f=256)
                            Wd_ = wbuf[:, ws, 2].rearrange("p (k d) -> p k d", d=1024)
                            for mi, (dst, srcw) in enumerate(((Wg_, weg_d[l, e][:, fh * 256:(fh + 1) * 256]),
                                                              (Wu_, weu_d[l, e][:, fh * 256:(fh + 1) * 256]))):
                                sl = srot.next()
                                sv_ = stage[:, sl, :].rearrange("p (k f) -> p k f", f=256)
                                P.dma(sv_, srcw.rearrange("(k p) f -> p k f", p=128))
                                P.copy(dst, sv_, eng="act")
                            sl = srot.next()
                            sv_ = stage[:, sl, :].rearrange("p (k d) -> p k d", d=1024)
                            P.dma(sv_, wed_d[l, e][fh * 256:(fh + 1) * 256, :].rearrange("(k p) d -> p k d", p=128))
                            P.copy(Wd_, sv_, eng="act")
                            for (c0, w, who) in lchunks:
                                ab = abuf[:, it % 2]
                                it += 1
                                P.mm(ps[:, 4, :w], esel[0:16, e, :], GT[0:16, c0:c0 + w], True, True)
                                for ft in range(2):
                                    bG = rG.next()
                                    bU = rU.next()
                                    for k in range(8):
                                        P.mm(ps[:, bG, :w], Wg_[:, k, ft * 128:(ft + 1) * 128], h2T[:, k, c0:c0 + w], k == 0, k == 7)
                                    for k in range(8):
                                        P.mm(ps[:, bU, :w], Wu_[:, k, ft * 128:(ft + 1) * 128], h2T[:, k, c0:c0 + w], k == 0, k == 7)
                                    P.act(sg[:, ft, :w], ps[:, bG, :w], AF.Silu)
                                    P.tt(t1[:, ft, :w], sg[:, ft, :w], ps[:, bU, :w], ALU.mult)
                                    P.tt(ab[:, ft, :w], t1[:, ft, :w], ps[:, 4, :w], ALU.mult)
                                if pending is not None:
                                    emit_down(pending)
                                pending = (Wd_, ab, c0, w, who)
                    emit_down(pending)
                P.barrier()

        for l in range(n_layers):
            last = (l == n_layers - 1) and (n_layers == 2)
            lchunks = CHUNKS[1:] if last else CHUNKS

            with ExitStack() as s1:
                sq = sb("b_sq", [128, 8, 512], BF16, s1)
                xn = sb("b_xn", [128, 8, 512], F32, s1)
                rs = sb("b_rs", [128, 512], F32, s1)
                hc = sb("b_hc", [128, 2, 8, 512], BF16, s1)
                for ci, (c0, w, who) in enumerate(CHUNKS):
                    norm_chunk(l, 0, c0, w, who, sq, xn, rs, ci % 2)
                    for j in range(8):
                        if j % 2 == 0:
                            P.ts(hc[:, ci % 2, j, :w], xn[:, j, :w], A_of(l, 0, j, who), B_of(l, 0, j, who), ALU.mult, ALU.add)
                        else:
                            P.act(hc[:, ci % 2, j, :w], xn[:, j, :w], AF.Identity, bias=B_of(l, 0, j, who), scale=A_of(l, 0, j, who))
                    P.dma(hT_d[:, :, c0:c0 + w], hc[:, ci % 2, :, :w])
                P.barrier()
            if stop_at == ("B", l):
                break

            with ExitStack() as s1:
                Wu = sb("f_Wu", [128, 8, 256], BF16, s1)
                Wo = sb("f_Wo", [128, 2, 1024], BF16, s1)
                cs64 = sb("f_cs64", [128, 256], BF16, s1)
                uT = sb("f_uT", [128, 2, T], BF16, s1)
                ucs = sb("f_ucs", [128, NT, 512], BF16, s1)
                fmix = sb("f_mix", [128, 2, T], BF16, s1)
                hcb = sb("f_hc", [128, 2, 8, 512], BF16, s1)
                tab = sb("f_tab", [128, 1, 2, 16, 512], BF16, s1)
                P.dma(cs64[:], cs64_d)
                load_w(Wu, win_d[l][:, 0:256], 256, srot)
                load_w(Wo, wout_d[l][0:256, :], 1024, srot)
                rb = _Rot([0, 1, 2, 3])
                for ci, (c0, w, who) in enumerate(CHUNKS):
                    P.dma(hcb[:, ci % 2, :, :w], hT_d[:, :, c0:c0 + w])
                    for m in range(2):
                        b = rb.next()
                        for k in range(8):
                            P.mm(ps[:, b, :w], Wu[:, k, m * 128:(m + 1) * 128], hcb[:, ci % 2, k, :w], k == 0, k == 7)
                        P.copy(uT[:, m, c0:c0 + w], ps[:, b, :w], eng=("act" if m == 0 else "dve"))
                t_lo = 2 if last else 0
                for t in range(t_lo, NT):
                    b = rb.next()
                    for j in range(2):
                        P.mm(ps[:, b, j * 256:(j + 1) * 256], uT[:, j, t * 128:(t + 1) * 128], cs64[:], True, True)
                    P.copy(ucs[:, t, :], ps[:, b, :], eng=("act" if t % 2 == 0 else "dve"))
                tab2 = tab.rearrange("p a c t n -> p (a c t n)").rearrange("p (b c t n) -> p b c t n", b=2, c=2, t=16)
                for pc in range(8):
                    bufi = pc % 2
                    P.dma(tab2[:, bufi, 0], cn_d[:, pc * 256:(pc + 1) * 256].rearrange("(t p) c -> p t c", p=128))
                    P.dma(tab2[:, bufi, 1], sn_d[:, pc * 256:(pc + 1) * 256].rearrange("(t p) c -> p t c", p=128))
                    for j in range(2):
                        b = rb.next()
                        for t in range(16):
                            P.mm(ps[:, b, 0:256], ucs[:, 2 + t, j * 256:j * 256 + 128], tab2[:, bufi, 0, t, :], t == 0, False)
                            P.mm(ps[:, b, 0:256], ucs[:, 2 + t, j * 256 + 128:j * 256 + 256], tab2[:, bufi, 1, t, :], False, t == 15)
                        P.copy(fmix[:, j, 256 + pc * 256:256 + (pc + 1) * 256], ps[:, b, 0:256], eng=("act" if j == 0 else "dve"))
                if not last:
                    c2 = sb("f_c2", [128, 2, 2, 256], BF16, s1)
                    P.dma(c2[:, 0], c256_d.rearrange("(t p) c -> p t c", p=128))
                    P.dma(c2[:, 1], s256_d.rearrange("(t p) c -> p t c", p=128))
                    for j in range(2):
                        b = rb.next()
                        for t in range(2):
                            P.mm(ps[:, b, 0:256], ucs[:, t, j * 256:j * 256 + 128], c2[:, 0, t, :], t == 0, False)
                            P.mm(ps[:, b, 0:256], ucs[:, t, j * 256 + 128:j * 256 + 256], c2[:, 1, t, :], False, t == 1)
                        P.copy(fmix[:, j, 0:256], ps[:, b, 0:256])
                out_proj(l, Wo, fmix, 2, lchunks, rb)
                P.barrier()
            dump_dbg(4 * l + 0)
            if stop_at == ("D", l):
                break

            if 'E' not in skip:
                gla_phase(l, last, lchunks)
            dump_dbg(4 * l + 1)
            if stop_at == ("E", l):
                break

            if 'F' not in skip:
                att_phase(l, last, lchunks)
            dump_dbg(4 * l + 2)
            if stop_at == ("F", l):
                break

            moe_phase(l, last, lchunks)
            dump_dbg(4 * l + 3)
            if stop_at == ("I", l):
                break

        if stop_at is None:
            with ExitStack() as s1:
                sq = sb("o_sq", [128, 8, 512], BF16, s1)
                xn = sb("o_xn", [128, 8, 512], F32, s1)
                rs = sb("o_rs", [128, 512], F32, s1)
                ot = sb("o_ot", [128, 2, 1024], F32, s1)
                fn32 = sb("o_fn", [128, 8], F32, s1)
                P.ts(fn32[:], V1T[:, 32:40], 32.0, None, ALU.mult)
                ev = 0
                for ci, (c0, w, who) in enumerate(CHUNKS[1:]):
                    norm_chunk(0, 0, c0, w, who, sq, xn, rs, 0)
                    for j in range(8):
                        if j % 2 == 0:
                            P.ts(xn[:, j, :w], xn[:, j, :w], fn32[:, j:j + 1], None, ALU.mult)
                        else:
                            P.act(xn[:, j, :w], xn[:, j, :w], AF.Identity, scale=fn32[:, j:j + 1])
                    for tt in range(4):
                        tg = ci * 4 + tt
                        for half in range(2):
                            b = 1 + (2 * tg + half) % 4
                            for jj in range(4):
                                j = half * 4 + jj
                                P.transpose(ps[:, b, jj * 128:(jj + 1) * 128], xn[:, j, tt * 128:(tt + 1) * 128], ident[:])
                            dst = ot[:, tg % 2, half * 512:(half + 1) * 512]
                            if ev % 2 == 0:
                                P.copy(dst, ps[:, b, :])
                            else:
                                P.copy(dst, ps[:, b, :], eng="act")
                            ev += 1
                        P.dma(y_d[tg * 128:(tg + 1) * 128, :], ot[:, tg % 2, :])
        P.emit()
        print("arena peak bytes", astate["peak"], "ops", P.nops)
    return nc


_CONST_CACHE = {}


def _consts():
    if _CONST_CACHE:
        return _CONST_CACHE
    bf = ml_dtypes.bfloat16
    c = {}
    c["ident"] = np.eye(128, dtype=np.float32)
    k = np.arange(64)
    ang = 2 * np.pi * np.outer(k, k) / 64.0
    C64 = np.cos(ang) / 8.0
    S64 = np.sin(ang) / 8.0
    cs = np.zeros((128, 256), np.float64)
    for g in range(2):
        cs[g * 64:(g + 1) * 64, g * 64:(g + 1) * 64] = C64
        cs[g * 64:(g + 1) * 64, 128 + g * 64:128 + (g + 1) * 64] = S64
    c["cs64"] = cs.astype(bf)
    for n, cn, sn in ((2048, "cn", "sn"), (256, "c256", "s256")):
        i = np.arange(n)
        a = 2 * np.pi * (np.outer(i, i) % n) / float(n)
        c[cn] = (np.cos(a) / np.sqrt(n)).astype(bf)
        c[sn] = (-np.sin(a) / np.sqrt(n)).astype(bf)
    inv = 10000.0 ** (-np.arange(16, dtype=np.float64) * 2.0 / 32.0)
    tok = np.arange(2048)
    row = tok // 64
    col = tok % 64
    rc = np.ones((128, T), np.float64)
    rs = np.zeros((128, T), np.float64)
    for hd in range(128):
        a = (hd % 64) // 32
        f = hd % 16
        pos = row if a == 0 else col
        rc[hd, 256:] = np.cos(pos * inv[f])
        rs[hd, 256:] = np.sin(pos * inv[f])
    c["ropec"] = rc.astype(np.float32)
    c["ropes"] = rs.astype(np.float32)
    psw = np.zeros((128, 128), np.float32)
    for hdp in range(128):
        half = (hdp % 32) // 16
        if half == 0:
            psw[hdp + 16, hdp] = -1.0
        else:
            psw[hdp - 16, hdp] = 1.0
    c["psw"] = psw
    j = np.arange(128)[:, None]
    i = np.arange(128)[None, :]
    same = (j // 64) == (i // 64)
    m = np.zeros((128, 6, 128), np.float32)
    m[:, 0, :] = np.where(same & (j <= i), -1.0 / 16.0, 0.0)
    m[:, 1, :] = np.where(same & (j >= i), -1.0 / 16.0, 0.0)
    m[:, 2, :] = np.where(same & (j > i), -1.0 / 16.0, 0.0)
    m[:, 3, :] = np.where(same & (j < i), -1.0 / 16.0, 0.0)
    m[:, 4, :] = np.where(same & (j <= i), 1.0, 0.0)
    m[:, 5, :] = np.where(same & (j >= i), 1.0, 0.0)
    c["masks"] = m
    c["bo64"] = same.astype(np.float32).astype(bf)
    es = np.zeros((16, 16, 128), np.float32)
    for e in range(16):
        es[e, e, :] = 1.0
    c["esel"] = es.astype(bf)
    _CONST_CACHE.update(c)
    return _CONST_CACHE


_NC_CACHE = {}


def _prep_inputs(inputs):
    f = lambda a: np.ascontiguousarray(np.asarray(a, dtype=np.float32))
    x = f(inputs["x"])
    c = f(inputs["c"])
    ctx = f(inputs["ctx"])
    c_ctx = f(inputs["c_ctx"])
    b_ada = f(inputs["b_ada"])
    cst = _consts()
    vecs1 = np.concatenate([
        f(inputs["norm_mix"]).reshape(16, 128),
        f(inputs["norm_ffn"]).reshape(16, 128),
        f(inputs["final_norm"]).reshape(8, 128),
        np.tile(f(inputs["gla_norm"]), (1, 2)),
        np.tile(f(inputs["q_norm"]), (1, 2)),
        np.tile(f(inputs["k_norm"]), (1, 2)),
    ], axis=0)
    wg_ = f(inputs["w_gla_gate_up"])
    bg_ = f(inputs["b_gla_gate"])
    wup = np.zeros((2, 2, 33, 128), np.float32)
    for l_ in range(2):
        for d_ in range(2):
            wup[l_, d_, 16 * d_:16 * d_ + 16, :] = wg_[l_, d_]
            wup[l_, d_, 32, :] = bg_[l_, d_]
    brep = np.ascontiguousarray(np.broadcast_to(np.tile(f(inputs["b_router"]), 18)[None, :], (128, 288)))
    shared = {
        "vecs1": np.ascontiguousarray(vecs1),
        "w_ada": f(inputs["w_ada"]), "w_in": f(inputs["w_in"]), "w_out": f(inputs["w_out"]),
        "wup": np.ascontiguousarray(wup), "w_router": f(inputs["w_router"]), "brep": brep,
        "w_exp_gate": f(inputs["w_exp_gate"]), "w_exp_up": f(inputs["w_exp_up"]), "w_exp_down": f(inputs["w_exp_down"]),
    }
    for k_ in ("ident", "cs64", "cn", "sn", "c256", "s256", "ropec", "ropes", "psw", "masks", "bo64", "esel"):
        shared[k_] = cst[k_]
    in_maps = []
    for b in range(8):
        vecs0 = np.concatenate([c[b].reshape(8, 128), c_ctx.reshape(8, 128),
                                b_ada[0].reshape(48, 128), b_ada[1].reshape(48, 128)], axis=0)
        m = dict(shared)
        m["x"] = x[b]
        m["ctx"] = ctx[b]
        m["vecs0"] = np.ascontiguousarray(vecs0)
        in_maps.append(m)
    return in_maps


def kernel(**inputs):
    in_maps = _prep_inputs(inputs)
    if "nc" not in _NC_CACHE:
        _NC_CACHE["nc"] = build()
    nc = _NC_CACHE["nc"]
    res = run_bass_kernel_spmd(nc, in_maps, core_ids=list(range(8)))
    out = np.stack([np.asarray(r["y"], dtype=np.float32) for r in res.results], axis=0)
    return out
```

```python
import numpy as np
import ml_dtypes
import concourse.bass as bass
import concourse.mybir as mybir
from concourse.bass_utils import run_bass_kernel_spmd

F32 = mybir.dt.float32
BF16 = mybir.dt.bfloat16
AF = mybir.ActivationFunctionType
ALU = mybir.AluOpType
AX = mybir.AxisListType

_DSZ = {F32: 4, BF16: 2}


def _dsz(dt):
    if dt in _DSZ:
        return _DSZ[dt]
    s = str(dt)
    if "32" in s:
        return 4
    if "16" in s:
        return 2
    if "64" in s:
        return 8
    return 1


def _region(ap):
    t = ap.tensor
    name = t.name
    dsz = _dsz(ap.dtype)
    space = str(ap.space)
    off = int(ap.offset)
    if "DRAM" in space.upper() or "HBM" in space.upper():
        ext = 0
        for (s, c) in ap.ap:
            ext += abs(int(s)) * (int(c) - 1)
        return (name, 0, 1, off * dsz, (off + ext + 1) * dsz)
    shape = list(t.shape)
    fsz = 1
    for d in shape[1:]:
        fsz *= int(d)
    p0 = off // fsz
    f0 = off % fsz
    pext = 0
    fext = 0
    for (s, c) in ap.ap:
        s = int(s)
        c = int(c)
        if c <= 1 or s == 0:
            continue
        if s % fsz == 0:
            pext += (s // fsz) * (c - 1)
        else:
            fext += abs(s) * (c - 1)
    b0 = f0 * dsz
    b1 = (f0 + fext + 1) * dsz
    if "PSUM" in space.upper():
        return (name, 0, 128, (b0 // 2048) * 2048, ((b1 + 2047) // 2048) * 2048)
    return (name, p0, p0 + pext + 1, b0, b1)


class _Op:
    __slots__ = ("eng", "fn", "k", "is_dma", "deps_eng", "deps_dma", "need_signal", "sig_val",
                 "dma_sem", "dma_val", "dma_prev", "name")

    def __init__(self, eng, fn, is_dma, name=""):
        self.eng = eng
        self.fn = fn
        self.is_dma = is_dma
        self.k = -1
        self.deps_eng = {}
        self.deps_dma = []
        self.need_signal = False
        self.sig_val = 0
        self.dma_sem = None
        self.dma_val = 0
        self.dma_prev = None
        self.name = name


class Prog:
    ENGS = ("pe", "act", "dve", "pool", "sp")
    NDMA = {"sp": 40, "pool": 8, "act": 8}

    def __init__(self, nc):
        self.nc = nc
        self.eng_ops = {e: [] for e in self.ENGS}
        self.recs = {}
        self.dma_count = {q: 0 for q in self.NDMA}
        self.dma_last = {q: [None] * n for q, n in self.NDMA.items()}
        self.nops = 0

    def _dep(self, x, y):
        if y is x:
            return
        if y.is_dma:
            if y not in x.deps_dma:
                x.deps_dma.append(y)
            return
        if (not x.is_dma) and y.eng == x.eng:
            if x.eng == "pe":
                return
        cur = x.deps_eng.get(y.eng, -1)
        if y.k > cur:
            x.deps_eng[y.eng] = y.k
        y.need_signal = True

    def add(self, eng, fn, reads, writes, is_dma=False, name=""):
        op = _Op(eng, fn, is_dma, name)
        rr = [_region(a) for a in reads if a is not None]
        ww = [_region(a) for a in writes if a is not None]
        for (nm, p0, p1, b0, b1) in rr:
            is_ps = (nm == "ps")
            for rec in self.recs.get(nm, ()):
                if rec[0] < p1 and p0 < rec[1] and rec[2] < b1 and b0 < rec[3]:
                    if rec[4] or (is_ps and rec[5].eng != eng):
                        self._dep(op, rec[5])
        for (nm, p0, p1, b0, b1) in ww:
            for rec in self.recs.get(nm, ()):
                if rec[0] < p1 and p0 < rec[1] and rec[2] < b1 and b0 < rec[3]:
                    self._dep(op, rec[5])
        for (nm, p0, p1, b0, b1) in ww:
            lst = self.recs.setdefault(nm, [])
            lst[:] = [r for r in lst if not (p0 <= r[0] and r[1] <= p1 and b0 <= r[2] and r[3] <= b1)]
            lst.append((p0, p1, b0, b1, True, op))
        for (nm, p0, p1, b0, b1) in rr:
            lst = self.recs.setdefault(nm, [])
            if not is_dma:
                lst[:] = [r for r in lst if not ((not r[4]) and (not r[5].is_dma) and r[5].eng == eng
                                                 and p0 <= r[0] and r[1] <= p1 and b0 <= r[2] and r[3] <= b1)]
            lst.append((p0, p1, b0, b1, False, op))
        op.k = len(self.eng_ops[eng])
        self.eng_ops[eng].append(op)
        if is_dma:
            n = self.NDMA[eng]
            slot = self.dma_count[eng] % n
            self.dma_count[eng] += 1
            prev = self.dma_last[eng][slot]
            op.dma_sem = (eng, slot)
            op.dma_prev = prev
            op.dma_val = (prev.dma_val if prev is not None else 0) + 16
            self.dma_last[eng][slot] = op
        self.nops += 1
        return op

    def barrier(self):
        bop = _Op("sp", lambda e: e.nop(), False, "barrier")
        for e in self.ENGS:
            lst = [o for o in self.eng_ops[e] if not o.is_dma]
            if lst:
                y = lst[-1]
                if e == "sp":
                    continue
                bop.deps_eng[e] = y.k
                y.need_signal = True
        for q in self.NDMA:
            for y in self.dma_last[q]:
                if y is not None:
                    bop.deps_dma.append(y)
        bop.k = len(self.eng_ops["sp"])
        bop.need_signal = True
        self.eng_ops["sp"].append(bop)
        self.recs = {"__barrier__": [(0, 1, 0, 1, True, bop)]}
        self._barrier_op = bop
        self._barrier_seen = set()
        return bop

    def _barrier_dep(self, op):
        b = getattr(self, "_barrier_op", None)
        if b is None or op is b or op.eng == "sp" or op.eng in self._barrier_seen:
            return
        self._barrier_seen.add(op.eng)
        if b.k > op.deps_eng.get("sp", -1):
            op.deps_eng["sp"] = b.k

    def _rec(self, eng, fn, reads, writes, is_dma=False, name=""):
        op = self.add(eng, fn, reads, writes, is_dma, name)
        self._barrier_dep(op)
        if eng == "pe":
            l = reads[0]
            rr = lambda v: 32 if v <= 32 else (64 if v <= 64 else 128)
            op.name = (rr(int(l.shape[0])), rr(int(l.shape[-1])))
        return op

    def mm(self, out, lhsT, rhs, start=True, stop=True):
        return self._rec("pe", lambda e: e.matmul(out, lhsT, rhs, start=start, stop=stop), [lhsT, rhs], [out])

    def transpose(self, out, in_, ident):
        return self._rec("pe", lambda e: e.transpose(out, in_, ident), [in_, ident], [out])

    def act(self, out, in_, func, bias=None, scale=1.0, accum_out=None):
        reads = [in_]
        kw = {}
        if bias is not None:
            kw["bias"] = bias
            if not isinstance(bias, (int, float)):
                reads.append(bias)
        if not isinstance(scale, (int, float)):
            reads.append(scale)
        kw["scale"] = scale
        writes = [out]
        if accum_out is not None:
            kw["accum_out"] = accum_out
            writes.append(accum_out)
        return self._rec("act", lambda e: e.activation(out, in_, func, **kw), reads, writes)

    def tt(self, out, in0, in1, op, eng="dve"):
        return self._rec(eng, lambda e: e.tensor_tensor(out, in0, in1, op), [in0, in1], [out])

    def ts(self, out, in0, s1, s2, op0, op1=None, eng="dve", accum_out=None):
        reads = [in0]
        if not isinstance(s1, (int, float)):
            reads.append(s1)
        if s2 is not None and not isinstance(s2, (int, float)):
            reads.append(s2)
        writes = [out]
        kw = {}
        if accum_out is not None:
            kw["accum_out"] = accum_out
            writes.append(accum_out)
        if op1 is None:
            return self._rec(eng, lambda e: e.tensor_scalar(out, in0, s1, s2, op0, **kw), reads, writes)
        return self._rec(eng, lambda e: e.tensor_scalar(out, in0, s1, s2, op0, op1, **kw), reads, writes)

    def stt(self, out, in0, scalar, in1, op0, op1, eng="dve"):
        reads = [in0, in1]
        if not isinstance(scalar, (int, float)):
            reads.append(scalar)
        return self._rec(eng, lambda e: e.scalar_tensor_tensor(out, in0, scalar, in1, op0, op1), reads, [out])

    def copy(self, out, in_, eng="dve"):
        if eng == "act":
            return self._rec("act", lambda e: e.copy(out, in_), [in_], [out])
        return self._rec(eng, lambda e: e.tensor_copy(out, in_), [in_], [out])

    def recip(self, out, in_):
        return self._rec("dve", lambda e: e.reciprocal(out, in_), [in_], [out])

    def reduce(self, out, in_, op, axis=None, eng="dve"):
        ax = axis if axis is not None else AX.X
        return self._rec(eng, lambda e: e.tensor_reduce(out, in_, ax, op), [in_], [out])

    def memset(self, out, val, eng="dve"):
        return self._rec(eng, lambda e: e.memset(out, val), [], [out])

    def dma(self, out, in_, q="sp"):
        return self._rec(q, lambda e: e.dma_start(out=out, in_=in_), [in_], [out], is_dma=True)

    def emit(self):
        nc = self.nc
        self.barrier()
        from contextlib import ExitStack
        with ExitStack() as st:
            esem = {e: st.enter_context(nc.semaphore("c_" + e)) for e in self.ENGS}
            dsem = {}
            for q, n in self.NDMA.items():
                for i in range(n):
                    dsem[(q, i)] = st.enter_context(nc.semaphore("d_%s%d" % (q, i)))
            for e in self.ENGS:
                v = 0
                for op in self.eng_ops[e]:
                    if (not op.is_dma) and op.need_signal:
                        v += 1
                        op.sig_val = v
            prog = self

            pstate = {}

            def run(ename, eng):
                waited = {}
                for op in prog.eng_ops[ename]:
                    waits = []
                    for e2, k2 in op.deps_eng.items():
                        y = prog.eng_ops[e2][k2]
                        waits.append((("c", e2), esem[e2], y.sig_val))
                    for y in op.deps_dma:
                        waits.append((y.dma_sem, dsem[y.dma_sem], y.dma_val))
                    if op.is_dma and op.dma_prev is not None:
                        waits.append((op.dma_sem, dsem[op.dma_sem], op.dma_prev.dma_val))
                    for key, sem, val in waits:
                        if waited.get(key, 0) >= val:
                            continue
                        waited[key] = val
                        eng.wait_ge(sem, val)
                    if ename == "pe":
                        pass
                        pstate["mode"] = op.name
                    ins = op.fn(eng)
                    if op.is_dma:
                        ins.then_inc(dsem[op.dma_sem], 16)
                    elif op.need_signal:
                        ins.then_inc(esem[ename], 1)

            with nc.Block() as block:
                @block.tensor
                def _(eng):
                    run("pe", eng)

                @block.scalar
                def _(eng):
                    run("act", eng)

                @block.vector
                def _(eng):
                    run("dve", eng)

                @block.gpsimd
                def _(eng):
                    run("pool", eng)

                @block.sync
                def _(eng):
                    run("sp", eng)


T = 2304
NT = 18
KT = 8
EPS = 1e-6
CHUNKS = [(0, 256, 1), (256, 512, 0), (768, 512, 0), (1280, 512, 0), (1792, 512, 0)]


def _bc(ap, pos, n):
    shp = list(ap.shape)
    v = ap.unsqueeze(pos)
    shp.insert(pos, n)
    return v.to_broadcast(shp)


_DBG = {}


class _Rot:
    def __init__(self, items):
        self.items = list(items)
        self.i = 0

    def next(self):
        v = self.items[self.i % len(self.items)]
        self.i += 1
        return v


def build(n_layers=2, stop_at=None, dbg=False, skip=(), sub=None):
    from contextlib import ExitStack
    nc = bass.Bass("TRN2", target_bir_lowering=False)

    def din(name, shape, dt=F32):
        return nc.dram_tensor(name, shape, dt, kind="ExternalInput").ap()

    x_d = din("x", [2048, 1024])
    ctx_d = din("ctx", [256, 1024])
    vecs0_d = din("vecs0", [112, 128])
    vecs1_d = din("vecs1", [46, 128])
    wada_d = din("w_ada", [2, 1024, 6144])
    win_d = din("w_in", [2, 1024, 1824])
    wout_d = din("w_out", [2, 1024, 1024])
    wup_d = din("wup", [2, 2, 33, 128])
    wr_d = din("w_router", [1024, 16])
    brep_d = din("brep", [128, 288])
    need_moe = stop_at is None or stop_at[0] == "I" or stop_at[1] >= 1
    weg_d = weu_d = wed_d = None
    if need_moe:
        weg_d = din("w_exp_gate", [2, 16, 1024, 512])
        weu_d = din("w_exp_up", [2, 16, 1024, 512])
        wed_d = din("w_exp_down", [2, 16, 512, 1024])
    ident_d = din("ident", [128, 128])
    cs64_d = din("cs64", [128, 256], BF16)
    cn_d = din("cn", [2048, 2048], BF16)
    sn_d = din("sn", [2048, 2048], BF16)
    c256_d = din("c256", [256, 256], BF16)
    s256_d = din("s256", [256, 256], BF16)
    ropec_d = din("ropec", [128, T])
    ropes_d = din("ropes", [128, T])
    psw_d = din("psw", [128, 128])
    masks_d = din("masks", [128, 6, 128])
    bo64_d = din("bo64", [128, 128], BF16)
    esel_d = din("esel", [16, 16, 128], BF16)
    y_d = nc.dram_tensor("y", [2048, 1024], F32, kind="ExternalOutput").ap()
    hT_d = nc.dram_tensor("hT_scr", [128, 8, T], BF16).ap()
    dbg_d = None
    if dbg:
        dbg_d = nc.dram_tensor("dbg_x", [8, 128, 8, T], F32, kind="ExternalOutput").ap()

    st = ExitStack()
    with st:
        ARENA_BYTES = 210944
        arena_t = st.enter_context(nc.sbuf_tensor("arena", [128, ARENA_BYTES // 2], BF16))
        astate = {"off": 0, "peak": 0}

        def _release(m):
            astate["off"] = m

        def sb(name, shape, dt, stack=None):
            n = _dsz(dt)
            for d in shape[1:]:
                n *= int(d)
            n = (n + 63) // 64 * 64
            off = astate["off"]
            if stack is not None:
                stack.callback(_release, off)
            assert off + n <= ARENA_BYTES, ("arena overflow", name, off, n)
            astate["off"] = off + n
            astate["peak"] = max(astate["peak"], off + n)
            v = arena_t[:, off // 2:(off + n) // 2]
            if dt != BF16:
                v = v.bitcast(dt)
            tot = 1
            for d in shape[1:]:
                tot *= int(d)
            v = v[:, 0:tot]
            if len(shape) > 2:
                names = ["d%d" % i for i in range(len(shape) - 1)]
                v = v.rearrange("p (%s) -> p %s" % (" ".join(names), " ".join(names)),
                                **{nm: int(s) for nm, s in zip(names, shape[1:])})
            return v

        P = Prog(nc)
        ps = st.enter_context(nc.psum_tensor("ps", [128, 8, 512], F32))
        xT = sb("xT", [128, 8, T], F32)
        stage = sb("stage", [128, 4, 2048], F32)
        ident = sb("ident", [128, 128], F32)
        ones_bf = sb("ones_bf", [128, 128], BF16)
        bo64 = sb("bo64", [128, 128], BF16)
        V0T = sb("V0T", [128, 112], F32)
        V1T = sb("V1T", [128, 46], F32)
        cvec = sb("cvec", [128, 8, 2], F32)
        modT = sb("modT", [128, 2, 48, 2], F32)
        drv = sb("drv", [128, 2, 2, 8, 2], F32)

        P.dma(ident[:], ident_d)
        P.dma(bo64[:], bo64_d)
        P.memset(ones_bf[:], 1.0)
        P.dma(stage[0:112, 0, 0:128], vecs0_d)
        P.dma(stage[0:46, 1, 0:128], vecs1_d)
        P.transpose(ps[:, 0, 0:112], stage[0:112, 0, 0:128], ident[0:112, 0:112])
        P.transpose(ps[:, 1, 0:46], stage[0:46, 1, 0:128], ident[0:46, 0:46])
        P.copy(V0T[:], ps[:, 0, 0:112])
        P.copy(V1T[:], ps[:, 1, 0:46])
        for wh_ in range(2):
            P.act(cvec[:, :, wh_], V0T[:, 8 * wh_:8 * wh_ + 8], AF.Exp, scale=-1.0)
            P.ts(cvec[:, :, wh_], cvec[:, :, wh_], 1.0, None, ALU.add)
            P.recip(cvec[:, :, wh_], cvec[:, :, wh_])
            P.tt(cvec[:, :, wh_], V0T[:, 8 * wh_:8 * wh_ + 8], cvec[:, :, wh_], ALU.mult)

        s_ada = ExitStack()
        modrow = sb("modrow", [128, 6144], F32, s_ada)
        for l in range(n_layers):
            for s in range(12):
                slot = (s % 2) * 2
                for hlf in range(2):
                    P.dma(stage[:, slot + hlf, :].rearrange("p (k n) -> p k n", k=4),
                          wada_d[l, hlf * 512:(hlf + 1) * 512, s * 512:(s + 1) * 512].rearrange("(k p) n -> p k n", p=128))
                bA = s % 2
                for k in range(8):
                    wv = stage[:, slot + k // 4, :].rearrange("p (k n) -> p k n", k=4)
                    P.mm(ps[0:2, bA, 0:512], cvec[:, k, :], wv[:, k % 4, :], k == 0, k == 7)
                P.copy(modrow[0:2, s * 512:(s + 1) * 512], ps[0:2, bA, 0:512])
            for jg in range(48):
                P.transpose(ps[:, 2, 2 * jg:2 * jg + 2], modrow[0:2, jg * 128:(jg + 1) * 128], ident[0:2, 0:2])
            pv = ps[:, 2, 0:96].rearrange("p (a b) -> p a b", b=2)
            bias = V0T[:, 16 + 48 * l:16 + 48 * (l + 1)]
            P.tt(modT[:, l], pv, _bc(bias, 2, 2), ALU.add)
            for which in range(2):
                sc = modT[:, l, (1 + 3 * which) * 8:(2 + 3 * which) * 8, :]
                nw = V1T[:, (16 * which + 8 * l):(16 * which + 8 * l + 8)]
                P.ts(drv[:, l, which], sc, 1.0, 32.0, ALU.add, ALU.mult)
                P.tt(drv[:, l, which], drv[:, l, which], _bc(nw, 2, 2), ALU.mult)
        s_ada.close()
        P.barrier()

        def A_of(l, which, j, who):
            return drv[:, l, which, j, who:who + 1]

        def B_of(l, which, j, who):
            sec = 0 if which == 0 else 3
            return modT[:, l, sec * 8 + j, who:who + 1]

        def G_of(l, which, j, who):
            sec = 2 if which == 0 else 5
            return modT[:, l, sec * 8 + j, who:who + 1]

        with ExitStack() as s0:
            xin = sb("xin", [128, 2, 1024], F32, s0)
            ev = 0
            for t in range(NT):
                src = ctx_d[t * 128:(t + 1) * 128, :] if t < 2 else x_d[(t - 2) * 128:(t - 1) * 128, :]
                P.dma(xin[:, t % 2, :], src)
                for half in range(2):
                    b = (2 * t + half) % 4 + 3
                    for jj in range(4):
                        j = half * 4 + jj
                        P.transpose(ps[:, b, jj * 128:(jj + 1) * 128], xin[:, t % 2, j * 128:(j + 1) * 128], ident[:])
                    dst = xT[:, half * 4:half * 4 + 4, t * 128:(t + 1) * 128]
                    srcp = ps[:, b, :].rearrange("p (a b) -> p a b", b=128)
                    if ev % 2 == 0:
                        P.copy(dst, srcp)
                    else:
                        P.copy(dst, srcp, eng="act")
                    ev += 1
            P.barrier()

        def norm_chunk(l, which, c0, w, who, sq, xn, rs, bank):
            P.act(sq[:, :, :w], xT[:, :, c0:c0 + w], AF.Square)
            for j in range(8):
                P.mm(ps[:, bank, :w], ones_bf[:], sq[:, j, :w], j == 0, j == 7)
            P.act(rs[:, :w], ps[:, bank, :w], AF.Ln, bias=EPS * 1024.0, scale=1.0)
            P.act(rs[:, :w], rs[:, :w], AF.Exp, scale=-0.5)
            P.tt(xn[:, :, :w], xT[:, :, c0:c0 + w], _bc(rs[:, :w], 1, 8), ALU.mult)

        def load_w(dst_bf, src_rows_by_cols, ncols, slot_rot, eng="pool"):
            kt = dst_bf.shape[1]
            per = max(1, 2048 // ncols)
            k = 0
            while k < kt:
                n = min(per, kt - k)
                slot = slot_rot.next()
                sv = stage[:, slot, 0:n * ncols].rearrange("p (k n) -> p k n", n=ncols)
                P.dma(sv, src_rows_by_cols[k * 128:(k + n) * 128, :].rearrange("(k p) n -> p k n", p=128))
                P.copy(dst_bf[:, k:k + n, :], sv, eng=("act" if cast_rot.next() == 0 else "dve"))
                k += n

        srot = _Rot([0, 1, 2, 3])
        cast_rot = _Rot([0, 1])

        def out_proj(l, Wo, mix, nk, chunks, banks):
            for (c0, w, who) in chunks:
                for m in range(8):
                    b = banks.next()
                    for k in range(nk):
                        P.mm(ps[:, b, :w], Wo[:, k, m * 128:(m + 1) * 128], mix[:, k, c0:c0 + w], k == 0, k == nk - 1)
                    P.stt(xT[:, m, c0:c0 + w], ps[:, b, :w], G_of(l, 0, m, who), xT[:, m, c0:c0 + w], ALU.mult, ALU.add)

        def dump_dbg(idx):
            if dbg_d is not None:
                for j in range(8):
                    P.dma(dbg_d[idx, :, j, :], xT[:, j, :])

        def gla_phase(l, last, lchunks):
            with ExitStack() as s1:
                Wo = sb("g_Wo", [128, 2, 1024], BF16, s1)
                wup = sb("g_wup", [128, 2, 128], BF16, s1)
                msk = sb("g_msk", [128, 6, 128], F32, s1)
                qT = sb("g_qT", [128, T], BF16, s1)
                kT = sb("g_kT", [128, T], BF16, s1)
                ktok = sb("g_ktok", [128, NT, 128], BF16, s1)
                vtok = sb("g_vtok", [128, NT, 256], BF16, s1)
                sgT = sb("g_sg", [128, 2, T], BF16, s1)
                gd = sb("g_gd", [128, T], BF16, s1)
                oT = sb("g_oT", [128, 2, T], F32, s1)
                g8 = sb("g_g8", [128, 1], F32, s1)
                rb = _Rot([0, 1, 2, 3, 4, 5, 6, 7])
                P.dma(msk[:], masks_d)
                P.ts(g8[:], V1T[:, 40 + l:41 + l], 8.0, None, ALU.mult)
                load_w(Wo, wout_d[l][256:512, :], 1024, srot)
                sl = srot.next()
                P.dma(stage[0:33, sl, 0:256].rearrange("p (d c) -> p d c", d=2), wup_d[l].rearrange("d r c -> r d c"))
                P.copy(wup[0:33], stage[0:33, sl, 0:256].rearrange("p (d c) -> p d c", d=2))
                P.memset(gd[0:64], 1.0)
                if sub == "E1a":
                    return
                with ExitStack() as s2:
                    Wg = sb("g_W", [128, 8, 800], BF16, s2)
                    hcb = sb("g_hc", [128, 8, 512], BF16, s2)
                    sge = sb("g_sge", [128, 2, 512], F32, s2)
                    load_w(Wg[:, :, 0:512], win_d[l][:, 256:768], 512, srot)
                    load_w(Wg[:, :, 512:800], win_d[l][:, 768:1056], 288, srot)
                    if sub == "E1b":
                        return
                    for ci, (c0, w, who) in enumerate(CHUNKS):
                        P.dma(hcb[:, :, :w], hT_d[:, :, c0:c0 + w])
                        for (m0, msz, kind) in ((0, 128, "q"), (128, 128, "k"), (512, 128, "g0"), (640, 128, "g1"),
                                                (768, 32, "df")):
                            if _DBG.get("kinds") is not None and kind not in _DBG["kinds"]:
                                continue
                            b = rb.next()
                            for k in range(8):
                                P.mm(ps[0:msz, b, :w], Wg[:, k, m0:m0 + msz], hcb[:, k, :w], k == 0, k == 7)
                            if kind == "q":
                                P.ts(qT[:, c0:c0 + w], ps[:, b, :w], float(32.0 ** -0.5), None, ALU.mult)
                            elif kind == "k":
                                P.copy(kT[:, c0:c0 + w], ps[:, b, :w])
                            elif kind in ("g0", "g1"):
                                gi = 0 if kind == "g0" else 1
                                P.act(sgT[:, gi, c0:c0 + w], ps[:, b, :w], AF.Silu)
                            else:
                                P.copy(gd[0:32, c0:c0 + w], ps[0:32, b, :w])
                        if sub == "E1c" or _DBG.get("notm"):
                            continue
                        for tt in range(w // 128):
                            tg = c0 // 128 + tt
                            b = rb.next()
                            for k in range(8):
                                P.mm(ps[:, b, 0:384], hcb[:, k, tt * 128:(tt + 1) * 128], Wg[:, k, 128:512], k == 0, k == 7)
                            P.copy(ktok[:, tg, :], ps[:, b, 0:128])
                            P.copy(vtok[:, tg, :], ps[:, b, 128:384], eng="act")
                if sub == "E1":
                    return
                sflat = stage.rearrange("p a b -> p (a b)")
                sp_tok = sflat[:, 0:2304].rearrange("p (t c) -> p t c", c=128)
                sbf = sflat[:, 2304:8192].bitcast(BF16)
                q_t = sbf[:, 0:2304]
                k_t = sbf[:, 2304:4608]
                kk = sbf[:, 4608:6912].rearrange("p (t c) -> p t c", c=128)
                Sb = sbf[:, 6912:9216].rearrange("p (i c) -> p i c", c=64)
                with ExitStack() as s2:
                    te = sb("g_te", [128, 512], F32, s2)
                    ec = sb("g_ec", [128, 512], F32, s2)
                    en = sb("g_en", [128, 512], F32, s2)
                    esf = sb("g_esf", [128, 512], F32, s2)
                    dec = sb("g_dec", [128, 36], F32, s2)
                    Sall = sb("g_Sall", [128, 37, 64], F32, s2)
                    attb = sb("g_attb", [128, 2, 4, 128], BF16, s2)
                    groups = [(0, 4), (4, 4), (8, 4), (12, 4), (16, 2)]
                    for d in range(2):
                        for (t0, n) in groups:
                            b = rb.next()
                            for i in range(n):
                                t = t0 + i
                                P.mm(ps[:, b, i * 128:(i + 1) * 128], gd[0:33, t * 128:(t + 1) * 128], wup[0:33, d, :], True, True)
                            P.act(te[:, 0:n * 128], ps[:, b, 0:n * 128], AF.Exp, scale=-1.0)
                            P.act(sp_tok[:, t0:t0 + n, :], te[:, 0:n * 128].rearrange("p (t c) -> p t c", c=128), AF.Ln, bias=1.0)
                        for (t0, n) in groups:
                            bC = rb.next()
                            bS = rb.next()
                            for i in range(n):
                                t = t0 + i
                                P.mm(ps[:, bC, i * 128:(i + 1) * 128], sp_tok[:, t, :], msk[:, d, :], True, True)
                                P.mm(ps[:, bS, i * 128:(i + 1) * 128], msk[:, 2 + d, :], sp_tok[:, t, :], True, True)
                            nn = n * 128
                            cols = slice(t0 * 128, t0 * 128 + nn)
                            P.act(ec[:, 0:nn], ps[:, bC, 0:nn], AF.Exp)
                            P.act(en[:, 0:nn], ps[:, bC, 0:nn], AF.Exp, scale=-1.0)
                            P.act(esf[:, 0:nn], ps[:, bS, 0:nn], AF.Exp)
                            P.tt(q_t[:, cols], qT[:, cols], ec[:, 0:nn], ALU.mult)
                            P.tt(k_t[:, cols], kT[:, cols], en[:, 0:nn], ALU.mult, eng="pool")
                            P.tt(kk[:, t0:t0 + n, :], ktok[:, t0:t0 + n, :], esf[:, 0:nn].rearrange("p (t c) -> p t c", c=128), ALU.mult)
                            ecv = ec[:, 0:nn].rearrange("p (i c) -> p i c", c=64)
                            pick = 63 if d == 0 else 0
                            P.copy(dec[:, 2 * t0:2 * t0 + 2 * n], ecv[:, :, pick], eng="pool")
                        if sub == "E2":
                            continue
                        P.memset(Sall[:, 0, :], 0.0)
                        order = list(range(36)) if d == 0 else ([3, 2, 1, 0] + list(range(35, 3, -1)))
                        pos_of = {ci_: i_ for i_, ci_ in enumerate(order)}
                        for idx_, ci in enumerate(order):
                            t, c = ci // 2, ci % 2
                            r0 = 64 * c
                            b = rb.next()
                            for h in range(4):
                                o_ap = ps[32 * h:32 * h + 32, b, 0:64]
                                l_ap = kk[r0:r0 + 64, t, 32 * h:32 * h + 32]
                                r_ap = vtok[r0:r0 + 64, t, 64 * h:64 * h + 64]
                                P._rec("pe", (lambda e, o_ap=o_ap, l_ap=l_ap, r_ap=r_ap, r0=r0, h=h:
                                              e.matmul(o_ap, l_ap, r_ap, start=True, stop=True, tile_position=(r0, 32 * h))),
                                       [l_ap, r_ap], [o_ap])
                            P.stt(Sall[:, idx_ + 1, :], Sall[:, idx_, :], dec[:, ci:ci + 1], ps[:, b, 0:64], ALU.mult, ALU.add)
                        P.copy(Sb[:, :, :], Sall[:, 0:36, :], eng="pool")
                        if sub == "E3":
                            continue
                        rbo = _Rot([4, 5, 6, 7])
                        for t in range(NT):
                            if last and t < 2:
                                continue
                            tc_ = slice(t * 128, (t + 1) * 128)
                            bA = 0
                            for h in range(4):
                                o_ap = ps[:, bA + h, 0:128]
                                l_ap = k_t[32 * h:32 * h + 32, tc_]
                                r_ap = q_t[32 * h:32 * h + 32, tc_]
                                P._rec("pe", (lambda e, o_ap=o_ap, l_ap=l_ap, r_ap=r_ap, h=h:
                                              e.matmul(o_ap, l_ap, r_ap, start=True, stop=True, tile_position=(32 * h, 0))),
                                       [l_ap, r_ap], [o_ap])
                            ab = attb[:, t % 2]
                            P.tt(ab, ps[:, bA:bA + 4, 0:128], _bc(msk[:, 4 + d, :], 1, 4), ALU.mult)
                            bO = rbo.next()
                            for h in range(4):
                                po = 64 * (h % 2)
                                cb = (h // 2) * 128
                                o_ap = ps[po:po + 64, bO, cb:cb + 128]
                                l_ap = vtok[:, t, 64 * h:64 * h + 64]
                                r_ap = ab[:, h, :]
                                P._rec("pe", (lambda e, o_ap=o_ap, l_ap=l_ap, r_ap=r_ap, po=po:
                                              e.matmul(o_ap, l_ap, r_ap, start=True, stop=False, tile_position=(0, po))),
                                       [l_ap, r_ap], [o_ap])
                                for c in range(2):
                                    ci = 2 * t + c
                                    o2 = ps[po:po + 64, bO, cb + 64 * c:cb + 64 * c + 64]
                                    l2 = Sb[32 * h:32 * h + 32, pos_of[ci], :]
                                    r2 = q_t[32 * h:32 * h + 32, t * 128 + 64 * c:t * 128 + 64 * c + 64]
                                    P._rec("pe", (lambda e, o2=o2, l2=l2, r2=r2, h=h, po=po, c=c:
                                                  e.matmul(o2, l2, r2, start=False, stop=(c == 1), tile_position=(32 * h, po))),
                                           [l2, r2], [o2])
                            pso = ps[:, bO, 0:256].rearrange("p (a c) -> p a c", c=128)
                            if d == 0:
                                P.copy(oT[:, :, tc_], pso, eng="act")
                            else:
                                P.tt(oT[:, :, tc_], oT[:, :, tc_], pso, ALU.add)
                    if sub in ("E2", "E3"):
                        return
                    sq = sb("g_sq", [128, 2, 512], BF16, s2)
                    rs = sb("g_rs", [128, 512], F32, s2)
                    tmp = sb("g_tmp", [128, 512], F32, s2)
                    for (c0, w, who) in lchunks:
                        P.act(sq[:, :, :w], oT[:, :, c0:c0 + w], AF.Square)
                        for m in range(2):
                            b = rb.next()
                            P.mm(ps[:, b, :w], bo64[:], sq[:, m, :w], True, True)
                            P.act(rs[:, :w], ps[:, b, :w], AF.Ln, bias=EPS * 64.0, scale=1.0)
                            P.act(rs[:, :w], rs[:, :w], AF.Exp, scale=-0.5)
                            P.stt(tmp[:, :w], oT[:, m, c0:c0 + w], g8[:, 0:1], rs[:, :w], ALU.mult, ALU.mult)
                            P.tt(sgT[:, m, c0:c0 + w], tmp[:, :w], sgT[:, m, c0:c0 + w], ALU.mult, eng="pool")
                out_proj(l, Wo, sgT, 2, lchunks, rb)
                P.barrier()

        def att_phase(l, last, lchunks):
            with ExitStack() as s1:
                Wo = sb("a_Wo", [128, 4, 1024], BF16, s1)
                qr = sb("a_qr", [128, 4, T], BF16, s1)
                kd = sb("a_kd", [128, 2, T], BF16, s1)
                vt = sb("a_vt", [128, NT, 128], BF16, s1)
                g8 = sb("a_g8", [128, 2], F32, s1)
                rb = _Rot([0, 1, 2, 3, 4, 5, 6, 7])
                P.ts(g8[:, 0:1], V1T[:, 42 + l:43 + l], 8.0, None, ALU.mult)
                P.ts(g8[:, 1:2], V1T[:, 44 + l:45 + l], 8.0, None, ALU.mult)
                load_w(Wo, wout_d[l][512:1024, :], 1024, srot)
                with ExitStack() as s2:
                    Wq = sb("a_Wq", [128, 8, 512], BF16, s2)
                    Wk = sb("a_Wk", [128, 8, 256], BF16, s2)
                    Wv = sb("a_Wv", [128, 8, 128], BF16, s2)
                    psw = sb("a_psw", [128, 128], F32, s2)
                    sflat_a = stage.rearrange("p a b -> p (a b)")
                    rc = sflat_a[:, 0:T]
                    rsn = sflat_a[:, T:2 * T]
                    hcb = sb("a_hc", [128, 8, 512], BF16, s2)
                    qg2 = sb("a_qg", [128, 2, 512], F32, s2)
                    sq2 = sb("a_sq", [128, 2, 512], BF16, s2)
                    rs2 = sb("a_rsd", [128, 2, 512], F32, s2)
                    t12 = sb("a_t1", [128, 2, 512], F32, s2)
                    t22 = sb("a_t2", [128, 2, 512], F32, s2)
                    P.dma(psw[:], psw_d)
                    load_w(Wq, win_d[l][:, 1056:1568], 512, srot)
                    load_w(Wv, win_d[l][:, 1696:1824], 128, srot)
                    sl = srot.next()
                    sv = stage[:, sl, :].rearrange("p (k n) -> p k n", n=256)
                    for g in range(2):
                        for r in range(2):
                            P.dma(sv[:, :, (2 * g + r) * 64:(2 * g + r + 1) * 64],
                                  win_d[l][:, 1568 + 64 * g:1568 + 64 * g + 64].rearrange("(k p) n -> p k n", p=128))
                    P.copy(Wk[:], sv, eng="act")
                    P.dma(rc, ropec_d)
                    P.dma(rsn, ropes_d)
                    tcnt = 0
                    for ci, (c0, w, who) in enumerate(CHUNKS):
                        P.dma(hcb[:, :, :w], hT_d[:, :, c0:c0 + w])
                        for i in range(6):
                            qg = qg2[:, tcnt % 2]
                            sq = sq2[:, tcnt % 2]
                            rs = rs2[:, tcnt % 2]
                            t1 = t12[:, tcnt % 2]
                            t2 = t22[:, tcnt % 2]
                            tcnt += 1
                            isq = i < 4
                            b0 = rb.next()
                            for k in range(8):
                                wsl = Wq[:, k, i * 128:(i + 1) * 128] if isq else Wk[:, k, (i - 4) * 128:(i - 3) * 128]
                                P.mm(ps[:, b0, :w], wsl, hcb[:, k, :w], k == 0, k == 7)
                            gcol = g8[:, 0:1] if isq else g8[:, 1:2]
                            P.act(qg[:, :w], ps[:, b0, :w], AF.Identity, scale=gcol)
                            P.act(sq[:, :w], ps[:, b0, :w], AF.Square)
                            b1 = rb.next()
                            P.mm(ps[:, b1, :w], bo64[:], sq[:, :w], True, True)
                            P.act(rs[:, :w], ps[:, b1, :w], AF.Ln, bias=EPS * 64.0, scale=1.0)
                            P.act(rs[:, :w], rs[:, :w], AF.Exp, scale=-0.5)
                            b2 = rb.next()
                            P.mm(ps[:, b2, :w], psw[:], qg[:, :w], True, True)
                            P.tt(t1[:, :w], qg[:, :w], rc[:, c0:c0 + w], ALU.mult, eng="pool")
                            P.tt(t2[:, :w], ps[:, b2, :w], rsn[:, c0:c0 + w], ALU.mult)
                            P.tt(t1[:, :w], t1[:, :w], t2[:, :w], ALU.add, eng="pool")
                            dst = qr[:, i, c0:c0 + w] if isq else kd[:, i - 4, c0:c0 + w]
                            P.tt(dst, t1[:, :w], rs[:, :w], ALU.mult)
                        for tt in range(w // 128):
                            tg = c0 // 128 + tt
                            b = rb.next()
                            for k in range(8):
                                P.mm(ps[:, b, 0:128], hcb[:, k, tt * 128:(tt + 1) * 128], Wv[:, k, :], k == 0, k == 7)
                            P.copy(vt[:, tg, :], ps[:, b, 0:128], eng="act")
                with ExitStack() as s2:
                    amix = sb("a_mix", [128, 4, T], BF16, s2)
                    PT = sb("a_PT", [128, 4, 512], BF16, s2)
                    rd = sb("a_rd", [128, 2, 512], F32, s2)
                    rS = _Rot([0, 1, 2, 3])
                    rO = _Rot([(4, 5), (6, 7)])
                    rP = _Rot([0, 1, 2, 3])
                    for (c0, w, who) in lchunks:
                        kts = list(range(2)) if who == 1 else list(range(NT))
                        for h in range(8):
                            g = h // 4
                            qt = h // 2
                            po = 64 * (h % 2)
                            bO, bD = rO.next()
                            sbank = {}

                            def score(kt_):
                                bS_ = rS.next()
                                sbank[kt_] = bS_
                                P.mm(ps[:, bS_, :w], kd[po:po + 64, g, kt_ * 128:(kt_ + 1) * 128], qr[po:po + 64, qt, c0:c0 + w], True, True)

                            for kt_ in kts[:3]:
                                score(kt_)
                            for idx, kt in enumerate(kts):
                                bS = sbank[kt]
                                pt = PT[:, rP.next(), :w]
                                P.act(pt, ps[:, bS, :w], AF.Exp, scale=0.125)
                                if idx + 3 < len(kts):
                                    score(kts[idx + 3])
                                P.mm(ps[po:po + 64, bO, :w], vt[:, kt, g * 64:(g + 1) * 64], pt, idx == 0, idx == len(kts) - 1)
                                P.mm(ps[po:po + 64, bD, :w], ones_bf[:, 0:64], pt, idx == 0, idx == len(kts) - 1)
                            P.recip(rd[po:po + 64, h % 2, :w], ps[po:po + 64, bD, :w])
                            P.tt(amix[po:po + 64, qt, c0:c0 + w], ps[po:po + 64, bO, :w], rd[po:po + 64, h % 2, :w], ALU.mult)
                    out_proj(l, Wo, amix, 4, lchunks, rb)
                P.barrier()

        def moe_phase(l, last, lchunks):
            with ExitStack() as s1:
                h2T = sb("m_h2T", [128, 8, T], BF16, s1)
                GT = sb("m_GT", [128, T], BF16, s1)
                esel = sb("m_esel", [128, 16, 128], BF16, s1)
                P.dma(esel[0:16], esel_d)
                tiles = list(range(2, NT)) if last else list(range(NT))
                with ExitStack() as s2:
                    sq = sb("m_sq", [128, 8, 512], BF16, s2)
                    xn = sb("m_xn", [128, 8, 512], F32, s2)
                    rs = sb("m_rs", [128, 512], F32, s2)
                    wr = sb("m_wr", [128, 8, 16], F32, s2)
                    brep = sb("m_brep", [128, 288], F32, s2)
                    s_tok = sb("m_s", [128, NT, 16], F32, s2)
                    sel2 = sb("m_sel2", [128, 72, 8], F32, s2)
                    p1 = sb("m_p1", [128, 72, 4], F32, s2)
                    p2 = sb("m_p2", [128, 72, 2], F32, s2)
                    gs = sb("m_gs", [128, 72], F32, s2)
                    gs2 = sb("m_gs2", [128, 72], F32, s2)
                    gmax = sb("m_gmax", [128, NT], F32, s2)
                    oh = sb("m_oh", [128, 72], F32, s2)
                    cnt = sb("m_cnt", [128, 72, 4], F32, s2)
                    c2 = sb("m_c2", [128, 72, 4], F32, s2)
                    wsum = sb("m_wsum", [128, NT], F32, s2)
                    gate = sb("m_gate", [128, NT, 16], F32, s2)
                    P.dma(wr[:], wr_d.rearrange("(k p) e -> p k e", p=128))
                    P.dma(brep[:], brep_d)
                    P.memset(s_tok[:], 0.0)
                    for ci, (c0, w, who) in enumerate(lchunks):
                        norm_chunk(l, 1, c0, w, who, sq, xn, rs, ci % 2)
                        for j in range(8):
                            if j % 2 == 0:
                                P.ts(xn[:, j, :w], xn[:, j, :w], A_of(l, 1, j, who), B_of(l, 1, j, who), ALU.mult, ALU.add)
                            else:
                                P.act(xn[:, j, :w], xn[:, j, :w], AF.Identity, bias=B_of(l, 1, j, who), scale=A_of(l, 1, j, who))
                        P.copy(h2T[:, :, c0:c0 + w], xn[:, :, :w], eng="act")
                        b = 2 + ci % 2
                        for tt in range(w // 128):
                            tg = c0 // 128 + tt
                            for k in range(8):
                                P.mm(ps[:, b, tt * 16:(tt + 1) * 16], xn[:, k, tt * 128:(tt + 1) * 128], wr[:, k, :], k == 0, k == 7)
                        nt_ = w // 128
                        tg0 = c0 // 128
                        P.act(s_tok[:, tg0:tg0 + nt_, :], ps[:, b, 0:nt_ * 16].rearrange("p (t e) -> p t e", e=16), AF.Exp, scale=-1.0)
                        P.ts(s_tok[:, tg0:tg0 + nt_, :], s_tok[:, tg0:tg0 + nt_, :], 1.0, None, ALU.add)
                        P.recip(s_tok[:, tg0:tg0 + nt_, :], s_tok[:, tg0:tg0 + nt_, :])
                    sv = s_tok.rearrange("p t (g e) -> p (t g) e", e=4)
                    bv = brep.rearrange("p (a e) -> p a e", e=4)
                    P.tt(sel2[:, :, 0:4], sv, bv, ALU.add)
                    P.tt(sel2[:, :, 4:8], sv, bv, ALU.add)
                    P.tt(p1[:], sel2[:, :, 0:4], sel2[:, :, 1:5], ALU.add)
                    P.tt(p2[:], sel2[:, :, 0:2], sel2[:, :, 2:4], ALU.add)
                    P.reduce(gs[:], p1[:], ALU.max)
                    P.reduce(gs2[:], p2[:], ALU.max)
                    P.tt(gs[:], gs[:], gs2[:], ALU.max)
                    P.reduce(gmax[:], gs.rearrange("p (t g) -> p t g", g=4), ALU.max)
                    P.tt(oh.rearrange("p (t g) -> p t g", g=4), gs.rearrange("p (t g) -> p t g", g=4), _bc(gmax[:], 2, 4), ALU.is_equal)
                    P.tt(cnt[:], sel2[:, :, 1:5], sel2[:, :, 0:4], ALU.is_gt)
                    P.tt(c2[:], sel2[:, :, 2:6], sel2[:, :, 0:4], ALU.is_gt)
                    P.tt(cnt[:], cnt[:], c2[:], ALU.add)
                    P.tt(c2[:], sel2[:, :, 3:7], sel2[:, :, 0:4], ALU.is_gt)
                    P.tt(cnt[:], cnt[:], c2[:], ALU.add)
                    P.ts(cnt[:], cnt[:], 1.0, None, ALU.is_le)
                    P.tt(cnt[:], cnt[:], _bc(oh[:], 2, 4), ALU.mult)
                    gv = gate.rearrange("p t (g e) -> p (t g) e", e=4)
                    P.tt(gv, sv, cnt[:], ALU.mult)
                    P.reduce(wsum[:], gate[:], ALU.add)
                    P.ts(wsum[:], wsum[:], 1e-30, None, ALU.max)
                    P.recip(wsum[:], wsum[:])
                    P.tt(gate[:], gate[:], _bc(wsum[:], 2, 16), ALU.mult)
                    for t in tiles:
                        b = 4 + (t // 4) % 2
                        P.transpose(ps[0:16, b, (t % 4) * 128:(t % 4 + 1) * 128], gate[:, t, :], ident[:])
                        P.copy(GT[0:16, t * 128:(t + 1) * 128], ps[0:16, b, (t % 4) * 128:(t % 4 + 1) * 128])
                with ExitStack() as s2:
                    wbuf = sb("m_wbuf", [128, 3, 3, 2048], BF16, s2)
                    sg = sb("m_sg", [128, 2, 512], F32, s2)
                    t1 = sb("m_t1", [128, 2, 512], F32, s2)
                    abuf = sb("m_a", [128, 2, 2, 512], BF16, s2)
                    rG = _Rot([0, 1])
                    rU = _Rot([2, 3])
                    rD = _Rot([5, 6, 7])
                    it = 0
                    pending = None

                    def emit_down(pd):
                        Wd_p, ab_p, c0p, wp, whop = pd
                        for m in range(8):
                            bD = rD.next()
                            for ft in range(2):
                                P.mm(ps[:, bD, :wp], Wd_p[:, ft, m * 128:(m + 1) * 128], ab_p[:, ft, :wp], ft == 0, ft == 1)
                            P.stt(xT[:, m, c0p:c0p + wp], ps[:, bD, :wp], G_of(l, 1, m, whop), xT[:, m, c0p:c0p + wp], ALU.mult, ALU.add)

                    for e in range(16):
                        for fh in range(2):
                            u = e * 2 + fh
                            ws = u % 3
                            Wg_ = wbuf[:, ws, 0].rearrange("p (k f) -> p k f", f=256)
                            Wu_ = wbuf[:, ws, 1].rearrange("p (k f) -> p k f", f=256)
                            Wd_ = wbuf[:, ws, 2].rearrange("p (k d) -> p k d", d=1024)
                            for mi, (dst, srcw) in enumerate(((Wg_, weg_d[l, e][:, fh * 256:(fh + 1) * 256]),
                                                              (Wu_, weu_d[l, e][:, fh * 256:(fh + 1) * 256]))):
                                sl = srot.next()
                                sv_ = stage[:, sl, :].rearrange("p (k f) -> p k f", f=256)
                                P.dma(sv_, srcw.rearrange("(k p) f -> p k f", p=128))
                                P.copy(dst, sv_, eng="act")
                            sl = srot.next()
                            sv_ = stage[:, sl, :].rearrange("p (k d) -> p k d", d=1024)
                            P.dma(sv_, wed_d[l, e][fh * 256:(fh + 1) * 256, :].rearrange("(k p) d -> p k d", p=128))
                            P.copy(Wd_, sv_, eng="act")
                            for (c0, w, who) in lchunks:
                                ab = abuf[:, it % 2]
                                it += 1
                                P.mm(ps[:, 4, :w], esel[0:16, e, :], GT[0:16, c0:c0 + w], True, True)
                                for ft in range(2):
                                    bG = rG.next()
                                    bU = rU.next()
                                    for k in range(8):
                                        P.mm(ps[:, bG, :w], Wg_[:, k, ft * 128:(ft + 1) * 128], h2T[:, k, c0:c0 + w], k == 0, k == 7)
                                    for k in range(8):
                                        P.mm(ps[:, bU, :w], Wu_[:, k, ft * 128:(ft + 1) * 128], h2T[:, k, c0:c0 + w], k == 0, k == 7)
                                    P.act(sg[:, ft, :w], ps[:, bG, :w], AF.Silu)
                                    P.tt(t1[:, ft, :w], sg[:, ft, :w], ps[:, bU, :w], ALU.mult)
                                    P.tt(ab[:, ft, :w], t1[:, ft, :w], ps[:, 4, :w], ALU.mult)
                                if pending is not None:
                                    emit_down(pending)
                                pending = (Wd_, ab, c0, w, who)
                    emit_down(pending)
                P.barrier()

        for l in range(n_layers):
            last = (l == n_layers - 1) and (n_layers == 2)
            lchunks = CHUNKS[1:] if last else CHUNKS

            with ExitStack() as s1:
                sq = sb("b_sq", [128, 8, 512], BF16, s1)
                xn = sb("b_xn", [128, 8, 512], F32, s1)
                rs = sb("b_rs", [128, 512], F32, s1)
                hc = sb("b_hc", [128, 2, 8, 512], BF16, s1)
                for ci, (c0, w, who) in enumerate(CHUNKS):
                    norm_chunk(l, 0, c0, w, who, sq, xn, rs, ci % 2)
                    for j in range(8):
                        if j % 2 == 0:
                            P.ts(hc[:, ci % 2, j, :w], xn[:, j, :w], A_of(l, 0, j, who), B_of(l, 0, j, who), ALU.mult, ALU.add)
                        else:
                            P.act(hc[:, ci % 2, j, :w], xn[:, j, :w], AF.Identity, bias=B_of(l, 0, j, who), scale=A_of(l, 0, j, who))
                    P.dma(hT_d[:, :, c0:c0 + w], hc[:, ci % 2, :, :w])
                P.barrier()
            if stop_at == ("B", l):
                break

            with ExitStack() as s1:
                Wu = sb("f_Wu", [128, 8, 256], BF16, s1)
                Wo = sb("f_Wo", [128, 2, 1024], BF16, s1)
                cs64 = sb("f_cs64", [128, 256], BF16, s1)
                uT = sb("f_uT", [128, 2, T], BF16, s1)
                ucs = sb("f_ucs", [128, NT, 512], BF16, s1)
                fmix = sb("f_mix", [128, 2, T], BF16, s1)
                hcb = sb("f_hc", [128, 2, 8, 512], BF16, s1)
                tab = sb("f_tab", [128, 1, 2, 16, 512], BF16, s1)
                P.dma(cs64[:], cs64_d)
                load_w(Wu, win_d[l][:, 0:256], 256, srot)
                load_w(Wo, wout_d[l][0:256, :], 1024, srot)
                rb = _Rot([0, 1, 2, 3])
                for ci, (c0, w, who) in enumerate(CHUNKS):
                    P.dma(hcb[:, ci % 2, :, :w], hT_d[:, :, c0:c0 + w])
                    for m in range(2):
                        b = rb.next()
                        for k in range(8):
                            P.mm(ps[:, b, :w], Wu[:, k, m * 128:(m + 1) * 128], hcb[:, ci % 2, k, :w], k == 0, k == 7)
                        P.copy(uT[:, m, c0:c0 + w], ps[:, b, :w], eng=("act" if m == 0 else "dve"))
                t_lo = 2 if last else 0
                for t in range(t_lo, NT):
                    b = rb.next()
                    for j in range(2):
                        P.mm(ps[:, b, j * 256:(j + 1) * 256], uT[:, j, t * 128:(t + 1) * 128], cs64[:], True, True)
                    P.copy(ucs[:, t, :], ps[:, b, :], eng=("act" if t % 2 == 0 else "dve"))
                tab2 = tab.rearrange("p a c t n -> p (a c t n)").rearrange("p (b c t n) -> p b c t n", b=2, c=2, t=16)
                for pc in range(8):
                    bufi = pc % 2
                    P.dma(tab2[:, bufi, 0], cn_d[:, pc * 256:(pc + 1) * 256].rearrange("(t p) c -> p t c", p=128))
                    P.dma(tab2[:, bufi, 1], sn_d[:, pc * 256:(pc + 1) * 256].rearrange("(t p) c -> p t c", p=128))
                    for j in range(2):
                        b = rb.next()
                        for t in range(16):
                            P.mm(ps[:, b, 0:256], ucs[:, 2 + t, j * 256:j * 256 + 128], tab2[:, bufi, 0, t, :], t == 0, False)
                            P.mm(ps[:, b, 0:256], ucs[:, 2 + t, j * 256 + 128:j * 256 + 256], tab2[:, bufi, 1, t, :], False, t == 15)
                        P.copy(fmix[:, j, 256 + pc * 256:256 + (pc + 1) * 256], ps[:, b, 0:256], eng=("act" if j == 0 else "dve"))
                if not last:
                    c2 = sb("f_c2", [128, 2, 2, 256], BF16, s1)
                    P.dma(c2[:, 0], c256_d.rearrange("(t p) c -> p t c", p=128))
                    P.dma(c2[:, 1], s256_d.rearrange("(t p) c -> p t c", p=128))
                    for j in range(2):
                        b = rb.next()
                        for t in range(2):
                            P.mm(ps[:, b, 0:256], ucs[:, t, j * 256:j * 256 + 128], c2[:, 0, t, :], t == 0, False)
                            P.mm(ps[:, b, 0:256], ucs[:, t, j * 256 + 128:j * 256 + 256], c2[:, 1, t, :], False, t == 1)
                        P.copy(fmix[:, j, 0:256], ps[:, b, 0:256])
                out_proj(l, Wo, fmix, 2, lchunks, rb)
                P.barrier()
            dump_dbg(4 * l + 0)
            if stop_at == ("D", l):
                break

            if 'E' not in skip:
                gla_phase(l, last, lchunks)
            dump_dbg(4 * l + 1)
            if stop_at == ("E", l):
                break

            if 'F' not in skip:
                att_phase(l, last, lchunks)
            dump_dbg(4 * l + 2)
            if stop_at == ("F", l):
                break

            moe_phase(l, last, lchunks)
            dump_dbg(4 * l + 3)
            if stop_at == ("I", l):
                break

        if stop_at is None:
            with ExitStack() as s1:
                sq = sb("o_sq", [128, 8, 512], BF16, s1)
                xn = sb("o_xn", [128, 8, 512], F32, s1)
                rs = sb("o_rs", [128, 512], F32, s1)
                ot = sb("o_ot", [128, 2, 1024], F32, s1)
                fn32 = sb("o_fn", [128, 8], F32, s1)
                P.ts(fn32[:], V1T[:, 32:40], 32.0, None, ALU.mult)
                ev = 0
                for ci, (c0, w, who) in enumerate(CHUNKS[1:]):
                    norm_chunk(0, 0, c0, w, who, sq, xn, rs, 0)
                    for j in range(8):
                        if j % 2 == 0:
                            P.ts(xn[:, j, :w], xn[:, j, :w], fn32[:, j:j + 1], None, ALU.mult)
                        else:
                            P.act(xn[:, j, :w], xn[:, j, :w], AF.Identity, scale=fn32[:, j:j + 1])
                    for tt in range(4):
                        tg = ci * 4 + tt
                        for half in range(2):
                            b = 1 + (2 * tg + half) % 4
                            for jj in range(4):
                                j = half * 4 + jj
                                P.transpose(ps[:, b, jj * 128:(jj + 1) * 128], xn[:, j, tt * 128:(tt + 1) * 128], ident[:])
                            dst = ot[:, tg % 2, half * 512:(half + 1) * 512]
                            if ev % 2 == 0:
                                P.copy(dst, ps[:, b, :])
                            else:
                                P.copy(dst, ps[:, b, :], eng="act")
                            ev += 1
                        P.dma(y_d[tg * 128:(tg + 1) * 128, :], ot[:, tg % 2, :])
        P.emit()
        print("arena peak bytes", astate["peak"], "ops", P.nops)
    return nc


_CONST_CACHE = {}


def _consts():
    if _CONST_CACHE:
        return _CONST_CACHE
    bf = ml_dtypes.bfloat16
    c = {}
    c["ident"] = np.eye(128, dtype=np.float32)
    k = np.arange(64)
    ang = 2 * np.pi * np.outer(k, k) / 64.0
    C64 = np.cos(ang) / 8.0
    S64 = np.sin(ang) / 8.0
    cs = np.zeros((128, 256), np.float64)
    for g in range(2):
        cs[g * 64:(g + 1) * 64, g * 64:(g + 1) * 64] = C64
        cs[g * 64:(g + 1) * 64, 128 + g * 64:128 + (g + 1) * 64] = S64
    c["cs64"] = cs.astype(bf)
    for n, cn, sn in ((2048, "cn", "sn"), (256, "c256", "s256")):
        i = np.arange(n)
        a = 2 * np.pi * (np.outer(i, i) % n) / float(n)
        c[cn] = (np.cos(a) / np.sqrt(n)).astype(bf)
        c[sn] = (-np.sin(a) / np.sqrt(n)).astype(bf)
    inv = 10000.0 ** (-np.arange(16, dtype=np.float64) * 2.0 / 32.0)
    tok = np.arange(2048)
    row = tok // 64
    col = tok % 64
    rc = np.ones((128, T), np.float64)
    rs = np.zeros((128, T), np.float64)
    for hd in range(128):
        a = (hd % 64) // 32
        f = hd % 16
        pos = row if a == 0 else col
        rc[hd, 256:] = np.cos(pos * inv[f])
        rs[hd, 256:] = np.sin(pos * inv[f])
    c["ropec"] = rc.astype(np.float32)
    c["ropes"] = rs.astype(np.float32)
    psw = np.zeros((128, 128), np.float32)
    for hdp in range(128):
        half = (hdp % 32) // 16
        if half == 0:
            psw[hdp + 16, hdp] = -1.0
        else:
            psw[hdp - 16, hdp] = 1.0
    c["psw"] = psw
    j = np.arange(128)[:, None]
    i = np.arange(128)[None, :]
    same = (j // 64) == (i // 64)
    m = np.zeros((128, 6, 128), np.float32)
    m[:, 0, :] = np.where(same & (j <= i), -1.0 / 16.0, 0.0)
    m[:, 1, :] = np.where(same & (j >= i), -1.0 / 16.0, 0.0)
    m[:, 2, :] = np.where(same & (j > i), -1.0 / 16.0, 0.0)
    m[:, 3, :] = np.where(same & (j < i), -1.0 / 16.0, 0.0)
    m[:, 4, :] = np.where(same & (j <= i), 1.0, 0.0)
    m[:, 5, :] = np.where(same & (j >= i), 1.0, 0.0)
    c["masks"] = m
    c["bo64"] = same.astype(np.float32).astype(bf)
    es = np.zeros((16, 16, 128), np.float32)
    for e in range(16):
        es[e, e, :] = 1.0
    c["esel"] = es.astype(bf)
    _CONST_CACHE.update(c)
    return _CONST_CACHE


_NC_CACHE = {}


def _prep_inputs(inputs):
    f = lambda a: np.ascontiguousarray(np.asarray(a, dtype=np.float32))
    x = f(inputs["x"])
    c = f(inputs["c"])
    ctx = f(inputs["ctx"])
    c_ctx = f(inputs["c_ctx"])
    b_ada = f(inputs["b_ada"])
    cst = _consts()
    vecs1 = np.concatenate([
        f(inputs["norm_mix"]).reshape(16, 128),
        f(inputs["norm_ffn"]).reshape(16, 128),
        f(inputs["final_norm"]).reshape(8, 128),
        np.tile(f(inputs["gla_norm"]), (1, 2)),
        np.tile(f(inputs["q_norm"]), (1, 2)),
        np.tile(f(inputs["k_norm"]), (1, 2)),
    ], axis=0)
    wg_ = f(inputs["w_gla_gate_up"])
    bg_ = f(inputs["b_gla_gate"])
    wup = np.zeros((2, 2, 33, 128), np.float32)
    for l_ in range(2):
        for d_ in range(2):
            wup[l_, d_, 16 * d_:16 * d_ + 16, :] = wg_[l_, d_]
            wup[l_, d_, 32, :] = bg_[l_, d_]
    brep = np.ascontiguousarray(np.broadcast_to(np.tile(f(inputs["b_router"]), 18)[None, :], (128, 288)))
    shared = {
        "vecs1": np.ascontiguousarray(vecs1),
        "w_ada": f(inputs["w_ada"]), "w_in": f(inputs["w_in"]), "w_out": f(inputs["w_out"]),
        "wup": np.ascontiguousarray(wup), "w_router": f(inputs["w_router"]), "brep": brep,
        "w_exp_gate": f(inputs["w_exp_gate"]), "w_exp_up": f(inputs["w_exp_up"]), "w_exp_down": f(inputs["w_exp_down"]),
    }
    for k_ in ("ident", "cs64", "cn", "sn", "c256", "s256", "ropec", "ropes", "psw", "masks", "bo64", "esel"):
        shared[k_] = cst[k_]
    in_maps = []
    for b in range(8):
        vecs0 = np.concatenate([c[b].reshape(8, 128), c_ctx.reshape(8, 128),
                                b_ada[0].reshape(48, 128), b_ada[1].reshape(48, 128)], axis=0)
        m = dict(shared)
        m["x"] = x[b]
        m["ctx"] = ctx[b]
        m["vecs0"] = np.ascontiguousarray(vecs0)
        in_maps.append(m)
    return in_maps


def kernel(**inputs):
    in_maps = _prep_inputs(inputs)
    if "nc" not in _NC_CACHE:
        _NC_CACHE["nc"] = build()
    nc = _NC_CACHE["nc"]
    res = run_bass_kernel_spmd(nc, in_maps, core_ids=list(range(8)))
    out = np.stack([np.asarray(r["y"], dtype=np.float32) for r in res.results], axis=0)
    return out
```

```python
import numpy as np
import ml_dtypes
import concourse.bass as bass
import concourse.mybir as mybir
from concourse.bass_utils import run_bass_kernel_spmd

F32 = mybir.dt.float32
BF16 = mybir.dt.bfloat16
AF = mybir.ActivationFunctionType
ALU = mybir.AluOpType
AX = mybir.AxisListType

_DSZ = {F32: 4, BF16: 2}


def _dsz(dt):
    if dt in _DSZ:
        return _DSZ[dt]
    s = str(dt)
    if "32" in s:
        return 4
    if "16" in s:
        return 2
    if "64" in s:
        return 8
    return 1


def _region(ap):
    t = ap.tensor
    name = t.name
    dsz = _dsz(ap.dtype)
    space = str(ap.space)
    off = int(ap.offset)
    if "DRAM" in space.upper() or "HBM" in space.upper():
        ext = 0
        for (s, c) in ap.ap:
            ext += abs(int(s)) * (int(c) - 1)
        return (name, 0, 1, off * dsz, (off + ext + 1) * dsz)
    shape = list(t.shape)
    fsz = 1
    for d in shape[1:]:
        fsz *= int(d)
    p0 = off // fsz
    f0 = off % fsz
    pext = 0
    fext = 0
    for (s, c) in ap.ap:
        s = int(s)
        c = int(c)
        if c <= 1 or s == 0:
            continue
        if s % fsz == 0:
            pext += (s // fsz) * (c - 1)
        else:
            fext += abs(s) * (c - 1)
    b0 = f0 * dsz
    b1 = (f0 + fext + 1) * dsz
    if "PSUM" in space.upper():
        return (name, 0, 128, (b0 // 2048) * 2048, ((b1 + 2047) // 2048) * 2048)
    return (name, p0, p0 + pext + 1, b0, b1)


class _Op:
    __slots__ = ("eng", "fn", "k", "is_dma", "deps_eng", "deps_dma", "need_signal", "sig_val",
                 "dma_sem", "dma_val", "dma_prev", "name")

    def __init__(self, eng, fn, is_dma, name=""):
        self.eng = eng
        self.fn = fn
        self.is_dma = is_dma
        self.k = -1
        self.deps_eng = {}
        self.deps_dma = []
        self.need_signal = False
        self.sig_val = 0
        self.dma_sem = None
        self.dma_val = 0
        self.dma_prev = None
        self.name = name


class Prog:
    ENGS = ("pe", "act", "dve", "pool", "sp")
    NDMA = {"sp": 40, "pool": 8, "act": 8}

    def __init__(self, nc):
        self.nc = nc
        self.eng_ops = {e: [] for e in self.ENGS}
        self.recs = {}
        self.dma_count = {q: 0 for q in self.NDMA}
        self.dma_last = {q: [None] * n for q, n in self.NDMA.items()}
        self.nops = 0

    def _dep(self, x, y):
        if y is x:
            return
        if y.is_dma:
            if y not in x.deps_dma:
                x.deps_dma.append(y)
            return
        if (not x.is_dma) and y.eng == x.eng:
            if x.eng == "pe":
                return
        cur = x.deps_eng.get(y.eng, -1)
        if y.k > cur:
            x.deps_eng[y.eng] = y.k
        y.need_signal = True

    def add(self, eng, fn, reads, writes, is_dma=False, name=""):
        op = _Op(eng, fn, is_dma, name)
        rr = [_region(a) for a in reads if a is not None]
        ww = [_region(a) for a in writes if a is not None]
        for (nm, p0, p1, b0, b1) in rr:
            is_ps = (nm == "ps")
            for rec in self.recs.get(nm, ()):
                if rec[0] < p1 and p0 < rec[1] and rec[2] < b1 and b0 < rec[3]:
                    if rec[4] or (is_ps and rec[5].eng != eng):
                        self._dep(op, rec[5])
        for (nm, p0, p1, b0, b1) in ww:
            for rec in self.recs.get(nm, ()):
                if rec[0] < p1 and p0 < rec[1] and rec[2] < b1 and b0 < rec[3]:
                    self._dep(op, rec[5])
        for (nm, p0, p1, b0, b1) in ww:
            lst = self.recs.setdefault(nm, [])
            lst[:] = [r for r in lst if not (p0 <= r[0] and r[1] <= p1 and b0 <= r[2] and r[3] <= b1)]
            lst.append((p0, p1, b0, b1, True, op))
        for (nm, p0, p1, b0, b1) in rr:
            lst = self.recs.setdefault(nm, [])
            if not is_dma:
                lst[:] = [r for r in lst if not ((not r[4]) and (not r[5].is_dma) and r[5].eng == eng
                                                 and p0 <= r[0] and r[1] <= p1 and b0 <= r[2] and r[3] <= b1)]
            lst.append((p0, p1, b0, b1, False, op))
        op.k = len(self.eng_ops[eng])
        self.eng_ops[eng].append(op)
        if is_dma:
            n = self.NDMA[eng]
            slot = self.dma_count[eng] % n
            self.dma_count[eng] += 1
            prev = self.dma_last[eng][slot]
            op.dma_sem = (eng, slot)
            op.dma_prev = prev
            op.dma_val = (prev.dma_val if prev is not None else 0) + 16
            self.dma_last[eng][slot] = op
        self.nops += 1
        return op

    def barrier(self):
        bop = _Op("sp", lambda e: e.nop(), False, "barrier")
        for e in self.ENGS:
            lst = [o for o in self.eng_ops[e] if not o.is_dma]
            if lst:
                y = lst[-1]
                if e == "sp":
                    continue
                bop.deps_eng[e] = y.k
                y.need_signal = True
        for q in self.NDMA:
            for y in self.dma_last[q]:
                if y is not None:
                    bop.deps_dma.append(y)
        bop.k = len(self.eng_ops["sp"])
        bop.need_signal = True
        self.eng_ops["sp"].append(bop)
        self.recs = {"__barrier__": [(0, 1, 0, 1, True, bop)]}
        self._barrier_op = bop
        self._barrier_seen = set()
        return bop

    def _barrier_dep(self, op):
        b = getattr(self, "_barrier_op", None)
        if b is None or op is b or op.eng == "sp" or op.eng in self._barrier_seen:
            return
        self._barrier_seen.add(op.eng)
        if b.k > op.deps_eng.get("sp", -1):
            op.deps_eng["sp"] = b.k

    def _rec(self, eng, fn, reads, writes, is_dma=False, name=""):
        op = self.add(eng, fn, reads, writes, is_dma, name)
        self._barrier_dep(op)
        if eng == "pe":
            l = reads[0]
            rr = lambda v: 32 if v <= 32 else (64 if v <= 64 else 128)
            op.name = (rr(int(l.shape[0])), rr(int(l.shape[-1])))
        return op

    def mm(self, out, lhsT, rhs, start=True, stop=True):
        return self._rec("pe", lambda e: e.matmul(out, lhsT, rhs, start=start, stop=stop), [lhsT, rhs], [out])

    def transpose(self, out, in_, ident):
        return self._rec("pe", lambda e: e.transpose(out, in_, ident), [in_, ident], [out])

    def act(self, out, in_, func, bias=None, scale=1.0, accum_out=None):
        reads = [in_]
        kw = {}
        if bias is not None:
            kw["bias"] = bias
            if not isinstance(bias, (int, float)):
                reads.append(bias)
        if not isinstance(scale, (int, float)):
            reads.append(scale)
        kw["scale"] = scale
        writes = [out]
        if accum_out is not None:
            kw["accum_out"] = accum_out
            writes.append(accum_out)
        return self._rec("act", lambda e: e.activation(out, in_, func, **kw), reads, writes)

    def tt(self, out, in0, in1, op, eng="dve"):
        return self._rec(eng, lambda e: e.tensor_tensor(out, in0, in1, op), [in0, in1], [out])

    def ts(self, out, in0, s1, s2, op0, op1=None, eng="dve", accum_out=None):
        reads = [in0]
        if not isinstance(s1, (int, float)):
            reads.append(s1)
        if s2 is not None and not isinstance(s2, (int, float)):
            reads.append(s2)
        writes = [out]
        kw = {}
        if accum_out is not None:
            kw["accum_out"] = accum_out
            writes.append(accum_out)
        if op1 is None:
            return self._rec(eng, lambda e: e.tensor_scalar(out, in0, s1, s2, op0, **kw), reads, writes)
        return self._rec(eng, lambda e: e.tensor_scalar(out, in0, s1, s2, op0, op1, **kw), reads, writes)

    def stt(self, out, in0, scalar, in1, op0, op1, eng="dve"):
        reads = [in0, in1]
        if not isinstance(scalar, (int, float)):
            reads.append(scalar)
        return self._rec(eng, lambda e: e.scalar_tensor_tensor(out, in0, scalar, in1, op0, op1), reads, [out])

    def copy(self, out, in_, eng="dve"):
        if eng == "act":
            return self._rec("act", lambda e: e.copy(out, in_), [in_], [out])
        return self._rec(eng, lambda e: e.tensor_copy(out, in_), [in_], [out])

    def recip(self, out, in_):
        return self._rec("dve", lambda e: e.reciprocal(out, in_), [in_], [out])

    def reduce(self, out, in_, op, axis=None, eng="dve"):
        ax = axis if axis is not None else AX.X
        return self._rec(eng, lambda e: e.tensor_reduce(out, in_, ax, op), [in_], [out])

    def memset(self, out, val, eng="dve"):
        return self._rec(eng, lambda e: e.memset(out, val), [], [out])

    def dma(self, out, in_, q="sp"):
        return self._rec(q, lambda e: e.dma_start(out=out, in_=in_), [in_], [out], is_dma=True)

    def emit(self):
        nc = self.nc
        self.barrier()
        from contextlib import ExitStack
        with ExitStack() as st:
            esem = {e: st.enter_context(nc.semaphore("c_" + e)) for e in self.ENGS}
            dsem = {}
            for q, n in self.NDMA.items():
                for i in range(n):
                    dsem[(q, i)] = st.enter_context(nc.semaphore("d_%s%d" % (q, i)))
            for e in self.ENGS:
                v = 0
                for op in self.eng_ops[e]:
                    if (not op.is_dma) and op.need_signal:
                        v += 1
                        op.sig_val = v
            prog = self

            pstate = {}

            def run(ename, eng):
                waited = {}
                for op in prog.eng_ops[ename]:
                    waits = []
                    for e2, k2 in op.deps_eng.items():
                        y = prog.eng_ops[e2][k2]
                        waits.append((("c", e2), esem[e2], y.sig_val))
                    for y in op.deps_dma:
                        waits.append((y.dma_sem, dsem[y.dma_sem], y.dma_val))
                    if op.is_dma and op.dma_prev is not None:
                        waits.append((op.dma_sem, dsem[op.dma_sem], op.dma_prev.dma_val))
                    for key, sem, val in waits:
                        if waited.get(key, 0) >= val:
                            continue
                        waited[key] = val
                        eng.wait_ge(sem, val)
                    if ename == "pe":
                        pass
                        pstate["mode"] = op.name
                    ins = op.fn(eng)
                    if op.is_dma:
                        ins.then_inc(dsem[op.dma_sem], 16)
                    elif op.need_signal:
                        ins.then_inc(esem[ename], 1)

            with nc.Block() as block:
                @block.tensor
                def _(eng):
                    run("pe", eng)

                @block.scalar
                def _(eng):
                    run("act", eng)

                @block.vector
                def _(eng):
                    run("dve", eng)

                @block.gpsimd
                def _(eng):
                    run("pool", eng)

                @block.sync
                def _(eng):
                    run("sp", eng)


T = 2304
NT = 18
KT = 8
EPS = 1e-6
CHUNKS = [(0, 256, 1), (256, 512, 0), (768, 512, 0), (1280, 512, 0), (1792, 512, 0)]


def _bc(ap, pos, n):
    shp = list(ap.shape)
    v = ap.unsqueeze(pos)
    shp.insert(pos, n)
    return v.to_broadcast(shp)


_DBG = {}


class _Rot:
    def __init__(self, items):
        self.items = list(items)
        self.i = 0

    def next(self):
        v = self.items[self.i % len(self.items)]
        self.i += 1
        return v


def build(n_layers=2, stop_at=None, dbg=False, skip=(), sub=None):
    from contextlib import ExitStack
    nc = bass.Bass("TRN2", target_bir_lowering=False)

    def din(name, shape, dt=F32):
        return nc.dram_tensor(name, shape, dt, kind="ExternalInput").ap()

    x_d = din("x", [2048, 1024])
    ctx_d = din("ctx", [256, 1024])
    vecs0_d = din("vecs0", [112, 128])
    vecs1_d = din("vecs1", [46, 128])
    wada_d = din("w_ada", [2, 1024, 6144])
    win_d = din("w_in", [2, 1024, 1824])
    wout_d = din("w_out", [2, 1024, 1024])
    wup_d = din("wup", [2, 2, 33, 128])
    wr_d = din("w_router", [1024, 16])
    brep_d = din("brep", [128, 288])
    need_moe = stop_at is None or stop_at[0] == "I" or stop_at[1] >= 1
    weg_d = weu_d = wed_d = None
    if need_moe:
        weg_d = din("w_exp_gate", [2, 16, 1024, 512])
        weu_d = din("w_exp_up", [2, 16, 1024, 512])
        wed_d = din("w_exp_down", [2, 16, 512, 1024])
    ident_d = din("ident", [128, 128])
    cs64_d = din("cs64", [128, 256], BF16)
    cn_d = din("cn", [2048, 2048], BF16)
    sn_d = din("sn", [2048, 2048], BF16)
    c256_d = din("c256", [256, 256], BF16)
    s256_d = din("s256", [256, 256], BF16)
    ropec_d = din("ropec", [128, T])
    ropes_d = din("ropes", [128, T])
    psw_d = din("psw", [128, 128])
    masks_d = din("masks", [128, 6, 128])
    bo64_d = din("bo64", [128, 128], BF16)
    esel_d = din("esel", [16, 16, 128], BF16)
    y_d = nc.dram_tensor("y", [2048, 1024], F32, kind="ExternalOutput").ap()
    hT_d = nc.dram_tensor("hT_scr", [128, 8, T], BF16).ap()
    dbg_d = None
    if dbg:
        dbg_d = nc.dram_tensor("dbg_x", [8, 128, 8, T], F32, kind="ExternalOutput").ap()

    st = ExitStack()
    with st:
        ARENA_BYTES = 210944
        arena_t = st.enter_context(nc.sbuf_tensor("arena", [128, ARENA_BYTES // 2], BF16))
        astate = {"off": 0, "peak": 0}

        def _release(m):
            astate["off"] = m

        def sb(name, shape, dt, stack=None):
            n = _dsz(dt)
            for d in shape[1:]:
                n *= int(d)
            n = (n + 63) // 64 * 64
            off = astate["off"]
            if stack is not None:
                stack.callback(_release, off)
            assert off + n <= ARENA_BYTES, ("arena overflow", name, off, n)
            astate["off"] = off + n
            astate["peak"] = max(astate["peak"], off + n)
            v = arena_t[:, off // 2:(off + n) // 2]
            if dt != BF16:
                v = v.bitcast(dt)
            tot = 1
            for d in shape[1:]:
                tot *= int(d)
            v = v[:, 0:tot]
            if len(shape) > 2:
                names = ["d%d" % i for i in range(len(shape) - 1)]
                v = v.rearrange("p (%s) -> p %s" % (" ".join(names), " ".join(names)),
                                **{nm: int(s) for nm, s in zip(names, shape[1:])})
            return v

        P = Prog(nc)
        ps = st.enter_context(nc.psum_tensor("ps", [128, 8, 512], F32))
        xT = sb("xT", [128, 8, T], F32)
        stage = sb("stage", [128, 4, 2048], F32)
        ident = sb("ident", [128, 128], F32)
        ones_bf = sb("ones_bf", [128, 128], BF16)
        bo64 = sb("bo64", [128, 128], BF16)
        V0T = sb("V0T", [128, 112], F32)
        V1T = sb("V1T", [128, 46], F32)
        cvec = sb("cvec", [128, 8, 2], F32)
        modT = sb("modT", [128, 2, 48, 2], F32)
        drv = sb("drv", [128, 2, 2, 8, 2], F32)

        P.dma(ident[:], ident_d)
        P.dma(bo64[:], bo64_d)
        P.memset(ones_bf[:], 1.0)
        P.dma(stage[0:112, 0, 0:128], vecs0_d)
        P.dma(stage[0:46, 1, 0:128], vecs1_d)
        P.transpose(ps[:, 0, 0:112], stage[0:112, 0, 0:128], ident[0:112, 0:112])
        P.transpose(ps[:, 1, 0:46], stage[0:46, 1, 0:128], ident[0:46, 0:46])
        P.copy(V0T[:], ps[:, 0, 0:112])
        P.copy(V1T[:], ps[:, 1, 0:46])
        for wh_ in range(2):
            P.act(cvec[:, :, wh_], V0T[:, 8 * wh_:8 * wh_ + 8], AF.Exp, scale=-1.0)
            P.ts(cvec[:, :, wh_], cvec[:, :, wh_], 1.0, None, ALU.add)
            P.recip(cvec[:, :, wh_], cvec[:, :, wh_])
            P.tt(cvec[:, :, wh_], V0T[:, 8 * wh_:8 * wh_ + 8], cvec[:, :, wh_], ALU.mult)

        s_ada = ExitStack()
        modrow = sb("modrow", [128, 6144], F32, s_ada)
        for l in range(n_layers):
            for s in range(12):
                slot = (s % 2) * 2
                for hlf in range(2):
                    P.dma(stage[:, slot + hlf, :].rearrange("p (k n) -> p k n", k=4),
                          wada_d[l, hlf * 512:(hlf + 1) * 512, s * 512:(s + 1) * 512].rearrange("(k p) n -> p k n", p=128))
                bA = s % 2
                for k in range(8):
                    wv = stage[:, slot + k // 4, :].rearrange("p (k n) -> p k n", k=4)
                    P.mm(ps[0:2, bA, 0:512], cvec[:, k, :], wv[:, k % 4, :], k == 0, k == 7)
                P.copy(modrow[0:2, s * 512:(s + 1) * 512], ps[0:2, bA, 0:512])
            for jg in range(48):
                P.transpose(ps[:, 2, 2 * jg:2 * jg + 2], modrow[0:2, jg * 128:(jg + 1) * 128], ident[0:2, 0:2])
            pv = ps[:, 2, 0:96].rearrange("p (a b) -> p a b", b=2)
            bias = V0T[:, 16 + 48 * l:16 + 48 * (l + 1)]
            P.tt(modT[:, l], pv, _bc(bias, 2, 2), ALU.add)
            for which in range(2):
                sc = modT[:, l, (1 + 3 * which) * 8:(2 + 3 * which) * 8, :]
                nw = V1T[:, (16 * which + 8 * l):(16 * which + 8 * l + 8)]
                P.ts(drv[:, l, which], sc, 1.0, 32.0, ALU.add, ALU.mult)
                P.tt(drv[:, l, which], drv[:, l, which], _bc(nw, 2, 2), ALU.mult)
        s_ada.close()
        P.barrier()

        def A_of(l, which, j, who):
            return drv[:, l, which, j, who:who + 1]

        def B_of(l, which, j, who):
            sec = 0 if which == 0 else 3
            return modT[:, l, sec * 8 + j, who:who + 1]

        def G_of(l, which, j, who):
            sec = 2 if which == 0 else 5
            return modT[:, l, sec * 8 + j, who:who + 1]

        with ExitStack() as s0:
            xin = sb("xin", [128, 2, 1024], F32, s0)
            ev = 0
            for t in range(NT):
                src = ctx_d[t * 128:(t + 1) * 128, :] if t < 2 else x_d[(t - 2) * 128:(t - 1) * 128, :]
                P.dma(xin[:, t % 2, :], src)
                for half in range(2):
                    b = (2 * t + half) % 4 + 3
                    for jj in range(4):
                        j = half * 4 + jj
                        P.transpose(ps[:, b, jj * 128:(jj + 1) * 128], xin[:, t % 2, j * 128:(j + 1) * 128], ident[:])
                    dst = xT[:, half * 4:half * 4 + 4, t * 128:(t + 1) * 128]
                    srcp = ps[:, b, :].rearrange("p (a b) -> p a b", b=128)
                    if ev % 2 == 0:
                        P.copy(dst, srcp)
                    else:
                        P.copy(dst, srcp, eng="act")
                    ev += 1
            P.barrier()

        def norm_chunk(l, which, c0, w, who, sq, xn, rs, bank):
            P.act(sq[:, :, :w], xT[:, :, c0:c0 + w], AF.Square)
            for j in range(8):
                P.mm(ps[:, bank, :w], ones_bf[:], sq[:, j, :w], j == 0, j == 7)
            P.act(rs[:, :w], ps[:, bank, :w], AF.Ln, bias=EPS * 1024.0, scale=1.0)
            P.act(rs[:, :w], rs[:, :w], AF.Exp, scale=-0.5)
            P.tt(xn[:, :, :w], xT[:, :, c0:c0 + w], _bc(rs[:, :w], 1, 8), ALU.mult)

        def load_w(dst_bf, src_rows_by_cols, ncols, slot_rot, eng="pool"):
            kt = dst_bf.shape[1]
            per = max(1, 2048 // ncols)
            k = 0
            while k < kt:
                n = min(per, kt - k)
                slot = slot_rot.next()
                sv = stage[:, slot, 0:n * ncols].rearrange("p (k n) -> p k n", n=ncols)
                P.dma(sv, src_rows_by_cols[k * 128:(k + n) * 128, :].rearrange("(k p) n -> p k n", p=128))
                P.copy(dst_bf[:, k:k + n, :], sv, eng=("act" if cast_rot.next() == 0 else "dve"))
                k += n

        srot = _Rot([0, 1, 2, 3])
        cast_rot = _Rot([0, 1])

        def out_proj(l, Wo, mix, nk, chunks, banks):
            for (c0, w, who) in chunks:
                for m in range(8):
                    b = banks.next()
                    for k in range(nk):
                        P.mm(ps[:, b, :w], Wo[:, k, m * 128:(m + 1) * 128], mix[:, k, c0:c0 + w], k == 0, k == nk - 1)
                    P.stt(xT[:, m, c0:c0 + w], ps[:, b, :w], G_of(l, 0, m, who), xT[:, m, c0:c0 + w], ALU.mult, ALU.add)

        def dump_dbg(idx):
            if dbg_d is not None:
                for j in range(8):
                    P.dma(dbg_d[idx, :, j, :], xT[:, j, :])

        def gla_phase(l, last, lchunks):
            with ExitStack() as s1:
                Wo = sb("g_Wo", [128, 2, 1024], BF16, s1)
                wup = sb("g_wup", [128, 2, 128], BF16, s1)
                msk = sb("g_msk", [128, 6, 128], F32, s1)
                qT = sb("g_qT", [128, T], BF16, s1)
                kT = sb("g_kT", [128, T], BF16, s1)
                ktok = sb("g_ktok", [128, NT, 128], BF16, s1)
                vtok = sb("g_vtok", [128, NT, 256], BF16, s1)
                sgT = sb("g_sg", [128, 2, T], BF16, s1)
                gd = sb("g_gd", [128, T], BF16, s1)
                oT = sb("g_oT", [128, 2, T], F32, s1)
                g8 = sb("g_g8", [128, 1], F32, s1)
                rb = _Rot([0, 1, 2, 3, 4, 5, 6, 7])
                P.dma(msk[:], masks_d)
                P.ts(g8[:], V1T[:, 40 + l:41 + l], 8.0, None, ALU.mult)
                load_w(Wo, wout_d[l][256:512, :], 1024, srot)
                sl = srot.next()
                P.dma(stage[0:33, sl, 0:256].rearrange("p (d c) -> p d c", d=2), wup_d[l].rearrange("d r c -> r d c"))
                P.copy(wup[0:33], stage[0:33, sl, 0:256].rearrange("p (d c) -> p d c", d=2))
                P.memset(gd[0:64], 1.0)
                if sub == "E1a":
                    return
                with ExitStack() as s2:
                    Wg = sb("g_W", [128, 8, 800], BF16, s2)
                    hcb = sb("g_hc", [128, 8, 512], BF16, s2)
                    sge = sb("g_sge", [128, 2, 512], F32, s2)
                    load_w(Wg[:, :, 0:512], win_d[l][:, 256:768], 512, srot)
                    load_w(Wg[:, :, 512:800], win_d[l][:, 768:1056], 288, srot)
                    if sub == "E1b":
                        return
                    for ci, (c0, w, who) in enumerate(CHUNKS):
                        P.dma(hcb[:, :, :w], hT_d[:, :, c0:c0 + w])
                        for (m0, msz, kind) in ((0, 128, "q"), (128, 128, "k"), (512, 128, "g0"), (640, 128, "g1"),
                                                (768, 32, "df")):
                            if _DBG.get("kinds") is not None and kind not in _DBG["kinds"]:
                                continue
                            b = rb.next()
                            for k in range(8):
                                P.mm(ps[0:msz, b, :w], Wg[:, k, m0:m0 + msz], hcb[:, k, :w], k == 0, k == 7)
                            if kind == "q":
                                P.ts(qT[:, c0:c0 + w], ps[:, b, :w], float(32.0 ** -0.5), None, ALU.mult)
                            elif kind == "k":
                                P.copy(kT[:, c0:c0 + w], ps[:, b, :w])
                            elif kind in ("g0", "g1"):
                                gi = 0 if kind == "g0" else 1
                                P.act(sgT[:, gi, c0:c0 + w], ps[:, b, :w], AF.Silu)
                            else:
                                P.copy(gd[0:32, c0:c0 + w], ps[0:32, b, :w])
                        if sub == "E1c" or _DBG.get("notm"):
                            continue
                        for tt in range(w // 128):
                            tg = c0 // 128 + tt
                            b = rb.next()
                            for k in range(8):
                                P.mm(ps[:, b, 0:384], hcb[:, k, tt * 128:(tt + 1) * 128], Wg[:, k, 128:512], k == 0, k == 7)
                            P.copy(ktok[:, tg, :], ps[:, b, 0:128])
                            P.copy(vtok[:, tg, :], ps[:, b, 128:384], eng="act")
                if sub == "E1":
                    return
                sflat = stage.rearrange("p a b -> p (a b)")
                sp_tok = sflat[:, 0:2304].rearrange("p (t c) -> p t c", c=128)
                sbf = sflat[:, 2304:8192].bitcast(BF16)
                q_t = sbf[:, 0:2304]
                k_t = sbf[:, 2304:4608]
                kk = sbf[:, 4608:6912].rearrange("p (t c) -> p t c", c=128)
                Sb = sbf[:, 6912:9216].rearrange("p (i c) -> p i c", c=64)
                with ExitStack() as s2:
                    te = sb("g_te", [128, 512], F32, s2)
                    ec = sb("g_ec", [128, 512], F32, s2)
                    en = sb("g_en", [128, 512], F32, s2)
                    esf = sb("g_esf", [128, 512], F32, s2)
                    dec = sb("g_dec", [128, 36], F32, s2)
                    Sall = sb("g_Sall", [128, 37, 64], F32, s2)
                    attb = sb("g_attb", [128, 2, 4, 128], BF16, s2)
                    groups = [(0, 4), (4, 4), (8, 4), (12, 4), (16, 2)]
                    for d in range(2):
                        for (t0, n) in groups:
                            b = rb.next()
                            for i in range(n):
                                t = t0 + i
                                P.mm(ps[:, b, i * 128:(i + 1) * 128], gd[0:33, t * 128:(t + 1) * 128], wup[0:33, d, :], True, True)
                            P.act(te[:, 0:n * 128], ps[:, b, 0:n * 128], AF.Exp, scale=-1.0)
                            P.act(sp_tok[:, t0:t0 + n, :], te[:, 0:n * 128].rearrange("p (t c) -> p t c", c=128), AF.Ln, bias=1.0)
                        for (t0, n) in groups:
                            bC = rb.next()
                            bS = rb.next()
                            for i in range(n):
                                t = t0 + i
                                P.mm(ps[:, bC, i * 128:(i + 1) * 128], sp_tok[:, t, :], msk[:, d, :], True, True)
                                P.mm(ps[:, bS, i * 128:(i + 1) * 128], msk[:, 2 + d, :], sp_tok[:, t, :], True, True)
                            nn = n * 128
                            cols = slice(t0 * 128, t0 * 128 + nn)
                            P.act(ec[:, 0:nn], ps[:, bC, 0:nn], AF.Exp)
                            P.act(en[:, 0:nn], ps[:, bC, 0:nn], AF.Exp, scale=-1.0)
                            P.act(esf[:, 0:nn], ps[:, bS, 0:nn], AF.Exp)
                            P.tt(q_t[:, cols], qT[:, cols], ec[:, 0:nn], ALU.mult)
                            P.tt(k_t[:, cols], kT[:, cols], en[:, 0:nn], ALU.mult, eng="pool")
                            P.tt(kk[:, t0:t0 + n, :], ktok[:, t0:t0 + n, :], esf[:, 0:nn].rearrange("p (t c) -> p t c", c=128), ALU.mult)
                            ecv = ec[:, 0:nn].rearrange("p (i c) -> p i c", c=64)
                            pick = 63 if d == 0 else 0
                            P.copy(dec[:, 2 * t0:2 * t0 + 2 * n], ecv[:, :, pick], eng="pool")
                        if sub == "E2":
                            continue
                        P.memset(Sall[:, 0, :], 0.0)
                        order = list(range(36)) if d == 0 else ([3, 2, 1, 0] + list(range(35, 3, -1)))
                        pos_of = {ci_: i_ for i_, ci_ in enumerate(order)}
                        for idx_, ci in enumerate(order):
                            t, c = ci // 2, ci % 2
                            r0 = 64 * c
                            b = rb.next()
                            for h in range(4):
                                o_ap = ps[32 * h:32 * h + 32, b, 0:64]
                                l_ap = kk[r0:r0 + 64, t, 32 * h:32 * h + 32]
                                r_ap = vtok[r0:r0 + 64, t, 64 * h:64 * h + 64]
                                P._rec("pe", (lambda e, o_ap=o_ap, l_ap=l_ap, r_ap=r_ap, r0=r0, h=h:
                                              e.matmul(o_ap, l_ap, r_ap, start=True, stop=True, tile_position=(r0, 32 * h))),
                                       [l_ap, r_ap], [o_ap])
                            P.stt(Sall[:, idx_ + 1, :], Sall[:, idx_, :], dec[:, ci:ci + 1], ps[:, b, 0:64], ALU.mult, ALU.add)
                        P.copy(Sb[:, :, :], Sall[:, 0:36, :], eng="act")
                        if sub == "E3":
                            continue
                        rbo = _Rot([4, 5, 6, 7])
                        for t in range(NT):
                            if last and t < 2:
                                continue
                            tc_ = slice(t * 128, (t + 1) * 128)
                            bA = 0
                            for h in range(4):
                                o_ap = ps[:, bA + h, 0:128]
                                l_ap = k_t[32 * h:32 * h + 32, tc_]
                                r_ap = q_t[32 * h:32 * h + 32, tc_]
                                P._rec("pe", (lambda e, o_ap=o_ap, l_ap=l_ap, r_ap=r_ap, h=h:
                                              e.matmul(o_ap, l_ap, r_ap, start=True, stop=True, tile_position=(32 * h, 0))),
                                       [l_ap, r_ap], [o_ap])
                            ab = attb[:, t % 2]
                            P.tt(ab, ps[:, bA:bA + 4, 0:128], _bc(msk[:, 4 + d, :], 1, 4), ALU.mult)
                            bO = rbo.next()
                            for h in range(4):
                                po = 64 * (h % 2)
                                cb = (h // 2) * 128
                                o_ap = ps[po:po + 64, bO, cb:cb + 128]
                                l_ap = vtok[:, t, 64 * h:64 * h + 64]
                                r_ap = ab[:, h, :]
                                P._rec("pe", (lambda e, o_ap=o_ap, l_ap=l_ap, r_ap=r_ap, po=po:
                                              e.matmul(o_ap, l_ap, r_ap, start=True, stop=False, tile_position=(0, po))),
                                       [l_ap, r_ap], [o_ap])
                                for c in range(2):
                                    ci = 2 * t + c
                                    o2 = ps[po:po + 64, bO, cb + 64 * c:cb + 64 * c + 64]
                                    l2 = Sb[32 * h:32 * h + 32, pos_of[ci], :]
                                    r2 = q_t[32 * h:32 * h + 32, t * 128 + 64 * c:t * 128 + 64 * c + 64]
                                    P._rec("pe", (lambda e, o2=o2, l2=l2, r2=r2, h=h, po=po, c=c:
                                                  e.matmul(o2, l2, r2, start=False, stop=(c == 1), tile_position=(32 * h, po))),
                                           [l2, r2], [o2])
                            pso = ps[:, bO, 0:256].rearrange("p (a c) -> p a c", c=128)
                            if d == 0:
                                P.copy(oT[:, :, tc_], pso, eng="act")
                            else:
                                P.tt(oT[:, :, tc_], oT[:, :, tc_], pso, ALU.add)
                    if sub in ("E2", "E3"):
                        return
                    sq = sb("g_sq", [128, 2, 512], BF16, s2)
                    rs = sb("g_rs", [128, 512], F32, s2)
                    tmp = sb("g_tmp", [128, 512], F32, s2)
                    for (c0, w, who) in lchunks:
                        P.act(sq[:, :, :w], oT[:, :, c0:c0 + w], AF.Square)
                        for m in range(2):
                            b = rb.next()
                            P.mm(ps[:, b, :w], bo64[:], sq[:, m, :w], True, True)
                            P.act(rs[:, :w], ps[:, b, :w], AF.Ln, bias=EPS * 64.0, scale=1.0)
                            P.act(rs[:, :w], rs[:, :w], AF.Exp, scale=-0.5)
                            P.stt(tmp[:, :w], oT[:, m, c0:c0 + w], g8[:, 0:1], rs[:, :w], ALU.mult, ALU.mult)
                            P.tt(sgT[:, m, c0:c0 + w], tmp[:, :w], sgT[:, m, c0:c0 + w], ALU.mult, eng="pool")
                out_proj(l, Wo, sgT, 2, lchunks, rb)
                P.barrier()

        def att_phase(l, last, lchunks):
            with ExitStack() as s1:
                Wo = sb("a_Wo", [128, 4, 1024], BF16, s1)
                qr = sb("a_qr", [128, 4, T], BF16, s1)
                kd = sb("a_kd", [128, 2, T], BF16, s1)
                vt = sb("a_vt", [128, NT, 128], BF16, s1)
                g8 = sb("a_g8", [128, 2], F32, s1)
                rb = _Rot([0, 1, 2, 3, 4, 5, 6, 7])
                P.ts(g8[:, 0:1], V1T[:, 42 + l:43 + l], 8.0, None, ALU.mult)
                P.ts(g8[:, 1:2], V1T[:, 44 + l:45 + l], 8.0, None, ALU.mult)
                with ExitStack() as s2:
                    Wq = sb("a_Wq", [128, 8, 512], BF16, s2)
                    Wk = sb("a_Wk", [128, 8, 256], BF16, s2)
                    Wv = sb("a_Wv", [128, 8, 128], BF16, s2)
                    psw = sb("a_psw", [128, 128], F32, s2)
                    sflat_a = stage.rearrange("p a b -> p (a b)")
                    rc = sflat_a[:, 0:T]
                    rsn = sflat_a[:, T:2 * T]
                    hcb = sb("a_hc", [128, 8, 512], BF16, s2)
                    qg2 = sb("a_qg", [128, 2, 512], F32, s2)
                    sq2 = sb("a_sq", [128, 2, 512], BF16, s2)
                    rs2 = sb("a_rsd", [128, 2, 512], F32, s2)
                    t12 = sb("a_t1", [128, 2, 512], F32, s2)
                    t22 = sb("a_t2", [128, 2, 512], F32, s2)
                    P.dma(psw[:], psw_d)
                    load_w(Wq, win_d[l][:, 1056:1568], 512, srot)
                    load_w(Wv, win_d[l][:, 1696:1824], 128, srot)
                    sl = srot.next()
                    sv = stage[:, sl, :].rearrange("p (k n) -> p k n", n=256)
                    for g in range(2):
                        for r in range(2):
                            P.dma(sv[:, :, (2 * g + r) * 64:(2 * g + r + 1) * 64],
                                  win_d[l][:, 1568 + 64 * g:1568 + 64 * g + 64].rearrange("(k p) n -> p k n", p=128))
                    P.copy(Wk[:], sv, eng="act")
                    P.dma(rc, ropec_d)
                    P.dma(rsn, ropes_d)
                    tcnt = 0
                    for ci, (c0, w, who) in enumerate(CHUNKS):
                        P.dma(hcb[:, :, :w], hT_d[:, :, c0:c0 + w])
                        for i in range(6):
                            qg = qg2[:, tcnt % 2]
                            sq = sq2[:, tcnt % 2]
                            rs = rs2[:, tcnt % 2]
                            t1 = t12[:, tcnt % 2]
                            t2 = t22[:, tcnt % 2]
                            tcnt += 1
                            isq = i < 4
                            b0 = rb.next()
                            for k in range(8):
                                wsl = Wq[:, k, i * 128:(i + 1) * 128] if isq else Wk[:, k, (i - 4) * 128:(i - 3) * 128]
                                P.mm(ps[:, b0, :w], wsl, hcb[:, k, :w], k == 0, k == 7)
                            gcol = g8[:, 0:1] if isq else g8[:, 1:2]
                            P.act(qg[:, :w], ps[:, b0, :w], AF.Identity, scale=gcol)
                            P.act(sq[:, :w], ps[:, b0, :w], AF.Square)
                            b1 = rb.next()
                            P.mm(ps[:, b1, :w], bo64[:], sq[:, :w], True, True)
                            P.act(rs[:, :w], ps[:, b1, :w], AF.Ln, bias=EPS * 64.0, scale=1.0)
                            P.act(rs[:, :w], rs[:, :w], AF.Exp, scale=-0.5)
                            b2 = rb.next()
                            P.mm(ps[:, b2, :w], psw[:], qg[:, :w], True, True)
                            P.tt(t1[:, :w], qg[:, :w], rc[:, c0:c0 + w], ALU.mult, eng="pool")
                            P.tt(t2[:, :w], ps[:, b2, :w], rsn[:, c0:c0 + w], ALU.mult)
                            P.tt(t1[:, :w], t1[:, :w], t2[:, :w], ALU.add, eng="pool")
                            dst = qr[:, i, c0:c0 + w] if isq else kd[:, i - 4, c0:c0 + w]
                            P.tt(dst, t1[:, :w], rs[:, :w], ALU.mult)
                        for tt in range(w // 128):
                            tg = c0 // 128 + tt
                            b = rb.next()
                            for k in range(8):
                                P.mm(ps[:, b, 0:128], hcb[:, k, tt * 128:(tt + 1) * 128], Wv[:, k, :], k == 0, k == 7)
                            P.copy(vt[:, tg, :], ps[:, b, 0:128], eng="act")
                load_w(Wo, wout_d[l][512:1024, :], 1024, srot)
                with ExitStack() as s2:
                    amix = sb("a_mix", [128, 4, T], BF16, s2)
                    PT = sb("a_PT", [128, 4, 512], BF16, s2)
                    rd = sb("a_rd", [128, 2, 512], F32, s2)
                    rS = _Rot([0, 1, 2, 3])
                    rO = _Rot([(4, 5), (6, 7)])
                    rP = _Rot([0, 1, 2, 3])
                    for (c0, w, who) in lchunks:
                        kts = list(range(2)) if who == 1 else list(range(NT))
                        for h in range(8):
                            g = h // 4
                            qt = h // 2
                            po = 64 * (h % 2)
                            bO, bD = rO.next()
                            sbank = {}

                            def score(kt_):
                                bS_ = rS.next()
                                sbank[kt_] = bS_
                                P.mm(ps[:, bS_, :w], kd[po:po + 64, g, kt_ * 128:(kt_ + 1) * 128], qr[po:po + 64, qt, c0:c0 + w], True, True)

                            for kt_ in kts[:3]:
                                score(kt_)
                            for idx, kt in enumerate(kts):
                                bS = sbank[kt]
                                pt = PT[:, rP.next(), :w]
                                P.act(pt, ps[:, bS, :w], AF.Exp, scale=0.125)
                                if idx + 3 < len(kts):
                                    score(kts[idx + 3])
                                P.mm(ps[po:po + 64, bO, :w], vt[:, kt, g * 64:(g + 1) * 64], pt, idx == 0, idx == len(kts) - 1)
                                P.mm(ps[po:po + 64, bD, :w], ones_bf[:, 0:64], pt, idx == 0, idx == len(kts) - 1)
                            P.recip(rd[po:po + 64, h % 2, :w], ps[po:po + 64, bD, :w])
                            P.tt(amix[po:po + 64, qt, c0:c0 + w], ps[po:po + 64, bO, :w], rd[po:po + 64, h % 2, :w], ALU.mult)
                    out_proj(l, Wo, amix, 4, lchunks, rb)
                P.barrier()

        def moe_phase(l, last, lchunks):
            with ExitStack() as s1:
                h2T = sb("m_h2T", [128, 8, T], BF16, s1)
                GT = sb("m_GT", [128, T], BF16, s1)
                esel = sb("m_esel", [128, 16, 128], BF16, s1)
                P.dma(esel[0:16], esel_d)
                tiles = list(range(2, NT)) if last else list(range(NT))
                with ExitStack() as s2:
                    sq = sb("m_sq", [128, 8, 512], BF16, s2)
                    xn = sb("m_xn", [128, 8, 512], F32, s2)
                    rs = sb("m_rs", [128, 512], F32, s2)
                    wr = sb("m_wr", [128, 8, 16], F32, s2)
                    brep = sb("m_brep", [128, 288], F32, s2)
                    s_tok = sb("m_s", [128, NT, 16], F32, s2)
                    sel2 = sb("m_sel2", [128, 72, 8], F32, s2)
                    p1 = sb("m_p1", [128, 72, 4], F32, s2)
                    p2 = sb("m_p2", [128, 72, 2], F32, s2)
                    gs = sb("m_gs", [128, 72], F32, s2)
                    gs2 = sb("m_gs2", [128, 72], F32, s2)
                    gmax = sb("m_gmax", [128, NT], F32, s2)
                    oh = sb("m_oh", [128, 72], F32, s2)
                    cnt = sb("m_cnt", [128, 72, 4], F32, s2)
                    c2 = sb("m_c2", [128, 72, 4], F32, s2)
                    wsum = sb("m_wsum", [128, NT], F32, s2)
                    gate = sb("m_gate", [128, NT, 16], F32, s2)
                    P.dma(wr[:], wr_d.rearrange("(k p) e -> p k e", p=128))
                    P.dma(brep[:], brep_d)
                    P.memset(s_tok[:], 0.0)
                    for ci, (c0, w, who) in enumerate(lchunks):
                        norm_chunk(l, 1, c0, w, who, sq, xn, rs, ci % 2)
                        for j in range(8):
                            if j % 2 == 0:
                                P.ts(xn[:, j, :w], xn[:, j, :w], A_of(l, 1, j, who), B_of(l, 1, j, who), ALU.mult, ALU.add)
                            else:
                                P.act(xn[:, j, :w], xn[:, j, :w], AF.Identity, bias=B_of(l, 1, j, who), scale=A_of(l, 1, j, who))
                        P.copy(h2T[:, :, c0:c0 + w], xn[:, :, :w], eng="act")
                        b = 2 + ci % 2
                        for tt in range(w // 128):
                            tg = c0 // 128 + tt
                            for k in range(8):
                                P.mm(ps[:, b, tt * 16:(tt + 1) * 16], xn[:, k, tt * 128:(tt + 1) * 128], wr[:, k, :], k == 0, k == 7)
                        nt_ = w // 128
                        tg0 = c0 // 128
                        P.act(s_tok[:, tg0:tg0 + nt_, :], ps[:, b, 0:nt_ * 16].rearrange("p (t e) -> p t e", e=16), AF.Exp, scale=-1.0)
                        P.ts(s_tok[:, tg0:tg0 + nt_, :], s_tok[:, tg0:tg0 + nt_, :], 1.0, None, ALU.add)
                        P.recip(s_tok[:, tg0:tg0 + nt_, :], s_tok[:, tg0:tg0 + nt_, :])
                    sv = s_tok.rearrange("p t (g e) -> p (t g) e", e=4)
                    bv = brep.rearrange("p (a e) -> p a e", e=4)
                    P.tt(sel2[:, :, 0:4], sv, bv, ALU.add)
                    P.tt(sel2[:, :, 4:8], sv, bv, ALU.add)
                    P.tt(p1[:], sel2[:, :, 0:4], sel2[:, :, 1:5], ALU.add)
                    P.tt(p2[:], sel2[:, :, 0:2], sel2[:, :, 2:4], ALU.add)
                    P.reduce(gs[:], p1[:], ALU.max)
                    P.reduce(gs2[:], p2[:], ALU.max)
                    P.tt(gs[:], gs[:], gs2[:], ALU.max)
                    P.reduce(gmax[:], gs.rearrange("p (t g) -> p t g", g=4), ALU.max)
                    P.tt(oh.rearrange("p (t g) -> p t g", g=4), gs.rearrange("p (t g) -> p t g", g=4), _bc(gmax[:], 2, 4), ALU.is_equal)
                    P.tt(cnt[:], sel2[:, :, 1:5], sel2[:, :, 0:4], ALU.is_gt)
                    P.tt(c2[:], sel2[:, :, 2:6], sel2[:, :, 0:4], ALU.is_gt)
                    P.tt(cnt[:], cnt[:], c2[:], ALU.add)
                    P.tt(c2[:], sel2[:, :, 3:7], sel2[:, :, 0:4], ALU.is_gt)
                    P.tt(cnt[:], cnt[:], c2[:], ALU.add)
                    P.ts(cnt[:], cnt[:], 1.0, None, ALU.is_le)
                    P.tt(cnt[:], cnt[:], _bc(oh[:], 2, 4), ALU.mult)
                    gv = gate.rearrange("p t (g e) -> p (t g) e", e=4)
                    P.tt(gv, sv, cnt[:], ALU.mult)
                    P.reduce(wsum[:], gate[:], ALU.add)
                    P.ts(wsum[:], wsum[:], 1e-30, None, ALU.max)
                    P.recip(wsum[:], wsum[:])
                    P.tt(gate[:], gate[:], _bc(wsum[:], 2, 16), ALU.mult)
                    for t in tiles:
                        b = 4 + (t // 4) % 2
                        P.transpose(ps[0:16, b, (t % 4) * 128:(t % 4 + 1) * 128], gate[:, t, :], ident[:])
                        P.copy(GT[0:16, t * 128:(t + 1) * 128], ps[0:16, b, (t % 4) * 128:(t % 4 + 1) * 128])
                with ExitStack() as s2:
                    wbuf = sb("m_wbuf", [128, 3, 3, 2048], BF16, s2)
                    sg = sb("m_sg", [128, 2, 512], F32, s2)
                    t1 = sb("m_t1", [128, 2, 512], F32, s2)
                    abuf = sb("m_a", [128, 2, 2, 512], BF16, s2)
                    rG = _Rot([0, 1])
                    rU = _Rot([2, 3])
                    rD = _Rot([5, 6, 7])
                    it = 0
                    pending = None

                    def emit_down(pd):
                        Wd_p, ab_p, c0p, wp, whop = pd
                        for m in range(8):
                            bD = rD.next()
                            for ft in range(2):
                                P.mm(ps[:, bD, :wp], Wd_p[:, ft, m * 128:(m + 1) * 128], ab_p[:, ft, :wp], ft == 0, ft == 1)
                            P.stt(xT[:, m, c0p:c0p + wp], ps[:, bD, :wp], G_of(l, 1, m, whop), xT[:, m, c0p:c0p + wp], ALU.mult, ALU.add)

                    for e in range(16):
                        for fh in range(2):
                            u = e * 2 + fh
                            ws = u % 3
                            Wg_ = wbuf[:, ws, 0].rearrange("p (k f) -> p k f", f=256)
                            Wu_ = wbuf[:, ws, 1].rearrange("p (k f) -> p k f", f=256)
                            Wd_ = wbuf[:, ws, 2].rearrange("p (k d) -> p k d", d=1024)
                            for mi, (dst, srcw) in enumerate(((Wg_, weg_d[l, e][:, fh * 256:(fh + 1) * 256]),
                                                              (Wu_, weu_d[l, e][:, fh * 256:(fh + 1) * 256]))):
                                sl = srot.next()
                                sv_ = stage[:, sl, :].rearrange("p (k f) -> p k f", f=256)
                                P.dma(sv_, srcw.rearrange("(k p) f -> p k f", p=128))
                                P.copy(dst, sv_, eng="act")
                            sl = srot.next()
                            sv_ = stage[:, sl, :].rearrange("p (k d) -> p k d", d=1024)
                            P.dma(sv_, wed_d[l, e][fh * 256:(fh + 1) * 256, :].rearrange("(k p) d -> p k d", p=128))
                            P.copy(Wd_, sv_, eng="act")
                            for (c0, w, who) in lchunks:
                                ab = abuf[:, it % 2]
                                it += 1
                                P.mm(ps[:, 4, :w], esel[0:16, e, :], GT[0:16, c0:c0 + w], True, True)
                                for ft in range(2):
                                    bG = rG.next()
                                    bU = rU.next()
                                    for k in range(8):
                                        P.mm(ps[:, bG, :w], Wg_[:, k, ft * 128:(ft + 1) * 128], h2T[:, k, c0:c0 + w], k == 0, k == 7)
                                    for k in range(8):
                                        P.mm(ps[:, bU, :w], Wu_[:, k, ft * 128:(ft + 1) * 128], h2T[:, k, c0:c0 + w], k == 0, k == 7)
                                    P.act(sg[:, ft, :w], ps[:, bG, :w], AF.Silu)
                                    P.tt(t1[:, ft, :w], sg[:, ft, :w], ps[:, bU, :w], ALU.mult)
                                    P.tt(ab[:, ft, :w], t1[:, ft, :w], ps[:, 4, :w], ALU.mult)
                                if pending is not None:
                                    emit_down(pending)
                                pending = (Wd_, ab, c0, w, who)
                    emit_down(pending)
                P.barrier()

        for l in range(n_layers):
            last = (l == n_layers - 1) and (n_layers == 2)
            lchunks = CHUNKS[1:] if last else CHUNKS

            with ExitStack() as s1:
                sq = sb("b_sq", [128, 8, 512], BF16, s1)
                xn = sb("b_xn", [128, 8, 512], F32, s1)
                rs = sb("b_rs", [128, 512], F32, s1)
                hc = sb("b_hc", [128, 2, 8, 512], BF16, s1)
                for ci, (c0, w, who) in enumerate(CHUNKS):
                    norm_chunk(l, 0, c0, w, who, sq, xn, rs, ci % 2)
                    for j in range(8):
                        if j % 2 == 0:
                            P.ts(hc[:, ci % 2, j, :w], xn[:, j, :w], A_of(l, 0, j, who), B_of(l, 0, j, who), ALU.mult, ALU.add)
                        else:
                            P.act(hc[:, ci % 2, j, :w], xn[:, j, :w], AF.Identity, bias=B_of(l, 0, j, who), scale=A_of(l, 0, j, who))
                    P.dma(hT_d[:, :, c0:c0 + w], hc[:, ci % 2, :, :w])
                P.barrier()
            if stop_at == ("B", l):
                break

            with ExitStack() as s1:
                Wu = sb("f_Wu", [128, 8, 256], BF16, s1)
                Wo = sb("f_Wo", [128, 2, 1024], BF16, s1)
                cs64 = sb("f_cs64", [128, 256], BF16, s1)
                uT = sb("f_uT", [128, 2, T], BF16, s1)
                ucs = sb("f_ucs", [128, NT, 512], BF16, s1)
                fmix = sb("f_mix", [128, 2, T], BF16, s1)
                hcb = sb("f_hc", [128, 2, 8, 512], BF16, s1)
                tab = sb("f_tab", [128, 1, 2, 16, 512], BF16, s1)
                P.dma(cs64[:], cs64_d)
                load_w(Wu, win_d[l][:, 0:256], 256, srot)
                load_w(Wo, wout_d[l][0:256, :], 1024, srot)
                rb = _Rot([0, 1, 2, 3])
                for ci, (c0, w, who) in enumerate(CHUNKS):
                    P.dma(hcb[:, ci % 2, :, :w], hT_d[:, :, c0:c0 + w])
                    for m in range(2):
                        b = rb.next()
                        for k in range(8):
                            P.mm(ps[:, b, :w], Wu[:, k, m * 128:(m + 1) * 128], hcb[:, ci % 2, k, :w], k == 0, k == 7)
                        P.copy(uT[:, m, c0:c0 + w], ps[:, b, :w], eng=("act" if m == 0 else "dve"))
                t_lo = 2 if last else 0
                for t in range(t_lo, NT):
                    b = rb.next()
                    for j in range(2):
                        P.mm(ps[:, b, j * 256:(j + 1) * 256], uT[:, j, t * 128:(t + 1) * 128], cs64[:], True, True)
                    P.copy(ucs[:, t, :], ps[:, b, :], eng=("act" if t % 2 == 0 else "dve"))
                tab2 = tab.rearrange("p a c t n -> p (a c t n)").rearrange("p (b c t n) -> p b c t n", b=2, c=2, t=16)
                for pc in range(8):
                    bufi = pc % 2
                    P.dma(tab2[:, bufi, 0], cn_d[:, pc * 256:(pc + 1) * 256].rearrange("(t p) c -> p t c", p=128))
                    P.dma(tab2[:, bufi, 1], sn_d[:, pc * 256:(pc + 1) * 256].rearrange("(t p) c -> p t c", p=128))
                    for j in range(2):
                        b = rb.next()
                        for t in range(16):
                            P.mm(ps[:, b, 0:256], ucs[:, 2 + t, j * 256:j * 256 + 128], tab2[:, bufi, 0, t, :], t == 0, False)
                            P.mm(ps[:, b, 0:256], ucs[:, 2 + t, j * 256 + 128:j * 256 + 256], tab2[:, bufi, 1, t, :], False, t == 15)
                        P.copy(fmix[:, j, 256 + pc * 256:256 + (pc + 1) * 256], ps[:, b, 0:256], eng=("act" if j == 0 else "dve"))
                if not last:
                    c2 = sb("f_c2", [128, 2, 2, 256], BF16, s1)
                    P.dma(c2[:, 0], c256_d.rearrange("(t p) c -> p t c", p=128))
                    P.dma(c2[:, 1], s256_d.rearrange("(t p) c -> p t c", p=128))
                    for j in range(2):
                        b = rb.next()
                        for t in range(2):
                            P.mm(ps[:, b, 0:256], ucs[:, t, j * 256:j * 256 + 128], c2[:, 0, t, :], t == 0, False)
                            P.mm(ps[:, b, 0:256], ucs[:, t, j * 256 + 128:j * 256 + 256], c2[:, 1, t, :], False, t == 1)
                        P.copy(fmix[:, j, 0:256], ps[:, b, 0:256])
                out_proj(l, Wo, fmix, 2, lchunks, rb)
                P.barrier()
            dump_dbg(4 * l + 0)
            if stop_at == ("D", l):
                break

            if 'E' not in skip:
                gla_phase(l, last, lchunks)
            dump_dbg(4 * l + 1)
            if stop_at == ("E", l):
                break

            if 'F' not in skip:
                att_phase(l, last, lchunks)
            dump_dbg(4 * l + 2)
            if stop_at == ("F", l):
                break

            moe_phase(l, last, lchunks)
            dump_dbg(4 * l + 3)
            if stop_at == ("I", l):
                break

        if stop_at is None:
            with ExitStack() as s1:
                sq = sb("o_sq", [128, 8, 512], BF16, s1)
                xn = sb("o_xn", [128, 8, 512], F32, s1)
                rs = sb("o_rs", [128, 512], F32, s1)
                ot = sb("o_ot", [128, 2, 1024], F32, s1)
                fn32 = sb("o_fn", [128, 8], F32, s1)
                P.ts(fn32[:], V1T[:, 32:40], 32.0, None, ALU.mult)
                ev = 0
                for ci, (c0, w, who) in enumerate(CHUNKS[1:]):
                    norm_chunk(0, 0, c0, w, who, sq, xn, rs, 0)
                    for j in range(8):
                        if j % 2 == 0:
                            P.ts(xn[:, j, :w], xn[:, j, :w], fn32[:, j:j + 1], None, ALU.mult)
                        else:
                            P.act(xn[:, j, :w], xn[:, j, :w], AF.Identity, scale=fn32[:, j:j + 1])
                    for tt in range(4):
                        tg = ci * 4 + tt
                        for half in range(2):
                            b = 1 + (2 * tg + half) % 4
                            for jj in range(4):
                                j = half * 4 + jj
                                P.transpose(ps[:, b, jj * 128:(jj + 1) * 128], xn[:, j, tt * 128:(tt + 1) * 128], ident[:])
                            dst = ot[:, tg % 2, half * 512:(half + 1) * 512]
                            if ev % 2 == 0:
                                P.copy(dst, ps[:, b, :])
                            else:
                                P.copy(dst, ps[:, b, :], eng="act")
                            ev += 1
                        P.dma(y_d[tg * 128:(tg + 1) * 128, :], ot[:, tg % 2, :])
        P.emit()
        print("arena peak bytes", astate["peak"], "ops", P.nops)
    return nc


_CONST_CACHE = {}


def _consts():
    if _CONST_CACHE:
        return _CONST_CACHE
    bf = ml_dtypes.bfloat16
    c = {}
    c["ident"] = np.eye(128, dtype=np.float32)
    k = np.arange(64)
    ang = 2 * np.pi * np.outer(k, k) / 64.0
    C64 = np.cos(ang) / 8.0
    S64 = np.sin(ang) / 8.0
    cs = np.zeros((128, 256), np.float64)
    for g in range(2):
        cs[g * 64:(g + 1) * 64, g * 64:(g + 1) * 64] = C64
        cs[g * 64:(g + 1) * 64, 128 + g * 64:128 + (g + 1) * 64] = S64
    c["cs64"] = cs.astype(bf)
    for n, cn, sn in ((2048, "cn", "sn"), (256, "c256", "s256")):
        i = np.arange(n)
        a = 2 * np.pi * (np.outer(i, i) % n) / float(n)
        c[cn] = (np.cos(a) / np.sqrt(n)).astype(bf)
        c[sn] = (-np.sin(a) / np.sqrt(n)).astype(bf)
    inv = 10000.0 ** (-np.arange(16, dtype=np.float64) * 2.0 / 32.0)
    tok = np.arange(2048)
    row = tok // 64
    col = tok % 64
    rc = np.ones((128, T), np.float64)
    rs = np.zeros((128, T), np.float64)
    for hd in range(128):
        a = (hd % 64) // 32
        f = hd % 16
        pos = row if a == 0 else col
        rc[hd, 256:] = np.cos(pos * inv[f])
        rs[hd, 256:] = np.sin(pos * inv[f])
    c["ropec"] = rc.astype(np.float32)
    c["ropes"] = rs.astype(np.float32)
    psw = np.zeros((128, 128), np.float32)
    for hdp in range(128):
        half = (hdp % 32) // 16
        if half == 0:
            psw[hdp + 16, hdp] = -1.0
        else:
            psw[hdp - 16, hdp] = 1.0
    c["psw"] = psw
    j = np.arange(128)[:, None]
    i = np.arange(128)[None, :]
    same = (j // 64) == (i // 64)
    m = np.zeros((128, 6, 128), np.float32)
    m[:, 0, :] = np.where(same & (j <= i), -1.0 / 16.0, 0.0)
    m[:, 1, :] = np.where(same & (j >= i), -1.0 / 16.0, 0.0)
    m[:, 2, :] = np.where(same & (j > i), -1.0 / 16.0, 0.0)
    m[:, 3, :] = np.where(same & (j < i), -1.0 / 16.0, 0.0)
    m[:, 4, :] = np.where(same & (j <= i), 1.0, 0.0)
    m[:, 5, :] = np.where(same & (j >= i), 1.0, 0.0)
    c["masks"] = m
    c["bo64"] = same.astype(np.float32).astype(bf)
    es = np.zeros((16, 16, 128), np.float32)
    for e in range(16):
        es[e, e, :] = 1.0
    c["esel"] = es.astype(bf)
    _CONST_CACHE.update(c)
    return _CONST_CACHE


_NC_CACHE = {}


def _prep_inputs(inputs):
    f = lambda a: np.ascontiguousarray(np.asarray(a, dtype=np.float32))
    x = f(inputs["x"])
    c = f(inputs["c"])
    ctx = f(inputs["ctx"])
    c_ctx = f(inputs["c_ctx"])
    b_ada = f(inputs["b_ada"])
    cst = _consts()
    vecs1 = np.concatenate([
        f(inputs["norm_mix"]).reshape(16, 128),
        f(inputs["norm_ffn"]).reshape(16, 128),
        f(inputs["final_norm"]).reshape(8, 128),
        np.tile(f(inputs["gla_norm"]), (1, 2)),
        np.tile(f(inputs["q_norm"]), (1, 2)),
        np.tile(f(inputs["k_norm"]), (1, 2)),
    ], axis=0)
    wg_ = f(inputs["w_gla_gate_up"])
    bg_ = f(inputs["b_gla_gate"])
    wup = np.zeros((2, 2, 33, 128), np.float32)
    for l_ in range(2):
        for d_ in range(2):
            wup[l_, d_, 16 * d_:16 * d_ + 16, :] = wg_[l_, d_]
            wup[l_, d_, 32, :] = bg_[l_, d_]
    brep = np.ascontiguousarray(np.broadcast_to(np.tile(f(inputs["b_router"]), 18)[None, :], (128, 288)))
    shared = {
        "vecs1": np.ascontiguousarray(vecs1),
        "w_ada": f(inputs["w_ada"]), "w_in": f(inputs["w_in"]), "w_out": f(inputs["w_out"]),
        "wup": np.ascontiguousarray(wup), "w_router": f(inputs["w_router"]), "brep": brep,
        "w_exp_gate": f(inputs["w_exp_gate"]), "w_exp_up": f(inputs["w_exp_up"]), "w_exp_down": f(inputs["w_exp_down"]),
    }
    for k_ in ("ident", "cs64", "cn", "sn", "c256", "s256", "ropec", "ropes", "psw", "masks", "bo64", "esel"):
        shared[k_] = cst[k_]
    in_maps = []
    for b in range(8):
        vecs0 = np.concatenate([c[b].reshape(8, 128), c_ctx.reshape(8, 128),
                                b_ada[0].reshape(48, 128), b_ada[1].reshape(48, 128)], axis=0)
        m = dict(shared)
        m["x"] = x[b]
        m["ctx"] = ctx[b]
        m["vecs0"] = np.ascontiguousarray(vecs0)
        in_maps.append(m)
    return in_maps


def kernel(**inputs):
    in_maps = _prep_inputs(inputs)
    if "nc" not in _NC_CACHE:
        _NC_CACHE["nc"] = build()
    nc = _NC_CACHE["nc"]
    res = run_bass_kernel_spmd(nc, in_maps, core_ids=list(range(8)))
    out = np.stack([np.asarray(r["y"], dtype=np.float32) for r in res.results], axis=0)
    return out
```

```python
import numpy as np
import ml_dtypes
import concourse.bass as bass
import concourse.mybir as mybir
from concourse.bass_utils import run_bass_kernel_spmd

F32 = mybir.dt.float32
BF16 = mybir.dt.bfloat16
AF = mybir.ActivationFunctionType
ALU = mybir.AluOpType
AX = mybir.AxisListType

_DSZ = {F32: 4, BF16: 2}


def _dsz(dt):
    if dt in _DSZ:
        return _DSZ[dt]
    s = str(dt)
    if "32" in s:
        return 4
    if "16" in s:
        return 2
    if "64" in s:
        return 8
    return 1


def _region(ap):
    t = ap.tensor
    name = t.name
    dsz = _dsz(ap.dtype)
    space = str(ap.space)
    off = int(ap.offset)
    if "DRAM" in space.upper() or "HBM" in space.upper():
        ext = 0
        for (s, c) in ap.ap:
            ext += abs(int(s)) * (int(c) - 1)
        return (name, 0, 1, off * dsz, (off + ext + 1) * dsz)
    shape = list(t.shape)
    fsz = 1
    for d in shape[1:]:
        fsz *= int(d)
    p0 = off // fsz
    f0 = off % fsz
    pext = 0
    fext = 0
    for (s, c) in ap.ap:
        s = int(s)
        c = int(c)
        if c <= 1 or s == 0:
            continue
        if s % fsz == 0:
            pext += (s // fsz) * (c - 1)
        else:
            fext += abs(s) * (c - 1)
    b0 = f0 * dsz
    b1 = (f0 + fext + 1) * dsz
    if "PSUM" in space.upper():
        return (name, 0, 128, (b0 // 2048) * 2048, ((b1 + 2047) // 2048) * 2048)
    return (name, p0, p0 + pext + 1, b0, b1)


class _Op:
    __slots__ = ("eng", "fn", "k", "is_dma", "deps_eng", "deps_dma", "need_signal", "sig_val",
                 "dma_sem", "dma_val", "dma_prev", "name")

    def __init__(self, eng, fn, is_dma, name=""):
        self.eng = eng
        self.fn = fn
        self.is_dma = is_dma
        self.k = -1
        self.deps_eng = {}
        self.deps_dma = []
        self.need_signal = False
        self.sig_val = 0
        self.dma_sem = None
        self.dma_val = 0
        self.dma_prev = None
        self.name = name


class Prog:
    ENGS = ("pe", "act", "dve", "pool", "sp")
    NDMA = {"sp": 40, "pool": 8, "act": 8}

    def __init__(self, nc):
        self.nc = nc
        self.eng_ops = {e: [] for e in self.ENGS}
        self.recs = {}
        self.dma_count = {q: 0 for q in self.NDMA}
        self.dma_last = {q: [None] * n for q, n in self.NDMA.items()}
        self.nops = 0

    def _dep(self, x, y):
        if y is x:
            return
        if y.is_dma:
            if y not in x.deps_dma:
                x.deps_dma.append(y)
            return
        if (not x.is_dma) and y.eng == x.eng:
            if x.eng == "pe":
                return
        cur = x.deps_eng.get(y.eng, -1)
        if y.k > cur:
            x.deps_eng[y.eng] = y.k
        y.need_signal = True

    def add(self, eng, fn, reads, writes, is_dma=False, name=""):
        op = _Op(eng, fn, is_dma, name)
        rr = [_region(a) for a in reads if a is not None]
        ww = [_region(a) for a in writes if a is not None]
        for (nm, p0, p1, b0, b1) in rr:
            is_ps = (nm == "ps")
            for rec in self.recs.get(nm, ()):
                if rec[0] < p1 and p0 < rec[1] and rec[2] < b1 and b0 < rec[3]:
                    if rec[4] or (is_ps and rec[5].eng != eng):
                        self._dep(op, rec[5])
        for (nm, p0, p1, b0, b1) in ww:
            for rec in self.recs.get(nm, ()):
                if rec[0] < p1 and p0 < rec[1] and rec[2] < b1 and b0 < rec[3]:
                    self._dep(op, rec[5])
        for (nm, p0, p1, b0, b1) in ww:
            lst = self.recs.setdefault(nm, [])
            lst[:] = [r for r in lst if not (p0 <= r[0] and r[1] <= p1 and b0 <= r[2] and r[3] <= b1)]
            lst.append((p0, p1, b0, b1, True, op))
        for (nm, p0, p1, b0, b1) in rr:
            lst = self.recs.setdefault(nm, [])
            if not is_dma:
                lst[:] = [r for r in lst if not ((not r[4]) and (not r[5].is_dma) and r[5].eng == eng
                                                 and p0 <= r[0] and r[1] <= p1 and b0 <= r[2] and r[3] <= b1)]
            lst.append((p0, p1, b0, b1, False, op))
        op.k = len(self.eng_ops[eng])
        self.eng_ops[eng].append(op)
        if is_dma:
            n = self.NDMA[eng]
            slot = self.dma_count[eng] % n
            self.dma_count[eng] += 1
            prev = self.dma_last[eng][slot]
            op.dma_sem = (eng, slot)
            op.dma_prev = prev
            op.dma_val = (prev.dma_val if prev is not None else 0) + 16
            self.dma_last[eng][slot] = op
        self.nops += 1
        return op

    def barrier(self):
        bop = _Op("sp", lambda e: e.nop(), False, "barrier")
        for e in self.ENGS:
            lst = [o for o in self.eng_ops[e] if not o.is_dma]
            if lst:
                y = lst[-1]
                if e == "sp":
                    continue
                bop.deps_eng[e] = y.k
                y.need_signal = True
        for q in self.NDMA:
            for y in self.dma_last[q]:
                if y is not None:
                    bop.deps_dma.append(y)
        bop.k = len(self.eng_ops["sp"])
        bop.need_signal = True
        self.eng_ops["sp"].append(bop)
        self.recs = {"__barrier__": [(0, 1, 0, 1, True, bop)]}
        self._barrier_op = bop
        self._barrier_seen = set()
        return bop

    def _barrier_dep(self, op):
        b = getattr(self, "_barrier_op", None)
        if b is None or op is b or op.eng == "sp" or op.eng in self._barrier_seen:
            return
        self._barrier_seen.add(op.eng)
        if b.k > op.deps_eng.get("sp", -1):
            op.deps_eng["sp"] = b.k

    def _rec(self, eng, fn, reads, writes, is_dma=False, name=""):
        op = self.add(eng, fn, reads, writes, is_dma, name)
        self._barrier_dep(op)
        if eng == "pe":
            l = reads[0]
            rr = lambda v: 32 if v <= 32 else (64 if v <= 64 else 128)
            op.name = (rr(int(l.shape[0])), rr(int(l.shape[-1])))
        return op

    def mm(self, out, lhsT, rhs, start=True, stop=True):
        return self._rec("pe", lambda e: e.matmul(out, lhsT, rhs, start=start, stop=stop), [lhsT, rhs], [out])

    def transpose(self, out, in_, ident):
        return self._rec("pe", lambda e: e.transpose(out, in_, ident), [in_, ident], [out])

    def act(self, out, in_, func, bias=None, scale=1.0, accum_out=None):
        reads = [in_]
        kw = {}
        if bias is not None:
            kw["bias"] = bias
            if not isinstance(bias, (int, float)):
                reads.append(bias)
        if not isinstance(scale, (int, float)):
            reads.append(scale)
        kw["scale"] = scale
        writes = [out]
        if accum_out is not None:
            kw["accum_out"] = accum_out
            writes.append(accum_out)
        return self._rec("act", lambda e: e.activation(out, in_, func, **kw), reads, writes)

    def tt(self, out, in0, in1, op, eng="dve"):
        return self._rec(eng, lambda e: e.tensor_tensor(out, in0, in1, op), [in0, in1], [out])

    def ts(self, out, in0, s1, s2, op0, op1=None, eng="dve", accum_out=None):
        reads = [in0]
        if not isinstance(s1, (int, float)):
            reads.append(s1)
        if s2 is not None and not isinstance(s2, (int, float)):
            reads.append(s2)
        writes = [out]
        kw = {}
        if accum_out is not None:
            kw["accum_out"] = accum_out
            writes.append(accum_out)
        if op1 is None:
            return self._rec(eng, lambda e: e.tensor_scalar(out, in0, s1, s2, op0, **kw), reads, writes)
        return self._rec(eng, lambda e: e.tensor_scalar(out, in0, s1, s2, op0, op1, **kw), reads, writes)

    def stt(self, out, in0, scalar, in1, op0, op1, eng="dve"):
        reads = [in0, in1]
        if not isinstance(scalar, (int, float)):
            reads.append(scalar)
        return self._rec(eng, lambda e: e.scalar_tensor_tensor(out, in0, scalar, in1, op0, op1), reads, [out])

    def copy(self, out, in_, eng="dve"):
        if eng == "act":
            return self._rec("act", lambda e: e.copy(out, in_), [in_], [out])
        return self._rec(eng, lambda e: e.tensor_copy(out, in_), [in_], [out])

    def recip(self, out, in_):
        return self._rec("dve", lambda e: e.reciprocal(out, in_), [in_], [out])

    def reduce(self, out, in_, op, axis=None, eng="dve"):
        ax = axis if axis is not None else AX.X
        return self._rec(eng, lambda e: e.tensor_reduce(out, in_, ax, op), [in_], [out])

    def memset(self, out, val, eng="dve"):
        return self._rec(eng, lambda e: e.memset(out, val), [], [out])

    def dma(self, out, in_, q="sp"):
        return self._rec(q, lambda e: e.dma_start(out=out, in_=in_), [in_], [out], is_dma=True)

    def emit(self):
        nc = self.nc
        self.barrier()
        from contextlib import ExitStack
        with ExitStack() as st:
            esem = {e: st.enter_context(nc.semaphore("c_" + e)) for e in self.ENGS}
            dsem = {}
            for q, n in self.NDMA.items():
                for i in range(n):
                    dsem[(q, i)] = st.enter_context(nc.semaphore("d_%s%d" % (q, i)))
            for e in self.ENGS:
                v = 0
                for op in self.eng_ops[e]:
                    if (not op.is_dma) and op.need_signal:
                        v += 1
                        op.sig_val = v
            prog = self

            pstate = {}

            def run(ename, eng):
                waited = {}
                for op in prog.eng_ops[ename]:
                    waits = []
                    for e2, k2 in op.deps_eng.items():
                        y = prog.eng_ops[e2][k2]
                        waits.append((("c", e2), esem[e2], y.sig_val))
                    for y in op.deps_dma:
                        waits.append((y.dma_sem, dsem[y.dma_sem], y.dma_val))
                    if op.is_dma and op.dma_prev is not None:
                        waits.append((op.dma_sem, dsem[op.dma_sem], op.dma_prev.dma_val))
                    for key, sem, val in waits:
                        if waited.get(key, 0) >= val:
                            continue
                        waited[key] = val
                        eng.wait_ge(sem, val)
                    if ename == "pe":
                        pass
                        pstate["mode"] = op.name
                    ins = op.fn(eng)
                    if op.is_dma:
                        ins.then_inc(dsem[op.dma_sem], 16)
                    elif op.need_signal:
                        ins.then_inc(esem[ename], 1)

            with nc.Block() as block:
                @block.tensor
                def _(eng):
                    run("pe", eng)

                @block.scalar
                def _(eng):
                    run("act", eng)

                @block.vector
                def _(eng):
                    run("dve", eng)

                @block.gpsimd
                def _(eng):
                    run("pool", eng)

                @block.sync
                def _(eng):
                    run("sp", eng)


T = 2304
NT = 18
KT = 8
EPS = 1e-6
CHUNKS = [(0, 256, 1), (256, 512, 0), (768, 512, 0), (1280, 512, 0), (1792, 512, 0)]


def _bc(ap, pos, n):
    shp = list(ap.shape)
    v = ap.unsqueeze(pos)
    shp.insert(pos, n)
    return v.to_broadcast(shp)


_DBG = {}


class _Rot:
    def __init__(self, items):
        self.items = list(items)
        self.i = 0

    def next(self):
        v = self.items[self.i % len(self.items)]
        self.i += 1
        return v


def build(n_layers=2, stop_at=None, dbg=False, skip=(), sub=None):
    from contextlib import ExitStack
    nc = bass.Bass("TRN2", target_bir_lowering=False)

    def din(name, shape, dt=F32):
        return nc.dram_tensor(name, shape, dt, kind="ExternalInput").ap()

    x_d = din("x", [2048, 1024])
    ctx_d = din("ctx", [256, 1024])
    vecs0_d = din("vecs0", [112, 128])
    vecs1_d = din("vecs1", [46, 128])
    wada_d = din("w_ada", [2, 1024, 6144])
    win_d = din("w_in", [2, 1024, 1824])
    wout_d = din("w_out", [2, 1024, 1024])
    wup_d = din("wup", [2, 2, 33, 128])
    wr_d = din("w_router", [1024, 16])
    brep_d = din("brep", [128, 288])
    need_moe = stop_at is None or stop_at[0] == "I" or stop_at[1] >= 1
    weg_d = weu_d = wed_d = None
    if need_moe:
        weg_d = din("w_exp_gate", [2, 16, 1024, 512])
        weu_d = din("w_exp_up", [2, 16, 1024, 512])
        wed_d = din("w_exp_down", [2, 16, 512, 1024])
    ident_d = din("ident", [128, 128])
    cs64_d = din("cs64", [128, 256], BF16)
    cn_d = din("cn", [2048, 2048], BF16)
    sn_d = din("sn", [2048, 2048], BF16)
    c256_d = din("c256", [256, 256], BF16)
    s256_d = din("s256", [256, 256], BF16)
    ropec_d = din("ropec", [128, T])
    ropes_d = din("ropes", [128, T])
    psw_d = din("psw", [128, 128])
    masks_d = din("masks", [128, 6, 128])
    bo64_d = din("bo64", [128, 128], BF16)
    esel_d = din("esel", [16, 16, 128], BF16)
    y_d = nc.dram_tensor("y", [2048, 1024], F32, kind="ExternalOutput").ap()
    hT_d = nc.dram_tensor("hT_scr", [128, 8, T], BF16).ap()
    dbg_d = None
    if dbg:
        dbg_d = nc.dram_tensor("dbg_x", [8, 128, 8, T], F32, kind="ExternalOutput").ap()

    st = ExitStack()
    with st:
        ARENA_BYTES = 210944
        arena_t = st.enter_context(nc.sbuf_tensor("arena", [128, ARENA_BYTES // 2], BF16))
        astate = {"off": 0, "peak": 0}

        def _release(m):
            astate["off"] = m

        def sb(name, shape, dt, stack=None):
            n = _dsz(dt)
            for d in shape[1:]:
                n *= int(d)
            n = (n + 63) // 64 * 64
            off = astate["off"]
            if stack is not None:
                stack.callback(_release, off)
            assert off + n <= ARENA_BYTES, ("arena overflow", name, off, n)
            astate["off"] = off + n
            astate["peak"] = max(astate["peak"], off + n)
            v = arena_t[:, off // 2:(off + n) // 2]
            if dt != BF16:
                v = v.bitcast(dt)
            tot = 1
            for d in shape[1:]:
                tot *= int(d)
            v = v[:, 0:tot]
            if len(shape) > 2:
                names = ["d%d" % i for i in range(len(shape) - 1)]
                v = v.rearrange("p (%s) -> p %s" % (" ".join(names), " ".join(names)),
                                **{nm: int(s) for nm, s in zip(names, shape[1:])})
            return v

        P = Prog(nc)
        ps = st.enter_context(nc.psum_tensor("ps", [128, 8, 512], F32))
        xT = sb("xT", [128, 8, T], F32)
        stage = sb("stage", [128, 4, 2048], F32)
        ident = sb("ident", [128, 128], F32)
        ones_bf = sb("ones_bf", [128, 128], BF16)
        bo64 = sb("bo64", [128, 128], BF16)
        V0T = sb("V0T", [128, 112], F32)
        V1T = sb("V1T", [128, 46], F32)
        cvec = sb("cvec", [128, 8, 2], F32)
        modT = sb("modT", [128, 2, 48, 2], F32)
        drv = sb("drv", [128, 2, 2, 8, 2], F32)

        P.dma(ident[:], ident_d)
        P.dma(bo64[:], bo64_d)
        P.memset(ones_bf[:], 1.0)
        P.dma(stage[0:112, 0, 0:128], vecs0_d)
        P.dma(stage[0:46, 1, 0:128], vecs1_d)
        P.transpose(ps[:, 0, 0:112], stage[0:112, 0, 0:128], ident[0:112, 0:112])
        P.transpose(ps[:, 1, 0:46], stage[0:46, 1, 0:128], ident[0:46, 0:46])
        P.copy(V0T[:], ps[:, 0, 0:112])
        P.copy(V1T[:], ps[:, 1, 0:46])
        for wh_ in range(2):
            P.act(cvec[:, :, wh_], V0T[:, 8 * wh_:8 * wh_ + 8], AF.Exp, scale=-1.0)
            P.ts(cvec[:, :, wh_], cvec[:, :, wh_], 1.0, None, ALU.add)
            P.recip(cvec[:, :, wh_], cvec[:, :, wh_])
            P.tt(cvec[:, :, wh_], V0T[:, 8 * wh_:8 * wh_ + 8], cvec[:, :, wh_], ALU.mult)

        s_ada = ExitStack()
        modrow = sb("modrow", [128, 6144], F32, s_ada)
        for l in range(n_layers):
            for s in range(12):
                slot = (s % 2) * 2
                for hlf in range(2):
                    P.dma(stage[:, slot + hlf, :].rearrange("p (k n) -> p k n", k=4),
                          wada_d[l, hlf * 512:(hlf + 1) * 512, s * 512:(s + 1) * 512].rearrange("(k p) n -> p k n", p=128))
                bA = s % 2
                for k in range(8):
                    wv = stage[:, slot + k // 4, :].rearrange("p (k n) -> p k n", k=4)
                    P.mm(ps[0:2, bA, 0:512], cvec[:, k, :], wv[:, k % 4, :], k == 0, k == 7)
                P.copy(modrow[0:2, s * 512:(s + 1) * 512], ps[0:2, bA, 0:512])
            for jg in range(48):
                P.transpose(ps[:, 2, 2 * jg:2 * jg + 2], modrow[0:2, jg * 128:(jg + 1) * 128], ident[0:2, 0:2])
            pv = ps[:, 2, 0:96].rearrange("p (a b) -> p a b", b=2)
            bias = V0T[:, 16 + 48 * l:16 + 48 * (l + 1)]
            P.tt(modT[:, l], pv, _bc(bias, 2, 2), ALU.add)
            for which in range(2):
                sc = modT[:, l, (1 + 3 * which) * 8:(2 + 3 * which) * 8, :]
                nw = V1T[:, (16 * which + 8 * l):(16 * which + 8 * l + 8)]
                P.ts(drv[:, l, which], sc, 1.0, 32.0, ALU.add, ALU.mult)
                P.tt(drv[:, l, which], drv[:, l, which], _bc(nw, 2, 2), ALU.mult)
        s_ada.close()
        P.barrier()

        def A_of(l, which, j, who):
            return drv[:, l, which, j, who:who + 1]

        def B_of(l, which, j, who):
            sec = 0 if which == 0 else 3
            return modT[:, l, sec * 8 + j, who:who + 1]

        def G_of(l, which, j, who):
            sec = 2 if which == 0 else 5
            return modT[:, l, sec * 8 + j, who:who + 1]

        with ExitStack() as s0:
            xin = sb("xin", [128, 2, 1024], F32, s0)
            ev = 0
            for t in range(NT):
                src = ctx_d[t * 128:(t + 1) * 128, :] if t < 2 else x_d[(t - 2) * 128:(t - 1) * 128, :]
                P.dma(xin[:, t % 2, :], src)
                for half in range(2):
                    b = (2 * t + half) % 4 + 3
                    for jj in range(4):
                        j = half * 4 + jj
                        P.transpose(ps[:, b, jj * 128:(jj + 1) * 128], xin[:, t % 2, j * 128:(j + 1) * 128], ident[:])
                    dst = xT[:, half * 4:half * 4 + 4, t * 128:(t + 1) * 128]
                    srcp = ps[:, b, :].rearrange("p (a b) -> p a b", b=128)
                    if ev % 2 == 0:
                        P.copy(dst, srcp)
                    else:
                        P.copy(dst, srcp, eng="act")
                    ev += 1
            P.barrier()

        def norm_chunk(l, which, c0, w, who, sq, xn, rs, bank):
            P.act(sq[:, :, :w], xT[:, :, c0:c0 + w], AF.Square)
            for j in range(8):
                P.mm(ps[:, bank, :w], ones_bf[:], sq[:, j, :w], j == 0, j == 7)
            P.act(rs[:, :w], ps[:, bank, :w], AF.Ln, bias=EPS * 1024.0, scale=1.0)
            P.act(rs[:, :w], rs[:, :w], AF.Exp, scale=-0.5)
            P.tt(xn[:, :, :w], xT[:, :, c0:c0 + w], _bc(rs[:, :w], 1, 8), ALU.mult)

        def load_w(dst_bf, src_rows_by_cols, ncols, slot_rot, eng="pool"):
            kt = dst_bf.shape[1]
            per = max(1, 2048 // ncols)
            k = 0
            while k < kt:
                n = min(per, kt - k)
                slot = slot_rot.next()
                sv = stage[:, slot, 0:n * ncols].rearrange("p (k n) -> p k n", n=ncols)
                P.dma(sv, src_rows_by_cols[k * 128:(k + n) * 128, :].rearrange("(k p) n -> p k n", p=128))
                P.copy(dst_bf[:, k:k + n, :], sv, eng=("act" if cast_rot.next() == 0 else "dve"))
                k += n

        srot = _Rot([0, 1, 2, 3])
        cast_rot = _Rot([0, 1])

        def out_proj(l, Wo, mix, nk, chunks, banks):
            for (c0, w, who) in chunks:
                for m in range(8):
                    b = banks.next()
                    for k in range(nk):
                        P.mm(ps[:, b, :w], Wo[:, k, m * 128:(m + 1) * 128], mix[:, k, c0:c0 + w], k == 0, k == nk - 1)
                    P.stt(xT[:, m, c0:c0 + w], ps[:, b, :w], G_of(l, 0, m, who), xT[:, m, c0:c0 + w], ALU.mult, ALU.add)

        def dump_dbg(idx):
            if dbg_d is not None:
                for j in range(8):
                    P.dma(dbg_d[idx, :, j, :], xT[:, j, :])

        def gla_phase(l, last, lchunks):
            with ExitStack() as s1:
                Wo = sb("g_Wo", [128, 2, 1024], BF16, s1)
                wup = sb("g_wup", [128, 2, 128], BF16, s1)
                msk = sb("g_msk", [128, 6, 128], F32, s1)
                qT = sb("g_qT", [128, T], BF16, s1)
                kT = sb("g_kT", [128, T], BF16, s1)
                ktok = sb("g_ktok", [128, NT, 128], BF16, s1)
                vtok = sb("g_vtok", [128, NT, 256], BF16, s1)
                sgT = sb("g_sg", [128, 2, T], BF16, s1)
                gd = sb("g_gd", [128, T], BF16, s1)
                oT = sb("g_oT", [128, 2, T], F32, s1)
                g8 = sb("g_g8", [128, 1], F32, s1)
                rb = _Rot([0, 1, 2, 3, 4, 5, 6, 7])
                P.dma(msk[:], masks_d)
                P.ts(g8[:], V1T[:, 40 + l:41 + l], 8.0, None, ALU.mult)
                load_w(Wo, wout_d[l][256:512, :], 1024, srot)
                sl = srot.next()
                P.dma(stage[0:33, sl, 0:256].rearrange("p (d c) -> p d c", d=2), wup_d[l].rearrange("d r c -> r d c"))
                P.copy(wup[0:33], stage[0:33, sl, 0:256].rearrange("p (d c) -> p d c", d=2))
                P.memset(gd[0:64], 1.0)
                if sub == "E1a":
                    return
                with ExitStack() as s2:
                    Wg = sb("g_W", [128, 8, 800], BF16, s2)
                    hcb = sb("g_hc", [128, 8, 512], BF16, s2)
                    sge = sb("g_sge", [128, 2, 512], F32, s2)
                    load_w(Wg[:, :, 0:512], win_d[l][:, 256:768], 512, srot)
                    load_w(Wg[:, :, 512:800], win_d[l][:, 768:1056], 288, srot)
                    if sub == "E1b":
                        return
                    for ci, (c0, w, who) in enumerate(CHUNKS):
                        P.dma(hcb[:, :, :w], hT_d[:, :, c0:c0 + w])
                        for (m0, msz, kind) in ((0, 128, "q"), (128, 128, "k"), (512, 128, "g0"), (640, 128, "g1"),
                                                (768, 32, "df")):
                            if _DBG.get("kinds") is not None and kind not in _DBG["kinds"]:
                                continue
                            b = rb.next()
                            for k in range(8):
                                P.mm(ps[0:msz, b, :w], Wg[:, k, m0:m0 + msz], hcb[:, k, :w], k == 0, k == 7)
                            if kind == "q":
                                P.ts(qT[:, c0:c0 + w], ps[:, b, :w], float(32.0 ** -0.5), None, ALU.mult)
                            elif kind == "k":
                                P.copy(kT[:, c0:c0 + w], ps[:, b, :w])
                            elif kind in ("g0", "g1"):
                                gi = 0 if kind == "g0" else 1
                                P.act(sgT[:, gi, c0:c0 + w], ps[:, b, :w], AF.Silu)
                            else:
                                P.copy(gd[0:32, c0:c0 + w], ps[0:32, b, :w])
                        if sub == "E1c" or _DBG.get("notm"):
                            continue
                        for tt in range(w // 128):
                            tg = c0 // 128 + tt
                            b = rb.next()
                            for k in range(8):
                                P.mm(ps[:, b, 0:384], hcb[:, k, tt * 128:(tt + 1) * 128], Wg[:, k, 128:512], k == 0, k == 7)
                            P.copy(ktok[:, tg, :], ps[:, b, 0:128])
                            P.copy(vtok[:, tg, :], ps[:, b, 128:384], eng="act")
                if sub == "E1":
                    return
                sflat = stage.rearrange("p a b -> p (a b)")
                sp_tok = sflat[:, 0:2304].rearrange("p (t c) -> p t c", c=128)
                sbf = sflat[:, 2304:8192].bitcast(BF16)
                q_t = sbf[:, 0:2304]
                k_t = sbf[:, 2304:4608]
                kk = sbf[:, 4608:6912].rearrange("p (t c) -> p t c", c=128)
                Sb = sbf[:, 6912:9216].rearrange("p (i c) -> p i c", c=64)
                with ExitStack() as s2:
                    te = sb("g_te", [128, 512], F32, s2)
                    ec = sb("g_ec", [128, 512], F32, s2)
                    en = sb("g_en", [128, 512], F32, s2)
                    esf = sb("g_esf", [128, 512], F32, s2)
                    dec = sb("g_dec", [128, 36], F32, s2)
                    Sall = sb("g_Sall", [128, 37, 64], F32, s2)
                    attb = sb("g_attb", [128, 2, 4, 128], BF16, s2)
                    groups = [(0, 4), (4, 4), (8, 4), (12, 4), (16, 2)]
                    for d in range(2):
                        for (t0, n) in groups:
                            b = rb.next()
                            for i in range(n):
                                t = t0 + i
                                P.mm(ps[:, b, i * 128:(i + 1) * 128], gd[0:33, t * 128:(t + 1) * 128], wup[0:33, d, :], True, True)
                            P.act(te[:, 0:n * 128], ps[:, b, 0:n * 128], AF.Exp, scale=-1.0)
                            P.act(sp_tok[:, t0:t0 + n, :], te[:, 0:n * 128].rearrange("p (t c) -> p t c", c=128), AF.Ln, bias=1.0)
                        for (t0, n) in groups:
                            bC = rb.next()
                            bS = rb.next()
                            for i in range(n):
                                t = t0 + i
                                P.mm(ps[:, bC, i * 128:(i + 1) * 128], sp_tok[:, t, :], msk[:, d, :], True, True)
                                P.mm(ps[:, bS, i * 128:(i + 1) * 128], msk[:, 2 + d, :], sp_tok[:, t, :], True, True)
                            nn = n * 128
                            cols = slice(t0 * 128, t0 * 128 + nn)
                            P.act(ec[:, 0:nn], ps[:, bC, 0:nn], AF.Exp)
                            P.act(en[:, 0:nn], ps[:, bC, 0:nn], AF.Exp, scale=-1.0)
                            P.act(esf[:, 0:nn], ps[:, bS, 0:nn], AF.Exp)
                            P.tt(q_t[:, cols], qT[:, cols], ec[:, 0:nn], ALU.mult)
                            P.tt(k_t[:, cols], kT[:, cols], en[:, 0:nn], ALU.mult, eng="pool")
                            P.tt(kk[:, t0:t0 + n, :], ktok[:, t0:t0 + n, :], esf[:, 0:nn].rearrange("p (t c) -> p t c", c=128), ALU.mult)
                            ecv = ec[:, 0:nn].rearrange("p (i c) -> p i c", c=64)
                            pick = 63 if d == 0 else 0
                            P.copy(dec[:, 2 * t0:2 * t0 + 2 * n], ecv[:, :, pick], eng="pool")
                        if sub == "E2":
                            continue
                        P.memset(Sall[:, 0, :], 0.0)
                        order = list(range(36)) if d == 0 else ([3, 2, 1, 0] + list(range(35, 3, -1)))
                        pos_of = {ci_: i_ for i_, ci_ in enumerate(order)}
                        for idx_, ci in enumerate(order):
                            t, c = ci // 2, ci % 2
                            r0 = 64 * c
                            b = rb.next()
                            for h in range(4):
                                o_ap = ps[32 * h:32 * h + 32, b, 0:64]
                                l_ap = kk[r0:r0 + 64, t, 32 * h:32 * h + 32]
                                r_ap = vtok[r0:r0 + 64, t, 64 * h:64 * h + 64]
                                P._rec("pe", (lambda e, o_ap=o_ap, l_ap=l_ap, r_ap=r_ap, r0=r0, h=h:
                                              e.matmul(o_ap, l_ap, r_ap, start=True, stop=True, tile_position=(r0, 32 * h))),
                                       [l_ap, r_ap], [o_ap])
                            P.stt(Sall[:, idx_ + 1, :], Sall[:, idx_, :], dec[:, ci:ci + 1], ps[:, b, 0:64], ALU.mult, ALU.add)
                        P.copy(Sb[:, :, :], Sall[:, 0:36, :], eng="act")
                        if sub == "E3":
                            continue
                        rbo = _Rot([4, 5, 6, 7])
                        for t in range(NT):
                            if last and t < 2:
                                continue
                            tc_ = slice(t * 128, (t + 1) * 128)
                            bA = 0
                            for h in range(4):
                                o_ap = ps[:, bA + h, 0:128]
                                l_ap = k_t[32 * h:32 * h + 32, tc_]
                                r_ap = q_t[32 * h:32 * h + 32, tc_]
                                P._rec("pe", (lambda e, o_ap=o_ap, l_ap=l_ap, r_ap=r_ap, h=h:
                                              e.matmul(o_ap, l_ap, r_ap, start=True, stop=True, tile_position=(32 * h, 0))),
                                       [l_ap, r_ap], [o_ap])
                            ab = attb[:, t % 2]
                            P.tt(ab, ps[:, bA:bA + 4, 0:128], _bc(msk[:, 4 + d, :], 1, 4), ALU.mult)
                            bO = rbo.next()
                            for h in range(4):
                                po = 64 * (h % 2)
                                cb = (h // 2) * 128
                                o_ap = ps[po:po + 64, bO, cb:cb + 128]
                                l_ap = vtok[:, t, 64 * h:64 * h + 64]
                                r_ap = ab[:, h, :]
                                P._rec("pe", (lambda e, o_ap=o_ap, l_ap=l_ap, r_ap=r_ap, po=po:
                                              e.matmul(o_ap, l_ap, r_ap, start=True, stop=False, tile_position=(0, po))),
                                       [l_ap, r_ap], [o_ap])
                                for c in range(2):
                                    ci = 2 * t + c
                                    o2 = ps[po:po + 64, bO, cb + 64 * c:cb + 64 * c + 64]
                                    l2 = Sb[32 * h:32 * h + 32, pos_of[ci], :]
                                    r2 = q_t[32 * h:32 * h + 32, t * 128 + 64 * c:t * 128 + 64 * c + 64]
                                    P._rec("pe", (lambda e, o2=o2, l2=l2, r2=r2, h=h, po=po, c=c:
                                                  e.matmul(o2, l2, r2, start=False, stop=(c == 1), tile_position=(32 * h, po))),
                                           [l2, r2], [o2])
                            pso = ps[:, bO, 0:256].rearrange("p (a c) -> p a c", c=128)
                            if d == 0:
                                P.copy(oT[:, :, tc_], pso, eng="act")
                            else:
                                P.tt(oT[:, :, tc_], oT[:, :, tc_], pso, ALU.add)
                    if sub in ("E2", "E3"):
                        return
                    sq = sb("g_sq", [128, 2, 512], BF16, s2)
                    rs = sb("g_rs", [128, 512], F32, s2)
                    tmp = sb("g_tmp", [128, 512], F32, s2)
                    for (c0, w, who) in lchunks:
                        P.act(sq[:, :, :w], oT[:, :, c0:c0 + w], AF.Square)
                        for m in range(2):
                            b = rb.next()
                            P.mm(ps[:, b, :w], bo64[:], sq[:, m, :w], True, True)
                            P.act(rs[:, :w], ps[:, b, :w], AF.Ln, bias=EPS * 64.0, scale=1.0)
                            P.act(rs[:, :w], rs[:, :w], AF.Exp, scale=-0.5)
                            P.stt(tmp[:, :w], oT[:, m, c0:c0 + w], g8[:, 0:1], rs[:, :w], ALU.mult, ALU.mult)
                            P.tt(sgT[:, m, c0:c0 + w], tmp[:, :w], sgT[:, m, c0:c0 + w], ALU.mult, eng="pool")
                out_proj(l, Wo, sgT, 2, lchunks, rb)
                P.barrier()

        def att_phase(l, last, lchunks):
            with ExitStack() as s1:
                Wo = sb("a_Wo", [128, 4, 1024], BF16, s1)
                qr = sb("a_qr", [128, 4, T], BF16, s1)
                kd = sb("a_kd", [128, 2, T], BF16, s1)
                vt = sb("a_vt", [128, NT, 128], BF16, s1)
                g8 = sb("a_g8", [128, 2], F32, s1)
                rb = _Rot([0, 1, 2, 3, 4, 5, 6, 7])
                P.ts(g8[:, 0:1], V1T[:, 42 + l:43 + l], 8.0, None, ALU.mult)
                P.ts(g8[:, 1:2], V1T[:, 44 + l:45 + l], 8.0, None, ALU.mult)
                with ExitStack() as s2:
                    Wq = sb("a_Wq", [128, 8, 512], BF16, s2)
                    Wk = sb("a_Wk", [128, 8, 256], BF16, s2)
                    Wv = sb("a_Wv", [128, 8, 128], BF16, s2)
                    psw = sb("a_psw", [128, 128], F32, s2)
                    sflat_a = stage.rearrange("p a b -> p (a b)")
                    rc = sflat_a[:, 0:T]
                    rsn = sflat_a[:, T:2 * T]
                    hcb = sb("a_hc", [128, 8, 512], BF16, s2)
                    qg2 = sb("a_qg", [128, 2, 512], F32, s2)
                    sq2 = sb("a_sq", [128, 2, 512], BF16, s2)
                    rs2 = sb("a_rsd", [128, 2, 512], F32, s2)
                    t12 = sb("a_t1", [128, 2, 512], F32, s2)
                    t22 = sb("a_t2", [128, 2, 512], F32, s2)
                    P.dma(psw[:], psw_d)
                    load_w(Wq, win_d[l][:, 1056:1568], 512, srot)
                    load_w(Wv, win_d[l][:, 1696:1824], 128, srot)
                    sl = srot.next()
                    sv = stage[:, sl, :].rearrange("p (k n) -> p k n", n=256)
                    for g in range(2):
                        for r in range(2):
                            P.dma(sv[:, :, (2 * g + r) * 64:(2 * g + r + 1) * 64],
                                  win_d[l][:, 1568 + 64 * g:1568 + 64 * g + 64].rearrange("(k p) n -> p k n", p=128))
                    P.copy(Wk[:], sv, eng="act")
                    P.dma(rc, ropec_d)
                    P.dma(rsn, ropes_d)
                    tcnt = 0
                    for ci, (c0, w, who) in enumerate(CHUNKS):
                        P.dma(hcb[:, :, :w], hT_d[:, :, c0:c0 + w])
                        for i in range(6):
                            qg = qg2[:, tcnt % 2]
                            sq = sq2[:, tcnt % 2]
                            rs = rs2[:, tcnt % 2]
                            t1 = t12[:, tcnt % 2]
                            t2 = t22[:, tcnt % 2]
                            tcnt += 1
                            isq = i < 4
                            b0 = rb.next()
                            for k in range(8):
                                wsl = Wq[:, k, i * 128:(i + 1) * 128] if isq else Wk[:, k, (i - 4) * 128:(i - 3) * 128]
                                P.mm(ps[:, b0, :w], wsl, hcb[:, k, :w], k == 0, k == 7)
                            gcol = g8[:, 0:1] if isq else g8[:, 1:2]
                            P.act(qg[:, :w], ps[:, b0, :w], AF.Identity, scale=gcol)
                            P.act(sq[:, :w], ps[:, b0, :w], AF.Square)
                            b1 = rb.next()
                            P.mm(ps[:, b1, :w], bo64[:], sq[:, :w], True, True)
                            P.act(rs[:, :w], ps[:, b1, :w], AF.Ln, bias=EPS * 64.0, scale=1.0)
                            P.act(rs[:, :w], rs[:, :w], AF.Exp, scale=-0.5)
                            b2 = rb.next()
                            P.mm(ps[:, b2, :w], psw[:], qg[:, :w], True, True)
                            P.tt(t1[:, :w], qg[:, :w], rc[:, c0:c0 + w], ALU.mult, eng="pool")
                            P.tt(t2[:, :w], ps[:, b2, :w], rsn[:, c0:c0 + w], ALU.mult)
                            P.tt(t1[:, :w], t1[:, :w], t2[:, :w], ALU.add, eng="pool")
                            dst = qr[:, i, c0:c0 + w] if isq else kd[:, i - 4, c0:c0 + w]
                            P.tt(dst, t1[:, :w], rs[:, :w], ALU.mult)
                        for tt in range(w // 128):
                            tg = c0 // 128 + tt
                            b = rb.next()
                            for k in range(8):
                                P.mm(ps[:, b, 0:128], hcb[:, k, tt * 128:(tt + 1) * 128], Wv[:, k, :], k == 0, k == 7)
                            P.copy(vt[:, tg, :], ps[:, b, 0:128], eng="act")
                load_w(Wo, wout_d[l][512:1024, :], 1024, srot)
                with ExitStack() as s2:
                    amix = sb("a_mix", [128, 4, T], BF16, s2)
                    PT = sb("a_PT", [128, 4, 512], BF16, s2)
                    rd = sb("a_rd", [128, 2, 512], F32, s2)
                    rS = _Rot([0, 1, 2, 3])
                    rO = _Rot([(4, 5), (6, 7)])
                    rP = _Rot([0, 1, 2, 3])
                    for (c0, w, who) in lchunks:
                        kts = list(range(2)) if who == 1 else list(range(NT))
                        for h in range(8):
                            g = h // 4
                            qt = h // 2
                            po = 64 * (h % 2)
                            bO, bD = rO.next()
                            sbank = {}

                            def score(kt_):
                                bS_ = rS.next()
                                sbank[kt_] = bS_
                                P.mm(ps[:, bS_, :w], kd[po:po + 64, g, kt_ * 128:(kt_ + 1) * 128], qr[po:po + 64, qt, c0:c0 + w], True, True)

                            for kt_ in kts[:3]:
                                score(kt_)
                            for idx, kt in enumerate(kts):
                                bS = sbank[kt]
                                pt = PT[:, rP.next(), :w]
                                P.act(pt, ps[:, bS, :w], AF.Exp, scale=0.125)
                                if idx + 3 < len(kts):
                                    score(kts[idx + 3])
                                P.mm(ps[po:po + 64, bO, :w], vt[:, kt, g * 64:(g + 1) * 64], pt, idx == 0, idx == len(kts) - 1)
                                P.mm(ps[po:po + 64, bD, :w], ones_bf[:, 0:64], pt, idx == 0, idx == len(kts) - 1)
                            P.recip(rd[po:po + 64, h % 2, :w], ps[po:po + 64, bD, :w])
                            P.tt(amix[po:po + 64, qt, c0:c0 + w], ps[po:po + 64, bO, :w], rd[po:po + 64, h % 2, :w], ALU.mult)
                    out_proj(l, Wo, amix, 4, lchunks, rb)
                P.barrier()

        def moe_phase(l, last, lchunks):
            with ExitStack() as s1:
                h2T = sb("m_h2T", [128, 8, T], BF16, s1)
                GT = sb("m_GT", [128, T], BF16, s1)
                esel = sb("m_esel", [128, 16, 128], BF16, s1)
                P.memset(esel[:], 0.0)
                P.memset(GT[:], 0.0)
                P.dma(esel[0:16], esel_d)
                tiles = list(range(2, NT)) if last else list(range(NT))
                with ExitStack() as s2:
                    sq = sb("m_sq", [128, 8, 512], BF16, s2)
                    xn = sb("m_xn", [128, 8, 512], F32, s2)
                    rs = sb("m_rs", [128, 512], F32, s2)
                    wr = sb("m_wr", [128, 8, 16], F32, s2)
                    brep = sb("m_brep", [128, 288], F32, s2)
                    s_tok = sb("m_s", [128, NT, 16], F32, s2)
                    sel2 = sb("m_sel2", [128, 72, 8], F32, s2)
                    p1 = sb("m_p1", [128, 72, 4], F32, s2)
                    p2 = sb("m_p2", [128, 72, 2], F32, s2)
                    gs = sb("m_gs", [128, 72], F32, s2)
                    gs2 = sb("m_gs2", [128, 72], F32, s2)
                    gmax = sb("m_gmax", [128, NT], F32, s2)
                    oh = sb("m_oh", [128, 72], F32, s2)
                    cnt = sb("m_cnt", [128, 72, 4], F32, s2)
                    c2 = sb("m_c2", [128, 72, 4], F32, s2)
                    wsum = sb("m_wsum", [128, NT], F32, s2)
                    gate = sb("m_gate", [128, NT, 16], F32, s2)
                    P.dma(wr[:], wr_d.rearrange("(k p) e -> p k e", p=128))
                    P.dma(brep[:], brep_d)
                    P.memset(s_tok[:], 0.0)
                    for ci, (c0, w, who) in enumerate(lchunks):
                        norm_chunk(l, 1, c0, w, who, sq, xn, rs, ci % 2)
                        for j in range(8):
                            if j % 2 == 0:
                                P.ts(xn[:, j, :w], xn[:, j, :w], A_of(l, 1, j, who), B_of(l, 1, j, who), ALU.mult, ALU.add)
                            else:
                                P.act(xn[:, j, :w], xn[:, j, :w], AF.Identity, bias=B_of(l, 1, j, who), scale=A_of(l, 1, j, who))
                        P.copy(h2T[:, :, c0:c0 + w], xn[:, :, :w], eng="act")
                        b = 2 + ci % 2
                        for tt in range(w // 128):
                            tg = c0 // 128 + tt
                            for k in range(8):
                                P.mm(ps[:, b, tt * 16:(tt + 1) * 16], xn[:, k, tt * 128:(tt + 1) * 128], wr[:, k, :], k == 0, k == 7)
                        nt_ = w // 128
                        tg0 = c0 // 128
                        P.act(s_tok[:, tg0:tg0 + nt_, :], ps[:, b, 0:nt_ * 16].rearrange("p (t e) -> p t e", e=16), AF.Exp, scale=-1.0)
                        P.ts(s_tok[:, tg0:tg0 + nt_, :], s_tok[:, tg0:tg0 + nt_, :], 1.0, None, ALU.add)
                        P.recip(s_tok[:, tg0:tg0 + nt_, :], s_tok[:, tg0:tg0 + nt_, :])
                    sv = s_tok.rearrange("p t (g e) -> p (t g) e", e=4)
                    bv = brep.rearrange("p (a e) -> p a e", e=4)
                    P.tt(sel2[:, :, 0:4], sv, bv, ALU.add)
                    P.tt(sel2[:, :, 4:8], sv, bv, ALU.add)
                    P.tt(p1[:], sel2[:, :, 0:4], sel2[:, :, 1:5], ALU.add)
                    P.tt(p2[:], sel2[:, :, 0:2], sel2[:, :, 2:4], ALU.add)
                    P.reduce(gs[:], p1[:], ALU.max)
                    P.reduce(gs2[:], p2[:], ALU.max)
                    P.tt(gs[:], gs[:], gs2[:], ALU.max)
                    P.reduce(gmax[:], gs.rearrange("p (t g) -> p t g", g=4), ALU.max)
                    P.tt(oh.rearrange("p (t g) -> p t g", g=4), gs.rearrange("p (t g) -> p t g", g=4), _bc(gmax[:], 2, 4), ALU.is_equal)
                    P.tt(cnt[:], sel2[:, :, 1:5], sel2[:, :, 0:4], ALU.is_gt)
                    P.tt(c2[:], sel2[:, :, 2:6], sel2[:, :, 0:4], ALU.is_gt)
                    P.tt(cnt[:], cnt[:], c2[:], ALU.add)
                    P.tt(c2[:], sel2[:, :, 3:7], sel2[:, :, 0:4], ALU.is_gt)
                    P.tt(cnt[:], cnt[:], c2[:], ALU.add)
                    P.ts(cnt[:], cnt[:], 1.0, None, ALU.is_le)
                    P.tt(cnt[:], cnt[:], _bc(oh[:], 2, 4), ALU.mult)
                    gv = gate.rearrange("p t (g e) -> p (t g) e", e=4)
                    P.tt(gv, sv, cnt[:], ALU.mult)
                    P.reduce(wsum[:], gate[:], ALU.add)
                    P.ts(wsum[:], wsum[:], 1e-30, None, ALU.max)
                    P.recip(wsum[:], wsum[:])
                    P.tt(gate[:], gate[:], _bc(wsum[:], 2, 16), ALU.mult)
                    for t in tiles:
                        b = 4 + (t // 4) % 2
                        P.transpose(ps[0:16, b, (t % 4) * 128:(t % 4 + 1) * 128], gate[:, t, :], ident[:])
                        P.copy(GT[0:16, t * 128:(t + 1) * 128], ps[0:16, b, (t % 4) * 128:(t % 4 + 1) * 128])
                with ExitStack() as s2:
                    wbuf = sb("m_wbuf", [128, 3, 3, 2048], BF16, s2)
                    sg = sb("m_sg", [128, 2, 512], F32, s2)
                    t1 = sb("m_t1", [128, 2, 512], F32, s2)
                    abuf = sb("m_a", [128, 2, 2, 512], BF16, s2)
                    rG = _Rot([0, 1])
                    rU = _Rot([2, 3])
                    rD = _Rot([5, 6, 7])
                    it = 0
                    pending = None

                    def emit_down(pd):
                        Wd_p, ab_p, c0p, wp, whop = pd
                        for m in range(8):
                            bD = rD.next()
                            for ft in range(2):
                                P.mm(ps[:, bD, :wp], Wd_p[:, ft, m * 128:(m + 1) * 128], ab_p[:, ft, :wp], ft == 0, ft == 1)
                            P.stt(xT[:, m, c0p:c0p + wp], ps[:, bD, :wp], G_of(l, 1, m, whop), xT[:, m, c0p:c0p + wp], ALU.mult, ALU.add)

                    for e in range(16):
                        for fh in range(2):
                            u = e * 2 + fh
                            ws = u % 3
                            Wg_ = wbuf[:, ws, 0].rearrange("p (k f) -> p k f", f=256)
                            Wu_ = wbuf[:, ws, 1].rearrange("p (k f) -> p k f", f=256)
                            Wd_ = wbuf[:, ws, 2].rearrange("p (k d) -> p k d", d=1024)
                            for mi, (dst, srcw) in enumerate(((Wg_, weg_d[l, e][:, fh * 256:(fh + 1) * 256]),
                                                              (Wu_, weu_d[l, e][:, fh * 256:(fh + 1) * 256]))):
                                sl = srot.next()
                                sv_ = stage[:, sl, :].rearrange("p (k f) -> p k f", f=256)
                                P.dma(sv_, srcw.rearrange("(k p) f -> p k f", p=128))
                                P.copy(dst, sv_, eng="act")
                            sl = srot.next()
                            sv_ = stage[:, sl, :].rearrange("p (k d) -> p k d", d=1024)
                            P.dma(sv_, wed_d[l, e][fh * 256:(fh + 1) * 256, :].rearrange("(k p) d -> p k d", p=128))
                            P.copy(Wd_, sv_, eng="act")
                            for (c0, w, who) in lchunks:
                                ab = abuf[:, it % 2]
                                it += 1
                                P.mm(ps[:, 4, :w], esel[:, e, :], GT[:, c0:c0 + w], True, True)
                                for ft in range(2):
                                    bG = rG.next()
                                    bU = rU.next()
                                    for k in range(8):
                                        P.mm(ps[:, bG, :w], Wg_[:, k, ft * 128:(ft + 1) * 128], h2T[:, k, c0:c0 + w], k == 0, k == 7)
                                    for k in range(8):
                                        P.mm(ps[:, bU, :w], Wu_[:, k, ft * 128:(ft + 1) * 128], h2T[:, k, c0:c0 + w], k == 0, k == 7)
                                    P.act(sg[:, ft, :w], ps[:, bG, :w], AF.Silu)
                                    P.tt(t1[:, ft, :w], sg[:, ft, :w], ps[:, bU, :w], ALU.mult)
                                    P.tt(ab[:, ft, :w], t1[:, ft, :w], ps[:, 4, :w], ALU.mult)
                                if pending is not None:
                                    emit_down(pending)
                                pending = (Wd_, ab, c0, w, who)
                    emit_down(pending)
                P.barrier()

        for l in range(n_layers):
            last = (l == n_layers - 1) and (n_layers == 2)
            lchunks = CHUNKS[1:] if last else CHUNKS

            with ExitStack() as s1:
                sq = sb("b_sq", [128, 8, 512], BF16, s1)
                xn = sb("b_xn", [128, 8, 512], F32, s1)
                rs = sb("b_rs", [128, 512], F32, s1)
                hc = sb("b_hc", [128, 2, 8, 512], BF16, s1)
                for ci, (c0, w, who) in enumerate(CHUNKS):
                    norm_chunk(l, 0, c0, w, who, sq, xn, rs, ci % 2)
                    for j in range(8):
                        if j % 2 == 0:
                            P.ts(hc[:, ci % 2, j, :w], xn[:, j, :w], A_of(l, 0, j, who), B_of(l, 0, j, who), ALU.mult, ALU.add)
                        else:
                            P.act(hc[:, ci % 2, j, :w], xn[:, j, :w], AF.Identity, bias=B_of(l, 0, j, who), scale=A_of(l, 0, j, who))
                    P.dma(hT_d[:, :, c0:c0 + w], hc[:, ci % 2, :, :w])
                P.barrier()
            if stop_at == ("B", l):
                break

            with ExitStack() as s1:
                Wu = sb("f_Wu", [128, 8, 256], BF16, s1)
                Wo = sb("f_Wo", [128, 2, 1024], BF16, s1)
                cs64 = sb("f_cs64", [128, 256], BF16, s1)
                uT = sb("f_uT", [128, 2, T], BF16, s1)
                ucs = sb("f_ucs", [128, NT, 512], BF16, s1)
                fmix = sb("f_mix", [128, 2, T], BF16, s1)
                hcb = sb("f_hc", [128, 2, 8, 512], BF16, s1)
                tab = sb("f_tab", [128, 1, 2, 16, 512], BF16, s1)
                P.dma(cs64[:], cs64_d)
                load_w(Wu, win_d[l][:, 0:256], 256, srot)
                load_w(Wo, wout_d[l][0:256, :], 1024, srot)
                rb = _Rot([0, 1, 2, 3])
                for ci, (c0, w, who) in enumerate(CHUNKS):
                    P.dma(hcb[:, ci % 2, :, :w], hT_d[:, :, c0:c0 + w])
                    for m in range(2):
                        b = rb.next()
                        for k in range(8):
                            P.mm(ps[:, b, :w], Wu[:, k, m * 128:(m + 1) * 128], hcb[:, ci % 2, k, :w], k == 0, k == 7)
                        P.copy(uT[:, m, c0:c0 + w], ps[:, b, :w], eng=("act" if m == 0 else "dve"))
                t_lo = 2 if last else 0
                for t in range(t_lo, NT):
                    b = rb.next()
                    for j in range(2):
                        P.mm(ps[:, b, j * 256:(j + 1) * 256], uT[:, j, t * 128:(t + 1) * 128], cs64[:], True, True)
                    P.copy(ucs[:, t, :], ps[:, b, :], eng=("act" if t % 2 == 0 else "dve"))
                tab2 = tab.rearrange("p a c t n -> p (a c t n)").rearrange("p (b c t n) -> p b c t n", b=2, c=2, t=16)
                for pc in range(8):
                    bufi = pc % 2
                    P.dma(tab2[:, bufi, 0], cn_d[:, pc * 256:(pc + 1) * 256].rearrange("(t p) c -> p t c", p=128))
                    P.dma(tab2[:, bufi, 1], sn_d[:, pc * 256:(pc + 1) * 256].rearrange("(t p) c -> p t c", p=128))
                    for j in range(2):
                        b = rb.next()
                        for t in range(16):
                            P.mm(ps[:, b, 0:256], ucs[:, 2 + t, j * 256:j * 256 + 128], tab2[:, bufi, 0, t, :], t == 0, False)
                            P.mm(ps[:, b, 0:256], ucs[:, 2 + t, j * 256 + 128:j * 256 + 256], tab2[:, bufi, 1, t, :], False, t == 15)
                        P.copy(fmix[:, j, 256 + pc * 256:256 + (pc + 1) * 256], ps[:, b, 0:256], eng=("act" if j == 0 else "dve"))
                if not last:
                    c2 = sb("f_c2", [128, 2, 2, 256], BF16, s1)
                    P.dma(c2[:, 0], c256_d.rearrange("(t p) c -> p t c", p=128))
                    P.dma(c2[:, 1], s256_d.rearrange("(t p) c -> p t c", p=128))
                    for j in range(2):
                        b = rb.next()
                        for t in range(2):
                            P.mm(ps[:, b, 0:256], ucs[:, t, j * 256:j * 256 + 128], c2[:, 0, t, :], t == 0, False)
                            P.mm(ps[:, b, 0:256], ucs[:, t, j * 256 + 128:j * 256 + 256], c2[:, 1, t, :], False, t == 1)
                        P.copy(fmix[:, j, 0:256], ps[:, b, 0:256])
                out_proj(l, Wo, fmix, 2, lchunks, rb)
                P.barrier()
            dump_dbg(4 * l + 0)
            if stop_at == ("D", l):
                break

            if 'E' not in skip:
                gla_phase(l, last, lchunks)
            dump_dbg(4 * l + 1)
            if stop_at == ("E", l):
                break

            if 'F' not in skip:
                att_phase(l, last, lchunks)
            dump_dbg(4 * l + 2)
            if stop_at == ("F", l):
                break

            moe_phase(l, last, lchunks)
            dump_dbg(4 * l + 3)
            if stop_at == ("I", l):
                break

        if stop_at is None:
            with ExitStack() as s1:
                sq = sb("o_sq", [128, 8, 512], BF16, s1)
                xn = sb("o_xn", [128, 8, 512], F32, s1)
                rs = sb("o_rs", [128, 512], F32, s1)
                ot = sb("o_ot", [128, 2, 1024], F32, s1)
                fn32 = sb("o_fn", [128, 8], F32, s1)
                P.ts(fn32[:], V1T[:, 32:40], 32.0, None, ALU.mult)
                ev = 0
                for ci, (c0, w, who) in enumerate(CHUNKS[1:]):
                    norm_chunk(0, 0, c0, w, who, sq, xn, rs, 0)
                    for j in range(8):
                        if j % 2 == 0:
                            P.ts(xn[:, j, :w], xn[:, j, :w], fn32[:, j:j + 1], None, ALU.mult)
                        else:
                            P.act(xn[:, j, :w], xn[:, j, :w], AF.Identity, scale=fn32[:, j:j + 1])
                    for tt in range(4):
                        tg = ci * 4 + tt
                        for half in range(2):
                            b = 1 + (2 * tg + half) % 4
                            for jj in range(4):
                                j = half * 4 + jj
                                P.transpose(ps[:, b, jj * 128:(jj + 1) * 128], xn[:, j, tt * 128:(tt + 1) * 128], ident[:])
                            dst = ot[:, tg % 2, half * 512:(half + 1) * 512]
                            if ev % 2 == 0:
                                P.copy(dst, ps[:, b, :])
                            else:
                                P.copy(dst, ps[:, b, :], eng="act")
                            ev += 1
                        P.dma(y_d[tg * 128:(tg + 1) * 128, :], ot[:, tg % 2, :])
        P.emit()
        print("arena peak bytes", astate["peak"], "ops", P.nops)
    return nc


_CONST_CACHE = {}


def _consts():
    if _CONST_CACHE:
        return _CONST_CACHE
    bf = ml_dtypes.bfloat16
    c = {}
    c["ident"] = np.eye(128, dtype=np.float32)
    k = np.arange(64)
    ang = 2 * np.pi * np.outer(k, k) / 64.0
    C64 = np.cos(ang) / 8.0
    S64 = np.sin(ang) / 8.0
    cs = np.zeros((128, 256), np.float64)
    for g in range(2):
        cs[g * 64:(g + 1) * 64, g * 64:(g + 1) * 64] = C64
        cs[g * 64:(g + 1) * 64, 128 + g * 64:128 + (g + 1) * 64] = S64
    c["cs64"] = cs.astype(bf)
    for n, cn, sn in ((2048, "cn", "sn"), (256, "c256", "s256")):
        i = np.arange(n)
        a = 2 * np.pi * (np.outer(i, i) % n) / float(n)
        c[cn] = (np.cos(a) / np.sqrt(n)).astype(bf)
        c[sn] = (-np.sin(a) / np.sqrt(n)).astype(bf)
    inv = 10000.0 ** (-np.arange(16, dtype=np.float64) * 2.0 / 32.0)
    tok = np.arange(2048)
    row = tok // 64
    col = tok % 64
    rc = np.ones((128, T), np.float64)
    rs = np.zeros((128, T), np.float64)
    for hd in range(128):
        a = (hd % 64) // 32
        f = hd % 16
        pos = row if a == 0 else col
        rc[hd, 256:] = np.cos(pos * inv[f])
        rs[hd, 256:] = np.sin(pos * inv[f])
    c["ropec"] = rc.astype(np.float32)
    c["ropes"] = rs.astype(np.float32)
    psw = np.zeros((128, 128), np.float32)
    for hdp in range(128):
        half = (hdp % 32) // 16
        if half == 0:
            psw[hdp + 16, hdp] = -1.0
        else:
            psw[hdp - 16, hdp] = 1.0
    c["psw"] = psw
    j = np.arange(128)[:, None]
    i = np.arange(128)[None, :]
    same = (j // 64) == (i // 64)
    m = np.zeros((128, 6, 128), np.float32)
    m[:, 0, :] = np.where(same & (j <= i), -1.0 / 16.0, 0.0)
    m[:, 1, :] = np.where(same & (j >= i), -1.0 / 16.0, 0.0)
    m[:, 2, :] = np.where(same & (j > i), -1.0 / 16.0, 0.0)
    m[:, 3, :] = np.where(same & (j < i), -1.0 / 16.0, 0.0)
    m[:, 4, :] = np.where(same & (j <= i), 1.0, 0.0)
    m[:, 5, :] = np.where(same & (j >= i), 1.0, 0.0)
    c["masks"] = m
    c["bo64"] = same.astype(np.float32).astype(bf)
    es = np.zeros((16, 16, 128), np.float32)
    for e in range(16):
        es[e, e, :] = 1.0
    c["esel"] = es.astype(bf)
    _CONST_CACHE.update(c)
    return _CONST_CACHE


_NC_CACHE = {}


def _prep_inputs(inputs):
    f = lambda a: np.ascontiguousarray(np.asarray(a, dtype=np.float32))
    x = f(inputs["x"])
    c = f(inputs["c"])
    ctx = f(inputs["ctx"])
    c_ctx = f(inputs["c_ctx"])
    b_ada = f(inputs["b_ada"])
    cst = _consts()
    vecs1 = np.concatenate([
        f(inputs["norm_mix"]).reshape(16, 128),
        f(inputs["norm_ffn"]).reshape(16, 128),
        f(inputs["final_norm"]).reshape(8, 128),
        np.tile(f(inputs["gla_norm"]), (1, 2)),
        np.tile(f(inputs["q_norm"]), (1, 2)),
        np.tile(f(inputs["k_norm"]), (1, 2)),
    ], axis=0)
    wg_ = f(inputs["w_gla_gate_up"])
    bg_ = f(inputs["b_gla_gate"])
    wup = np.zeros((2, 2, 33, 128), np.float32)
    for l_ in range(2):
        for d_ in range(2):
            wup[l_, d_, 16 * d_:16 * d_ + 16, :] = wg_[l_, d_]
            wup[l_, d_, 32, :] = bg_[l_, d_]
    brep = np.ascontiguousarray(np.broadcast_to(np.tile(f(inputs["b_router"]), 18)[None, :], (128, 288)))
    shared = {
        "vecs1": np.ascontiguousarray(vecs1),
        "w_ada": f(inputs["w_ada"]), "w_in": f(inputs["w_in"]), "w_out": f(inputs["w_out"]),
        "wup": np.ascontiguousarray(wup), "w_router": f(inputs["w_router"]), "brep": brep,
        "w_exp_gate": f(inputs["w_exp_gate"]), "w_exp_up": f(inputs["w_exp_up"]), "w_exp_down": f(inputs["w_exp_down"]),
    }
    for k_ in ("ident", "cs64", "cn", "sn", "c256", "s256", "ropec", "ropes", "psw", "masks", "bo64", "esel"):
        shared[k_] = cst[k_]
    in_maps = []
    for b in range(8):
        vecs0 = np.concatenate([c[b].reshape(8, 128), c_ctx.reshape(8, 128),
                                b_ada[0].reshape(48, 128), b_ada[1].reshape(48, 128)], axis=0)
        m = dict(shared)
        m["x"] = x[b]
        m["ctx"] = ctx[b]
        m["vecs0"] = np.ascontiguousarray(vecs0)
        in_maps.append(m)
    return in_maps


def kernel(**inputs):
    in_maps = _prep_inputs(inputs)
    if "nc" not in _NC_CACHE:
        _NC_CACHE["nc"] = build()
    nc = _NC_CACHE["nc"]
    res = run_bass_kernel_spmd(nc, in_maps, core_ids=list(range(8)))
    out = np.stack([np.asarray(r["y"], dtype=np.float32) for r in res.results], axis=0)
    return out
```
